# Optimizing a Trainium2 kernel written in Bass

```python
import jax, jax.numpy as jnp
from jax import lax
import numpy as np

D_MODEL = 1024
BATCH = 16
SEQ = 2048
DEPTH = 1

CTX_LEN = 256
GRID_W = 64
CHUNK = 128
RET_HEADS = 4
RET_DK = 256
RET_DV = 256
RET_W = RET_HEADS * RET_DK
ML_HEADS = 4
ML_DK = 256
ML_DV = 256
ML_W = ML_HEADS * ML_DK
CONV_W = 3
N_ML_GATES = 4 * ML_HEADS
N_GROUPS = 4
EXP_PER_GROUP = 8
N_EXPERTS = N_GROUPS * EXP_PER_GROUP
TOP_K = 2
D_EXPERT = 512
MOE_BLOCK = 128
DN_ALPHA = (2 * DEPTH) ** 0.25
DN_BETA = (8 * DEPTH) ** -0.25
ROPE_BASE = 10000.0
LN_EPS = 1e-5
NEG_INF = -1e30
IN_SIZES = [RET_W, RET_W, RET_W, RET_W, ML_W, ML_W, ML_W, ML_W, N_ML_GATES, D_MODEL, D_MODEL]
IN_COLS = int(sum(IN_SIZES))
IN_SPLITS = np.cumsum(IN_SIZES)[:-1].tolist()

kernel_name = "hybrid_retention_mlstm_hmoe_dit"

F32 = jnp.float32


def layer_norm(x):
    xf = x.astype(F32)
    mu = xf.mean(-1, keepdims=True)
    var = jnp.mean(jnp.square(xf - mu), -1, keepdims=True)
    return ((xf - mu) * lax.rsqrt(var + LN_EPS)).astype(x.dtype)


def post_norm(x, g, b):
    return layer_norm(x) * g + b


def modulate(h, shift, scale):
    return h * (1.0 + scale) + shift


def heads(t, n_heads):
    B, T, _ = t.shape
    return t.reshape(B, T, n_heads, -1).transpose(0, 2, 1, 3)


def merge_heads(t):
    B, H, T, d = t.shape
    return t.transpose(0, 2, 1, 3).reshape(B, T, H * d)


def flip(t):
    return jnp.flip(t, axis=2)


def rotary_2d(t, pos_row, pos_col):
    half = t.shape[-1] // 2
    quarter = half // 2
    freqs = ROPE_BASE ** (-jnp.arange(quarter, dtype=F32) / quarter)

    def rot(u, pos):
        ang = pos.astype(F32)[:, None] * freqs[None, :]
        cos, sin = jnp.cos(ang), jnp.sin(ang)
        u1, u2 = u[..., :quarter], u[..., quarter:]
        return jnp.concatenate([u1 * cos - u2 * sin, u1 * sin + u2 * cos], -1)

    out = jnp.concatenate([rot(t[..., :half], pos_row), rot(t[..., half:], pos_col)], -1)
    return out.astype(t.dtype)


def centred_conv(x, w, b):
    pad = CONV_W // 2
    T = x.shape[1]
    xp = jnp.pad(x, ((0, 0), (pad, pad), (0, 0)))
    return sum(xp[:, j:j + T, :] * w[j] for j in range(CONV_W)) + b


def to_chunks(t):
    B, H, T = t.shape[:3]
    t = t.reshape((B, H, T // CHUNK, CHUNK) + t.shape[3:])
    return jnp.moveaxis(t, 2, 0)


def from_chunks(t):
    t = jnp.moveaxis(t, 0, 2)
    B, H, N, L = t.shape[:4]
    return t.reshape((B, H, N * L) + t.shape[4:])


def intra_mask(inclusive):
    idx = jnp.arange(CHUNK)
    rel = idx[:, None] - idx[None, :]
    return (rel >= 0) if inclusive else (rel > 0), rel.astype(F32)


def retention_scan(q, k, v, log_gamma, s0, inclusive):
    q, k, v = q.astype(F32), k.astype(F32), v.astype(F32)
    mask, rel = intra_mask(inclusive)
    idx = jnp.arange(CHUNK, dtype=F32)
    lg = log_gamma[:, None, None]
    intra_decay = jnp.where(mask, jnp.exp(jnp.where(mask, rel, 0.0) * lg), 0.0)
    q_decay = jnp.exp((idx + 1.0)[None, :] * log_gamma[:, None])
    k_decay = jnp.exp((CHUNK - 1.0 - idx)[None, :] * log_gamma[:, None])
    chunk_decay = jnp.exp(CHUNK * log_gamma)

    def step(s, qkv):
        qc, kc, vc = qkv
        att = jnp.einsum('bhld,bhmd->bhlm', qc, kc) * intra_decay
        out = (jnp.einsum('bhlm,bhme->bhle', att, vc)
               + jnp.einsum('bhld,bhde->bhle', qc, s) * q_decay[None, :, :, None])
        s = (s * chunk_decay[None, :, None, None]
             + jnp.einsum('bhld,bhle->bhde', kc * k_decay[None, :, :, None], vc))
        return s, out

    s, out = lax.scan(step, s0, (to_chunks(q), to_chunks(k), to_chunks(v)))
    return from_chunks(out), s


def retention_state(k, v, log_gamma):
    T = k.shape[2]
    w = jnp.exp((T - 1.0 - jnp.arange(T, dtype=F32))[None, :] * log_gamma[:, None])
    return jnp.einsum('bhtd,bhte->bhde', k.astype(F32) * w[None, :, :, None], v.astype(F32))


def mlstm_scan(q, k, v, log_i, log_f, state0, inclusive):
    q, k, v = q.astype(F32), k.astype(F32), v.astype(F32)
    mask, _ = intra_mask(inclusive)

    def step(carry, inp):
        C, n, m = carry
        qc, kc, vc, ic, fc = inp
        b = jnp.cumsum(fc, axis=-1)
        d = jnp.where(mask, b[..., :, None] - b[..., None, :] + ic[..., None, :], NEG_INF)
        inter = b + m[..., None]
        m_row = jnp.maximum(d.max(-1), inter)
        w = jnp.exp(d - m_row[..., None])
        a = jnp.exp(inter - m_row)
        att = jnp.einsum('bhld,bhmd->bhlm', qc, kc) * w
        num = jnp.einsum('bhlm,bhme->bhle', att, vc) + a[..., None] * jnp.einsum('bhld,bhde->bhle', qc, C)
        den = att.sum(-1) + a * jnp.einsum('bhld,bhd->bhl', qc, n)
        h = num / jnp.maximum(jnp.abs(den), jnp.exp(-m_row))[..., None]
        b_last = b[..., -1]
        g = b_last[..., None] - b + ic
        m_new = jnp.maximum(b_last + m, g.max(-1))
        kw = kc * jnp.exp(g - m_new[..., None])[..., None]
        decay = jnp.exp(b_last + m - m_new)
        C = decay[..., None, None] * C + jnp.einsum('bhld,bhle->bhde', kw, vc)
        n = decay[..., None] * n + kw.sum(2)
        return (C, n, m_new), h

    state, h = lax.scan(step, state0, (to_chunks(q), to_chunks(k), to_chunks(v),
                                       to_chunks(log_i), to_chunks(log_f)))
    return from_chunks(h), state


def mlstm_state(k, v, log_i, log_f):
    k, v = k.astype(F32), v.astype(F32)
    F = jnp.cumsum(log_f, axis=-1)
    FT = F[..., -1]
    g = FT[..., None] - F + log_i
    m = jnp.maximum(FT, g.max(-1))
    kw = k * jnp.exp(g - m[..., None])[..., None]
    return (jnp.einsum('bhtd,bhte->bhde', kw, v), kw.sum(2), m)


def project_stream(u, w_in, b_mgate, conv_w, conv_b, pos):
    B, T, _ = u.shape
    rq, rk, rv, rg, mq, mk, mv, mo, mg, gate_r, gate_m = jnp.split(u @ w_in, IN_SPLITS, axis=-1)
    rq, rk, rv = heads(rq, RET_HEADS), heads(rk, RET_HEADS) * RET_DK ** -0.5, heads(rv, RET_HEADS)
    if pos is not None:
        rq, rk = rotary_2d(rq, *pos), rotary_2d(rk, *pos)
    mq = jax.nn.silu(centred_conv(mq, conv_w[:, :ML_W], conv_b[:ML_W]))
    mk = jax.nn.silu(centred_conv(mk, conv_w[:, ML_W:], conv_b[ML_W:]))
    mq, mk, mv = heads(mq, ML_HEADS), heads(mk, ML_HEADS) * ML_DK ** -0.5, heads(mv, ML_HEADS)
    g = (mg + b_mgate).astype(F32).reshape(B, T, 4, ML_HEADS).transpose(2, 0, 3, 1)
    ml_gates = (g[0], jax.nn.log_sigmoid(g[1]), g[2], jax.nn.log_sigmoid(g[3]))
    return (rq, rk, rv, rg), (mq, mk, mv, mo), ml_gates, (gate_r, gate_m)


def merge_branches(ret, mls, rg, mo, gate_r, gate_m, w_ret_branch, w_ml_branch, w_out):
    r = merge_heads(layer_norm(ret)).astype(rg.dtype) * jax.nn.silu(rg)
    m = merge_heads(layer_norm(mls)).astype(mo.dtype) * jax.nn.sigmoid(mo)
    y = jax.nn.sigmoid(gate_r) * (r @ w_ret_branch) + jax.nn.sigmoid(gate_m) * (m @ w_ml_branch)
    return y @ w_out


def token_mixer(u_lat, u_ctx, pos, w_in, b_mgate, conv_w, conv_b, decay_logit,
                w_ret_branch, w_ml_branch, w_out, need_ctx_out):
    B = u_lat.shape[0]
    lg = jax.nn.log_sigmoid(decay_logit.astype(F32))
    (crq, crk, crv, crg), (cmq, cmk, cmv, cmo), (ci_f, clf_f, ci_b, clf_b), (cgr, cgm) = \
        project_stream(u_ctx, w_in, b_mgate, conv_w, conv_b, None)
    (lrq, lrk, lrv, lrg), (lmq, lmk, lmv, lmo), (li_f, llf_f, li_b, llf_b), (lgr, lgm) = \
        project_stream(u_lat, w_in, b_mgate, conv_w, conv_b, pos)
    merge = lambda ret, mls, rg, mo, gr, gm: merge_branches(ret, mls, rg, mo, gr, gm,
                                                           w_ret_branch, w_ml_branch, w_out)
    if need_ctx_out:
        zr = jnp.zeros((B, RET_HEADS, RET_DK, RET_DV), F32)
        zm = (jnp.zeros((B, ML_HEADS, ML_DK, ML_DV), F32), jnp.zeros((B, ML_HEADS, ML_DK), F32),
              jnp.zeros((B, ML_HEADS), F32))
        cr_f, s_f = retention_scan(crq, crk, crv, lg[0], zr, True)
        cr_b, s_b = retention_scan(flip(crq), flip(crk), flip(crv), lg[1], zr, False)
        cm_f, m_f = mlstm_scan(cmq, cmk, cmv, ci_f, clf_f, zm, True)
        cm_b, m_b = mlstm_scan(flip(cmq), flip(cmk), flip(cmv), flip(ci_b), flip(clf_b), zm, False)
        y_ctx = merge(cr_f + flip(cr_b), cm_f + flip(cm_b), crg, cmo, cgr, cgm)
    else:
        s_f = retention_state(crk, crv, lg[0])
        s_b = retention_state(flip(crk), flip(crv), lg[1])
        m_f = mlstm_state(cmk, cmv, ci_f, clf_f)
        m_b = mlstm_state(flip(cmk), flip(cmv), flip(ci_b), flip(clf_b))
        y_ctx = None
    lr_f, _ = retention_scan(lrq, lrk, lrv, lg[0], s_f, True)
    lr_b, _ = retention_scan(flip(lrq), flip(lrk), flip(lrv), lg[1], s_b, False)
    lm_f, _ = mlstm_scan(lmq, lmk, lmv, li_f, llf_f, m_f, True)
    lm_b, _ = mlstm_scan(flip(lmq), flip(lmk), flip(lmv), flip(li_b), flip(llf_b), m_b, False)
    y_lat = merge(lr_f + flip(lr_b), lm_f + flip(lm_b), lrg, lmo, lgr, lgm)
    return y_lat, y_ctx


def hier_moe(u, w_rg, b_rg, w_re, b_re, w_e1, w_e3, w_e2):
    N, D = u.shape
    p_g = jax.nn.softmax((u @ w_rg + b_rg).astype(F32), axis=-1)
    pg_top, g_idx = lax.top_k(p_g, 1)
    le = (u @ w_re + b_re).astype(F32).reshape(N, N_GROUPS, EXP_PER_GROUP)
    le = jnp.take_along_axis(le, g_idx[:, :, None], axis=1)[:, 0]
    pe_top, e_idx = lax.top_k(jax.nn.softmax(le, axis=-1), TOP_K)
    w = pg_top * pe_top / pe_top.sum(-1, keepdims=True)
    e_flat = (g_idx * EXP_PER_GROUP + e_idx).reshape(-1)
    w_flat = w.reshape(-1)
    t_flat = jnp.repeat(jnp.arange(N, dtype=jnp.int32), TOP_K)
    n_assign = N * TOP_K
    n_blocks = -(-n_assign // MOE_BLOCK) + N_EXPERTS
    n_slots = n_blocks * MOE_BLOCK
    order = jnp.argsort(e_flat)
    se, st, sw = e_flat[order], t_flat[order], w_flat[order]
    counts = jnp.zeros((N_EXPERTS,), jnp.int32).at[e_flat].add(1)
    offsets = jnp.cumsum(counts) - counts
    padded = (counts + MOE_BLOCK - 1) // MOE_BLOCK * MOE_BLOCK
    pad_end = jnp.cumsum(padded)
    pad_off = pad_end - padded
    dest = pad_off[se] + jnp.arange(n_assign, dtype=jnp.int32) - offsets[se]
    slot_tok = jnp.zeros((n_slots,), jnp.int32).at[dest].set(st)
    slot_w = jnp.zeros((n_slots,), F32).at[dest].set(sw)
    block_start = jnp.arange(n_blocks, dtype=jnp.int32) * MOE_BLOCK
    block_exp = jnp.minimum((block_start[:, None] >= pad_end[None, :]).sum(1), N_EXPERTS - 1)
    xs = u[slot_tok].reshape(n_blocks, MOE_BLOCK, D)

    def expert_block(args):
        xb, e = args
        return (jax.nn.silu(xb @ w_e1[e]) * (xb @ w_e3[e])) @ w_e2[e]

    ys = lax.map(expert_block, (xs, block_exp)).reshape(n_slots, D)
    out = jnp.zeros((N, D), F32).at[slot_tok].add(slot_w[:, None] * ys)
    return out.astype(u.dtype)


def setup_inputs(seed: int = 0) -> dict:
    key = jax.random.key(seed)
    ks = iter(jax.random.split(key, 32))
    nrm = lambda shape, scale: scale * jax.random.normal(next(ks), shape, F32)
    D = D_MODEL
    lin = jnp.linspace(3.0, 6.0, ML_HEADS, dtype=F32)
    zh = jnp.zeros((ML_HEADS,), F32)
    gate_base = jnp.concatenate([zh, lin, zh, lin])[None, :]
    decay_base = jnp.log(2.0 ** (5.0 + jnp.arange(RET_HEADS, dtype=F32)) - 1.0)
    return {
        "x": nrm((BATCH, SEQ, D), 1.0),
        "c": nrm((BATCH, D), 1.0),
        "ctx": nrm((BATCH, CTX_LEN, D), 1.0),
        "c_ctx": nrm((D,), 1.0),
        "w_ada": nrm((DEPTH, D, 6 * D), D ** -0.5),
        "b_ada": nrm((DEPTH, 6 * D), 0.02),
        "w_in": nrm((DEPTH, D, IN_COLS), D ** -0.5),
        "b_mgate": gate_base + nrm((DEPTH, N_ML_GATES), 0.1),
        "ml_conv_w": nrm((DEPTH, CONV_W, 2 * ML_W), CONV_W ** -0.5),
        "ml_conv_b": nrm((DEPTH, 2 * ML_W), 0.02),
        "ret_decay_logit": decay_base[None, None, :] + nrm((DEPTH, 2, RET_HEADS), 0.01),
        "w_ret_branch": nrm((DEPTH, RET_HEADS * RET_DV, D), (RET_HEADS * RET_DV) ** -0.5),
        "w_ml_branch": nrm((DEPTH, ML_HEADS * ML_DV, D), (ML_HEADS * ML_DV) ** -0.5),
        "w_out": nrm((DEPTH, D, D), DN_BETA * D ** -0.5),
        "ln1_g": 1.0 + nrm((DEPTH, D), 0.02),
        "ln1_b": nrm((DEPTH, D), 0.02),
        "w_rg": nrm((DEPTH, D, N_GROUPS), D ** -0.5),
        "b_rg": nrm((DEPTH, N_GROUPS), 0.01),
        "w_re": nrm((DEPTH, D, N_EXPERTS), D ** -0.5),
        "b_re": nrm((DEPTH, N_EXPERTS), 0.01),
        "w_e1": nrm((DEPTH, N_EXPERTS, D, D_EXPERT), D ** -0.5),
        "w_e3": nrm((DEPTH, N_EXPERTS, D, D_EXPERT), D ** -0.5),
        "w_e2": nrm((DEPTH, N_EXPERTS, D_EXPERT, D), DN_BETA * D_EXPERT ** -0.5),
        "ln2_g": 1.0 + nrm((DEPTH, D), 0.02),
        "ln2_b": nrm((DEPTH, D), 0.02),
    }


def reference(x, c, ctx, c_ctx, w_ada, b_ada, w_in, b_mgate, ml_conv_w, ml_conv_b, ret_decay_logit,
              w_ret_branch, w_ml_branch, w_out, ln1_g, ln1_b, w_rg, b_rg, w_re, b_re,
              w_e1, w_e3, w_e2, ln2_g, ln2_b):
    B, T, D = x.shape
    ROWS = T // GRID_W
    pos = (jnp.repeat(jnp.arange(ROWS), GRID_W), jnp.tile(jnp.arange(GRID_W), ROWS))
    h_ctx = ctx
    for l in range(DEPTH):
        last = l == DEPTH - 1
        mod = jax.nn.silu(c) @ w_ada[l] + b_ada[l]
        sh1, sc1, g1, sh2, sc2, g2 = jnp.split(mod[:, None, :], 6, axis=-1)
        cmod = jax.nn.silu(c_ctx) @ w_ada[l] + b_ada[l]
        csh1, csc1, cg1, csh2, csc2, cg2 = jnp.split(cmod[None, None, :], 6, axis=-1)
        u_lat = modulate(layer_norm(x), sh1, sc1)
        u_ctx = modulate(layer_norm(h_ctx), csh1, csc1)
        y_lat, y_ctx = token_mixer(u_lat, u_ctx, pos, w_in[l], b_mgate[l], ml_conv_w[l], ml_conv_b[l],
                                   ret_decay_logit[l], w_ret_branch[l], w_ml_branch[l], w_out[l],
                                   not last)
        x = post_norm(DN_ALPHA * x + g1 * y_lat, ln1_g[l], ln1_b[l])
        moe_p = (w_rg[l], b_rg[l], w_re[l], b_re[l], w_e1[l], w_e3[l], w_e2[l])
        u2 = modulate(layer_norm(x), sh2, sc2)
        if last:
            f_lat = hier_moe(u2.reshape(-1, D), *moe_p).reshape(x.shape)
        else:
            h_ctx = post_norm(DN_ALPHA * h_ctx + cg1 * y_ctx, ln1_g[l], ln1_b[l])
            u2c = modulate(layer_norm(h_ctx), csh2, csc2)
            f_all = hier_moe(jnp.concatenate([u2.reshape(-1, D), u2c.reshape(-1, D)], axis=0), *moe_p)
            f_lat = f_all[:B * T].reshape(x.shape)
            h_ctx = post_norm(DN_ALPHA * h_ctx + cg2 * f_all[B * T:].reshape(h_ctx.shape),
                              ln2_g[l], ln2_b[l])
        x = post_norm(DN_ALPHA * x + g2 * f_lat, ln2_g[l], ln2_b[l])
    return x
```

```python
import contextlib
import math
import numpy as np
import concourse.bass as bass
import concourse.mybir as mybir
from concourse.bass_utils import run_bass_kernel_spmd

F32 = mybir.dt.float32
BF16 = mybir.dt.bfloat16
I32 = mybir.dt.int32
AF = mybir.ActivationFunctionType
ALU = mybir.AluOpType
AX = mybir.AxisListType

ENGS = ["tensor", "vector", "scalar", "gpsimd", "sync"]
LN_EPS = 1e-5
DN_ALPHA = 2.0 ** 0.25
NEG = -30000.0
LN16 = math.log(16.0)
NCORES = 8


import types


def _freeze(fn):
    if fn is None or fn.__closure__ is None:
        return fn
    cells = []
    for c in fn.__closure__:
        try:
            cells.append(types.CellType(c.cell_contents))
        except ValueError:
            cells.append(c)
    return types.FunctionType(fn.__code__, fn.__globals__, fn.__name__, fn.__defaults__, tuple(cells))


class Buf:
    __slots__ = ("w", "r", "pr")

    def __init__(self):
        self.w = []
        self.r = []
        self.pr = []


class Prog:
    def __init__(self, nc):
        self.nc = nc
        self.es = contextlib.ExitStack()
        self.ops = {e: [] for e in ENGS}
        self.cnt = {e: 0 for e in ENGS}
        self.dsem = {}
        self.known = {e: {} for e in ENGS}
        self.semobj = {}
        self.stack = [self.es]

    def _nm(self, name):
        self.uid = getattr(self, "uid", 0) + 1
        return f"t{self.uid}_{name}"

    def sb(self, name, shape, dt):
        return self.stack[-1].enter_context(self.nc.sbuf_tensor(self._nm(name), list(shape), dt))

    def ps(self, name, shape, dt=F32):
        return self.stack[-1].enter_context(self.nc.psum_tensor(self._nm(name), list(shape), dt))

    @contextlib.contextmanager
    def phase(self):
        es = contextlib.ExitStack()
        self.stack.append(es)
        with es:
            yield
            self.barrier()
        self.stack.pop()

    def _deps(self, eng, reads, writes, nowaw):
        deps = {}

        def add(ev):
            k, v = ev
            if deps.get(k, 0) < v:
                deps[k] = v
        for b in reads:
            for ev in b.w:
                add(ev)
        for b in writes:
            for ev in b.r:
                add(ev)
            if nowaw:
                for ev in b.pr:
                    add(ev)
            else:
                for ev in b.w:
                    add(ev)
        out = []
        kn = self.known[eng]
        for k, v in deps.items():
            if k == ("E", "tensor") and eng == "tensor":
                continue
            if kn.get(k, 0) >= v:
                continue
            kn[k] = v
            out.append((k, v))
        return out

    def _commit(self, ev, reads, writes, nowaw):
        for b in reads:
            b.r.append(ev)
        for b in writes:
            if nowaw:
                b.w.append(ev)
            else:
                b.pr = b.r
                b.r = []
                b.w = [ev]

    def op(self, eng, fn, reads=(), writes=(), nowaw=False):
        waits = self._deps(eng, reads, writes, nowaw)
        self.cnt[eng] += 1
        ev = (("E", eng), self.cnt[eng])
        self.ops[eng].append((waits, _freeze(fn), ("E", eng), 1))
        self._commit(ev, reads, writes, nowaw)
        return ev

    def dma(self, eng, fn, slot, reads=(), writes=(), nowaw=False):
        waits = self._deps(eng, reads, writes, nowaw)
        self.dsem[slot] = self.dsem.get(slot, 0) + 16
        ev = (("D", slot), self.dsem[slot])
        self.ops[eng].append((waits, _freeze(fn), ("D", slot), 16))
        self._commit(ev, reads, writes, nowaw)
        return ev

    def barrier(self):
        for e in ENGS:
            waits = []
            kn = self.known[e]
            for e2 in ENGS:
                k = ("E", e2)
                v = self.cnt[e2]
                if e2 != e and v > 0 and kn.get(k, 0) < v:
                    kn[k] = v
                    waits.append((k, v))
            for slot, v in self.dsem.items():
                k = ("D", slot)
                if kn.get(k, 0) < v:
                    kn[k] = v
                    waits.append((k, v))
            if waits:
                self.ops[e].append((waits, None, None, 0))

    def emit(self):
        nc = self.nc
        for e in ENGS:
            self.semobj[("E", e)] = self.es.enter_context(nc.semaphore("e_" + e))
        for slot in self.dsem:
            self.semobj[("D", slot)] = self.es.enter_context(nc.semaphore("d_" + str(slot)))
        block = self.es.enter_context(nc.Block())
        for e in ENGS:
            ops = self.ops[e]
            if not ops:
                continue

            def body(engine, ops=ops):
                for waits, fn, sk, inc in ops:
                    for k, v in waits:
                        engine.wait_ge(self.semobj[k], v)
                    if fn is not None:
                        fn(engine).then_inc(self.semobj[sk], inc)
            getattr(block, e)(body)


class Ring:
    def __init__(self, P, name, n, shape, dt, psum=False):
        self.t = [(P.ps if psum else P.sb)(f"{name}{i}", shape, dt) for i in range(n)]
        self.b = [Buf() for _ in range(n)]
        self.i = 0
        self.n = n

    def next(self):
        i = self.i
        self.i = (i + 1) % self.n
        self.last = i
        return self.t[i], self.b[i]


C_RQ, C_RK, C_RV, C_RG, C_MQ, C_MK, C_MV, C_MO, C_MG, C_GR, C_GM = (
    0, 1024, 2048, 3072, 4096, 5120, 6144, 7168, 8192, 8208, 9232)

K_ID = 0
K_IOTA = 128
K_PF = 640
K_PB = 658
K_COSR = 676
K_SINR = 708
K_COSC = 740
K_SINC = 804
K_TRI = 868
K_BLK = 996
K_MF = 1092
K_MB = 1988
K_ONE = 2884
NCONST = 3012


def make_consts():
    c = np.zeros((128, NCONST), np.float32)
    p = np.arange(128)
    c[:, K_ID:K_ID + 128] = np.eye(128, dtype=np.float32)
    c[:, K_IOTA:K_IOTA + 512] = np.arange(512, dtype=np.float32)[None, :]
    for jf in range(18):
        c[:, K_PF + jf] = 128 * jf + p
        c[:, K_PB + jf] = (2048 + 128 * jf + p) if jf < 2 else (128 * (jf - 2) + p)
    q = (p % 64).astype(np.float64)
    freq = 10000.0 ** (-q / 64.0)
    sgn = np.where(p < 64, -1.0, 1.0)
    rows = np.arange(32, dtype=np.float64)
    cols = np.arange(64, dtype=np.float64)
    c[:, K_COSR:K_COSR + 32] = np.cos(freq[:, None] * rows[None, :])
    c[:, K_SINR:K_SINR + 32] = sgn[:, None] * np.sin(freq[:, None] * rows[None, :])
    c[:, K_COSC:K_COSC + 64] = np.cos(freq[:, None] * cols[None, :])
    c[:, K_SINC:K_SINC + 64] = sgn[:, None] * np.sin(freq[:, None] * cols[None, :])
    c[:, K_TRI:K_TRI + 128] = (p[:, None] < p[None, :]).astype(np.float32)
    c[:, K_BLK:K_BLK + 96] = 512.0 * np.arange(96, dtype=np.float32)[None, :]
    cc = np.arange(896)[None, :]
    c[:, K_MF:K_MF + 896] = np.where(cc - 384 - p[:, None] >= 0, 0.0, NEG)
    c[:, K_MB:K_MB + 896] = np.where(p[:, None] - (cc - 384) > 0, 0.0, NEG)
    c[:, K_ONE:K_ONE + 128] = 1.0
    return c


def build(dbg=None):
    nc = bass.Bass("TRN2", target_bir_lowering=False)
    P = Prog(nc)

    def din(name, shape, dt=F32):
        return nc.dram_tensor(name, list(shape), dt, kind="ExternalInput").ap()

    def dscr(name, shape, dt=F32):
        return nc.dram_tensor(name, list(shape), dt).ap()

    x_d = din("x", [2, 2048, 1024])
    ctx_d = din("ctx", [2, 256, 1024])
    cT_d = din("cT", [128, 8, 3])
    w_ada_d = din("w_ada", [1024, 6144])
    b_ada_d = din("b_ada", [1, 6144])
    w_in_d = din("w_in", [1024, 10256])
    bmg_d = din("bmg", [4, 4])
    cw_d = din("cw", [128, 16, 3])
    cb_d = din("cb", [128, 16])
    rdl_d = din("rdl", [1, 8])
    w_rb_d = din("w_rb", [1024, 1024])
    w_mb_d = din("w_mb", [1024, 1024])
    w_out_d = din("w_out", [1024, 1024])
    lnp_d = din("lnp", [4, 1024])
    w_rt_d = din("w_rt", [1024, 36])
    b_rt_d = din("b_rt", [1, 36])
    w_e1_d = din("w_e1", [32, 1024, 512])
    w_e3_d = din("w_e3", [32, 1024, 512])
    w_e2_d = din("w_e2", [32, 512, 1024])
    consts_d = din("consts", [128, NCONST])
    out_d = nc.dram_tensor("out", [2, 2048, 1024], F32, kind="ExternalOutput").ap()

    mod_d = dscr("mod_s", [3, 6144])
    r_d = dscr("r_s", [2, 8, 128, 2048], BF16)
    y_d = dscr("y_s", [8, 128, 2048], BF16)
    x1_d = dscr("x1_s", [4096, 1024])
    u2_d = dscr("u2_s", [4096, 1024], BF16)
    xs_d = dscr("xs_s", [24576, 1024], BF16)
    ys_d = dscr("ys_s", [24576, 1024])
    dbg_outs = {}
    if dbg:
        for name, shape in dbg.items():
            dbg_outs[name] = nc.dram_tensor(name, list(shape), F32, kind="ExternalOutput").ap()

    bmod = Buf()
    br_d = [Buf(), Buf()]
    bx1_d = Buf()
    by_d = Buf()
    bu2_d = Buf()
    bxs_d = Buf()
    bys_d = Buf()
    bout = Buf()
    bdbg = Buf()

    with P.es:
        cst = P.sb("cst", [128, NCONST], F32)
        bcst = Buf()
        P.dma("sync", lambda e: e.dma_start(out=cst[:], in_=consts_d), "cst", writes=[bcst])
        identb = P.sb("identb", [128, 128], BF16)
        bidb = Buf()
        P.op("vector", lambda e: e.tensor_copy(out=identb[:], in_=cst[:, K_ID:K_ID + 128]), reads=[bcst], writes=[bidb])
        identf = cst[:, K_ID:K_ID + 128]
        trib = P.sb("trib", [128, 128], BF16)
        onesb = P.sb("onesb", [128, 128], BF16)
        P.op("vector", lambda e: e.tensor_copy(out=trib[:], in_=cst[:, K_TRI:K_TRI + 128]), reads=[bcst], writes=[bidb], nowaw=True)
        P.op("vector", lambda e: e.tensor_copy(out=onesb[:], in_=cst[:, K_ONE:K_ONE + 128]), reads=[bcst], writes=[bidb], nowaw=True)
        uT = P.sb("uT", [128, 8, 2304], BF16)
        buT = [Buf() for _ in range(5)]
        modc = P.sb("modc", [128, 6, 8], F32)
        bmodc = Buf()
        lgc = P.sb("lgc", [128, 8], F32)
        nlgc = P.sb("nlgc", [128, 8], F32)
        blgc = Buf()
        cw = P.sb("cw", [128, 16, 3], F32)
        cbias = P.sb("cbias", [128, 16], F32)
        bcw = Buf()
        P.dma("sync", lambda e: e.dma_start(out=cw[:], in_=cw_d), "cw", writes=[bcw])
        P.dma("sync", lambda e: e.dma_start(out=cbias[:], in_=cb_d), "cw", writes=[bcw], nowaw=True)
        bmg = P.sb("bmg", [4, 4], F32)
        bbmg = Buf()
        P.dma("sync", lambda e: e.dma_start(out=bmg[:], in_=bmg_d), "bmg", writes=[bbmg])
        logits = P.sb("logits", [128, 32, 36], F32)
        blog = Buf()
        wrt = P.sb("wrt", [128, 8, 36], F32)
        brt = P.sb("brt", [128, 36], F32)
        bwrt = Buf()
        P.dma("sync", lambda e: e.dma_start(out=wrt[:], in_=w_rt_d.rearrange("(c p) n -> p c n", p=128)), "wrt", writes=[bwrt])
        P.dma("sync", lambda e: e.dma_start(out=brt[:], in_=b_rt_d.partition_broadcast(128)), "wrt", writes=[bwrt], nowaw=True)

        P.dma("sync", lambda e: e.dma_start(out=lgc[:], in_=rdl_d.partition_broadcast(128)), "lgc", writes=[blgc])
        P.op("scalar", lambda e: e.activation(out=nlgc[:], in_=lgc[:], func=AF.Exp, scale=-1.0), reads=[blgc], writes=[blgc])
        P.op("scalar", lambda e: e.activation(out=nlgc[:], in_=nlgc[:], func=AF.Ln, bias=1.0, scale=1.0), reads=[blgc], writes=[blgc])
        P.op("vector", lambda e: e.tensor_scalar(out=lgc[:], in0=nlgc[:], scalar1=-1.0, scalar2=None, op0=ALU.mult), reads=[blgc], writes=[blgc])

        with P.phase():
            cT = P.sb("cT", [128, 8, 3], F32)
            bcT = Buf()
            P.dma("sync", lambda e: e.dma_start(out=cT[:], in_=cT_d), "cT", writes=[bcT])
            P.op("scalar", lambda e: e.activation(out=cT[:], in_=cT[:], func=AF.Silu), reads=[bcT], writes=[bcT])
            bada = P.sb("bada", [3, 6144], F32)
            bbada = Buf()
            P.dma("sync", lambda e: e.dma_start(out=bada[:], in_=b_ada_d.partition_broadcast(3)), "bada", writes=[bbada])
            modsb = P.sb("modsb", [3, 6144], F32)
            bmodsb = Buf()
            wa = Ring(P, "wa", 2, [128, 8, 512], F32)
            pmod = Ring(P, "pmod", 2, [128, 512], F32, psum=True)
            for cg in range(12):
                wt, bw = wa.next()
                P.dma("sync", lambda e, wt=wt, cg=cg: e.dma_start(
                    out=wt[:], in_=w_ada_d[:, 512 * cg:512 * cg + 512].rearrange("(c p) n -> p c n", p=128)),
                    f"wa{cg % 2}", writes=[bw])
                pt, bp = pmod.next()
                for k in range(8):
                    P.op("tensor", lambda e, pt=pt, wt=wt, k=k: e.matmul(pt[0:3, :], lhsT=cT[:, k, :], rhs=wt[:, k, :], start=(k == 0), stop=(k == 7)),
                         reads=[bcT, bw], writes=[bp])
                P.op("vector", lambda e, pt=pt, cg=cg: e.tensor_tensor(out=modsb[:, 512 * cg:512 * cg + 512], in0=pt[0:3, :], in1=bada[:, 512 * cg:512 * cg + 512], op=ALU.add),
                     reads=[bp, bbada], writes=[bmodsb], nowaw=True)
            P.dma("gpsimd", lambda e: e.dma_start(out=mod_d, in_=modsb[:]), "mod", reads=[bmodsb], writes=[bmod])
            for v in range(3):
                for which in range(2):
                    P.dma("gpsimd", lambda e, v=v, which=which: e.dma_start(
                        out=modc[:, 2 * v + which, :], in_=mod_d[v, 1024 * which:1024 * which + 1024].rearrange("(c p) -> p c", p=128),
                        allow_slow_non_contiguous=True), "modc", reads=[bmod], writes=[bmodc], nowaw=True)
            for v in range(3):
                P.op("vector", lambda e, v=v: e.tensor_scalar(out=modc[:, 2 * v + 1, :], in0=modc[:, 2 * v + 1, :], scalar1=1.0, scalar2=None, op0=ALU.add),
                     reads=[bmodc], writes=[bmodc])


        selp = P.sb("selp", [4, 4, 128], F32)
        seln = P.sb("seln", [4, 4, 128], F32)
        bsel = Buf()
        P.op("vector", lambda e: e.tensor_copy(out=selp[:], in_=cst[0:4, K_ID:K_ID + 4].unsqueeze(2).broadcast_to([4, 4, 128])), reads=[bcst], writes=[bsel])
        P.op("vector", lambda e: e.tensor_scalar(out=seln[:], in0=selp[:], scalar1=-1.0, scalar2=None, op0=ALU.mult), reads=[bsel], writes=[bsel])

        def V(fn, r=(), w=(), **kw):
            return P.op("vector", fn, reads=r, writes=w, **kw)

        def S(fn, r=(), w=(), **kw):
            return P.op("scalar", fn, reads=r, writes=w, **kw)

        def G(fn, r=(), w=(), **kw):
            return P.op("gpsimd", fn, reads=r, writes=w, **kw)

        def T(fn, r=(), w=(), **kw):
            return P.op("tensor", fn, reads=r, writes=w, **kw)

        def ln_stats(src, bsrc, s6, bs6, m, bm, r, brs):
            V(lambda e: e.bn_stats(out=s6[:, 0, :], in_=src[:, 0:512]), [bsrc], [bs6])
            V(lambda e: e.bn_stats(out=s6[:, 1, :], in_=src[:, 512:1024]), [bsrc], [bs6], nowaw=True)
            V(lambda e: e.bn_aggr(out=m[:], in_=s6[:].rearrange("p a b -> p (a b)")), [bs6], [bm])
            S(lambda e: e.activation(out=r[:], in_=m[:, 1:2], func=AF.Sqrt, bias=LN_EPS, scale=1.0), [bm], [brs])
            V(lambda e: e.reciprocal(out=r[:], in_=r[:]), [brs], [brs])

        def make_wload(wst, wbf):
            def load_w(src, c0, swap=False):
                wt, bw = wst.next()
                P.dma("sync", lambda e: e.dma_start(out=wt[:], in_=src[:, c0:c0 + 128].rearrange("(c p) n -> p c n", p=128)),
                      f"wst{wst.last}", writes=[bw])
                wb, bwb = wbf.next()
                S(lambda e: e.copy(out=wb[:], in_=wt[:]), [bw], [bwb])
                if swap:
                    wb2, bwb2 = wbf.next()
                    S(lambda e: e.copy(out=wb2[:, :, 0:64], in_=wt[:, :, 64:128]), [bw], [bwb2])
                    S(lambda e: e.copy(out=wb2[:, :, 64:128], in_=wt[:, :, 0:64]), [bw], [bwb2], nowaw=True)
                    return (wb, bwb), (wb2, bwb2)
                return wb, bwb
            return load_w

        for b in range(2):
            with P.phase():
                T2 = [P.sb(f"T2_{d}", [4, 2304], F32) for d in range(2)]
                bT2 = [Buf(), Buf()]
                acol = [P.sb(f"acol{d}", [128, 18, 4], F32) for d in range(2)]
                bacol = [Buf(), Buf()]
                em = [P.sb(f"em{d}", [128, 4], F32) for d in range(2)]
                bem = [Buf(), Buf()]
                pA = Ring(P, "pA", 3, [128, 512], F32, psum=True)
                pO = [P.ps(f"pO{i}", [128, 512], F32) for i in range(4)]
                bpO = [Buf() for _ in range(4)]
                pT = P.ps("pT", [128, 8, 128], BF16)
                bpT = Buf()
                with P.phase():
                    xr = Ring(P, "xr", 2, [128, 1024], F32)
                    xn = Ring(P, "xn", 2, [128, 1024], BF16)
                    st6 = Ring(P, "st6", 2, [128, 2, 6], F32)
                    mv = Ring(P, "mv", 2, [128, 2], F32)
                    rs = Ring(P, "rs", 2, [128, 1], F32)
                    tmpm = Ring(P, "tmpm", 2, [128, 8, 128], F32)
                    for j in range(18):
                        xt, bx = xr.next()
                        src = ctx_d[b, 128 * j:128 * j + 128, :] if j < 2 else x_d[b, 128 * (j - 2):128 * (j - 2) + 128, :]
                        P.dma("sync", lambda e, xt=xt, src=src: e.dma_start(out=xt[:], in_=src), f"xr{j % 2}", writes=[bx])
                        s6, bs6 = st6.next()
                        m, bm = mv.next()
                        r, brs = rs.next()
                        ln_stats(xt, bx, s6, bs6, m, bm, r, brs)
                        xb, bxb = xn.next()
                        V(lambda e, xb=xb, xt=xt, m=m, r=r: e.tensor_scalar(out=xb[:], in0=xt[:], scalar1=m[:, 0:1], scalar2=r[:, 0:1], op0=ALU.subtract, op1=ALU.mult),
                          [bx, bm, brs], [bxb])
                        for k in range(8):
                            T(lambda e, xb=xb, k=k: e.transpose(out=pT[:, k, :], in_=xb[:, 128 * k:128 * k + 128], identity=identb[:]),
                              [bxb, bidb], [bpT], nowaw=(k > 0))
                        v = 2 if j < 2 else b
                        tm, btm = tmpm.next()
                        V(lambda e, tm=tm, v=v: e.tensor_tensor(out=tm[:], in0=pT[:], in1=modc[:, 2 * v + 1, :].unsqueeze(2).broadcast_to([128, 8, 128]), op=ALU.mult),
                          [bpT, bmodc], [btm])
                        ch = 0 if j < 2 else 1 + (j - 2) // 4
                        G(lambda e, tm=tm, v=v, j=j: e.tensor_tensor(out=uT[:, :, 128 * j:128 * j + 128], in0=tm[:], in1=modc[:, 2 * v, :].unsqueeze(2).broadcast_to([128, 8, 128]), op=ALU.add),
                          [btm, bmodc], [buT[ch]], nowaw=True)

                    wgs = P.sb("wgs", [128, 8, 16], F32)
                    wgb = P.sb("wgb", [128, 8, 16], BF16)
                    bwg = Buf()
                    P.dma("sync", lambda e: e.dma_start(out=wgs[:], in_=w_in_d[:, C_MG:C_MG + 16].rearrange("(c p) n -> p c n", p=128)), "wg", writes=[bwg])
                    V(lambda e: e.tensor_copy(out=wgb[:], in_=wgs[:]), [bwg], [bwg])
                    T0 = P.sb("T0", [4, 2304], F32)
                    T1 = P.sb("T1", [4, 2304], F32)
                    ones4 = P.sb("ones4", [4, 2304], F32)
                    bT0 = Buf()
                    bT1 = Buf()
                    bones4 = Buf()
                    V(lambda e: e.memset(ones4[:], 1.0), [], [bones4])
                    mx = P.sb("mx", [4, 2], F32)
                    bmx = Buf()
                    mrow = P.sb("mrow", [4, 128], F32)
                    bmrow = Buf()
                    chunks = [(0, 256)] + [(256 + 512 * n, 512) for n in range(4)]
                    for d in range(2):
                        for gi, Tt, bT in ((2 * d, T0, bT0), (2 * d + 1, T1, bT1)):
                            for ci, (c0, cl) in enumerate(chunks):
                                pt, bp = pA.next()
                                for k in range(8):
                                    T(lambda e, pt=pt, k=k, gi=gi, c0=c0, cl=cl: e.matmul(pt[0:4, 0:cl], lhsT=wgb[:, k, 4 * gi:4 * gi + 4], rhs=uT[:, k, c0:c0 + cl], start=(k == 0), stop=(k == 7)),
                                      [bwg, buT[ci]], [bp])
                                if d == 0:
                                    o0 = c0
                                else:
                                    o0 = 2048 if ci == 0 else c0 - 256
                                V(lambda e, pt=pt, Tt=Tt, gi=gi, o0=o0, cl=cl: e.tensor_scalar(out=Tt[:, o0:o0 + cl], in0=pt[0:4, 0:cl], scalar1=bmg[:, gi:gi + 1], scalar2=None, op0=ALU.add),
                                  [bp, bbmg], [bT], nowaw=(ci > 0))
                        S(lambda e: e.activation(out=T1[:], in_=T1[:], func=AF.Exp, scale=-1.0), [bT1], [bT1])
                        S(lambda e: e.activation(out=T1[:], in_=T1[:], func=AF.Ln, bias=1.0, scale=1.0), [bT1], [bT1])
                        V(lambda e, d=d: e.tensor_tensor_scan(out=T2[d][:], data0=ones4[:], data1=T1[:], initial=0.0, op0=ALU.mult, op1=ALU.add),
                          [bT1, bones4], [bT2[d]])
                        if d == 1:
                            V(lambda e: e.tensor_tensor(out=T2[1][:], in0=T2[1][:], in1=T1[:], op=ALU.subtract), [bT2[1], bT1], [bT2[1]])
                        V(lambda e: e.tensor_reduce(out=mx[:, 0:1], in_=T0[:], axis=AX.X, op=ALU.max), [bT0], [bmx])
                        V(lambda e: e.tensor_scalar(out=mx[:, 1:2], in0=mx[:, 0:1], scalar1=LN16, scalar2=None, op0=ALU.add), [bmx], [bmx])
                        V(lambda e, d=d: e.scalar_tensor_tensor(out=T0[:], in0=T0[:], scalar=mx[:, 1:2], in1=T2[d][:], op0=ALU.subtract, op1=(ALU.add if d == 0 else ALU.subtract)),
                          [bT0, bmx, bT2[d]], [bT0])
                        pt, bp = pA.next()
                        for jo in range(18):
                            jf = jo if d == 0 else (jo + 2 if jo < 16 else jo - 16)
                            T(lambda e, pt=pt, jo=jo, jf=jf: e.transpose(out=pt[:, 4 * jf:4 * jf + 4], in_=T0[0:4, 128 * jo:128 * jo + 128], identity=identf[0:4, 0:4]),
                              [bT0, bcst], [bp], nowaw=(jo > 0))
                        S(lambda e, pt=pt, d=d: e.copy(out=acol[d][:].rearrange("p a b -> p (a b)"), in_=pt[:, 0:72]), [bp], [bacol[d]])
                        V(lambda e: e.tensor_scalar(out=mrow[:], in0=ones4[:, 0:128], scalar1=mx[:, 0:1], scalar2=-1.0, op0=ALU.mult, op1=ALU.mult), [bmx, bones4], [bmrow])
                        pt, bp = pA.next()
                        T(lambda e, pt=pt: e.transpose(out=pt[:, 0:4], in_=mrow[0:4, :], identity=identf[0:4, 0:4]), [bmrow, bcst], [bp])
                        S(lambda e, pt=pt, d=d: e.activation(out=em[d][:], in_=pt[:, 0:4], func=AF.Exp), [bp], [bem[d]])

                if dbg and "uT" in dbg and b == 0:
                    du = P.sb("dbg_u", [128, 2304], F32)
                    bdu = Buf()
                    V(lambda e: e.tensor_copy(out=du[:], in_=uT[:, 0, :]), buT, [bdu])
                    P.dma("gpsimd", lambda e: e.dma_start(out=dbg_outs["uT"], in_=du[:]), "dbg", reads=[bdu], writes=[bdbg], nowaw=True)
                    da = P.sb("dbg_a", [128, 2, 76], F32)
                    bda = Buf()
                    for d in range(2):
                        V(lambda e, d=d: e.tensor_copy(out=da[:, d, 0:72], in_=acol[d][:].rearrange("p a b -> p (a b)")), [bacol[d]], [bda], nowaw=True)
                        V(lambda e, d=d: e.tensor_copy(out=da[:, d, 72:76], in_=em[d][:]), [bem[d]], [bda], nowaw=True)
                    P.dma("gpsimd", lambda e: e.dma_start(out=dbg_outs["acol"], in_=da[:].rearrange("p a b -> p (a b)")), "dbg", reads=[bda], writes=[bdbg], nowaw=True)
                    P.dma("gpsimd", lambda e: e.dma_start(out=dbg_outs["T2"][0:4, :], in_=T2[0][:]), "dbg", reads=[bT2[0]], writes=[bdbg], nowaw=True)
                    P.dma("gpsimd", lambda e: e.dma_start(out=dbg_outs["T2"][4:8, :], in_=T2[1][:]), "dbg", reads=[bT2[1]], writes=[bdbg], nowaw=True)

                with P.phase():
                    wst = Ring(P, "wst", 3, [128, 8, 128], F32)
                    wbf = Ring(P, "wbf", 4, [128, 8, 128], BF16)
                    load_w = make_wload(wst, wbf)
                    wvt = P.sb("wvt", [128, 8, 256], BF16)
                    bwv = Buf()
                    qT = P.sb("qT", [128, 2, 2048], BF16)
                    kT = P.sb("kT", [128, 2, 2304], BF16)
                    vv = P.sb("vv", [128, 18, 257], BF16)
                    gT = P.sb("gT", [128, 2, 2048], BF16)
                    bq, bk, bv, bg = Buf(), Buf(), Buf(), Buf()
                    V(lambda e: e.memset(vv[:, :, 256:257], 1.0), [], [bv])
                    raw = P.sb("raw", [128, 2050], F32)
                    rawc = P.sb("rawc", [128, 258], F32)
                    acc = P.sb("acc", [128, 2048], F32)
                    braw, brawc, bacc = Buf(), Buf(), Buf()
                    V(lambda e: e.memset(raw[:], 0.0), [], [braw])
                    V(lambda e: e.memset(rawc[:], 0.0), [], [brawc])
                    tA = Ring(P, "tA", 2, [128, 512], F32)
                    tB = Ring(P, "tB", 2, [128, 512], F32)
                    rowr = Ring(P, "rowr", 2, [128, 512], F32)
                    prer = Ring(P, "prer", 2, [128, 512], F32)
                    Dr = Ring(P, "Dr", 3, [128, 512], BF16)
                    atr = Ring(P, "atr", 3, [128, 512], BF16)
                    mhalf = P.sb("mhalf", [128, 4], F32)
                    bmhalf = Buf()
                    V(lambda e: e.memset(mhalf[:], -0.5), [], [bmhalf])
                    acolr = [P.sb(f"acolr{d}", [128, 18], F32) for d in range(2)]
                    bacolr = [Buf(), Buf()]
                    rawr = Ring(P, "rawr", 2, [128, 4, 257], F32)
                    dn = P.sb("dn", [128, 4], F32)
                    bdn = Buf()
                    hf = P.sb("hf", [128, 4, 256], F32)
                    hs = P.sb("hs", [128, 4, 256], F32)
                    hn = P.sb("hn", [128, 4, 256], BF16)
                    bhf, bhs, bhn = Buf(), Buf(), Buf()
                    s6h = P.sb("s6h", [128, 4, 6], F32)
                    mvh = P.sb("mvh", [128, 4, 2], F32)
                    rsh = P.sb("rsh", [128, 4], F32)
                    bs6h, bmvh, brsh = Buf(), Buf(), Buf()
                    ofm = Ring(P, "ofm", 2, [128, 2, 512], BF16)

                    def proj_fm(wb, bwb, c0, cl, ci):
                        pt, bp = pA.next()
                        for k in range(8):
                            T(lambda e, k=k: e.matmul(pt[:, 0:cl], lhsT=wb[:, k, :], rhs=uT[:, k, c0:c0 + cl], start=(k == 0), stop=(k == 7)),
                              [bwb, buT[ci]], [bp])
                        return pt, bp

                    def proj_v(base):
                        for vu in range(2):
                            wt, bw = wst.next()
                            P.dma("sync", lambda e, wt=wt, vu=vu: e.dma_start(out=wt[:], in_=w_in_d[:, base + 128 * vu:base + 128 * vu + 128].rearrange("(c p) n -> p c n", p=128)),
                                  f"wst{wst.last}", writes=[bw])
                            S(lambda e, wt=wt, vu=vu: e.copy(out=wvt[:, :, 128 * vu:128 * vu + 128], in_=wt[:]), [bw], [bwv], nowaw=(vu > 0))
                        for q2 in range(9):
                            pt, bp = pA.next()
                            for jj in range(2):
                                j = 2 * q2 + jj
                                ci = 0 if j < 2 else 1 + (j - 2) // 4
                                for k in range(8):
                                    T(lambda e, pt=pt, jj=jj, j=j, k=k: e.matmul(pt[:, 256 * jj:256 * jj + 256], lhsT=uT[:, k, 128 * j:128 * j + 128], rhs=wvt[:, k, :], start=(k == 0), stop=(k == 7)),
                                      [bwv, buT[ci]], [bp], nowaw=not (jj == 0 and k == 0))
                            S(lambda e, pt=pt, q2=q2: e.copy(out=vv[:, 2 * q2:2 * q2 + 2, 0:256], in_=pt[:, :].rearrange("p (a b) -> p a b", b=256)),
                              [bp], [bv], nowaw=True)

                    def proj_g(base, func):
                        for dc in range(2):
                            wb, bwb = load_w(w_in_d, base + 128 * dc)
                            for n in range(4):
                                pt, bp = proj_fm(wb, bwb, 256 + 512 * n, 512, n + 1)
                                S(lambda e, pt=pt, dc=dc, n=n: e.activation(out=gT[:, dc, 512 * n:512 * n + 512], in_=pt[:], func=func), [bp], [bg], nowaw=True)

                    def proj_rot(base, dstT, bdst, is_k):
                        for dc in range(2):
                            (wb, bwb), (wb2, bwb2) = load_w(w_in_d, base + 128 * dc, swap=True)
                            if is_k:
                                pt, bp = proj_fm(wb, bwb, 0, 256, 0)
                                S(lambda e, pt=pt, dc=dc: e.copy(out=dstT[:, dc, 0:256], in_=pt[:, 0:256]), [bp], [bdst], nowaw=True)
                            for n in range(4):
                                p1, bp1 = proj_fm(wb, bwb, 256 + 512 * n, 512, n + 1)
                                p2, bp2 = proj_fm(wb2, bwb2, 256 + 512 * n, 512, n + 1)
                                if dc == 0:
                                    cosv = cst[:, K_COSR + 8 * n:K_COSR + 8 * n + 8].unsqueeze(2).broadcast_to([128, 8, 64])
                                    sinv = cst[:, K_SINR + 8 * n:K_SINR + 8 * n + 8].unsqueeze(2).broadcast_to([128, 8, 64])
                                else:
                                    cosv = cst[:, K_COSC:K_COSC + 64].unsqueeze(1).broadcast_to([128, 8, 64])
                                    sinv = cst[:, K_SINC:K_SINC + 64].unsqueeze(1).broadcast_to([128, 8, 64])
                                t1, bt1 = tA.next()
                                t2, bt2 = tB.next()
                                V(lambda e, t1=t1, p1=p1, cosv=cosv: e.tensor_tensor(out=t1[:].rearrange("p (a b) -> p a b", b=64), in0=p1[:].rearrange("p (a b) -> p a b", b=64), in1=cosv, op=ALU.mult),
                                  [bp1, bcst], [bt1])
                                V(lambda e, t2=t2, p2=p2, sinv=sinv: e.tensor_tensor(out=t2[:].rearrange("p (a b) -> p a b", b=64), in0=p2[:].rearrange("p (a b) -> p a b", b=64), in1=sinv, op=ALU.mult),
                                  [bp2, bcst], [bt2])
                                off = (256 if is_k else 0) + 512 * n
                                G(lambda e, t1=t1, t2=t2, dc=dc, off=off: e.tensor_tensor(out=dstT[:, dc, off:off + 512], in0=t1[:], in1=t2[:], op=ALU.add),
                                  [bt1, bt2], [bdst], nowaw=True)

                    def conv_silu(rw, brw, L, ch, dst_ap, bdst):
                        a = acc[:, 0:L]
                        V(lambda e: e.tensor_scalar(out=a, in0=rw[:, 0:L], scalar1=cw[:, ch, 0:1], scalar2=None, op0=ALU.mult), [brw, bcw], [bacc])
                        V(lambda e: e.scalar_tensor_tensor(out=a, in0=rw[:, 1:L + 1], scalar=cw[:, ch, 1:2], in1=a, op0=ALU.mult, op1=ALU.add), [brw, bcw, bacc], [bacc])
                        V(lambda e: e.scalar_tensor_tensor(out=a, in0=rw[:, 2:L + 2], scalar=cw[:, ch, 2:3], in1=a, op0=ALU.mult, op1=ALU.add), [brw, bcw, bacc], [bacc])
                        S(lambda e: e.activation(out=dst_ap, in_=a, func=AF.Silu, bias=cbias[:, ch:ch + 1], scale=1.0), [bacc, bcw], [bdst], nowaw=True)

                    def proj_conv(base, dstT, bdst, is_k, chbase):
                        for dc in range(2):
                            wb, bwb = load_w(w_in_d, base + 128 * dc)
                            ch = chbase + dc
                            if is_k:
                                pt, bp = proj_fm(wb, bwb, 0, 256, 0)
                                S(lambda e, pt=pt: e.copy(out=rawc[:, 1:257], in_=pt[:, 0:256]), [bp], [brawc], nowaw=True)
                                conv_silu(rawc, brawc, 256, ch, dstT[:, dc, 0:256], bdst)
                            for n in range(4):
                                pt, bp = proj_fm(wb, bwb, 256 + 512 * n, 512, n + 1)
                                S(lambda e, pt=pt, n=n: e.copy(out=raw[:, 1 + 512 * n:1 + 512 * n + 512], in_=pt[:]), [bp], [braw], nowaw=True)
                            off = 256 if is_k else 0
                            conv_silu(raw, braw, 2048, ch, dstT[:, dc, off:off + 2048], bdst)

                    def attention(h, is_ml, br):
                        dq = []
                        if not is_ml:
                            V(lambda e: e.tensor_scalar(out=acolr[0][:], in0=cst[:, K_PF:K_PF + 18], scalar1=nlgc[:, h:h + 1], scalar2=-LN16, op0=ALU.mult, op1=ALU.add),
                              [bcst, blgc], [bacolr[0]])
                            V(lambda e: e.tensor_scalar(out=acolr[1][:], in0=cst[:, K_PB:K_PB + 18], scalar1=lgc[:, 4 + h:5 + h], scalar2=-LN16, op0=ALU.mult, op1=ALU.add),
                              [bcst, blgc], [bacolr[1]])
                        ctxs = {}

                        def group_ctx(g, d):
                            rowt, brow = rowr.next()
                            if is_ml:
                                pt, bp = pA.next()
                                c0 = 256 + 512 * g if d == 0 else 512 * g
                                sl = seln if d == 0 else selp
                                T(lambda e: e.matmul(pt[:, :], lhsT=sl[:, h, :], rhs=T2[d][:, c0:c0 + 512], start=True, stop=True), [bsel, bT2[d]], [bp])
                                S(lambda e: e.copy(out=rowt[:], in_=pt[:]), [bp], [brow])
                            else:
                                base = float(256 + 512 * g) if d == 0 else float(512 * g)
                                sc = lgc[:, h:h + 1] if d == 0 else nlgc[:, 4 + h:5 + h]
                                G(lambda e: e.tensor_scalar(out=rowt[:], in0=cst[:, K_IOTA:K_IOTA + 512], scalar1=base, scalar2=sc, op0=ALU.add, op1=ALU.mult), [bcst, blgc], [brow])
                            keys = list(range(0, 4 * g + 6)) if d == 0 else [0, 1] + list(range(4 * g + 2, 18))

                            def applies(jf, sub):
                                if jf < 2:
                                    return True
                                jl = jf - 2
                                qi = 4 * g + sub
                                return jl <= qi if d == 0 else jl >= qi
                            first = {sub: [jf for jf in keys if applies(jf, sub)][0] for sub in range(4)}
                            last = {sub: [jf for jf in keys if applies(jf, sub)][-1] for sub in range(4)}
                            ctxs[(g, d)] = (rowt, brow, keys, applies, first, last)

                        def emit_S(g, d, jf):
                            rowt, brow, keys, applies, first, last = ctxs[(g, d)]
                            ps, bps = pA.next()
                            for dc in range(2):
                                T(lambda e, dc=dc: e.matmul(ps[:, :], lhsT=kT[:, dc, 128 * jf:128 * jf + 128], rhs=qT[:, dc, 512 * g:512 * g + 512], start=(dc == 0), stop=(dc == 1)),
                                  [bk, bq], [bps])
                            jl = jf - 2
                            masked = jf >= 2 and 4 * g <= jl <= 4 * g + 3
                            src, bsrc = rowt, brow
                            if masked:
                                jj = jl - 4 * g
                                mk = (K_MF if d == 0 else K_MB) + 384 - 128 * jj
                                pre, bpre = prer.next()
                                G(lambda e: e.tensor_tensor(out=pre[:], in0=rowt[:], in1=cst[:, mk:mk + 512], op=ALU.add), [brow, bcst], [bpre])
                                src, bsrc = pre, bpre
                            Dt, bD = Dr.next()
                            if is_ml:
                                bias_ap, bbias = acol[d][:, jf, h:h + 1], bacol[d]
                            else:
                                bias_ap, bbias = acolr[d][:, jf:jf + 1], bacolr[d]
                            S(lambda e: e.activation(out=Dt[:], in_=src[:], func=AF.Exp, bias=bias_ap, scale=1.0), [bsrc, bbias], [bD])
                            at, bat = atr.next()
                            V(lambda e: e.tensor_tensor(out=at[:], in0=ps[:], in1=Dt[:], op=ALU.mult), [bps, bD], [bat])
                            return at, bat

                        def emit_AV(g, d, jf, at, bat):
                            rowt, brow, keys, applies, first, last = ctxs[(g, d)]
                            for sub in range(4):
                                if not applies(jf, sub):
                                    continue
                                T(lambda e, sub=sub, st=(jf == first[sub]), sp=(jf == last[sub]): e.matmul(pO[sub][:, 0:257], lhsT=at[:, 128 * sub:128 * sub + 128], rhs=vv[:, jf, :], start=st, stop=sp),
                                  [bat, bv], [bpO[sub]])
                            if jf == keys[-1]:
                                group_done(g, d)

                        def group_done(g, d):
                            raw, braw_ = rawr.next()
                            for sub in range(4):
                                S(lambda e, sub=sub: e.copy(out=raw[:, sub, :], in_=pO[sub][:, 0:257]), [bpO[sub]], [braw_], nowaw=(sub > 0))
                            if is_ml:
                                dq.append(lambda: S(lambda e: e.activation(out=dn[:], in_=raw[:, :, 256], func=AF.Abs), [braw_], [bdn]))
                                dq.append(lambda: V(lambda e: e.tensor_tensor(out=dn[:], in0=dn[:], in1=em[d][:, h:h + 1].broadcast_to([128, 4]), op=ALU.max), [bdn, bem[d]], [bdn]))
                                dq.append(lambda: V(lambda e: e.reciprocal(out=dn[:], in_=dn[:]), [bdn], [bdn]))
                                if d == 0:
                                    dq.append(lambda: G(lambda e: e.tensor_tensor(out=hf[:], in0=raw[:, :, 0:256], in1=dn[:].unsqueeze(2).broadcast_to([128, 4, 256]), op=ALU.mult), [braw_, bdn], [bhf]))
                                else:
                                    dq.append(lambda: G(lambda e: e.tensor_tensor(out=hs[:], in0=raw[:, :, 0:256], in1=dn[:].unsqueeze(2).broadcast_to([128, 4, 256]), op=ALU.mult), [braw_, bdn], [bhs]))
                                    dq.append(lambda: G(lambda e: e.tensor_tensor(out=hs[:], in0=hs[:], in1=hf[:], op=ALU.add), [bhs, bhf], [bhs]))
                            else:
                                if d == 0:
                                    dq.append(lambda: G(lambda e: e.tensor_copy(out=hf[:], in_=raw[:, :, 0:256]), [braw_], [bhf]))
                                else:
                                    dq.append(lambda: G(lambda e: e.tensor_tensor(out=hs[:], in0=raw[:, :, 0:256], in1=hf[:], op=ALU.add), [braw_, bhf], [bhs]))
                            if d == 1:
                                def st_stats():
                                    for sub in range(4):
                                        V(lambda e, sub=sub: e.bn_stats(out=s6h[:, sub, :], in_=hs[:, sub, :]), [bhs], [bs6h], nowaw=(sub > 0))

                                def st_aggr():
                                    for sub in range(4):
                                        V(lambda e, sub=sub: e.bn_aggr(out=mvh[:, sub, :], in_=s6h[:, sub, :]), [bs6h], [bmvh], nowaw=(sub > 0))

                                def st_eps():
                                    G(lambda e: e.tensor_scalar(out=rsh[:], in0=mvh[:, :, 1], scalar1=LN_EPS, scalar2=None, op0=ALU.add), [bmvh], [brsh])

                                def st_pow():
                                    G(lambda e: e.tensor_tensor(out=rsh[:], in0=rsh[:], in1=mhalf[:], op=ALU.pow), [brsh, bmhalf], [brsh])

                                def st_norm():
                                    for sub in range(4):
                                        G(lambda e, sub=sub: e.tensor_scalar(out=hn[:, sub, :], in0=hs[:, sub, :], scalar1=mvh[:, sub, 0:1], scalar2=rsh[:, sub:sub + 1], op0=ALU.subtract, op1=ALU.mult),
                                          [bhs, bmvh, brsh], [bhn], nowaw=(sub > 0))

                                def st_tr():
                                    for sub in range(4):
                                        for dc in range(2):
                                            T(lambda e, sub=sub, dc=dc: e.transpose(out=pT[:, 4 * dc + sub, :], in_=hn[:, sub, 128 * dc:128 * dc + 128], identity=identb[:]),
                                              [bhn, bidb], [bpT], nowaw=not (sub == 0 and dc == 0))

                                def st_out():
                                    of, bof = ofm.next()
                                    for dc in range(2):
                                        V(lambda e, dc=dc: e.tensor_tensor(out=of[:, dc, :].rearrange("p (s c) -> p s c", c=128), in0=pT[:, 4 * dc:4 * dc + 4, :], in1=gT[:, dc, 512 * g:512 * g + 512].rearrange("p (s c) -> p s c", c=128), op=ALU.mult),
                                          [bpT, bg], [bof], nowaw=(dc > 0))
                                    P.dma("gpsimd", lambda e: e.dma_start(out=r_d[br, 2 * h:2 * h + 2, :, 512 * g:512 * g + 512].rearrange("c p t -> p c t"), in_=of[:]),
                                          f"rsp{ofm.last}", reads=[bof], writes=[br_d[br]], nowaw=True)
                                dq.extend([st_stats, st_aggr, st_eps, st_pow, st_norm, st_tr, st_out])

                        tiles = []
                        for g in range(4):
                            for d in range(2):
                                keys_ = list(range(0, 4 * g + 6)) if d == 0 else [0, 1] + list(range(4 * g + 2, 18))
                                for jf in keys_:
                                    tiles.append((g, d, jf))
                        queue = []
                        for ti_, (g, d, jf) in enumerate(tiles):
                            for (g2_, d2_, _) in tiles[ti_:ti_ + 4]:
                                if (g2_, d2_) not in ctxs:
                                    group_ctx(g2_, d2_)
                            at, bat = emit_S(g, d, jf)
                            queue.append((g, d, jf, at, bat))
                            if len(queue) > 2:
                                emit_AV(*queue.pop(0))
                            if dq:
                                dq.pop(0)()
                        while queue:
                            emit_AV(*queue.pop(0))
                        while dq:
                            dq.pop(0)()

                    for h in range(4):
                        proj_rot(C_RQ + 256 * h, qT, bq, False)
                        proj_rot(C_RK + 256 * h, kT, bk, True)
                        proj_v(C_RV + 256 * h)
                        proj_g(C_RG + 256 * h, AF.Silu)
                        attention(h, False, 0)
                    for h in range(4):
                        proj_conv(C_MQ + 256 * h, qT, bq, False, 2 * h)
                        proj_conv(C_MK + 256 * h, kT, bk, True, 8 + 2 * h)
                        proj_v(C_MV + 256 * h)
                        proj_g(C_MO + 256 * h, AF.Sigmoid)
                        attention(h, True, 1)

            if dbg and "r" in dbg and b == 0:
                with P.phase():
                    dr = P.sb("dbg_r", [128, 2048], BF16)
                    drf = P.sb("dbg_rf", [128, 2048], F32)
                    bdr = Buf()
                    for br in range(2):
                        for c in range(8):
                            P.dma("sync", lambda e, br=br, c=c: e.dma_start(out=dr[:], in_=r_d[br, c, :, :]), "dbgl", reads=[br_d[br]], writes=[bdr])
                            V(lambda e: e.tensor_copy(out=drf[:], in_=dr[:]), [bdr], [bdr])
                            P.dma("gpsimd", lambda e, br=br, c=c: e.dma_start(out=dbg_outs["r"][br, c, :, :], in_=drf[:]), "dbg", reads=[bdr], writes=[bdbg], nowaw=True)

            with P.phase():
                pA = Ring(P, "pA", 4, [128, 512], F32, psum=True)
                wst = Ring(P, "wst", 3, [128, 8, 128], F32)
                wbf = Ring(P, "wbf", 6, [128, 8, 128], BF16)
                load_w = make_wload(wst, wbf)
                rfull = P.sb("rfull", [128, 8, 2048], BF16)
                mfull = P.sb("mfull", [128, 8, 2048], BF16)
                brf, bmf = Buf(), Buf()
                for c in range(8):
                    P.dma("sync", lambda e, c=c: e.dma_start(out=rfull[:, c, :], in_=r_d[0, c, :, :]), "rfl", reads=[br_d[0]], writes=[brf], nowaw=True)
                    P.dma("sync", lambda e, c=c: e.dma_start(out=mfull[:, c, :], in_=r_d[1, c, :, :]), "mfl", reads=[br_d[1]], writes=[bmf], nowaw=True)
                sgr = Ring(P, "sgr", 2, [128, 512], F32)
                t1r = Ring(P, "t1r", 2, [128, 512], F32)
                yor = Ring(P, "yor", 2, [128, 512], BF16)
                for oc in range(8):
                    ws = []
                    for (wsrc, gbase) in ((w_rb_d, C_GR), (w_mb_d, C_GM)):
                        ws.append((load_w(wsrc, 128 * oc), load_w(w_in_d, gbase + 128 * oc)))
                    for n in range(4):
                        tt = []
                        for bi, (src_t, bsrc_t) in enumerate(((rfull, brf), (mfull, bmf))):
                            (wb, bwb), (wg_, bwg_) = ws[bi]
                            pa, bpa = pA.next()
                            for k in range(8):
                                T(lambda e, pa=pa, wb=wb, k=k, src_t=src_t, n=n: e.matmul(pa[:, :], lhsT=wb[:, k, :], rhs=src_t[:, k, 512 * n:512 * n + 512], start=(k == 0), stop=(k == 7)), [bwb, bsrc_t], [bpa])
                            pg, bpg = pA.next()
                            for k in range(8):
                                T(lambda e, pg=pg, wg_=wg_, k=k, n=n: e.matmul(pg[:, :], lhsT=wg_[:, k, :], rhs=uT[:, k, 256 + 512 * n:256 + 512 * n + 512], start=(k == 0), stop=(k == 7)), [bwg_, buT[n + 1]], [bpg])
                            sg, bsg = sgr.next()
                            S(lambda e, sg=sg, pg=pg: e.activation(out=sg[:], in_=pg[:], func=AF.Sigmoid), [bpg], [bsg])
                            t1, bt1 = t1r.next()
                            V(lambda e, t1=t1, pa=pa, sg=sg: e.tensor_tensor(out=t1[:], in0=pa[:], in1=sg[:], op=ALU.mult), [bpa, bsg], [bt1])
                            tt.append((t1, bt1))
                        yo, byo = yor.next()
                        G(lambda e, yo=yo, a=tt[0][0], c=tt[1][0]: e.tensor_tensor(out=yo[:], in0=a[:], in1=c[:], op=ALU.add), [tt[0][1], tt[1][1]], [byo])
                        P.dma("gpsimd", lambda e, yo=yo, oc=oc, n=n: e.dma_start(out=y_d[oc, :, 512 * n:512 * n + 512], in_=yo[:]), f"yspill{yor.last}", reads=[byo], writes=[by_d], nowaw=True)

            with P.phase():
                pA = Ring(P, "pA", 3, [128, 512], F32, psum=True)
                pO = [P.ps(f"pO{i}", [128, 512], F32) for i in range(4)]
                bpO = [Buf() for _ in range(4)]
                wst = Ring(P, "wst", 3, [128, 8, 128], F32)
                woutb = P.sb("woutb", [128, 8, 1024], BF16)
                bwout = Buf()
                for c in range(8):
                    wt, bw = wst.next()
                    P.dma("sync", lambda e, wt=wt, c=c: e.dma_start(out=wt[:], in_=w_out_d[:, 128 * c:128 * c + 128].rearrange("(c p) n -> p c n", p=128)), f"wst{wst.last}", writes=[bw])
                    S(lambda e, wt=wt, c=c: e.copy(out=woutb[:, :, 128 * c:128 * c + 128], in_=wt[:]), [bw], [bwout], nowaw=True)
                bct = {}
                bbc = Buf()
                for name, src in (("g1", mod_d[b:b + 1, 2048:3072]), ("sh2", mod_d[b:b + 1, 3072:4096]), ("sc2", mod_d[b:b + 1, 4096:5120]),
                                  ("l1g", lnp_d[0:1, :]), ("l1b", lnp_d[1:2, :])):
                    t = P.sb("bc_" + name, [128, 1024], F32)
                    bct[name] = t
                    P.dma("sync", lambda e, t=t, src=src: e.dma_start(out=t[:], in_=src.partition_broadcast(128)), "bc", reads=[bmod], writes=[bbc], nowaw=True)
                V(lambda e: e.tensor_scalar(out=bct["sc2"][:], in0=bct["sc2"][:], scalar1=1.0, scalar2=None, op0=ALU.add), [bbc], [bbc])
                yTr = Ring(P, "yT", 2, [128, 8, 512], BF16)
                xtr = Ring(P, "xt2", 2, [128, 1024], F32)
                ztr = Ring(P, "zt", 2, [128, 1024], F32)
                x1tr = Ring(P, "x1t", 2, [128, 1024], F32)
                u2tr = Ring(P, "u2t", 2, [128, 1024], F32)
                u2br = Ring(P, "u2b", 2, [128, 1024], BF16)
                u2Tr = Ring(P, "u2T", 2, [128, 8, 128], F32)
                s6r = Ring(P, "s6b", 2, [128, 2, 6], F32)
                m2r = Ring(P, "m2b", 2, [128, 2], F32)
                r2r = Ring(P, "r2b", 2, [128, 1], F32)
                yTs = {}
                u2s_ = {}

                def part1(i):
                    n, sub = i // 4, i % 4
                    gi = b * 16 + i
                    if sub == 0:
                        yT, byT = yTr.next()
                        P.dma("sync", lambda e: e.dma_start(out=yT[:], in_=y_d[:, :, 512 * n:512 * n + 512].rearrange("c p t -> p c t")), f"yT{yTr.last}", reads=[by_d], writes=[byT])
                        yTs[n] = (yT, byT)
                    yT, byT = yTs[n]
                    xt, bxt = xtr.next()
                    P.dma("sync", lambda e: e.dma_start(out=xt[:], in_=x_d[b, 128 * i:128 * i + 128, :]), f"xt2{xtr.last}", writes=[bxt])
                    for half in range(2):
                        for k in range(8):
                            T(lambda e, half=half, k=k: e.matmul(pO[half][:, :], lhsT=yT[:, k, 128 * sub:128 * sub + 128], rhs=woutb[:, k, 512 * half:512 * half + 512], start=(k == 0), stop=(k == 7)),
                              [byT, bwout], [bpO[half]])
                    zt, bzt = ztr.next()
                    for half in range(2):
                        V(lambda e, half=half: e.tensor_tensor(out=zt[:, 512 * half:512 * half + 512], in0=pO[half][:, :], in1=bct["g1"][:, 512 * half:512 * half + 512], op=ALU.mult),
                          [bpO[half], bbc], [bzt], nowaw=(half > 0))
                    V(lambda e: e.scalar_tensor_tensor(out=zt[:], in0=xt[:], scalar=DN_ALPHA, in1=zt[:], op0=ALU.mult, op1=ALU.add), [bxt, bzt], [bzt])
                    s6, bs6 = s6r.next()
                    m2, bm2 = m2r.next()
                    r2, br2 = r2r.next()
                    ln_stats(zt, bzt, s6, bs6, m2, bm2, r2, br2)
                    x1t, bx1t = x1tr.next()
                    V(lambda e: e.tensor_scalar(out=x1t[:], in0=zt[:], scalar1=m2[:, 0:1], scalar2=r2[:, 0:1], op0=ALU.subtract, op1=ALU.mult), [bzt, bm2, br2], [bx1t])
                    G(lambda e: e.tensor_tensor(out=x1t[:], in0=x1t[:], in1=bct["l1g"][:], op=ALU.mult), [bx1t, bbc], [bx1t])
                    G(lambda e: e.tensor_tensor(out=x1t[:], in0=x1t[:], in1=bct["l1b"][:], op=ALU.add), [bx1t, bbc], [bx1t])
                    P.dma("gpsimd", lambda e: e.dma_start(out=x1_d[128 * gi:128 * gi + 128, :], in_=x1t[:]), f"x1st{x1tr.last}", reads=[bx1t], writes=[bx1_d], nowaw=True)
                    s6, bs6 = s6r.next()
                    m2, bm2 = m2r.next()
                    r2, br2 = r2r.next()
                    ln_stats(x1t, bx1t, s6, bs6, m2, bm2, r2, br2)
                    u2t, bu2t = u2tr.next()
                    V(lambda e: e.tensor_scalar(out=u2t[:], in0=x1t[:], scalar1=m2[:, 0:1], scalar2=r2[:, 0:1], op0=ALU.subtract, op1=ALU.mult), [bx1t, bm2, br2], [bu2t])
                    G(lambda e: e.tensor_tensor(out=u2t[:], in0=u2t[:], in1=bct["sc2"][:], op=ALU.mult), [bu2t, bbc], [bu2t])
                    G(lambda e: e.tensor_tensor(out=u2t[:], in0=u2t[:], in1=bct["sh2"][:], op=ALU.add), [bu2t, bbc], [bu2t])
                    u2b, bu2b = u2br.next()
                    S(lambda e: e.copy(out=u2b[:], in_=u2t[:]), [bu2t], [bu2b])
                    P.dma("gpsimd", lambda e: e.dma_start(out=u2_d[128 * gi:128 * gi + 128, :], in_=u2b[:]), f"u2st{u2br.last}", reads=[bu2b], writes=[bu2_d], nowaw=True)
                    u2s_[i] = (u2t, bu2t)

                def part2(i):
                    gi = b * 16 + i
                    u2t, bu2t = u2s_.pop(i)
                    for k in range(8):
                        pp = pO[2 + k // 4]
                        T(lambda e, pp=pp, k=k: e.transpose(out=pp[:, 128 * (k % 4):128 * (k % 4) + 128], in_=u2t[:, 128 * k:128 * k + 128], identity=identf),
                          [bu2t, bcst], [bpO[2 + k // 4]], nowaw=(k % 4 > 0))
                    u2T, bu2T = u2Tr.next()
                    S(lambda e: e.copy(out=u2T[:, 0:4, :].rearrange("p a b -> p (a b)"), in_=pO[2][:, :]), [bpO[2]], [bu2T])
                    V(lambda e: e.tensor_copy(out=u2T[:, 4:8, :].rearrange("p a b -> p (a b)"), in_=pO[3][:, :]), [bpO[3]], [bu2T], nowaw=True)
                    pl, bpl = pA.next()
                    for k in range(8):
                        T(lambda e, k=k: e.matmul(pl[:, 0:36], lhsT=u2T[:, k, :], rhs=wrt[:, k, :], start=(k == 0), stop=(k == 7)), [bu2T, bwrt], [bpl])
                    V(lambda e: e.tensor_tensor(out=logits[:, gi, :], in0=pl[:, 0:36], in1=brt[:], op=ALU.add), [bpl, bwrt], [blog], nowaw=True)

                for i in range(17):
                    if i < 16:
                        part1(i)
                    if i >= 1:
                        part2(i - 1)

        if dbg and "x1" in dbg:
            with P.phase():
                t = P.sb("dbg_x1", [128, 1024], F32)
                bt = Buf()
                for gi in range(32):
                    P.dma("sync", lambda e, gi=gi: e.dma_start(out=t[:], in_=x1_d[128 * gi:128 * gi + 128, :]), "dbgl", reads=[bx1_d], writes=[bt])
                    P.dma("gpsimd", lambda e, gi=gi: e.dma_start(out=dbg_outs["x1"][128 * gi:128 * gi + 128, :], in_=t[:]), "dbg", reads=[bt], writes=[bdbg], nowaw=True)
                lt = P.sb("dbg_lg", [128, 32 * 36], F32)
                V(lambda e: e.tensor_copy(out=lt[:], in_=logits[:].rearrange("p a b -> p (a b)")), [blog], [bt])
                P.dma("gpsimd", lambda e: e.dma_start(out=dbg_outs["logits"], in_=lt[:]), "dbg", reads=[bt], writes=[bdbg], nowaw=True)

        MOE_PLACEHOLDER = True

        d1i = P.sb("d1i", [128, 32], I32)
        d2i = P.sb("d2i", [128, 32], I32)
        wt1 = P.sb("wt1", [128, 32], F32)
        wt2 = P.sb("wt2", [128, 32], F32)
        bei = P.sb("bei", [128, 96], I32)
        bd12, bwt12, bbe = Buf(), Buf(), Buf()
        with P.phase():
            pA = Ring(P, "pA", 3, [128, 512], F32, psum=True)
            ppos = [P.ps(f"ppos{i}", [128, 16, 32], F32) for i in range(2)]
            bppos = [Buf(), Buf()]

            def R(name, shape, dt=F32):
                return P.sb("rt_" + name, shape, dt), Buf()
            gmax, bgmax = R("gmax", [128, 32])
            ohg, bohg = R("ohg", [128, 32, 4])
            eg, beg = R("eg", [128, 32, 4])
            pg, bpg_ = R("pg", [128, 32])
            tmp4, btmp4 = R("tmp4", [128, 32, 4, 8])
            les, bles = R("les", [128, 32, 8])
            m1, bm1 = R("m1", [128, 32])
            mk1, bmk1 = R("mk1", [128, 32, 8])
            le2, ble2 = R("le2", [128, 32, 8])
            m2_, bm2_ = R("m2", [128, 32])
            mk2, bmk2 = R("mk2", [128, 32, 8])
            sg_, bsg_ = R("sg", [128, 32])
            A1, bA1 = R("A1", [128, 32, 4, 8])
            A2, bA2 = R("A2", [128, 32, 4, 8])
            Ab, bAb = R("Ab", [128, 32, 32], BF16)
            posf, bposf = R("posf", [128, 32, 32])
            cnt, bcnt = R("cnt", [128, 32])
            cnti, bcnti = R("cnti", [128, 32], I32)
            padf, bpadf = R("padf", [128, 32])
            pend, bpend = R("pend", [128, 32])
            poff, bpoff = R("poff", [128, 32])
            ones32, bones32 = R("ones32", [128, 32])
            dfl, bdfl = R("dfl", [128, 32])
            cmp, bcmp = R("cmp", [128, 48, 32])
            bef, bbef = R("bef", [128, 48])
            lgv = logits[:, :, 0:4]
            lev = logits[:, :, 4:36].rearrange("p t (g e) -> p t g e", e=8)

            def bc3(ap, n):
                return ap.unsqueeze(2).broadcast_to([128, 32, n])
            V(lambda e: e.memset(ones32[:], 1.0), [], [bones32])
            V(lambda e: e.tensor_reduce(out=gmax[:], in_=lgv, axis=AX.X, op=ALU.max), [blog], [bgmax])
            V(lambda e: e.tensor_tensor(out=ohg[:], in0=lgv, in1=bc3(gmax[:], 4), op=ALU.is_equal), [blog, bgmax], [bohg])
            V(lambda e: e.tensor_tensor(out=eg[:], in0=lgv, in1=bc3(gmax[:], 4), op=ALU.subtract), [blog, bgmax], [beg])
            S(lambda e: e.activation(out=eg[:], in_=eg[:], func=AF.Exp), [beg], [beg])
            V(lambda e: e.tensor_reduce(out=pg[:], in_=eg[:], axis=AX.X, op=ALU.add), [beg], [bpg_])
            V(lambda e: e.reciprocal(out=pg[:], in_=pg[:]), [bpg_], [bpg_])
            V(lambda e: e.tensor_tensor(out=tmp4[:], in0=lev, in1=ohg[:].unsqueeze(3).broadcast_to([128, 32, 4, 8]), op=ALU.mult), [blog, bohg], [btmp4])
            V(lambda e: e.tensor_reduce(out=les[:], in_=tmp4[:].rearrange("p t g e -> p t e g"), axis=AX.X, op=ALU.add), [btmp4], [bles])
            V(lambda e: e.tensor_reduce(out=m1[:], in_=les[:], axis=AX.X, op=ALU.max), [bles], [bm1])
            V(lambda e: e.tensor_tensor(out=mk1[:], in0=les[:], in1=bc3(m1[:], 8), op=ALU.is_equal), [bles, bm1], [bmk1])
            V(lambda e: e.scalar_tensor_tensor(out=le2[:], in0=mk1[:], scalar=-1e30, in1=les[:], op0=ALU.mult, op1=ALU.add), [bmk1, bles], [ble2])
            V(lambda e: e.tensor_reduce(out=m2_[:], in_=le2[:], axis=AX.X, op=ALU.max), [ble2], [bm2_])
            V(lambda e: e.tensor_tensor(out=mk2[:], in0=le2[:], in1=bc3(m2_[:], 8), op=ALU.is_equal), [ble2, bm2_], [bmk2])
            V(lambda e: e.tensor_tensor(out=sg_[:], in0=m1[:], in1=m2_[:], op=ALU.subtract), [bm1, bm2_], [bsg_])
            S(lambda e: e.activation(out=sg_[:], in_=sg_[:], func=AF.Sigmoid), [bsg_], [bsg_])
            V(lambda e: e.tensor_tensor(out=wt1[:], in0=pg[:], in1=sg_[:], op=ALU.mult), [bpg_, bsg_], [bwt12])
            V(lambda e: e.tensor_tensor(out=wt2[:], in0=pg[:], in1=wt1[:], op=ALU.subtract), [bpg_, bwt12], [bwt12])
            for (Ax, bAx, mk, bmk) in ((A1, bA1, mk1, bmk1), (A2, bA2, mk2, bmk2)):
                V(lambda e, Ax=Ax, mk=mk: e.tensor_tensor(out=Ax[:], in0=ohg[:].unsqueeze(3).broadcast_to([128, 32, 4, 8]), in1=mk[:].unsqueeze(2).broadcast_to([128, 32, 4, 8]), op=ALU.mult),
                  [bohg, bmk], [bAx])
            A1f = A1[:].rearrange("p t g e -> p t (g e)")
            A2f = A2[:].rearrange("p t g e -> p t (g e)")
            V(lambda e: e.tensor_tensor(out=Ab[:], in0=A1f, in1=A2f, op=ALU.add), [bA1, bA2], [bAb])
            for ti in range(32):
                pp = ppos[ti // 16]
                bpp = bppos[ti // 16]
                for tj in range(ti):
                    T(lambda e, pp=pp, ti=ti, tj=tj: e.matmul(pp[:, ti % 16, :], lhsT=onesb[:], rhs=Ab[:, tj, :], start=(tj == 0), stop=False), [bidb, bAb], [bpp], nowaw=True)
                T(lambda e, pp=pp, ti=ti: e.matmul(pp[:, ti % 16, :], lhsT=trib[:], rhs=Ab[:, ti, :], start=(ti == 0), stop=True), [bidb, bAb], [bpp], nowaw=True)
            pc, bpc = pA.next()
            for tj in range(32):
                T(lambda e, pc=pc, tj=tj: e.matmul(pc[:, 0:32], lhsT=onesb[:], rhs=Ab[:, tj, :], start=(tj == 0), stop=(tj == 31)), [bidb, bAb], [bpc])
            S(lambda e: e.copy(out=posf[:, 0:16, :], in_=ppos[0][:]), [bppos[0]], [bposf])
            V(lambda e: e.tensor_copy(out=posf[:, 16:32, :], in_=ppos[1][:]), [bppos[1]], [bposf], nowaw=True)
            V(lambda e: e.tensor_scalar(out=cnti[:], in0=pc[:, 0:32], scalar1=511.0, scalar2=None, op0=ALU.add), [bpc], [bcnti])
            V(lambda e: e.tensor_single_scalar(out=cnti[:], in_=cnti[:], scalar=9, op=ALU.arith_shift_right), [bcnti], [bcnti])
            V(lambda e: e.tensor_single_scalar(out=cnti[:], in_=cnti[:], scalar=9, op=ALU.logical_shift_left), [bcnti], [bcnti])
            V(lambda e: e.tensor_copy(out=padf[:], in_=cnti[:]), [bcnti], [bpadf])
            V(lambda e: e.tensor_tensor_scan(out=pend[:], data0=ones32[:], data1=padf[:], initial=0.0, op0=ALU.mult, op1=ALU.add), [bones32, bpadf], [bpend])
            V(lambda e: e.tensor_tensor(out=poff[:], in0=pend[:], in1=padf[:], op=ALU.subtract), [bpend, bpadf], [bpoff])
            V(lambda e: e.tensor_tensor(out=posf[:], in0=posf[:], in1=poff[:].unsqueeze(1).broadcast_to([128, 32, 32]), op=ALU.add), [bposf, bpoff], [bposf])
            for (Af, bAx, di) in ((A1f, bA1, d1i), (A2f, bA2, d2i)):
                V(lambda e, Af=Af: e.tensor_tensor(out=tmp4[:].rearrange("p t g e -> p t (g e)"), in0=Af, in1=posf[:], op=ALU.mult), [bAx, bposf], [btmp4])
                V(lambda e: e.tensor_reduce(out=dfl[:], in_=tmp4[:].rearrange("p t g e -> p t (g e)"), axis=AX.X, op=ALU.add), [btmp4], [bdfl])
                V(lambda e, di=di: e.tensor_copy(out=di[:], in_=dfl[:]), [bdfl], [bd12], nowaw=True)
            V(lambda e: e.tensor_tensor(out=cmp[:], in0=pend[:].unsqueeze(1).broadcast_to([128, 48, 32]), in1=cst[:, K_BLK:K_BLK + 48].unsqueeze(2).broadcast_to([128, 48, 32]), op=ALU.is_le),
              [bpend, bcst], [bcmp])
            V(lambda e: e.tensor_reduce(out=bef[:], in_=cmp[:], axis=AX.X, op=ALU.add), [bcmp], [bbef])
            V(lambda e: e.tensor_scalar(out=bei[:, 0:48], in0=bef[:], scalar1=31.0, scalar2=None, op0=ALU.min), [bbef], [bbe])
            if dbg and "route" in dbg:
                rt = P.sb("dbg_rt", [128, 4, 32], F32)
                brt_ = Buf()
                V(lambda e: e.tensor_copy(out=rt[:, 0, :], in_=d1i[:]), [bd12], [brt_], nowaw=True)
                V(lambda e: e.tensor_copy(out=rt[:, 1, :], in_=d2i[:]), [bd12], [brt_], nowaw=True)
                V(lambda e: e.tensor_copy(out=rt[:, 2, :], in_=wt1[:]), [bwt12], [brt_], nowaw=True)
                V(lambda e: e.tensor_copy(out=rt[:, 3, :], in_=wt2[:]), [bwt12], [brt_], nowaw=True)
                P.dma("gpsimd", lambda e: e.dma_start(out=dbg_outs["route"], in_=rt[:].rearrange("p a b -> p (a b)")), "dbg", reads=[brt_], writes=[bdbg], nowaw=True)
                bt_ = P.sb("dbg_be", [128, 96], F32)
                V(lambda e: e.memset(bt_[:], 0.0), [], [brt_], nowaw=True)
                V(lambda e: e.tensor_copy(out=bt_[:, 0:48], in_=bei[:, 0:48]), [bbe], [brt_])
                P.dma("gpsimd", lambda e: e.dma_start(out=dbg_outs["be"], in_=bt_[:]), "dbg", reads=[brt_], writes=[bdbg], nowaw=True)
            u2s = Ring(P, "u2s", 2, [128, 1024], BF16)
            for ti in range(32):
                ut, but = u2s.next()
                P.dma("sync", lambda e, ut=ut, ti=ti: e.dma_start(out=ut[:], in_=u2_d[128 * ti:128 * ti + 128, :]), f"u2s{ti % 2}", reads=[bu2_d], writes=[but])
                for di in (d1i, d2i):
                    P.dma("gpsimd", lambda e, ut=ut, ti=ti, di=di: e.indirect_dma_start(
                        out=xs_d, out_offset=bass.IndirectOffsetOnAxis(ap=di[:, ti:ti + 1], axis=0), in_=ut[:], in_offset=None),
                        f"xsc{ti % 2}", reads=[but, bd12], writes=[bxs_d], nowaw=True)

        with P.phase():
            pA = Ring(P, "pA", 2, [128, 512], F32, psum=True)
            pY = [P.ps(f"pY{i}", [128, 512], F32) for i in range(4)]
            bpY = [Buf(), Buf()]
            pT = P.ps("pT", [128, 8, 128], BF16)
            bpT = Buf()
            pT2 = P.ps("pT2", [128, 8, 128], BF16)
            bpT2 = Buf()
            wstg = Ring(P, "wstg", 4, [128, 2048], F32)
            w1b = Ring(P, "w1b", 2, [128, 8, 512], BF16)
            w3b = Ring(P, "w3b", 2, [128, 8, 512], BF16)
            w2b = Ring(P, "w2b", 2, [128, 4, 1024], BF16)
            xsb = Ring(P, "xsb", 4, [128, 1024], BF16)
            xsTr = Ring(P, "xsT", 2, [128, 8, 128], BF16)
            hsl = P.sb("hsl", [128, 512], F32)
            bhsl = Buf()
            hhr = Ring(P, "hh", 2, [128, 512], BF16)
            hT = P.sb("hT", [128, 4, 128], BF16)
            bhT = Buf()
            ysb = Ring(P, "ysb", 2, [128, 1024], F32)
            regs = {}
            blocks = [(sbk, sub_) for sbk in range(48) for sub_ in range(4)]
            wsets = {}
            hhs = {}

            def load_weights(sbk):
                w1t, bw1 = w1b.next()
                w3t, bw3 = w3b.next()
                w2t, bw2 = w2b.next()
                wsets[sbk] = (w1t, bw1, w3t, bw3, w2t, bw2)
                idx = 0
                for (wd, wt_, bwt_, eng, is2) in ((w_e1_d, w1t, bw1, "scalar", False), (w_e3_d, w3t, bw3, "scalar", False), (w_e2_d, w2t, bw2, "gpsimd", True)):
                    for hf_ in range(2):
                        stg, bstg = wstg.next()

                        def dma_fn(e, wd=wd, hf_=hf_, stg=stg, is2=is2, idx=idx, sbk=sbk):
                            if idx == 0:
                                if "r" not in regs:
                                    regs["r"] = e.alloc_register("r_exp")
                                    regs["a"] = e.alloc_register("r_expa")
                                    regs["b"] = e.alloc_register("r_expb")
                                e.reg_load(regs["r"], bei[0:1, sbk:sbk + 1])
                                e.reg_mul(regs["a"], regs["r"], 524288)
                                e.reg_add(regs["b"], regs["a"], 262144)
                            rr = regs["a"] if hf_ == 0 else regs["b"]
                            if not is2:
                                src = bass.AP(wd.tensor, rr, [[512, 128], [65536, 4], [1, 512]])
                                ins = e.dma_start(out=stg[:].rearrange("p (c n) -> p c n", n=512), in_=src)
                            else:
                                src = bass.AP(wd.tensor, rr, [[1024, 128], [131072, 2], [1, 1024]])
                                ins = e.dma_start(out=stg[:].rearrange("p (c n) -> p c n", n=1024), in_=src)
                            RH = type(rr)
                            tmpn = [nm for grp in ins.ins.regs_accessed() for nm in grp if "_tmp_" in nm][0]
                            kk = int(tmpn.split("_")[-1])
                            e.free_register(RH(tmpn, rr.engine))
                            e.free_register(RH(f"SP_{rr.name}_snap_{kk - 2}", rr.engine))
                            return ins
                        P.dma("sync", dma_fn, f"wstg{wstg.last}", reads=[bbe], writes=[bstg])
                        if not is2:
                            dst = wt_[:, 4 * hf_:4 * hf_ + 4, :].rearrange("p c n -> p (c n)")
                        else:
                            dst = wt_[:, 2 * hf_:2 * hf_ + 2, :].rearrange("p c n -> p (c n)")
                        if eng == "scalar":
                            S(lambda e, dst=dst, stg=stg: e.copy(out=dst, in_=stg[:]), [bstg], [bwt_], nowaw=(hf_ > 0))
                        else:
                            P.op(eng, lambda e, dst=dst, stg=stg: e.tensor_copy(out=dst, in_=stg[:]), reads=[bstg], writes=[bwt_], nowaw=(hf_ > 0))
                        idx += 1

            def stageA(i):
                sbk, sub_ = blocks[i]
                bk = 4 * sbk + sub_
                if sub_ == 0 and sbk == 0:
                    load_weights(0)
                if sub_ == 1 and sbk + 1 < 48:
                    load_weights(sbk + 1)
                w1t, bw1, w3t, bw3, w2t, bw2 = wsets[sbk]
                xb_, bxb_ = xs_tiles.pop(i)
                for k in range(8):
                    T(lambda e, k=k: e.transpose(out=pT[:, k, :], in_=xb_[:, 128 * k:128 * k + 128], identity=identb[:]), [bxb_, bidb], [bpT], nowaw=(k > 0))
                xsT, bxsT = xsTr.next()
                V(lambda e: e.tensor_copy(out=xsT[:], in_=pT[:]), [bpT], [bxsT])
                p1, bp1 = pA.next()
                p3, bp3 = pA.next()
                for k in range(8):
                    T(lambda e, k=k: e.matmul(p1[:, :], lhsT=xsT[:, k, :], rhs=w1t[:, k, :], start=(k == 0), stop=(k == 7)), [bxsT, bw1], [bp1])
                for k in range(8):
                    T(lambda e, k=k: e.matmul(p3[:, :], lhsT=xsT[:, k, :], rhs=w3t[:, k, :], start=(k == 0), stop=(k == 7)), [bxsT, bw3], [bp3])
                S(lambda e: e.activation(out=hsl[:], in_=p1[:], func=AF.Silu), [bp1], [bhsl])
                hh, bhh = hhr.next()
                V(lambda e: e.tensor_tensor(out=hh[:], in0=p3[:], in1=hsl[:], op=ALU.mult), [bp3, bhsl], [bhh])
                hhs[i] = (hh, bhh)

            def stageB(i):
                sbk, sub_ = blocks[i]
                bk = 4 * sbk + sub_
                w1t, bw1, w3t, bw3, w2t, bw2 = wsets[sbk]
                hh, bhh = hhs.pop(i)
                for f in range(4):
                    T(lambda e, f=f: e.transpose(out=pT2[:, f, :], in_=hh[:, 128 * f:128 * f + 128], identity=identb[:]), [bhh, bidb], [bpT2], nowaw=(f > 0))
                V(lambda e: e.tensor_copy(out=hT[:], in_=pT2[:, 0:4, :]), [bpT2], [bhT])
                par = bk % 2
                for half in range(2):
                    for f in range(4):
                        T(lambda e, half=half, f=f: e.matmul(pY[2 * par + half][:, :], lhsT=hT[:, f, :], rhs=w2t[:, f, 512 * half:512 * half + 512], start=(f == 0), stop=(f == 3)),
                          [bhT, bw2], [bpY[par]], nowaw=not (half == 0 and f == 0))
                yt_, byt_ = ysb.next()
                S(lambda e: e.copy(out=yt_[:, 0:512], in_=pY[2 * par][:, :]), [bpY[par]], [byt_])
                V(lambda e: e.tensor_copy(out=yt_[:, 512:1024], in_=pY[2 * par + 1][:, :]), [bpY[par]], [byt_], nowaw=True)
                P.dma("gpsimd", lambda e: e.dma_start(out=ys_d[128 * bk:128 * bk + 128, :], in_=yt_[:]), f"yst{ysb.last}", reads=[byt_], writes=[bys_d], nowaw=True)

            xs_tiles = {}

            def load_xs(i):
                sbk, sub_ = blocks[i]
                bk = 4 * sbk + sub_
                xb_, bxb_ = xsb.next()
                P.dma("gpsimd", lambda e: e.dma_start(out=xb_[:], in_=xs_d[128 * bk:128 * bk + 128, :]), f"xsb{xsb.last}", reads=[bxs_d], writes=[bxb_])
                xs_tiles[i] = (xb_, bxb_)
            load_xs(0)
            load_xs(1)
            for i in range(len(blocks) + 1):
                if i + 2 < len(blocks):
                    load_xs(i + 2)
                if i < len(blocks):
                    stageA(i)
                if i >= 1:
                    stageB(i - 1)

        with P.phase():
            bct = {}
            bbc = Buf()
            for name, src in (("g2_0", mod_d[0:1, 5120:6144]), ("g2_1", mod_d[1:2, 5120:6144]), ("l2g", lnp_d[2:3, :]), ("l2b", lnp_d[3:4, :])):
                t = P.sb("bc2_" + name, [128, 1024], F32)
                bct[name] = t
                P.dma("sync", lambda e, t=t, src=src: e.dma_start(out=t[:], in_=src.partition_broadcast(128)), "bc2", reads=[bmod], writes=[bbc], nowaw=True)
            y1r = Ring(P, "y1r", 3, [128, 1024], F32)
            y2r = Ring(P, "y2r", 3, [128, 1024], F32)
            x1r = Ring(P, "x1r", 3, [128, 1024], F32)
            outr = Ring(P, "outr", 3, [128, 1024], F32)
            s6, m2, r2 = P.sb("s6c", [128, 2, 6], F32), P.sb("m2c", [128, 2], F32), P.sb("r2c", [128, 1], F32)
            bs6, bm2, br2 = Buf(), Buf(), Buf()
            gt = {}

            def gathers(ti):
                y1, by1 = y1r.next()
                y2, by2 = y2r.next()
                x1, bx1 = x1r.next()
                P.dma("gpsimd", lambda e: e.indirect_dma_start(out=y1[:], out_offset=None, in_=ys_d, in_offset=bass.IndirectOffsetOnAxis(ap=d1i[:, ti:ti + 1], axis=0)),
                      f"y1g{y1r.last}", reads=[bys_d, bd12], writes=[by1])
                P.dma("gpsimd", lambda e: e.indirect_dma_start(out=y2[:], out_offset=None, in_=ys_d, in_offset=bass.IndirectOffsetOnAxis(ap=d2i[:, ti:ti + 1], axis=0)),
                      f"y2g{y2r.last}", reads=[bys_d, bd12], writes=[by2])
                P.dma("sync", lambda e: e.dma_start(out=x1[:], in_=x1_d[128 * ti:128 * ti + 128, :]), f"x1l{x1r.last}", reads=[bx1_d], writes=[bx1])
                gt[ti] = (y1, by1, y2, by2, x1, bx1)

            def combine(ti):
                b = ti // 16
                i = ti % 16
                y1, by1, y2, by2, x1, bx1 = gt.pop(ti)
                V(lambda e: e.tensor_scalar(out=y1[:], in0=y1[:], scalar1=wt1[:, ti:ti + 1], scalar2=None, op0=ALU.mult), [by1, bwt12], [by1])
                V(lambda e: e.scalar_tensor_tensor(out=y1[:], in0=y2[:], scalar=wt2[:, ti:ti + 1], in1=y1[:], op0=ALU.mult, op1=ALU.add), [by1, by2, bwt12], [by1])
                g2 = bct["g2_%d" % b]
                G(lambda e: e.tensor_tensor(out=y1[:], in0=y1[:], in1=g2[:], op=ALU.mult), [by1, bbc], [by1])
                V(lambda e: e.scalar_tensor_tensor(out=y1[:], in0=x1[:], scalar=DN_ALPHA, in1=y1[:], op0=ALU.mult, op1=ALU.add), [by1, bx1], [by1])
                s6, bs6 = s6r.next()
                m2, bm2 = m2r.next()
                r2, br2 = r2r.next()
                ln_stats(y1, by1, s6, bs6, m2, bm2, r2, br2)
                ot, bot = outr.next()
                V(lambda e: e.tensor_scalar(out=ot[:], in0=y1[:], scalar1=m2[:, 0:1], scalar2=r2[:, 0:1], op0=ALU.subtract, op1=ALU.mult), [by1, bm2, br2], [bot])
                G(lambda e: e.tensor_tensor(out=ot[:], in0=ot[:], in1=bct["l2g"][:], op=ALU.mult), [bot, bbc], [bot])
                G(lambda e: e.tensor_tensor(out=ot[:], in0=ot[:], in1=bct["l2b"][:], op=ALU.add), [bot, bbc], [bot])
                P.dma("gpsimd", lambda e: e.dma_start(out=out_d[b, 128 * i:128 * i + 128, :], in_=ot[:]), f"ost{outr.last}", reads=[bot], writes=[bout], nowaw=True)
            s6r = Ring(P, "s6cr", 2, [128, 2, 6], F32)
            m2r = Ring(P, "m2cr", 2, [128, 2], F32)
            r2r = Ring(P, "r2cr", 2, [128, 1], F32)
            gathers(0)
            gathers(1)
            for ti in range(32):
                if ti + 2 < 32:
                    gathers(ti + 2)
                combine(ti)
        P.barrier()
        P.emit()
    return nc


_CONSTS = None


def _prep_shared(inp):
    global _CONSTS
    if _CONSTS is None:
        _CONSTS = make_consts()
    f = lambda a: np.ascontiguousarray(a, dtype=np.float32)
    cwT = np.ascontiguousarray(inp["ml_conv_w"][0].reshape(3, 16, 128).transpose(2, 1, 0), dtype=np.float32)
    cbT = np.ascontiguousarray(inp["ml_conv_b"][0].reshape(16, 128).T, dtype=np.float32)
    return {
        "w_ada": f(inp["w_ada"][0]), "b_ada": f(inp["b_ada"][0].reshape(1, 6144)), "w_in": f(inp["w_in"][0]),
        "bmg": f(inp["b_mgate"][0].reshape(4, 4).T), "cw": cwT, "cb": cbT,
        "rdl": f(inp["ret_decay_logit"][0].reshape(1, 8)),
        "w_rb": f(inp["w_ret_branch"][0]), "w_mb": f(inp["w_ml_branch"][0]), "w_out": f(inp["w_out"][0]),
        "lnp": f(np.stack([inp["ln1_g"][0], inp["ln1_b"][0], inp["ln2_g"][0], inp["ln2_b"][0]], 0)),
        "w_rt": f(np.concatenate([inp["w_rg"][0], inp["w_re"][0]], 1)),
        "b_rt": f(np.concatenate([inp["b_rg"][0], inp["b_re"][0]], 0).reshape(1, 36)),
        "w_e1": f(inp["w_e1"][0]), "w_e3": f(inp["w_e3"][0]), "w_e2": f(inp["w_e2"][0]),
        "consts": _CONSTS,
    }


def _core_inputs(inp, shared, c):
    x = np.asarray(inp["x"], dtype=np.float32)
    ctx = np.asarray(inp["ctx"], dtype=np.float32)
    cc = np.asarray(inp["c"], dtype=np.float32)
    c_ctx = np.asarray(inp["c_ctx"], dtype=np.float32)
    vecs = np.stack([cc[2 * c], cc[2 * c + 1], c_ctx], 0)
    cT = np.ascontiguousarray(vecs.reshape(3, 8, 128).transpose(2, 1, 0))
    m = dict(shared)
    m["x"] = np.ascontiguousarray(x[2 * c:2 * c + 2])
    m["ctx"] = np.ascontiguousarray(ctx[2 * c:2 * c + 2])
    m["cT"] = cT
    return m


def kernel(**inputs):
    nc = build()
    shared = _prep_shared(inputs)
    in_maps = [_core_inputs(inputs, shared, c) for c in range(NCORES)]
    res = run_bass_kernel_spmd(nc, in_maps, core_ids=list(range(NCORES)))
    out = np.concatenate([np.asarray(r["out"]) for r in res.results], axis=0)
    return out.astype(np.float32)
```

```python
import contextlib
import math
import numpy as np
import concourse.bass as bass
import concourse.mybir as mybir
from concourse.bass_utils import run_bass_kernel_spmd

F32 = mybir.dt.float32
BF16 = mybir.dt.bfloat16
I32 = mybir.dt.int32
AF = mybir.ActivationFunctionType
ALU = mybir.AluOpType
AX = mybir.AxisListType

ENGS = ["tensor", "vector", "scalar", "gpsimd", "sync"]
LN_EPS = 1e-5
DN_ALPHA = 2.0 ** 0.25
NEG = -30000.0
LN16 = math.log(16.0)
NCORES = 8


import types


def _freeze(fn):
    if fn is None or fn.__closure__ is None:
        return fn
    cells = []
    for c in fn.__closure__:
        try:
            cells.append(types.CellType(c.cell_contents))
        except ValueError:
            cells.append(c)
    return types.FunctionType(fn.__code__, fn.__globals__, fn.__name__, fn.__defaults__, tuple(cells))


class Buf:
    __slots__ = ("w", "r", "pr")

    def __init__(self):
        self.w = []
        self.r = []
        self.pr = []


class Prog:
    def __init__(self, nc):
        self.nc = nc
        self.es = contextlib.ExitStack()
        self.ops = {e: [] for e in ENGS}
        self.cnt = {e: 0 for e in ENGS}
        self.dsem = {}
        self.known = {e: {} for e in ENGS}
        self.semobj = {}
        self.stack = [self.es]

    def _nm(self, name):
        self.uid = getattr(self, "uid", 0) + 1
        return f"t{self.uid}_{name}"

    def sb(self, name, shape, dt):
        return self.stack[-1].enter_context(self.nc.sbuf_tensor(self._nm(name), list(shape), dt))

    def ps(self, name, shape, dt=F32):
        return self.stack[-1].enter_context(self.nc.psum_tensor(self._nm(name), list(shape), dt))

    @contextlib.contextmanager
    def phase(self):
        es = contextlib.ExitStack()
        self.stack.append(es)
        with es:
            yield
            self.barrier()
        self.stack.pop()

    def _deps(self, eng, reads, writes, nowaw):
        deps = {}

        def add(ev):
            k, v = ev
            if deps.get(k, 0) < v:
                deps[k] = v
        for b in reads:
            for ev in b.w:
                add(ev)
        for b in writes:
            for ev in b.r:
                add(ev)
            if nowaw:
                for ev in b.pr:
                    add(ev)
            else:
                for ev in b.w:
                    add(ev)
        out = []
        kn = self.known[eng]
        for k, v in deps.items():
            if k == ("E", "tensor") and eng == "tensor":
                continue
            if kn.get(k, 0) >= v:
                continue
            kn[k] = v
            out.append((k, v))
        return out

    def _commit(self, ev, reads, writes, nowaw):
        for b in reads:
            b.r.append(ev)
        for b in writes:
            if nowaw:
                b.w.append(ev)
            else:
                b.pr = b.r
                b.r = []
                b.w = [ev]

    def op(self, eng, fn, reads=(), writes=(), nowaw=False):
        waits = self._deps(eng, reads, writes, nowaw)
        self.cnt[eng] += 1
        ev = (("E", eng), self.cnt[eng])
        self.ops[eng].append((waits, _freeze(fn), ("E", eng), 1))
        self._commit(ev, reads, writes, nowaw)
        return ev

    def dma(self, eng, fn, slot, reads=(), writes=(), nowaw=False):
        waits = self._deps(eng, reads, writes, nowaw)
        self.dsem[slot] = self.dsem.get(slot, 0) + 16
        ev = (("D", slot), self.dsem[slot])
        self.ops[eng].append((waits, _freeze(fn), ("D", slot), 16))
        self._commit(ev, reads, writes, nowaw)
        return ev

    def barrier(self):
        for e in ENGS:
            waits = []
            kn = self.known[e]
            for e2 in ENGS:
                k = ("E", e2)
                v = self.cnt[e2]
                if e2 != e and v > 0 and kn.get(k, 0) < v:
                    kn[k] = v
                    waits.append((k, v))
            for slot, v in self.dsem.items():
                k = ("D", slot)
                if kn.get(k, 0) < v:
                    kn[k] = v
                    waits.append((k, v))
            if waits:
                self.ops[e].append((waits, None, None, 0))

    def emit(self):
        nc = self.nc
        for e in ENGS:
            self.semobj[("E", e)] = self.es.enter_context(nc.semaphore("e_" + e))
        for slot in self.dsem:
            self.semobj[("D", slot)] = self.es.enter_context(nc.semaphore("d_" + str(slot)))
        block = self.es.enter_context(nc.Block())
        for e in ENGS:
            ops = self.ops[e]
            if not ops:
                continue

            def body(engine, ops=ops):
                for waits, fn, sk, inc in ops:
                    for k, v in waits:
                        engine.wait_ge(self.semobj[k], v)
                    if fn is not None:
                        fn(engine).then_inc(self.semobj[sk], inc)
            getattr(block, e)(body)


class Ring:
    def __init__(self, P, name, n, shape, dt, psum=False):
        self.t = [(P.ps if psum else P.sb)(f"{name}{i}", shape, dt) for i in range(n)]
        self.b = [Buf() for _ in range(n)]
        self.i = 0
        self.n = n

    def next(self):
        i = self.i
        self.i = (i + 1) % self.n
        self.last = i
        return self.t[i], self.b[i]


C_RQ, C_RK, C_RV, C_RG, C_MQ, C_MK, C_MV, C_MO, C_MG, C_GR, C_GM = (
    0, 1024, 2048, 3072, 4096, 5120, 6144, 7168, 8192, 8208, 9232)

K_ID = 0
K_IOTA = 128
K_PF = 640
K_PB = 658
K_COSR = 676
K_SINR = 708
K_COSC = 740
K_SINC = 804
K_TRI = 868
K_BLK = 996
K_MF = 1092
K_MB = 1988
K_ONE = 2884
NCONST = 3012


def make_consts():
    c = np.zeros((128, NCONST), np.float32)
    p = np.arange(128)
    c[:, K_ID:K_ID + 128] = np.eye(128, dtype=np.float32)
    c[:, K_IOTA:K_IOTA + 512] = np.arange(512, dtype=np.float32)[None, :]
    for jf in range(18):
        c[:, K_PF + jf] = 128 * jf + p
        c[:, K_PB + jf] = (2048 + 128 * jf + p) if jf < 2 else (128 * (jf - 2) + p)
    q = (p % 64).astype(np.float64)
    freq = 10000.0 ** (-q / 64.0)
    sgn = np.where(p < 64, -1.0, 1.0)
    rows = np.arange(32, dtype=np.float64)
    cols = np.arange(64, dtype=np.float64)
    c[:, K_COSR:K_COSR + 32] = np.cos(freq[:, None] * rows[None, :])
    c[:, K_SINR:K_SINR + 32] = sgn[:, None] * np.sin(freq[:, None] * rows[None, :])
    c[:, K_COSC:K_COSC + 64] = np.cos(freq[:, None] * cols[None, :])
    c[:, K_SINC:K_SINC + 64] = sgn[:, None] * np.sin(freq[:, None] * cols[None, :])
    c[:, K_TRI:K_TRI + 128] = (p[:, None] < p[None, :]).astype(np.float32)
    c[:, K_BLK:K_BLK + 96] = 512.0 * np.arange(96, dtype=np.float32)[None, :]
    cc = np.arange(896)[None, :]
    c[:, K_MF:K_MF + 896] = np.where(cc - 384 - p[:, None] >= 0, 0.0, NEG)
    c[:, K_MB:K_MB + 896] = np.where(p[:, None] - (cc - 384) > 0, 0.0, NEG)
    c[:, K_ONE:K_ONE + 128] = 1.0
    return c


def build(dbg=None):
    nc = bass.Bass("TRN2", target_bir_lowering=False)
    P = Prog(nc)

    def din(name, shape, dt=F32):
        return nc.dram_tensor(name, list(shape), dt, kind="ExternalInput").ap()

    def dscr(name, shape, dt=F32):
        return nc.dram_tensor(name, list(shape), dt).ap()

    x_d = din("x", [2, 2048, 1024])
    ctx_d = din("ctx", [2, 256, 1024])
    cT_d = din("cT", [128, 8, 3])
    w_ada_d = din("w_ada", [1024, 6144])
    b_ada_d = din("b_ada", [1, 6144])
    w_in_d = din("w_in", [1024, 10256])
    bmg_d = din("bmg", [4, 4])
    cw_d = din("cw", [128, 16, 3])
    cb_d = din("cb", [128, 16])
    rdl_d = din("rdl", [1, 8])
    w_rb_d = din("w_rb", [1024, 1024])
    w_mb_d = din("w_mb", [1024, 1024])
    w_out_d = din("w_out", [1024, 1024])
    lnp_d = din("lnp", [4, 1024])
    w_rt_d = din("w_rt", [1024, 36])
    b_rt_d = din("b_rt", [1, 36])
    w_e1_d = din("w_e1", [32, 1024, 512])
    w_e3_d = din("w_e3", [32, 1024, 512])
    w_e2_d = din("w_e2", [32, 512, 1024])
    consts_d = din("consts", [128, NCONST])
    out_d = nc.dram_tensor("out", [2, 2048, 1024], F32, kind="ExternalOutput").ap()

    mod_d = dscr("mod_s", [3, 6144])
    r_d = dscr("r_s", [2, 8, 128, 2048], BF16)
    y_d = dscr("y_s", [8, 128, 2048], BF16)
    x1_d = dscr("x1_s", [4096, 1024])
    u2_d = dscr("u2_s", [4096, 1024], BF16)
    xs_d = dscr("xs_s", [24576, 1024], BF16)
    ys_d = dscr("ys_s", [24576, 1024])
    dbg_outs = {}
    if dbg:
        for name, shape in dbg.items():
            dbg_outs[name] = nc.dram_tensor(name, list(shape), F32, kind="ExternalOutput").ap()

    bmod = Buf()
    br_d = [Buf(), Buf()]
    bx1_d = Buf()
    by_d = Buf()
    bu2_d = Buf()
    bxs_d = Buf()
    bys_d = Buf()
    bout = Buf()
    bdbg = Buf()

    with P.es:
        cst = P.sb("cst", [128, NCONST], F32)
        bcst = Buf()
        P.dma("sync", lambda e: e.dma_start(out=cst[:], in_=consts_d), "cst", writes=[bcst])
        identb = P.sb("identb", [128, 128], BF16)
        bidb = Buf()
        P.op("vector", lambda e: e.tensor_copy(out=identb[:], in_=cst[:, K_ID:K_ID + 128]), reads=[bcst], writes=[bidb])
        identf = cst[:, K_ID:K_ID + 128]
        trib = P.sb("trib", [128, 128], BF16)
        onesb = P.sb("onesb", [128, 128], BF16)
        P.op("vector", lambda e: e.tensor_copy(out=trib[:], in_=cst[:, K_TRI:K_TRI + 128]), reads=[bcst], writes=[bidb], nowaw=True)
        P.op("vector", lambda e: e.tensor_copy(out=onesb[:], in_=cst[:, K_ONE:K_ONE + 128]), reads=[bcst], writes=[bidb], nowaw=True)
        uT = P.sb("uT", [128, 8, 2304], BF16)
        buT = [Buf() for _ in range(5)]
        modc = P.sb("modc", [128, 6, 8], F32)
        bmodc = Buf()
        lgc = P.sb("lgc", [128, 8], F32)
        nlgc = P.sb("nlgc", [128, 8], F32)
        blgc = Buf()
        cw = P.sb("cw", [128, 16, 3], F32)
        cbias = P.sb("cbias", [128, 16], F32)
        bcw = Buf()
        P.dma("sync", lambda e: e.dma_start(out=cw[:], in_=cw_d), "cw", writes=[bcw])
        P.dma("sync", lambda e: e.dma_start(out=cbias[:], in_=cb_d), "cw", writes=[bcw], nowaw=True)
        bmg = P.sb("bmg", [4, 4], F32)
        bbmg = Buf()
        P.dma("sync", lambda e: e.dma_start(out=bmg[:], in_=bmg_d), "bmg", writes=[bbmg])
        logits = P.sb("logits", [128, 32, 36], F32)
        blog = Buf()
        wrt = P.sb("wrt", [128, 8, 36], F32)
        brt = P.sb("brt", [128, 36], F32)
        bwrt = Buf()
        P.dma("sync", lambda e: e.dma_start(out=wrt[:], in_=w_rt_d.rearrange("(c p) n -> p c n", p=128)), "wrt", writes=[bwrt])
        P.dma("sync", lambda e: e.dma_start(out=brt[:], in_=b_rt_d.partition_broadcast(128)), "wrt", writes=[bwrt], nowaw=True)

        P.dma("sync", lambda e: e.dma_start(out=lgc[:], in_=rdl_d.partition_broadcast(128)), "lgc", writes=[blgc])
        P.op("scalar", lambda e: e.activation(out=nlgc[:], in_=lgc[:], func=AF.Exp, scale=-1.0), reads=[blgc], writes=[blgc])
        P.op("scalar", lambda e: e.activation(out=nlgc[:], in_=nlgc[:], func=AF.Ln, bias=1.0, scale=1.0), reads=[blgc], writes=[blgc])
        P.op("vector", lambda e: e.tensor_scalar(out=lgc[:], in0=nlgc[:], scalar1=-1.0, scalar2=None, op0=ALU.mult), reads=[blgc], writes=[blgc])

        with P.phase():
            cT = P.sb("cT", [128, 8, 3], F32)
            bcT = Buf()
            P.dma("sync", lambda e: e.dma_start(out=cT[:], in_=cT_d), "cT", writes=[bcT])
            P.op("scalar", lambda e: e.activation(out=cT[:], in_=cT[:], func=AF.Silu), reads=[bcT], writes=[bcT])
            bada = P.sb("bada", [3, 6144], F32)
            bbada = Buf()
            P.dma("sync", lambda e: e.dma_start(out=bada[:], in_=b_ada_d.partition_broadcast(3)), "bada", writes=[bbada])
            modsb = P.sb("modsb", [3, 6144], F32)
            bmodsb = Buf()
            wa = Ring(P, "wa", 2, [128, 8, 512], F32)
            pmod = Ring(P, "pmod", 2, [128, 512], F32, psum=True)
            for cg in range(12):
                wt, bw = wa.next()
                P.dma("sync", lambda e, wt=wt, cg=cg: e.dma_start(
                    out=wt[:], in_=w_ada_d[:, 512 * cg:512 * cg + 512].rearrange("(c p) n -> p c n", p=128)),
                    f"wa{cg % 2}", writes=[bw])
                pt, bp = pmod.next()
                for k in range(8):
                    P.op("tensor", lambda e, pt=pt, wt=wt, k=k: e.matmul(pt[0:3, :], lhsT=cT[:, k, :], rhs=wt[:, k, :], start=(k == 0), stop=(k == 7)),
                         reads=[bcT, bw], writes=[bp])
                P.op("vector", lambda e, pt=pt, cg=cg: e.tensor_tensor(out=modsb[:, 512 * cg:512 * cg + 512], in0=pt[0:3, :], in1=bada[:, 512 * cg:512 * cg + 512], op=ALU.add),
                     reads=[bp, bbada], writes=[bmodsb], nowaw=True)
            P.dma("gpsimd", lambda e: e.dma_start(out=mod_d, in_=modsb[:]), "mod", reads=[bmodsb], writes=[bmod])
            for v in range(3):
                for which in range(2):
                    P.dma("gpsimd", lambda e, v=v, which=which: e.dma_start(
                        out=modc[:, 2 * v + which, :], in_=mod_d[v, 1024 * which:1024 * which + 1024].rearrange("(c p) -> p c", p=128),
                        allow_slow_non_contiguous=True), "modc", reads=[bmod], writes=[bmodc], nowaw=True)
            for v in range(3):
                P.op("vector", lambda e, v=v: e.tensor_scalar(out=modc[:, 2 * v + 1, :], in0=modc[:, 2 * v + 1, :], scalar1=1.0, scalar2=None, op0=ALU.add),
                     reads=[bmodc], writes=[bmodc])


        selp = P.sb("selp", [4, 4, 128], F32)
        seln = P.sb("seln", [4, 4, 128], F32)
        bsel = Buf()
        P.op("vector", lambda e: e.tensor_copy(out=selp[:], in_=cst[0:4, K_ID:K_ID + 4].unsqueeze(2).broadcast_to([4, 4, 128])), reads=[bcst], writes=[bsel])
        P.op("vector", lambda e: e.tensor_scalar(out=seln[:], in0=selp[:], scalar1=-1.0, scalar2=None, op0=ALU.mult), reads=[bsel], writes=[bsel])

        def V(fn, r=(), w=(), **kw):
            return P.op("vector", fn, reads=r, writes=w, **kw)

        def S(fn, r=(), w=(), **kw):
            return P.op("scalar", fn, reads=r, writes=w, **kw)

        def G(fn, r=(), w=(), **kw):
            return P.op("gpsimd", fn, reads=r, writes=w, **kw)

        def T(fn, r=(), w=(), **kw):
            return P.op("tensor", fn, reads=r, writes=w, **kw)

        def ln_stats(src, bsrc, s6, bs6, m, bm, r, brs):
            V(lambda e: e.bn_stats(out=s6[:, 0, :], in_=src[:, 0:512]), [bsrc], [bs6])
            V(lambda e: e.bn_stats(out=s6[:, 1, :], in_=src[:, 512:1024]), [bsrc], [bs6], nowaw=True)
            V(lambda e: e.bn_aggr(out=m[:], in_=s6[:].rearrange("p a b -> p (a b)")), [bs6], [bm])
            S(lambda e: e.activation(out=r[:], in_=m[:, 1:2], func=AF.Sqrt, bias=LN_EPS, scale=1.0), [bm], [brs])
            V(lambda e: e.reciprocal(out=r[:], in_=r[:]), [brs], [brs])

        def make_wload(wst, wbf):
            def load_w(src, c0, swap=False):
                wt, bw = wst.next()
                P.dma("sync", lambda e: e.dma_start(out=wt[:], in_=src[:, c0:c0 + 128].rearrange("(c p) n -> p c n", p=128)),
                      f"wst{wst.last}", writes=[bw])
                wb, bwb = wbf.next()
                S(lambda e: e.copy(out=wb[:], in_=wt[:]), [bw], [bwb])
                if swap:
                    wb2, bwb2 = wbf.next()
                    S(lambda e: e.copy(out=wb2[:, :, 0:64], in_=wt[:, :, 64:128]), [bw], [bwb2])
                    S(lambda e: e.copy(out=wb2[:, :, 64:128], in_=wt[:, :, 0:64]), [bw], [bwb2], nowaw=True)
                    return (wb, bwb), (wb2, bwb2)
                return wb, bwb
            return load_w

        for b in range(2):
            with P.phase():
                T2 = [P.sb(f"T2_{d}", [4, 2304], F32) for d in range(2)]
                bT2 = [Buf(), Buf()]
                acol = [P.sb(f"acol{d}", [128, 18, 4], F32) for d in range(2)]
                bacol = [Buf(), Buf()]
                em = [P.sb(f"em{d}", [128, 4], F32) for d in range(2)]
                bem = [Buf(), Buf()]
                pA = Ring(P, "pA", 3, [128, 512], F32, psum=True)
                pO = [P.ps(f"pO{i}", [128, 512], F32) for i in range(4)]
                bpO = [Buf() for _ in range(4)]
                pT = P.ps("pT", [128, 8, 128], BF16)
                bpT = Buf()
                with P.phase():
                    xr = Ring(P, "xr", 2, [128, 1024], F32)
                    xn = Ring(P, "xn", 2, [128, 1024], BF16)
                    st6 = Ring(P, "st6", 2, [128, 2, 6], F32)
                    mv = Ring(P, "mv", 2, [128, 2], F32)
                    rs = Ring(P, "rs", 2, [128, 1], F32)
                    tmpm = Ring(P, "tmpm", 2, [128, 8, 128], F32)
                    for j in range(18):
                        xt, bx = xr.next()
                        src = ctx_d[b, 128 * j:128 * j + 128, :] if j < 2 else x_d[b, 128 * (j - 2):128 * (j - 2) + 128, :]
                        P.dma("sync", lambda e, xt=xt, src=src: e.dma_start(out=xt[:], in_=src), f"xr{j % 2}", writes=[bx])
                        s6, bs6 = st6.next()
                        m, bm = mv.next()
                        r, brs = rs.next()
                        ln_stats(xt, bx, s6, bs6, m, bm, r, brs)
                        xb, bxb = xn.next()
                        V(lambda e, xb=xb, xt=xt, m=m, r=r: e.tensor_scalar(out=xb[:], in0=xt[:], scalar1=m[:, 0:1], scalar2=r[:, 0:1], op0=ALU.subtract, op1=ALU.mult),
                          [bx, bm, brs], [bxb])
                        for k in range(8):
                            T(lambda e, xb=xb, k=k: e.transpose(out=pT[:, k, :], in_=xb[:, 128 * k:128 * k + 128], identity=identb[:]),
                              [bxb, bidb], [bpT], nowaw=(k > 0))
                        v = 2 if j < 2 else b
                        tm, btm = tmpm.next()
                        V(lambda e, tm=tm, v=v: e.tensor_tensor(out=tm[:], in0=pT[:], in1=modc[:, 2 * v + 1, :].unsqueeze(2).broadcast_to([128, 8, 128]), op=ALU.mult),
                          [bpT, bmodc], [btm])
                        ch = 0 if j < 2 else 1 + (j - 2) // 4
                        G(lambda e, tm=tm, v=v, j=j: e.tensor_tensor(out=uT[:, :, 128 * j:128 * j + 128], in0=tm[:], in1=modc[:, 2 * v, :].unsqueeze(2).broadcast_to([128, 8, 128]), op=ALU.add),
                          [btm, bmodc], [buT[ch]], nowaw=True)

                    wgs = P.sb("wgs", [128, 8, 16], F32)
                    wgb = P.sb("wgb", [128, 8, 16], BF16)
                    bwg = Buf()
                    P.dma("sync", lambda e: e.dma_start(out=wgs[:], in_=w_in_d[:, C_MG:C_MG + 16].rearrange("(c p) n -> p c n", p=128)), "wg", writes=[bwg])
                    V(lambda e: e.tensor_copy(out=wgb[:], in_=wgs[:]), [bwg], [bwg])
                    T0 = P.sb("T0", [4, 2304], F32)
                    T1 = P.sb("T1", [4, 2304], F32)
                    ones4 = P.sb("ones4", [4, 2304], F32)
                    bT0 = Buf()
                    bT1 = Buf()
                    bones4 = Buf()
                    V(lambda e: e.memset(ones4[:], 1.0), [], [bones4])
                    mx = P.sb("mx", [4, 2], F32)
                    bmx = Buf()
                    mrow = P.sb("mrow", [4, 128], F32)
                    bmrow = Buf()
                    chunks = [(0, 256)] + [(256 + 512 * n, 512) for n in range(4)]
                    for d in range(2):
                        for gi, Tt, bT in ((2 * d, T0, bT0), (2 * d + 1, T1, bT1)):
                            for ci, (c0, cl) in enumerate(chunks):
                                pt, bp = pA.next()
                                for k in range(8):
                                    T(lambda e, pt=pt, k=k, gi=gi, c0=c0, cl=cl: e.matmul(pt[0:4, 0:cl], lhsT=wgb[:, k, 4 * gi:4 * gi + 4], rhs=uT[:, k, c0:c0 + cl], start=(k == 0), stop=(k == 7)),
                                      [bwg, buT[ci]], [bp])
                                if d == 0:
                                    o0 = c0
                                else:
                                    o0 = 2048 if ci == 0 else c0 - 256
                                V(lambda e, pt=pt, Tt=Tt, gi=gi, o0=o0, cl=cl: e.tensor_scalar(out=Tt[:, o0:o0 + cl], in0=pt[0:4, 0:cl], scalar1=bmg[:, gi:gi + 1], scalar2=None, op0=ALU.add),
                                  [bp, bbmg], [bT], nowaw=(ci > 0))
                        S(lambda e: e.activation(out=T1[:], in_=T1[:], func=AF.Exp, scale=-1.0), [bT1], [bT1])
                        S(lambda e: e.activation(out=T1[:], in_=T1[:], func=AF.Ln, bias=1.0, scale=1.0), [bT1], [bT1])
                        V(lambda e, d=d: e.tensor_tensor_scan(out=T2[d][:], data0=ones4[:], data1=T1[:], initial=0.0, op0=ALU.mult, op1=ALU.add),
                          [bT1, bones4], [bT2[d]])
                        if d == 1:
                            V(lambda e: e.tensor_tensor(out=T2[1][:], in0=T2[1][:], in1=T1[:], op=ALU.subtract), [bT2[1], bT1], [bT2[1]])
                        V(lambda e: e.tensor_reduce(out=mx[:, 0:1], in_=T0[:], axis=AX.X, op=ALU.max), [bT0], [bmx])
                        V(lambda e: e.tensor_scalar(out=mx[:, 1:2], in0=mx[:, 0:1], scalar1=LN16, scalar2=None, op0=ALU.add), [bmx], [bmx])
                        V(lambda e, d=d: e.scalar_tensor_tensor(out=T0[:], in0=T0[:], scalar=mx[:, 1:2], in1=T2[d][:], op0=ALU.subtract, op1=(ALU.add if d == 0 else ALU.subtract)),
                          [bT0, bmx, bT2[d]], [bT0])
                        pt, bp = pA.next()
                        for jo in range(18):
                            jf = jo if d == 0 else (jo + 2 if jo < 16 else jo - 16)
                            T(lambda e, pt=pt, jo=jo, jf=jf: e.transpose(out=pt[:, 4 * jf:4 * jf + 4], in_=T0[0:4, 128 * jo:128 * jo + 128], identity=identf[0:4, 0:4]),
                              [bT0, bcst], [bp], nowaw=(jo > 0))
                        S(lambda e, pt=pt, d=d: e.copy(out=acol[d][:].rearrange("p a b -> p (a b)"), in_=pt[:, 0:72]), [bp], [bacol[d]])
                        V(lambda e: e.tensor_scalar(out=mrow[:], in0=ones4[:, 0:128], scalar1=mx[:, 0:1], scalar2=-1.0, op0=ALU.mult, op1=ALU.mult), [bmx, bones4], [bmrow])
                        pt, bp = pA.next()
                        T(lambda e, pt=pt: e.transpose(out=pt[:, 0:4], in_=mrow[0:4, :], identity=identf[0:4, 0:4]), [bmrow, bcst], [bp])
                        S(lambda e, pt=pt, d=d: e.activation(out=em[d][:], in_=pt[:, 0:4], func=AF.Exp), [bp], [bem[d]])

                if dbg and "uT" in dbg and b == 0:
                    du = P.sb("dbg_u", [128, 2304], F32)
                    bdu = Buf()
                    V(lambda e: e.tensor_copy(out=du[:], in_=uT[:, 0, :]), buT, [bdu])
                    P.dma("gpsimd", lambda e: e.dma_start(out=dbg_outs["uT"], in_=du[:]), "dbg", reads=[bdu], writes=[bdbg], nowaw=True)
                    da = P.sb("dbg_a", [128, 2, 76], F32)
                    bda = Buf()
                    for d in range(2):
                        V(lambda e, d=d: e.tensor_copy(out=da[:, d, 0:72], in_=acol[d][:].rearrange("p a b -> p (a b)")), [bacol[d]], [bda], nowaw=True)
                        V(lambda e, d=d: e.tensor_copy(out=da[:, d, 72:76], in_=em[d][:]), [bem[d]], [bda], nowaw=True)
                    P.dma("gpsimd", lambda e: e.dma_start(out=dbg_outs["acol"], in_=da[:].rearrange("p a b -> p (a b)")), "dbg", reads=[bda], writes=[bdbg], nowaw=True)
                    P.dma("gpsimd", lambda e: e.dma_start(out=dbg_outs["T2"][0:4, :], in_=T2[0][:]), "dbg", reads=[bT2[0]], writes=[bdbg], nowaw=True)
                    P.dma("gpsimd", lambda e: e.dma_start(out=dbg_outs["T2"][4:8, :], in_=T2[1][:]), "dbg", reads=[bT2[1]], writes=[bdbg], nowaw=True)

                with P.phase():
                    wst = Ring(P, "wst", 3, [128, 8, 128], F32)
                    wbf = Ring(P, "wbf", 4, [128, 8, 128], BF16)
                    load_w = make_wload(wst, wbf)
                    wvt = P.sb("wvt", [128, 8, 256], BF16)
                    bwv = Buf()
                    qT = P.sb("qT", [128, 2, 2048], BF16)
                    kT = P.sb("kT", [128, 2, 2304], BF16)
                    vv = P.sb("vv", [128, 18, 257], BF16)
                    gT = P.sb("gT", [128, 2, 2048], BF16)
                    bq, bk, bv, bg = Buf(), Buf(), Buf(), Buf()
                    V(lambda e: e.memset(vv[:, :, 256:257], 1.0), [], [bv])
                    raw = P.sb("raw", [128, 2050], F32)
                    rawc = P.sb("rawc", [128, 258], F32)
                    acc = P.sb("acc", [128, 2048], F32)
                    braw, brawc, bacc = Buf(), Buf(), Buf()
                    V(lambda e: e.memset(raw[:], 0.0), [], [braw])
                    V(lambda e: e.memset(rawc[:], 0.0), [], [brawc])
                    tA = Ring(P, "tA", 2, [128, 512], F32)
                    tB = Ring(P, "tB", 2, [128, 512], F32)
                    rowr = Ring(P, "rowr", 2, [128, 512], F32)
                    prer = Ring(P, "prer", 2, [128, 512], F32)
                    Dr = Ring(P, "Dr", 3, [128, 512], BF16)
                    atr = Ring(P, "atr", 3, [128, 512], BF16)
                    mhalf = P.sb("mhalf", [128, 4], F32)
                    bmhalf = Buf()
                    V(lambda e: e.memset(mhalf[:], -0.5), [], [bmhalf])
                    acolr = [P.sb(f"acolr{d}", [128, 18], F32) for d in range(2)]
                    bacolr = [Buf(), Buf()]
                    rawr = Ring(P, "rawr", 2, [128, 4, 257], F32)
                    dn = P.sb("dn", [128, 4], F32)
                    bdn = Buf()
                    hf = P.sb("hf", [128, 4, 256], F32)
                    hs = P.sb("hs", [128, 4, 256], F32)
                    hn = P.sb("hn", [128, 4, 256], BF16)
                    bhf, bhs, bhn = Buf(), Buf(), Buf()
                    s6h = P.sb("s6h", [128, 4, 6], F32)
                    mvh = P.sb("mvh", [128, 4, 2], F32)
                    rsh = P.sb("rsh", [128, 4], F32)
                    bs6h, bmvh, brsh = Buf(), Buf(), Buf()
                    ofm = Ring(P, "ofm", 2, [128, 2, 512], BF16)

                    def proj_fm(wb, bwb, c0, cl, ci):
                        pt, bp = pA.next()
                        for k in range(8):
                            T(lambda e, k=k: e.matmul(pt[:, 0:cl], lhsT=wb[:, k, :], rhs=uT[:, k, c0:c0 + cl], start=(k == 0), stop=(k == 7)),
                              [bwb, buT[ci]], [bp])
                        return pt, bp

                    def proj_v(base):
                        for vu in range(2):
                            wt, bw = wst.next()
                            P.dma("sync", lambda e, wt=wt, vu=vu: e.dma_start(out=wt[:], in_=w_in_d[:, base + 128 * vu:base + 128 * vu + 128].rearrange("(c p) n -> p c n", p=128)),
                                  f"wst{wst.last}", writes=[bw])
                            S(lambda e, wt=wt, vu=vu: e.copy(out=wvt[:, :, 128 * vu:128 * vu + 128], in_=wt[:]), [bw], [bwv], nowaw=(vu > 0))
                        for q2 in range(9):
                            pt, bp = pA.next()
                            for jj in range(2):
                                j = 2 * q2 + jj
                                ci = 0 if j < 2 else 1 + (j - 2) // 4
                                for k in range(8):
                                    T(lambda e, pt=pt, jj=jj, j=j, k=k: e.matmul(pt[:, 256 * jj:256 * jj + 256], lhsT=uT[:, k, 128 * j:128 * j + 128], rhs=wvt[:, k, :], start=(k == 0), stop=(k == 7)),
                                      [bwv, buT[ci]], [bp], nowaw=not (jj == 0 and k == 0))
                            S(lambda e, pt=pt, q2=q2: e.copy(out=vv[:, 2 * q2:2 * q2 + 2, 0:256], in_=pt[:, :].rearrange("p (a b) -> p a b", b=256)),
                              [bp], [bv], nowaw=True)

                    def proj_g(base, func):
                        for dc in range(2):
                            wb, bwb = load_w(w_in_d, base + 128 * dc)
                            for n in range(4):
                                pt, bp = proj_fm(wb, bwb, 256 + 512 * n, 512, n + 1)
                                S(lambda e, pt=pt, dc=dc, n=n: e.activation(out=gT[:, dc, 512 * n:512 * n + 512], in_=pt[:], func=func), [bp], [bg], nowaw=True)

                    def proj_rot(base, dstT, bdst, is_k):
                        for dc in range(2):
                            (wb, bwb), (wb2, bwb2) = load_w(w_in_d, base + 128 * dc, swap=True)
                            if is_k:
                                pt, bp = proj_fm(wb, bwb, 0, 256, 0)
                                S(lambda e, pt=pt, dc=dc: e.copy(out=dstT[:, dc, 0:256], in_=pt[:, 0:256]), [bp], [bdst], nowaw=True)
                            for n in range(4):
                                p1, bp1 = proj_fm(wb, bwb, 256 + 512 * n, 512, n + 1)
                                p2, bp2 = proj_fm(wb2, bwb2, 256 + 512 * n, 512, n + 1)
                                if dc == 0:
                                    cosv = cst[:, K_COSR + 8 * n:K_COSR + 8 * n + 8].unsqueeze(2).broadcast_to([128, 8, 64])
                                    sinv = cst[:, K_SINR + 8 * n:K_SINR + 8 * n + 8].unsqueeze(2).broadcast_to([128, 8, 64])
                                else:
                                    cosv = cst[:, K_COSC:K_COSC + 64].unsqueeze(1).broadcast_to([128, 8, 64])
                                    sinv = cst[:, K_SINC:K_SINC + 64].unsqueeze(1).broadcast_to([128, 8, 64])
                                t1, bt1 = tA.next()
                                t2, bt2 = tB.next()
                                V(lambda e, t1=t1, p1=p1, cosv=cosv: e.tensor_tensor(out=t1[:].rearrange("p (a b) -> p a b", b=64), in0=p1[:].rearrange("p (a b) -> p a b", b=64), in1=cosv, op=ALU.mult),
                                  [bp1, bcst], [bt1])
                                V(lambda e, t2=t2, p2=p2, sinv=sinv: e.tensor_tensor(out=t2[:].rearrange("p (a b) -> p a b", b=64), in0=p2[:].rearrange("p (a b) -> p a b", b=64), in1=sinv, op=ALU.mult),
                                  [bp2, bcst], [bt2])
                                off = (256 if is_k else 0) + 512 * n
                                G(lambda e, t1=t1, t2=t2, dc=dc, off=off: e.tensor_tensor(out=dstT[:, dc, off:off + 512], in0=t1[:], in1=t2[:], op=ALU.add),
                                  [bt1, bt2], [bdst], nowaw=True)

                    def conv_silu(rw, brw, L, ch, dst_ap, bdst):
                        a = acc[:, 0:L]
                        V(lambda e: e.tensor_scalar(out=a, in0=rw[:, 0:L], scalar1=cw[:, ch, 0:1], scalar2=None, op0=ALU.mult), [brw, bcw], [bacc])
                        V(lambda e: e.scalar_tensor_tensor(out=a, in0=rw[:, 1:L + 1], scalar=cw[:, ch, 1:2], in1=a, op0=ALU.mult, op1=ALU.add), [brw, bcw, bacc], [bacc])
                        V(lambda e: e.scalar_tensor_tensor(out=a, in0=rw[:, 2:L + 2], scalar=cw[:, ch, 2:3], in1=a, op0=ALU.mult, op1=ALU.add), [brw, bcw, bacc], [bacc])
                        S(lambda e: e.activation(out=dst_ap, in_=a, func=AF.Silu, bias=cbias[:, ch:ch + 1], scale=1.0), [bacc, bcw], [bdst], nowaw=True)

                    def proj_conv(base, dstT, bdst, is_k, chbase):
                        for dc in range(2):
                            wb, bwb = load_w(w_in_d, base + 128 * dc)
                            ch = chbase + dc
                            if is_k:
                                pt, bp = proj_fm(wb, bwb, 0, 256, 0)
                                S(lambda e, pt=pt: e.copy(out=rawc[:, 1:257], in_=pt[:, 0:256]), [bp], [brawc], nowaw=True)
                                conv_silu(rawc, brawc, 256, ch, dstT[:, dc, 0:256], bdst)
                            for n in range(4):
                                pt, bp = proj_fm(wb, bwb, 256 + 512 * n, 512, n + 1)
                                S(lambda e, pt=pt, n=n: e.copy(out=raw[:, 1 + 512 * n:1 + 512 * n + 512], in_=pt[:]), [bp], [braw], nowaw=True)
                            off = 256 if is_k else 0
                            conv_silu(raw, braw, 2048, ch, dstT[:, dc, off:off + 2048], bdst)

                    def attention(h, is_ml, br):
                        dq = []
                        if not is_ml:
                            V(lambda e: e.tensor_scalar(out=acolr[0][:], in0=cst[:, K_PF:K_PF + 18], scalar1=nlgc[:, h:h + 1], scalar2=-LN16, op0=ALU.mult, op1=ALU.add),
                              [bcst, blgc], [bacolr[0]])
                            V(lambda e: e.tensor_scalar(out=acolr[1][:], in0=cst[:, K_PB:K_PB + 18], scalar1=lgc[:, 4 + h:5 + h], scalar2=-LN16, op0=ALU.mult, op1=ALU.add),
                              [bcst, blgc], [bacolr[1]])
                        ctxs = {}

                        def group_ctx(g, d):
                            rowt, brow = rowr.next()
                            if is_ml:
                                pt, bp = pA.next()
                                c0 = 256 + 512 * g if d == 0 else 512 * g
                                sl = seln if d == 0 else selp
                                T(lambda e: e.matmul(pt[:, :], lhsT=sl[:, h, :], rhs=T2[d][:, c0:c0 + 512], start=True, stop=True), [bsel, bT2[d]], [bp])
                                S(lambda e: e.copy(out=rowt[:], in_=pt[:]), [bp], [brow])
                            else:
                                base = float(256 + 512 * g) if d == 0 else float(512 * g)
                                sc = lgc[:, h:h + 1] if d == 0 else nlgc[:, 4 + h:5 + h]
                                V(lambda e: e.tensor_scalar(out=rowt[:], in0=cst[:, K_IOTA:K_IOTA + 512], scalar1=base, scalar2=sc, op0=ALU.add, op1=ALU.mult), [bcst, blgc], [brow])
                            keys = list(range(0, 4 * g + 6)) if d == 0 else [0, 1] + list(range(4 * g + 2, 18))

                            def applies(jf, sub):
                                if jf < 2:
                                    return True
                                jl = jf - 2
                                qi = 4 * g + sub
                                return jl <= qi if d == 0 else jl >= qi
                            first = {sub: [jf for jf in keys if applies(jf, sub)][0] for sub in range(4)}
                            last = {sub: [jf for jf in keys if applies(jf, sub)][-1] for sub in range(4)}
                            ctxs[(g, d)] = (rowt, brow, keys, applies, first, last)

                        def emit_S(g, d, jf):
                            rowt, brow, keys, applies, first, last = ctxs[(g, d)]
                            ps, bps = pA.next()
                            for dc in range(2):
                                T(lambda e, dc=dc: e.matmul(ps[:, :], lhsT=kT[:, dc, 128 * jf:128 * jf + 128], rhs=qT[:, dc, 512 * g:512 * g + 512], start=(dc == 0), stop=(dc == 1)),
                                  [bk, bq], [bps])
                            jl = jf - 2
                            masked = jf >= 2 and 4 * g <= jl <= 4 * g + 3
                            src, bsrc = rowt, brow
                            if masked:
                                jj = jl - 4 * g
                                mk = (K_MF if d == 0 else K_MB) + 384 - 128 * jj
                                pre, bpre = prer.next()
                                G(lambda e: e.tensor_tensor(out=pre[:], in0=rowt[:], in1=cst[:, mk:mk + 512], op=ALU.add), [brow, bcst], [bpre])
                                src, bsrc = pre, bpre
                            Dt, bD = Dr.next()
                            if is_ml:
                                bias_ap, bbias = acol[d][:, jf, h:h + 1], bacol[d]
                            else:
                                bias_ap, bbias = acolr[d][:, jf:jf + 1], bacolr[d]
                            S(lambda e: e.activation(out=Dt[:], in_=src[:], func=AF.Exp, bias=bias_ap, scale=1.0), [bsrc, bbias], [bD])
                            at, bat = atr.next()
                            V(lambda e: e.tensor_tensor(out=at[:], in0=ps[:], in1=Dt[:], op=ALU.mult), [bps, bD], [bat])
                            return at, bat

                        def emit_AV(g, d, jf, at, bat):
                            rowt, brow, keys, applies, first, last = ctxs[(g, d)]
                            for sub in range(4):
                                if not applies(jf, sub):
                                    continue
                                T(lambda e, sub=sub, st=(jf == first[sub]), sp=(jf == last[sub]): e.matmul(pO[sub][:, 0:257], lhsT=at[:, 128 * sub:128 * sub + 128], rhs=vv[:, jf, :], start=st, stop=sp),
                                  [bat, bv], [bpO[sub]])
                            if jf == keys[-1]:
                                group_done(g, d)

                        def group_done(g, d):
                            raw, braw_ = rawr.next()
                            for sub in range(4):
                                S(lambda e, sub=sub: e.copy(out=raw[:, sub, :], in_=pO[sub][:, 0:257]), [bpO[sub]], [braw_], nowaw=(sub > 0))
                            if is_ml:
                                dq.append(lambda: S(lambda e: e.activation(out=dn[:], in_=raw[:, :, 256], func=AF.Abs), [braw_], [bdn]))
                                dq.append(lambda: V(lambda e: e.tensor_tensor(out=dn[:], in0=dn[:], in1=em[d][:, h:h + 1].broadcast_to([128, 4]), op=ALU.max), [bdn, bem[d]], [bdn]))
                                dq.append(lambda: V(lambda e: e.reciprocal(out=dn[:], in_=dn[:]), [bdn], [bdn]))
                                if d == 0:
                                    dq.append(lambda: V(lambda e: e.tensor_tensor(out=hf[:], in0=raw[:, :, 0:256], in1=dn[:].unsqueeze(2).broadcast_to([128, 4, 256]), op=ALU.mult), [braw_, bdn], [bhf]))
                                else:
                                    dq.append(lambda: V(lambda e: e.tensor_tensor(out=hs[:], in0=raw[:, :, 0:256], in1=dn[:].unsqueeze(2).broadcast_to([128, 4, 256]), op=ALU.mult), [braw_, bdn], [bhs]))
                                    dq.append(lambda: V(lambda e: e.tensor_tensor(out=hs[:], in0=hs[:], in1=hf[:], op=ALU.add), [bhs, bhf], [bhs]))
                            else:
                                if d == 0:
                                    dq.append(lambda: S(lambda e: e.copy(out=hf[:], in_=raw[:, :, 0:256]), [braw_], [bhf]))
                                else:
                                    dq.append(lambda: V(lambda e: e.tensor_tensor(out=hs[:], in0=raw[:, :, 0:256], in1=hf[:], op=ALU.add), [braw_, bhf], [bhs]))
                            if d == 1:
                                def st_stats():
                                    for sub in range(4):
                                        V(lambda e, sub=sub: e.bn_stats(out=s6h[:, sub, :], in_=hs[:, sub, :]), [bhs], [bs6h], nowaw=(sub > 0))

                                def st_aggr():
                                    for sub in range(4):
                                        V(lambda e, sub=sub: e.bn_aggr(out=mvh[:, sub, :], in_=s6h[:, sub, :]), [bs6h], [bmvh], nowaw=(sub > 0))

                                def st_eps():
                                    V(lambda e: e.tensor_scalar(out=rsh[:], in0=mvh[:, :, 1], scalar1=LN_EPS, scalar2=None, op0=ALU.add), [bmvh], [brsh])

                                def st_pow():
                                    G(lambda e: e.tensor_tensor(out=rsh[:], in0=rsh[:], in1=mhalf[:], op=ALU.pow), [brsh, bmhalf], [brsh])

                                def st_norm():
                                    for sub in range(4):
                                        V(lambda e, sub=sub: e.tensor_scalar(out=hn[:, sub, :], in0=hs[:, sub, :], scalar1=mvh[:, sub, 0:1], scalar2=rsh[:, sub:sub + 1], op0=ALU.subtract, op1=ALU.mult),
                                          [bhs, bmvh, brsh], [bhn], nowaw=(sub > 0))

                                def st_tr():
                                    for sub in range(4):
                                        for dc in range(2):
                                            T(lambda e, sub=sub, dc=dc: e.transpose(out=pT[:, 4 * dc + sub, :], in_=hn[:, sub, 128 * dc:128 * dc + 128], identity=identb[:]),
                                              [bhn, bidb], [bpT], nowaw=not (sub == 0 and dc == 0))

                                def st_out():
                                    of, bof = ofm.next()
                                    for dc in range(2):
                                        V(lambda e, dc=dc: e.tensor_tensor(out=of[:, dc, :].rearrange("p (s c) -> p s c", c=128), in0=pT[:, 4 * dc:4 * dc + 4, :], in1=gT[:, dc, 512 * g:512 * g + 512].rearrange("p (s c) -> p s c", c=128), op=ALU.mult),
                                          [bpT, bg], [bof], nowaw=(dc > 0))
                                    P.dma("gpsimd", lambda e: e.dma_start(out=r_d[br, 2 * h:2 * h + 2, :, 512 * g:512 * g + 512].rearrange("c p t -> p c t"), in_=of[:]),
                                          f"rsp{ofm.last}", reads=[bof], writes=[br_d[br]], nowaw=True)
                                dq.extend([st_stats, st_aggr, st_eps, st_pow, st_norm, st_tr, st_out])

                        tiles = []
                        for g in range(4):
                            for d in range(2):
                                keys_ = list(range(0, 4 * g + 6)) if d == 0 else [0, 1] + list(range(4 * g + 2, 18))
                                for jf in keys_:
                                    tiles.append((g, d, jf))
                        queue = []
                        for ti_, (g, d, jf) in enumerate(tiles):
                            for (g2_, d2_, _) in tiles[ti_:ti_ + 4]:
                                if (g2_, d2_) not in ctxs:
                                    group_ctx(g2_, d2_)
                            at, bat = emit_S(g, d, jf)
                            queue.append((g, d, jf, at, bat))
                            if len(queue) > 2:
                                emit_AV(*queue.pop(0))
                            if dq:
                                dq.pop(0)()
                        while queue:
                            emit_AV(*queue.pop(0))
                        while dq:
                            dq.pop(0)()

                    for h in range(4):
                        proj_rot(C_RQ + 256 * h, qT, bq, False)
                        proj_rot(C_RK + 256 * h, kT, bk, True)
                        proj_v(C_RV + 256 * h)
                        proj_g(C_RG + 256 * h, AF.Silu)
                        attention(h, False, 0)
                    for h in range(4):
                        proj_conv(C_MQ + 256 * h, qT, bq, False, 2 * h)
                        proj_conv(C_MK + 256 * h, kT, bk, True, 8 + 2 * h)
                        proj_v(C_MV + 256 * h)
                        proj_g(C_MO + 256 * h, AF.Sigmoid)
                        attention(h, True, 1)

            if dbg and "r" in dbg and b == 0:
                with P.phase():
                    dr = P.sb("dbg_r", [128, 2048], BF16)
                    drf = P.sb("dbg_rf", [128, 2048], F32)
                    bdr = Buf()
                    for br in range(2):
                        for c in range(8):
                            P.dma("sync", lambda e, br=br, c=c: e.dma_start(out=dr[:], in_=r_d[br, c, :, :]), "dbgl", reads=[br_d[br]], writes=[bdr])
                            V(lambda e: e.tensor_copy(out=drf[:], in_=dr[:]), [bdr], [bdr])
                            P.dma("gpsimd", lambda e, br=br, c=c: e.dma_start(out=dbg_outs["r"][br, c, :, :], in_=drf[:]), "dbg", reads=[bdr], writes=[bdbg], nowaw=True)

            with P.phase():
                pA = Ring(P, "pA", 4, [128, 512], F32, psum=True)
                wst = Ring(P, "wst", 3, [128, 8, 128], F32)
                wbf = Ring(P, "wbf", 6, [128, 8, 128], BF16)
                load_w = make_wload(wst, wbf)
                rfull = P.sb("rfull", [128, 8, 2048], BF16)
                mfull = P.sb("mfull", [128, 8, 2048], BF16)
                brf, bmf = Buf(), Buf()
                for c in range(8):
                    P.dma("sync", lambda e, c=c: e.dma_start(out=rfull[:, c, :], in_=r_d[0, c, :, :]), "rfl", reads=[br_d[0]], writes=[brf], nowaw=True)
                    P.dma("sync", lambda e, c=c: e.dma_start(out=mfull[:, c, :], in_=r_d[1, c, :, :]), "mfl", reads=[br_d[1]], writes=[bmf], nowaw=True)
                sgr = Ring(P, "sgr", 2, [128, 512], F32)
                t1r = Ring(P, "t1r", 2, [128, 512], F32)
                yor = Ring(P, "yor", 2, [128, 512], BF16)
                for oc in range(8):
                    ws = []
                    for (wsrc, gbase) in ((w_rb_d, C_GR), (w_mb_d, C_GM)):
                        ws.append((load_w(wsrc, 128 * oc), load_w(w_in_d, gbase + 128 * oc)))
                    for n in range(4):
                        tt = []
                        for bi, (src_t, bsrc_t) in enumerate(((rfull, brf), (mfull, bmf))):
                            (wb, bwb), (wg_, bwg_) = ws[bi]
                            pa, bpa = pA.next()
                            for k in range(8):
                                T(lambda e, pa=pa, wb=wb, k=k, src_t=src_t, n=n: e.matmul(pa[:, :], lhsT=wb[:, k, :], rhs=src_t[:, k, 512 * n:512 * n + 512], start=(k == 0), stop=(k == 7)), [bwb, bsrc_t], [bpa])
                            pg, bpg = pA.next()
                            for k in range(8):
                                T(lambda e, pg=pg, wg_=wg_, k=k, n=n: e.matmul(pg[:, :], lhsT=wg_[:, k, :], rhs=uT[:, k, 256 + 512 * n:256 + 512 * n + 512], start=(k == 0), stop=(k == 7)), [bwg_, buT[n + 1]], [bpg])
                            sg, bsg = sgr.next()
                            S(lambda e, sg=sg, pg=pg: e.activation(out=sg[:], in_=pg[:], func=AF.Sigmoid), [bpg], [bsg])
                            t1, bt1 = t1r.next()
                            V(lambda e, t1=t1, pa=pa, sg=sg: e.tensor_tensor(out=t1[:], in0=pa[:], in1=sg[:], op=ALU.mult), [bpa, bsg], [bt1])
                            tt.append((t1, bt1))
                        yo, byo = yor.next()
                        G(lambda e, yo=yo, a=tt[0][0], c=tt[1][0]: e.tensor_tensor(out=yo[:], in0=a[:], in1=c[:], op=ALU.add), [tt[0][1], tt[1][1]], [byo])
                        P.dma("gpsimd", lambda e, yo=yo, oc=oc, n=n: e.dma_start(out=y_d[oc, :, 512 * n:512 * n + 512], in_=yo[:]), f"yspill{yor.last}", reads=[byo], writes=[by_d], nowaw=True)

            with P.phase():
                pA = Ring(P, "pA", 3, [128, 512], F32, psum=True)
                pO = [P.ps(f"pO{i}", [128, 512], F32) for i in range(4)]
                bpO = [Buf() for _ in range(4)]
                wst = Ring(P, "wst", 3, [128, 8, 128], F32)
                woutb = P.sb("woutb", [128, 8, 1024], BF16)
                bwout = Buf()
                for c in range(8):
                    wt, bw = wst.next()
                    P.dma("sync", lambda e, wt=wt, c=c: e.dma_start(out=wt[:], in_=w_out_d[:, 128 * c:128 * c + 128].rearrange("(c p) n -> p c n", p=128)), f"wst{wst.last}", writes=[bw])
                    S(lambda e, wt=wt, c=c: e.copy(out=woutb[:, :, 128 * c:128 * c + 128], in_=wt[:]), [bw], [bwout], nowaw=True)
                bct = {}
                bbc = Buf()
                for name, src in (("g1", mod_d[b:b + 1, 2048:3072]), ("sh2", mod_d[b:b + 1, 3072:4096]), ("sc2", mod_d[b:b + 1, 4096:5120]),
                                  ("l1g", lnp_d[0:1, :]), ("l1b", lnp_d[1:2, :])):
                    t = P.sb("bc_" + name, [128, 1024], F32)
                    bct[name] = t
                    P.dma("sync", lambda e, t=t, src=src: e.dma_start(out=t[:], in_=src.partition_broadcast(128)), "bc", reads=[bmod], writes=[bbc], nowaw=True)
                V(lambda e: e.tensor_scalar(out=bct["sc2"][:], in0=bct["sc2"][:], scalar1=1.0, scalar2=None, op0=ALU.add), [bbc], [bbc])
                yTr = Ring(P, "yT", 2, [128, 8, 512], BF16)
                xtr = Ring(P, "xt2", 2, [128, 1024], F32)
                ztr = Ring(P, "zt", 2, [128, 1024], F32)
                x1tr = Ring(P, "x1t", 2, [128, 1024], F32)
                u2tr = Ring(P, "u2t", 2, [128, 1024], F32)
                u2br = Ring(P, "u2b", 2, [128, 1024], BF16)
                u2Tr = Ring(P, "u2T", 2, [128, 8, 128], F32)
                s6r = Ring(P, "s6b", 2, [128, 2, 6], F32)
                m2r = Ring(P, "m2b", 2, [128, 2], F32)
                r2r = Ring(P, "r2b", 2, [128, 1], F32)
                yTs = {}
                u2s_ = {}

                def part1(i):
                    n, sub = i // 4, i % 4
                    gi = b * 16 + i
                    if sub == 0:
                        yT, byT = yTr.next()
                        P.dma("sync", lambda e: e.dma_start(out=yT[:], in_=y_d[:, :, 512 * n:512 * n + 512].rearrange("c p t -> p c t")), f"yT{yTr.last}", reads=[by_d], writes=[byT])
                        yTs[n] = (yT, byT)
                    yT, byT = yTs[n]
                    xt, bxt = xtr.next()
                    P.dma("sync", lambda e: e.dma_start(out=xt[:], in_=x_d[b, 128 * i:128 * i + 128, :]), f"xt2{xtr.last}", writes=[bxt])
                    for half in range(2):
                        for k in range(8):
                            T(lambda e, half=half, k=k: e.matmul(pO[half][:, :], lhsT=yT[:, k, 128 * sub:128 * sub + 128], rhs=woutb[:, k, 512 * half:512 * half + 512], start=(k == 0), stop=(k == 7)),
                              [byT, bwout], [bpO[half]])
                    zt, bzt = ztr.next()
                    for half in range(2):
                        V(lambda e, half=half: e.tensor_tensor(out=zt[:, 512 * half:512 * half + 512], in0=pO[half][:, :], in1=bct["g1"][:, 512 * half:512 * half + 512], op=ALU.mult),
                          [bpO[half], bbc], [bzt], nowaw=(half > 0))
                    V(lambda e: e.scalar_tensor_tensor(out=zt[:], in0=xt[:], scalar=DN_ALPHA, in1=zt[:], op0=ALU.mult, op1=ALU.add), [bxt, bzt], [bzt])
                    s6, bs6 = s6r.next()
                    m2, bm2 = m2r.next()
                    r2, br2 = r2r.next()
                    ln_stats(zt, bzt, s6, bs6, m2, bm2, r2, br2)
                    x1t, bx1t = x1tr.next()
                    V(lambda e: e.tensor_scalar(out=x1t[:], in0=zt[:], scalar1=m2[:, 0:1], scalar2=r2[:, 0:1], op0=ALU.subtract, op1=ALU.mult), [bzt, bm2, br2], [bx1t])
                    G(lambda e: e.tensor_tensor(out=x1t[:], in0=x1t[:], in1=bct["l1g"][:], op=ALU.mult), [bx1t, bbc], [bx1t])
                    G(lambda e: e.tensor_tensor(out=x1t[:], in0=x1t[:], in1=bct["l1b"][:], op=ALU.add), [bx1t, bbc], [bx1t])
                    P.dma("gpsimd", lambda e: e.dma_start(out=x1_d[128 * gi:128 * gi + 128, :], in_=x1t[:]), f"x1st{x1tr.last}", reads=[bx1t], writes=[bx1_d], nowaw=True)
                    s6, bs6 = s6r.next()
                    m2, bm2 = m2r.next()
                    r2, br2 = r2r.next()
                    ln_stats(x1t, bx1t, s6, bs6, m2, bm2, r2, br2)
                    u2t, bu2t = u2tr.next()
                    V(lambda e: e.tensor_scalar(out=u2t[:], in0=x1t[:], scalar1=m2[:, 0:1], scalar2=r2[:, 0:1], op0=ALU.subtract, op1=ALU.mult), [bx1t, bm2, br2], [bu2t])
                    G(lambda e: e.tensor_tensor(out=u2t[:], in0=u2t[:], in1=bct["sc2"][:], op=ALU.mult), [bu2t, bbc], [bu2t])
                    G(lambda e: e.tensor_tensor(out=u2t[:], in0=u2t[:], in1=bct["sh2"][:], op=ALU.add), [bu2t, bbc], [bu2t])
                    u2b, bu2b = u2br.next()
                    S(lambda e: e.copy(out=u2b[:], in_=u2t[:]), [bu2t], [bu2b])
                    P.dma("gpsimd", lambda e: e.dma_start(out=u2_d[128 * gi:128 * gi + 128, :], in_=u2b[:]), f"u2st{u2br.last}", reads=[bu2b], writes=[bu2_d], nowaw=True)
                    u2s_[i] = (u2t, bu2t)

                def part2(i):
                    gi = b * 16 + i
                    u2t, bu2t = u2s_.pop(i)
                    for k in range(8):
                        pp = pO[2 + k // 4]
                        T(lambda e, pp=pp, k=k: e.transpose(out=pp[:, 128 * (k % 4):128 * (k % 4) + 128], in_=u2t[:, 128 * k:128 * k + 128], identity=identf),
                          [bu2t, bcst], [bpO[2 + k // 4]], nowaw=(k % 4 > 0))
                    u2T, bu2T = u2Tr.next()
                    S(lambda e: e.copy(out=u2T[:, 0:4, :].rearrange("p a b -> p (a b)"), in_=pO[2][:, :]), [bpO[2]], [bu2T])
                    V(lambda e: e.tensor_copy(out=u2T[:, 4:8, :].rearrange("p a b -> p (a b)"), in_=pO[3][:, :]), [bpO[3]], [bu2T], nowaw=True)
                    pl, bpl = pA.next()
                    for k in range(8):
                        T(lambda e, k=k: e.matmul(pl[:, 0:36], lhsT=u2T[:, k, :], rhs=wrt[:, k, :], start=(k == 0), stop=(k == 7)), [bu2T, bwrt], [bpl])
                    V(lambda e: e.tensor_tensor(out=logits[:, gi, :], in0=pl[:, 0:36], in1=brt[:], op=ALU.add), [bpl, bwrt], [blog], nowaw=True)

                for i in range(17):
                    if i < 16:
                        part1(i)
                    if i >= 1:
                        part2(i - 1)

        if dbg and "x1" in dbg:
            with P.phase():
                t = P.sb("dbg_x1", [128, 1024], F32)
                bt = Buf()
                for gi in range(32):
                    P.dma("sync", lambda e, gi=gi: e.dma_start(out=t[:], in_=x1_d[128 * gi:128 * gi + 128, :]), "dbgl", reads=[bx1_d], writes=[bt])
                    P.dma("gpsimd", lambda e, gi=gi: e.dma_start(out=dbg_outs["x1"][128 * gi:128 * gi + 128, :], in_=t[:]), "dbg", reads=[bt], writes=[bdbg], nowaw=True)
                lt = P.sb("dbg_lg", [128, 32 * 36], F32)
                V(lambda e: e.tensor_copy(out=lt[:], in_=logits[:].rearrange("p a b -> p (a b)")), [blog], [bt])
                P.dma("gpsimd", lambda e: e.dma_start(out=dbg_outs["logits"], in_=lt[:]), "dbg", reads=[bt], writes=[bdbg], nowaw=True)

        MOE_PLACEHOLDER = True

        d1i = P.sb("d1i", [128, 32], I32)
        d2i = P.sb("d2i", [128, 32], I32)
        wt1 = P.sb("wt1", [128, 32], F32)
        wt2 = P.sb("wt2", [128, 32], F32)
        bei = P.sb("bei", [128, 96], I32)
        bd12, bwt12, bbe = Buf(), Buf(), Buf()
        with P.phase():
            pA = Ring(P, "pA", 3, [128, 512], F32, psum=True)
            ppos = [P.ps(f"ppos{i}", [128, 16, 32], F32) for i in range(2)]
            bppos = [Buf(), Buf()]

            def R(name, shape, dt=F32):
                return P.sb("rt_" + name, shape, dt), Buf()
            gmax, bgmax = R("gmax", [128, 32])
            ohg, bohg = R("ohg", [128, 32, 4])
            eg, beg = R("eg", [128, 32, 4])
            pg, bpg_ = R("pg", [128, 32])
            tmp4, btmp4 = R("tmp4", [128, 32, 4, 8])
            les, bles = R("les", [128, 32, 8])
            m1, bm1 = R("m1", [128, 32])
            mk1, bmk1 = R("mk1", [128, 32, 8])
            le2, ble2 = R("le2", [128, 32, 8])
            m2_, bm2_ = R("m2", [128, 32])
            mk2, bmk2 = R("mk2", [128, 32, 8])
            sg_, bsg_ = R("sg", [128, 32])
            A1, bA1 = R("A1", [128, 32, 4, 8])
            A2, bA2 = R("A2", [128, 32, 4, 8])
            Ab, bAb = R("Ab", [128, 32, 32], BF16)
            posf, bposf = R("posf", [128, 32, 32])
            cnt, bcnt = R("cnt", [128, 32])
            cnti, bcnti = R("cnti", [128, 32], I32)
            padf, bpadf = R("padf", [128, 32])
            pend, bpend = R("pend", [128, 32])
            poff, bpoff = R("poff", [128, 32])
            ones32, bones32 = R("ones32", [128, 32])
            dfl, bdfl = R("dfl", [128, 32])
            cmp, bcmp = R("cmp", [128, 48, 32])
            bef, bbef = R("bef", [128, 48])
            lgv = logits[:, :, 0:4]
            lev = logits[:, :, 4:36].rearrange("p t (g e) -> p t g e", e=8)

            def bc3(ap, n):
                return ap.unsqueeze(2).broadcast_to([128, 32, n])
            V(lambda e: e.memset(ones32[:], 1.0), [], [bones32])
            V(lambda e: e.tensor_reduce(out=gmax[:], in_=lgv, axis=AX.X, op=ALU.max), [blog], [bgmax])
            V(lambda e: e.tensor_tensor(out=ohg[:], in0=lgv, in1=bc3(gmax[:], 4), op=ALU.is_equal), [blog, bgmax], [bohg])
            V(lambda e: e.tensor_tensor(out=eg[:], in0=lgv, in1=bc3(gmax[:], 4), op=ALU.subtract), [blog, bgmax], [beg])
            S(lambda e: e.activation(out=eg[:], in_=eg[:], func=AF.Exp), [beg], [beg])
            V(lambda e: e.tensor_reduce(out=pg[:], in_=eg[:], axis=AX.X, op=ALU.add), [beg], [bpg_])
            V(lambda e: e.reciprocal(out=pg[:], in_=pg[:]), [bpg_], [bpg_])
            V(lambda e: e.tensor_tensor(out=tmp4[:], in0=lev, in1=ohg[:].unsqueeze(3).broadcast_to([128, 32, 4, 8]), op=ALU.mult), [blog, bohg], [btmp4])
            V(lambda e: e.tensor_reduce(out=les[:], in_=tmp4[:].rearrange("p t g e -> p t e g"), axis=AX.X, op=ALU.add), [btmp4], [bles])
            V(lambda e: e.tensor_reduce(out=m1[:], in_=les[:], axis=AX.X, op=ALU.max), [bles], [bm1])
            V(lambda e: e.tensor_tensor(out=mk1[:], in0=les[:], in1=bc3(m1[:], 8), op=ALU.is_equal), [bles, bm1], [bmk1])
            V(lambda e: e.scalar_tensor_tensor(out=le2[:], in0=mk1[:], scalar=-1e30, in1=les[:], op0=ALU.mult, op1=ALU.add), [bmk1, bles], [ble2])
            V(lambda e: e.tensor_reduce(out=m2_[:], in_=le2[:], axis=AX.X, op=ALU.max), [ble2], [bm2_])
            V(lambda e: e.tensor_tensor(out=mk2[:], in0=le2[:], in1=bc3(m2_[:], 8), op=ALU.is_equal), [ble2, bm2_], [bmk2])
            V(lambda e: e.tensor_tensor(out=sg_[:], in0=m1[:], in1=m2_[:], op=ALU.subtract), [bm1, bm2_], [bsg_])
            S(lambda e: e.activation(out=sg_[:], in_=sg_[:], func=AF.Sigmoid), [bsg_], [bsg_])
            V(lambda e: e.tensor_tensor(out=wt1[:], in0=pg[:], in1=sg_[:], op=ALU.mult), [bpg_, bsg_], [bwt12])
            V(lambda e: e.tensor_tensor(out=wt2[:], in0=pg[:], in1=wt1[:], op=ALU.subtract), [bpg_, bwt12], [bwt12])
            for (Ax, bAx, mk, bmk) in ((A1, bA1, mk1, bmk1), (A2, bA2, mk2, bmk2)):
                V(lambda e, Ax=Ax, mk=mk: e.tensor_tensor(out=Ax[:], in0=ohg[:].unsqueeze(3).broadcast_to([128, 32, 4, 8]), in1=mk[:].unsqueeze(2).broadcast_to([128, 32, 4, 8]), op=ALU.mult),
                  [bohg, bmk], [bAx])
            A1f = A1[:].rearrange("p t g e -> p t (g e)")
            A2f = A2[:].rearrange("p t g e -> p t (g e)")
            V(lambda e: e.tensor_tensor(out=Ab[:], in0=A1f, in1=A2f, op=ALU.add), [bA1, bA2], [bAb])
            for ti in range(32):
                pp = ppos[ti // 16]
                bpp = bppos[ti // 16]
                for tj in range(ti):
                    T(lambda e, pp=pp, ti=ti, tj=tj: e.matmul(pp[:, ti % 16, :], lhsT=onesb[:], rhs=Ab[:, tj, :], start=(tj == 0), stop=False), [bidb, bAb], [bpp], nowaw=True)
                T(lambda e, pp=pp, ti=ti: e.matmul(pp[:, ti % 16, :], lhsT=trib[:], rhs=Ab[:, ti, :], start=(ti == 0), stop=True), [bidb, bAb], [bpp], nowaw=True)
            pc, bpc = pA.next()
            for tj in range(32):
                T(lambda e, pc=pc, tj=tj: e.matmul(pc[:, 0:32], lhsT=onesb[:], rhs=Ab[:, tj, :], start=(tj == 0), stop=(tj == 31)), [bidb, bAb], [bpc])
            S(lambda e: e.copy(out=posf[:, 0:16, :], in_=ppos[0][:]), [bppos[0]], [bposf])
            V(lambda e: e.tensor_copy(out=posf[:, 16:32, :], in_=ppos[1][:]), [bppos[1]], [bposf], nowaw=True)
            V(lambda e: e.tensor_scalar(out=cnti[:], in0=pc[:, 0:32], scalar1=511.0, scalar2=None, op0=ALU.add), [bpc], [bcnti])
            V(lambda e: e.tensor_single_scalar(out=cnti[:], in_=cnti[:], scalar=9, op=ALU.arith_shift_right), [bcnti], [bcnti])
            V(lambda e: e.tensor_single_scalar(out=cnti[:], in_=cnti[:], scalar=9, op=ALU.logical_shift_left), [bcnti], [bcnti])
            V(lambda e: e.tensor_copy(out=padf[:], in_=cnti[:]), [bcnti], [bpadf])
            V(lambda e: e.tensor_tensor_scan(out=pend[:], data0=ones32[:], data1=padf[:], initial=0.0, op0=ALU.mult, op1=ALU.add), [bones32, bpadf], [bpend])
            V(lambda e: e.tensor_tensor(out=poff[:], in0=pend[:], in1=padf[:], op=ALU.subtract), [bpend, bpadf], [bpoff])
            V(lambda e: e.tensor_tensor(out=posf[:], in0=posf[:], in1=poff[:].unsqueeze(1).broadcast_to([128, 32, 32]), op=ALU.add), [bposf, bpoff], [bposf])
            for (Af, bAx, di) in ((A1f, bA1, d1i), (A2f, bA2, d2i)):
                V(lambda e, Af=Af: e.tensor_tensor(out=tmp4[:].rearrange("p t g e -> p t (g e)"), in0=Af, in1=posf[:], op=ALU.mult), [bAx, bposf], [btmp4])
                V(lambda e: e.tensor_reduce(out=dfl[:], in_=tmp4[:].rearrange("p t g e -> p t (g e)"), axis=AX.X, op=ALU.add), [btmp4], [bdfl])
                V(lambda e, di=di: e.tensor_copy(out=di[:], in_=dfl[:]), [bdfl], [bd12], nowaw=True)
            V(lambda e: e.tensor_tensor(out=cmp[:], in0=pend[:].unsqueeze(1).broadcast_to([128, 48, 32]), in1=cst[:, K_BLK:K_BLK + 48].unsqueeze(2).broadcast_to([128, 48, 32]), op=ALU.is_le),
              [bpend, bcst], [bcmp])
            V(lambda e: e.tensor_reduce(out=bef[:], in_=cmp[:], axis=AX.X, op=ALU.add), [bcmp], [bbef])
            V(lambda e: e.tensor_scalar(out=bei[:, 0:48], in0=bef[:], scalar1=31.0, scalar2=None, op0=ALU.min), [bbef], [bbe])
            if dbg and "route" in dbg:
                rt = P.sb("dbg_rt", [128, 4, 32], F32)
                brt_ = Buf()
                V(lambda e: e.tensor_copy(out=rt[:, 0, :], in_=d1i[:]), [bd12], [brt_], nowaw=True)
                V(lambda e: e.tensor_copy(out=rt[:, 1, :], in_=d2i[:]), [bd12], [brt_], nowaw=True)
                V(lambda e: e.tensor_copy(out=rt[:, 2, :], in_=wt1[:]), [bwt12], [brt_], nowaw=True)
                V(lambda e: e.tensor_copy(out=rt[:, 3, :], in_=wt2[:]), [bwt12], [brt_], nowaw=True)
                P.dma("gpsimd", lambda e: e.dma_start(out=dbg_outs["route"], in_=rt[:].rearrange("p a b -> p (a b)")), "dbg", reads=[brt_], writes=[bdbg], nowaw=True)
                bt_ = P.sb("dbg_be", [128, 96], F32)
                V(lambda e: e.memset(bt_[:], 0.0), [], [brt_], nowaw=True)
                V(lambda e: e.tensor_copy(out=bt_[:, 0:48], in_=bei[:, 0:48]), [bbe], [brt_])
                P.dma("gpsimd", lambda e: e.dma_start(out=dbg_outs["be"], in_=bt_[:]), "dbg", reads=[brt_], writes=[bdbg], nowaw=True)
            u2s = Ring(P, "u2s", 2, [128, 1024], BF16)
            for ti in range(32):
                ut, but = u2s.next()
                P.dma("sync", lambda e, ut=ut, ti=ti: e.dma_start(out=ut[:], in_=u2_d[128 * ti:128 * ti + 128, :]), f"u2s{ti % 2}", reads=[bu2_d], writes=[but])
                for di in (d1i, d2i):
                    P.dma("gpsimd", lambda e, ut=ut, ti=ti, di=di: e.indirect_dma_start(
                        out=xs_d, out_offset=bass.IndirectOffsetOnAxis(ap=di[:, ti:ti + 1], axis=0), in_=ut[:], in_offset=None),
                        f"xsc{ti % 2}", reads=[but, bd12], writes=[bxs_d], nowaw=True)

        with P.phase():
            pA = Ring(P, "pA", 2, [128, 512], F32, psum=True)
            pY = [P.ps(f"pY{i}", [128, 512], F32) for i in range(4)]
            bpY = [Buf(), Buf()]
            pT = P.ps("pT", [128, 8, 128], BF16)
            bpT = Buf()
            pT2 = P.ps("pT2", [128, 8, 128], BF16)
            bpT2 = Buf()
            wstg = Ring(P, "wstg", 4, [128, 2048], F32)
            w1b = Ring(P, "w1b", 2, [128, 8, 512], BF16)
            w3b = Ring(P, "w3b", 2, [128, 8, 512], BF16)
            w2b = Ring(P, "w2b", 2, [128, 4, 1024], BF16)
            xsb = Ring(P, "xsb", 4, [128, 1024], BF16)
            xsTr = Ring(P, "xsT", 2, [128, 8, 128], BF16)
            hsl = P.sb("hsl", [128, 512], F32)
            bhsl = Buf()
            hhr = Ring(P, "hh", 2, [128, 512], BF16)
            hT = P.sb("hT", [128, 4, 128], BF16)
            bhT = Buf()
            ysb = Ring(P, "ysb", 2, [128, 1024], F32)
            regs = {}
            blocks = [(sbk, sub_) for sbk in range(48) for sub_ in range(4)]
            wsets = {}
            hhs = {}

            def load_weights(sbk):
                w1t, bw1 = w1b.next()
                w3t, bw3 = w3b.next()
                w2t, bw2 = w2b.next()
                wsets[sbk] = (w1t, bw1, w3t, bw3, w2t, bw2)
                idx = 0
                for (wd, wt_, bwt_, eng, is2) in ((w_e1_d, w1t, bw1, "scalar", False), (w_e3_d, w3t, bw3, "scalar", False), (w_e2_d, w2t, bw2, "gpsimd", True)):
                    for hf_ in range(2):
                        stg, bstg = wstg.next()

                        def dma_fn(e, wd=wd, hf_=hf_, stg=stg, is2=is2, idx=idx, sbk=sbk):
                            if idx == 0:
                                if "r" not in regs:
                                    regs["r"] = e.alloc_register("r_exp")
                                    regs["a"] = e.alloc_register("r_expa")
                                    regs["b"] = e.alloc_register("r_expb")
                                e.reg_load(regs["r"], bei[0:1, sbk:sbk + 1])
                                e.reg_mul(regs["a"], regs["r"], 524288)
                                e.reg_add(regs["b"], regs["a"], 262144)
                            rr = regs["a"] if hf_ == 0 else regs["b"]
                            if not is2:
                                src = bass.AP(wd.tensor, rr, [[512, 128], [65536, 4], [1, 512]])
                                ins = e.dma_start(out=stg[:].rearrange("p (c n) -> p c n", n=512), in_=src)
                            else:
                                src = bass.AP(wd.tensor, rr, [[1024, 128], [131072, 2], [1, 1024]])
                                ins = e.dma_start(out=stg[:].rearrange("p (c n) -> p c n", n=1024), in_=src)
                            RH = type(rr)
                            tmpn = [nm for grp in ins.ins.regs_accessed() for nm in grp if "_tmp_" in nm][0]
                            kk = int(tmpn.split("_")[-1])
                            e.free_register(RH(tmpn, rr.engine))
                            e.free_register(RH(f"SP_{rr.name}_snap_{kk - 2}", rr.engine))
                            return ins
                        P.dma("sync", dma_fn, f"wstg{wstg.last}", reads=[bbe], writes=[bstg])
                        if not is2:
                            dst = wt_[:, 4 * hf_:4 * hf_ + 4, :].rearrange("p c n -> p (c n)")
                        else:
                            dst = wt_[:, 2 * hf_:2 * hf_ + 2, :].rearrange("p c n -> p (c n)")
                        if eng == "scalar":
                            S(lambda e, dst=dst, stg=stg: e.copy(out=dst, in_=stg[:]), [bstg], [bwt_], nowaw=(hf_ > 0))
                        else:
                            P.op(eng, lambda e, dst=dst, stg=stg: e.tensor_copy(out=dst, in_=stg[:]), reads=[bstg], writes=[bwt_], nowaw=(hf_ > 0))
                        idx += 1

            def stageA(i):
                sbk, sub_ = blocks[i]
                bk = 4 * sbk + sub_
                if sub_ == 0 and sbk == 0:
                    load_weights(0)
                if sub_ == 1 and sbk + 1 < 48:
                    load_weights(sbk + 1)
                w1t, bw1, w3t, bw3, w2t, bw2 = wsets[sbk]
                xb_, bxb_ = xs_tiles.pop(i)
                for k in range(8):
                    T(lambda e, k=k: e.transpose(out=pT[:, k, :], in_=xb_[:, 128 * k:128 * k + 128], identity=identb[:]), [bxb_, bidb], [bpT], nowaw=(k > 0))
                xsT, bxsT = xsTr.next()
                V(lambda e: e.tensor_copy(out=xsT[:], in_=pT[:]), [bpT], [bxsT])
                p1, bp1 = pA.next()
                p3, bp3 = pA.next()
                for k in range(8):
                    T(lambda e, k=k: e.matmul(p1[:, :], lhsT=xsT[:, k, :], rhs=w1t[:, k, :], start=(k == 0), stop=(k == 7)), [bxsT, bw1], [bp1])
                for k in range(8):
                    T(lambda e, k=k: e.matmul(p3[:, :], lhsT=xsT[:, k, :], rhs=w3t[:, k, :], start=(k == 0), stop=(k == 7)), [bxsT, bw3], [bp3])
                S(lambda e: e.activation(out=hsl[:], in_=p1[:], func=AF.Silu), [bp1], [bhsl])
                hh, bhh = hhr.next()
                V(lambda e: e.tensor_tensor(out=hh[:], in0=p3[:], in1=hsl[:], op=ALU.mult), [bp3, bhsl], [bhh])
                hhs[i] = (hh, bhh)

            def stageB(i):
                sbk, sub_ = blocks[i]
                bk = 4 * sbk + sub_
                w1t, bw1, w3t, bw3, w2t, bw2 = wsets[sbk]
                hh, bhh = hhs.pop(i)
                for f in range(4):
                    T(lambda e, f=f: e.transpose(out=pT2[:, f, :], in_=hh[:, 128 * f:128 * f + 128], identity=identb[:]), [bhh, bidb], [bpT2], nowaw=(f > 0))
                V(lambda e: e.tensor_copy(out=hT[:], in_=pT2[:, 0:4, :]), [bpT2], [bhT])
                par = bk % 2
                for half in range(2):
                    for f in range(4):
                        T(lambda e, half=half, f=f: e.matmul(pY[2 * par + half][:, :], lhsT=hT[:, f, :], rhs=w2t[:, f, 512 * half:512 * half + 512], start=(f == 0), stop=(f == 3)),
                          [bhT, bw2], [bpY[par]], nowaw=not (half == 0 and f == 0))
                yt_, byt_ = ysb.next()
                S(lambda e: e.copy(out=yt_[:, 0:512], in_=pY[2 * par][:, :]), [bpY[par]], [byt_])
                V(lambda e: e.tensor_copy(out=yt_[:, 512:1024], in_=pY[2 * par + 1][:, :]), [bpY[par]], [byt_], nowaw=True)
                P.dma("gpsimd", lambda e: e.dma_start(out=ys_d[128 * bk:128 * bk + 128, :], in_=yt_[:]), f"yst{ysb.last}", reads=[byt_], writes=[bys_d], nowaw=True)

            xs_tiles = {}

            def load_xs(i):
                sbk, sub_ = blocks[i]
                bk = 4 * sbk + sub_
                xb_, bxb_ = xsb.next()
                P.dma("gpsimd", lambda e: e.dma_start(out=xb_[:], in_=xs_d[128 * bk:128 * bk + 128, :]), f"xsb{xsb.last}", reads=[bxs_d], writes=[bxb_])
                xs_tiles[i] = (xb_, bxb_)
            load_xs(0)
            load_xs(1)
            for i in range(len(blocks) + 1):
                if i + 2 < len(blocks):
                    load_xs(i + 2)
                if i < len(blocks):
                    stageA(i)
                if i >= 1:
                    stageB(i - 1)

        with P.phase():
            bct = {}
            bbc = Buf()
            for name, src in (("g2_0", mod_d[0:1, 5120:6144]), ("g2_1", mod_d[1:2, 5120:6144]), ("l2g", lnp_d[2:3, :]), ("l2b", lnp_d[3:4, :])):
                t = P.sb("bc2_" + name, [128, 1024], F32)
                bct[name] = t
                P.dma("sync", lambda e, t=t, src=src: e.dma_start(out=t[:], in_=src.partition_broadcast(128)), "bc2", reads=[bmod], writes=[bbc], nowaw=True)
            y1r = Ring(P, "y1r", 3, [128, 1024], F32)
            y2r = Ring(P, "y2r", 3, [128, 1024], F32)
            x1r = Ring(P, "x1r", 3, [128, 1024], F32)
            outr = Ring(P, "outr", 3, [128, 1024], F32)
            s6, m2, r2 = P.sb("s6c", [128, 2, 6], F32), P.sb("m2c", [128, 2], F32), P.sb("r2c", [128, 1], F32)
            bs6, bm2, br2 = Buf(), Buf(), Buf()
            gt = {}

            def gathers(ti):
                y1, by1 = y1r.next()
                y2, by2 = y2r.next()
                x1, bx1 = x1r.next()
                P.dma("gpsimd", lambda e: e.indirect_dma_start(out=y1[:], out_offset=None, in_=ys_d, in_offset=bass.IndirectOffsetOnAxis(ap=d1i[:, ti:ti + 1], axis=0)),
                      f"y1g{y1r.last}", reads=[bys_d, bd12], writes=[by1])
                P.dma("gpsimd", lambda e: e.indirect_dma_start(out=y2[:], out_offset=None, in_=ys_d, in_offset=bass.IndirectOffsetOnAxis(ap=d2i[:, ti:ti + 1], axis=0)),
                      f"y2g{y2r.last}", reads=[bys_d, bd12], writes=[by2])
                P.dma("sync", lambda e: e.dma_start(out=x1[:], in_=x1_d[128 * ti:128 * ti + 128, :]), f"x1l{x1r.last}", reads=[bx1_d], writes=[bx1])
                gt[ti] = (y1, by1, y2, by2, x1, bx1)

            def combine(ti):
                b = ti // 16
                i = ti % 16
                y1, by1, y2, by2, x1, bx1 = gt.pop(ti)
                V(lambda e: e.tensor_scalar(out=y1[:], in0=y1[:], scalar1=wt1[:, ti:ti + 1], scalar2=None, op0=ALU.mult), [by1, bwt12], [by1])
                V(lambda e: e.scalar_tensor_tensor(out=y1[:], in0=y2[:], scalar=wt2[:, ti:ti + 1], in1=y1[:], op0=ALU.mult, op1=ALU.add), [by1, by2, bwt12], [by1])
                g2 = bct["g2_%d" % b]
                G(lambda e: e.tensor_tensor(out=y1[:], in0=y1[:], in1=g2[:], op=ALU.mult), [by1, bbc], [by1])
                V(lambda e: e.scalar_tensor_tensor(out=y1[:], in0=x1[:], scalar=DN_ALPHA, in1=y1[:], op0=ALU.mult, op1=ALU.add), [by1, bx1], [by1])
                s6, bs6 = s6r.next()
                m2, bm2 = m2r.next()
                r2, br2 = r2r.next()
                ln_stats(y1, by1, s6, bs6, m2, bm2, r2, br2)
                ot, bot = outr.next()
                V(lambda e: e.tensor_scalar(out=ot[:], in0=y1[:], scalar1=m2[:, 0:1], scalar2=r2[:, 0:1], op0=ALU.subtract, op1=ALU.mult), [by1, bm2, br2], [bot])
                G(lambda e: e.tensor_tensor(out=ot[:], in0=ot[:], in1=bct["l2g"][:], op=ALU.mult), [bot, bbc], [bot])
                G(lambda e: e.tensor_tensor(out=ot[:], in0=ot[:], in1=bct["l2b"][:], op=ALU.add), [bot, bbc], [bot])
                P.dma("gpsimd", lambda e: e.dma_start(out=out_d[b, 128 * i:128 * i + 128, :], in_=ot[:]), f"ost{outr.last}", reads=[bot], writes=[bout], nowaw=True)
            s6r = Ring(P, "s6cr", 2, [128, 2, 6], F32)
            m2r = Ring(P, "m2cr", 2, [128, 2], F32)
            r2r = Ring(P, "r2cr", 2, [128, 1], F32)
            gathers(0)
            gathers(1)
            for ti in range(32):
                if ti + 2 < 32:
                    gathers(ti + 2)
                combine(ti)
        P.barrier()
        P.emit()
    return nc


_CONSTS = None


def _prep_shared(inp):
    global _CONSTS
    if _CONSTS is None:
        _CONSTS = make_consts()
    f = lambda a: np.ascontiguousarray(a, dtype=np.float32)
    cwT = np.ascontiguousarray(inp["ml_conv_w"][0].reshape(3, 16, 128).transpose(2, 1, 0), dtype=np.float32)
    cbT = np.ascontiguousarray(inp["ml_conv_b"][0].reshape(16, 128).T, dtype=np.float32)
    return {
        "w_ada": f(inp["w_ada"][0]), "b_ada": f(inp["b_ada"][0].reshape(1, 6144)), "w_in": f(inp["w_in"][0]),
        "bmg": f(inp["b_mgate"][0].reshape(4, 4).T), "cw": cwT, "cb": cbT,
        "rdl": f(inp["ret_decay_logit"][0].reshape(1, 8)),
        "w_rb": f(inp["w_ret_branch"][0]), "w_mb": f(inp["w_ml_branch"][0]), "w_out": f(inp["w_out"][0]),
        "lnp": f(np.stack([inp["ln1_g"][0], inp["ln1_b"][0], inp["ln2_g"][0], inp["ln2_b"][0]], 0)),
        "w_rt": f(np.concatenate([inp["w_rg"][0], inp["w_re"][0]], 1)),
        "b_rt": f(np.concatenate([inp["b_rg"][0], inp["b_re"][0]], 0).reshape(1, 36)),
        "w_e1": f(inp["w_e1"][0]), "w_e3": f(inp["w_e3"][0]), "w_e2": f(inp["w_e2"][0]),
        "consts": _CONSTS,
    }


def _core_inputs(inp, shared, c):
    x = np.asarray(inp["x"], dtype=np.float32)
    ctx = np.asarray(inp["ctx"], dtype=np.float32)
    cc = np.asarray(inp["c"], dtype=np.float32)
    c_ctx = np.asarray(inp["c_ctx"], dtype=np.float32)
    vecs = np.stack([cc[2 * c], cc[2 * c + 1], c_ctx], 0)
    cT = np.ascontiguousarray(vecs.reshape(3, 8, 128).transpose(2, 1, 0))
    m = dict(shared)
    m["x"] = np.ascontiguousarray(x[2 * c:2 * c + 2])
    m["ctx"] = np.ascontiguousarray(ctx[2 * c:2 * c + 2])
    m["cT"] = cT
    return m


def kernel(**inputs):
    nc = build()
    shared = _prep_shared(inputs)
    in_maps = [_core_inputs(inputs, shared, c) for c in range(NCORES)]
    res = run_bass_kernel_spmd(nc, in_maps, core_ids=list(range(NCORES)))
    out = np.concatenate([np.asarray(r["out"]) for r in res.results], axis=0)
    return out.astype(np.float32)
```

```python
import contextlib
import math
import numpy as np
import concourse.bass as bass
import concourse.mybir as mybir
from concourse.bass_utils import run_bass_kernel_spmd

F32 = mybir.dt.float32
BF16 = mybir.dt.bfloat16
I32 = mybir.dt.int32
AF = mybir.ActivationFunctionType
ALU = mybir.AluOpType
AX = mybir.AxisListType

ENGS = ["tensor", "vector", "scalar", "gpsimd", "sync"]
LN_EPS = 1e-5
DN_ALPHA = 2.0 ** 0.25
NEG = -30000.0
LN16 = math.log(16.0)
NCORES = 8


import types


def _freeze(fn):
    if fn is None or fn.__closure__ is None:
        return fn
    cells = []
    for c in fn.__closure__:
        try:
            cells.append(types.CellType(c.cell_contents))
        except ValueError:
            cells.append(c)
    return types.FunctionType(fn.__code__, fn.__globals__, fn.__name__, fn.__defaults__, tuple(cells))


class Buf:
    __slots__ = ("w", "r", "pr")

    def __init__(self):
        self.w = []
        self.r = []
        self.pr = []


class Prog:
    def __init__(self, nc):
        self.nc = nc
        self.es = contextlib.ExitStack()
        self.ops = {e: [] for e in ENGS}
        self.cnt = {e: 0 for e in ENGS}
        self.dsem = {}
        self.known = {e: {} for e in ENGS}
        self.semobj = {}
        self.stack = [self.es]

    def _nm(self, name):
        self.uid = getattr(self, "uid", 0) + 1
        return f"t{self.uid}_{name}"

    def sb(self, name, shape, dt):
        return self.stack[-1].enter_context(self.nc.sbuf_tensor(self._nm(name), list(shape), dt))

    def ps(self, name, shape, dt=F32):
        return self.stack[-1].enter_context(self.nc.psum_tensor(self._nm(name), list(shape), dt))

    @contextlib.contextmanager
    def phase(self):
        es = contextlib.ExitStack()
        self.stack.append(es)
        with es:
            yield
            self.barrier()
        self.stack.pop()

    def _deps(self, eng, reads, writes, nowaw):
        deps = {}

        def add(ev):
            k, v = ev
            if deps.get(k, 0) < v:
                deps[k] = v
        for b in reads:
            for ev in b.w:
                add(ev)
        for b in writes:
            for ev in b.r:
                add(ev)
            if nowaw:
                for ev in b.pr:
                    add(ev)
            else:
                for ev in b.w:
                    add(ev)
        out = []
        kn = self.known[eng]
        for k, v in deps.items():
            if k == ("E", "tensor") and eng == "tensor":
                continue
            if kn.get(k, 0) >= v:
                continue
            kn[k] = v
            out.append((k, v))
        return out

    def _commit(self, ev, reads, writes, nowaw):
        for b in reads:
            b.r.append(ev)
        for b in writes:
            if nowaw:
                b.w.append(ev)
            else:
                b.pr = b.r
                b.r = []
                b.w = [ev]

    def op(self, eng, fn, reads=(), writes=(), nowaw=False):
        waits = self._deps(eng, reads, writes, nowaw)
        self.cnt[eng] += 1
        ev = (("E", eng), self.cnt[eng])
        self.ops[eng].append((waits, _freeze(fn), ("E", eng), 1))
        self._commit(ev, reads, writes, nowaw)
        return ev

    def dma(self, eng, fn, slot, reads=(), writes=(), nowaw=False):
        waits = self._deps(eng, reads, writes, nowaw)
        self.dsem[slot] = self.dsem.get(slot, 0) + 16
        ev = (("D", slot), self.dsem[slot])
        self.ops[eng].append((waits, _freeze(fn), ("D", slot), 16))
        self._commit(ev, reads, writes, nowaw)
        return ev

    def barrier(self):
        for e in ENGS:
            waits = []
            kn = self.known[e]
            for e2 in ENGS:
                k = ("E", e2)
                v = self.cnt[e2]
                if e2 != e and v > 0 and kn.get(k, 0) < v:
                    kn[k] = v
                    waits.append((k, v))
            for slot, v in self.dsem.items():
                k = ("D", slot)
                if kn.get(k, 0) < v:
                    kn[k] = v
                    waits.append((k, v))
            if waits:
                self.ops[e].append((waits, None, None, 0))

    def emit(self):
        nc = self.nc
        for e in ENGS:
            self.semobj[("E", e)] = self.es.enter_context(nc.semaphore("e_" + e))
        for slot in self.dsem:
            self.semobj[("D", slot)] = self.es.enter_context(nc.semaphore("d_" + str(slot)))
        block = self.es.enter_context(nc.Block())
        for e in ENGS:
            ops = self.ops[e]
            if not ops:
                continue

            def body(engine, ops=ops):
                for waits, fn, sk, inc in ops:
                    for k, v in waits:
                        engine.wait_ge(self.semobj[k], v)
                    if fn is not None:
                        fn(engine).then_inc(self.semobj[sk], inc)
            getattr(block, e)(body)


class Ring:
    def __init__(self, P, name, n, shape, dt, psum=False):
        self.t = [(P.ps if psum else P.sb)(f"{name}{i}", shape, dt) for i in range(n)]
        self.b = [Buf() for _ in range(n)]
        self.i = 0
        self.n = n

    def next(self):
        i = self.i
        self.i = (i + 1) % self.n
        self.last = i
        return self.t[i], self.b[i]


C_RQ, C_RK, C_RV, C_RG, C_MQ, C_MK, C_MV, C_MO, C_MG, C_GR, C_GM = (
    0, 1024, 2048, 3072, 4096, 5120, 6144, 7168, 8192, 8208, 9232)

K_ID = 0
K_IOTA = 128
K_PF = 640
K_PB = 658
K_COSR = 676
K_SINR = 708
K_COSC = 740
K_SINC = 804
K_TRI = 868
K_BLK = 996
K_MF = 1092
K_MB = 1988
K_ONE = 2884
NCONST = 3012


def make_consts():
    c = np.zeros((128, NCONST), np.float32)
    p = np.arange(128)
    c[:, K_ID:K_ID + 128] = np.eye(128, dtype=np.float32)
    c[:, K_IOTA:K_IOTA + 512] = np.arange(512, dtype=np.float32)[None, :]
    for jf in range(18):
        c[:, K_PF + jf] = 128 * jf + p
        c[:, K_PB + jf] = (2048 + 128 * jf + p) if jf < 2 else (128 * (jf - 2) + p)
    q = (p % 64).astype(np.float64)
    freq = 10000.0 ** (-q / 64.0)
    sgn = np.where(p < 64, -1.0, 1.0)
    rows = np.arange(32, dtype=np.float64)
    cols = np.arange(64, dtype=np.float64)
    c[:, K_COSR:K_COSR + 32] = np.cos(freq[:, None] * rows[None, :])
    c[:, K_SINR:K_SINR + 32] = sgn[:, None] * np.sin(freq[:, None] * rows[None, :])
    c[:, K_COSC:K_COSC + 64] = np.cos(freq[:, None] * cols[None, :])
    c[:, K_SINC:K_SINC + 64] = sgn[:, None] * np.sin(freq[:, None] * cols[None, :])
    c[:, K_TRI:K_TRI + 128] = (p[:, None] < p[None, :]).astype(np.float32)
    c[:, K_BLK:K_BLK + 96] = 512.0 * np.arange(96, dtype=np.float32)[None, :]
    cc = np.arange(896)[None, :]
    c[:, K_MF:K_MF + 896] = np.where(cc - 384 - p[:, None] >= 0, 0.0, NEG)
    c[:, K_MB:K_MB + 896] = np.where(p[:, None] - (cc - 384) > 0, 0.0, NEG)
    c[:, K_ONE:K_ONE + 128] = 1.0
    return c


def build(dbg=None):
    nc = bass.Bass("TRN2", target_bir_lowering=False)
    P = Prog(nc)

    def din(name, shape, dt=F32):
        return nc.dram_tensor(name, list(shape), dt, kind="ExternalInput").ap()

    def dscr(name, shape, dt=F32):
        return nc.dram_tensor(name, list(shape), dt).ap()

    x_d = din("x", [2, 2048, 1024])
    ctx_d = din("ctx", [2, 256, 1024])
    cT_d = din("cT", [128, 8, 3])
    w_ada_d = din("w_ada", [1024, 6144])
    b_ada_d = din("b_ada", [1, 6144])
    w_in_d = din("w_in", [1024, 10256])
    bmg_d = din("bmg", [4, 4])
    cw_d = din("cw", [128, 16, 3])
    cb_d = din("cb", [128, 16])
    rdl_d = din("rdl", [1, 8])
    w_rb_d = din("w_rb", [1024, 1024])
    w_mb_d = din("w_mb", [1024, 1024])
    w_out_d = din("w_out", [1024, 1024])
    lnp_d = din("lnp", [4, 1024])
    w_rt_d = din("w_rt", [1024, 36])
    b_rt_d = din("b_rt", [1, 36])
    w_e1_d = din("w_e1", [32, 1024, 512])
    w_e3_d = din("w_e3", [32, 1024, 512])
    w_e2_d = din("w_e2", [32, 512, 1024])
    consts_d = din("consts", [128, NCONST])
    out_d = nc.dram_tensor("out", [2, 2048, 1024], F32, kind="ExternalOutput").ap()

    mod_d = dscr("mod_s", [3, 6144])
    r_d = dscr("r_s", [2, 8, 128, 2048], BF16)
    y_d = dscr("y_s", [8, 128, 2048], BF16)
    x1_d = dscr("x1_s", [4096, 1024])
    u2_d = dscr("u2_s", [4096, 1024], BF16)
    xs_d = dscr("xs_s", [24576, 1024], BF16)
    ys_d = dscr("ys_s", [24576, 1024])
    dbg_outs = {}
    if dbg:
        for name, shape in dbg.items():
            dbg_outs[name] = nc.dram_tensor(name, list(shape), F32, kind="ExternalOutput").ap()

    bmod = Buf()
    br_d = [Buf(), Buf()]
    bx1_d = Buf()
    by_d = Buf()
    bu2_d = Buf()
    bxs_d = Buf()
    bys_d = Buf()
    bout = Buf()
    bdbg = Buf()

    with P.es:
        cst = P.sb("cst", [128, NCONST], F32)
        bcst = Buf()
        P.dma("sync", lambda e: e.dma_start(out=cst[:], in_=consts_d), "cst", writes=[bcst])
        identb = P.sb("identb", [128, 128], BF16)
        bidb = Buf()
        P.op("vector", lambda e: e.tensor_copy(out=identb[:], in_=cst[:, K_ID:K_ID + 128]), reads=[bcst], writes=[bidb])
        identf = cst[:, K_ID:K_ID + 128]
        trib = P.sb("trib", [128, 128], BF16)
        onesb = P.sb("onesb", [128, 128], BF16)
        P.op("vector", lambda e: e.tensor_copy(out=trib[:], in_=cst[:, K_TRI:K_TRI + 128]), reads=[bcst], writes=[bidb], nowaw=True)
        P.op("vector", lambda e: e.tensor_copy(out=onesb[:], in_=cst[:, K_ONE:K_ONE + 128]), reads=[bcst], writes=[bidb], nowaw=True)
        uT = P.sb("uT", [128, 8, 2304], BF16)
        buT = [Buf() for _ in range(5)]
        modc = P.sb("modc", [128, 6, 8], F32)
        bmodc = Buf()
        lgc = P.sb("lgc", [128, 8], F32)
        nlgc = P.sb("nlgc", [128, 8], F32)
        blgc = Buf()
        cw = P.sb("cw", [128, 16, 3], F32)
        cbias = P.sb("cbias", [128, 16], F32)
        bcw = Buf()
        P.dma("sync", lambda e: e.dma_start(out=cw[:], in_=cw_d), "cw", writes=[bcw])
        P.dma("sync", lambda e: e.dma_start(out=cbias[:], in_=cb_d), "cw", writes=[bcw], nowaw=True)
        bmg = P.sb("bmg", [4, 4], F32)
        bbmg = Buf()
        P.dma("sync", lambda e: e.dma_start(out=bmg[:], in_=bmg_d), "bmg", writes=[bbmg])
        logits = P.sb("logits", [128, 32, 36], F32)
        blog = Buf()
        wrt = P.sb("wrt", [128, 8, 36], F32)
        brt = P.sb("brt", [128, 36], F32)
        bwrt = Buf()
        P.dma("sync", lambda e: e.dma_start(out=wrt[:], in_=w_rt_d.rearrange("(c p) n -> p c n", p=128)), "wrt", writes=[bwrt])
        P.dma("sync", lambda e: e.dma_start(out=brt[:], in_=b_rt_d.partition_broadcast(128)), "wrt", writes=[bwrt], nowaw=True)

        P.dma("sync", lambda e: e.dma_start(out=lgc[:], in_=rdl_d.partition_broadcast(128)), "lgc", writes=[blgc])
        P.op("scalar", lambda e: e.activation(out=nlgc[:], in_=lgc[:], func=AF.Exp, scale=-1.0), reads=[blgc], writes=[blgc])
        P.op("scalar", lambda e: e.activation(out=nlgc[:], in_=nlgc[:], func=AF.Ln, bias=1.0, scale=1.0), reads=[blgc], writes=[blgc])
        P.op("vector", lambda e: e.tensor_scalar(out=lgc[:], in0=nlgc[:], scalar1=-1.0, scalar2=None, op0=ALU.mult), reads=[blgc], writes=[blgc])

        with P.phase():
            cT = P.sb("cT", [128, 8, 3], F32)
            bcT = Buf()
            P.dma("sync", lambda e: e.dma_start(out=cT[:], in_=cT_d), "cT", writes=[bcT])
            P.op("scalar", lambda e: e.activation(out=cT[:], in_=cT[:], func=AF.Silu), reads=[bcT], writes=[bcT])
            bada = P.sb("bada", [3, 6144], F32)
            bbada = Buf()
            P.dma("sync", lambda e: e.dma_start(out=bada[:], in_=b_ada_d.partition_broadcast(3)), "bada", writes=[bbada])
            modsb = P.sb("modsb", [3, 6144], F32)
            bmodsb = Buf()
            wa = Ring(P, "wa", 2, [128, 8, 512], F32)
            pmod = Ring(P, "pmod", 2, [128, 512], F32, psum=True)
            for cg in range(12):
                wt, bw = wa.next()
                P.dma("sync", lambda e, wt=wt, cg=cg: e.dma_start(
                    out=wt[:], in_=w_ada_d[:, 512 * cg:512 * cg + 512].rearrange("(c p) n -> p c n", p=128)),
                    f"wa{cg % 2}", writes=[bw])
                pt, bp = pmod.next()
                for k in range(8):
                    P.op("tensor", lambda e, pt=pt, wt=wt, k=k: e.matmul(pt[0:3, :], lhsT=cT[:, k, :], rhs=wt[:, k, :], start=(k == 0), stop=(k == 7)),
                         reads=[bcT, bw], writes=[bp])
                P.op("vector", lambda e, pt=pt, cg=cg: e.tensor_tensor(out=modsb[:, 512 * cg:512 * cg + 512], in0=pt[0:3, :], in1=bada[:, 512 * cg:512 * cg + 512], op=ALU.add),
                     reads=[bp, bbada], writes=[bmodsb], nowaw=True)
            P.dma("gpsimd", lambda e: e.dma_start(out=mod_d, in_=modsb[:]), "mod", reads=[bmodsb], writes=[bmod])
            for v in range(3):
                for which in range(2):
                    P.dma("gpsimd", lambda e, v=v, which=which: e.dma_start(
                        out=modc[:, 2 * v + which, :], in_=mod_d[v, 1024 * which:1024 * which + 1024].rearrange("(c p) -> p c", p=128),
                        allow_slow_non_contiguous=True), "modc", reads=[bmod], writes=[bmodc], nowaw=True)
            for v in range(3):
                P.op("vector", lambda e, v=v: e.tensor_scalar(out=modc[:, 2 * v + 1, :], in0=modc[:, 2 * v + 1, :], scalar1=1.0, scalar2=None, op0=ALU.add),
                     reads=[bmodc], writes=[bmodc])


        selp = P.sb("selp", [4, 4, 128], F32)
        seln = P.sb("seln", [4, 4, 128], F32)
        bsel = Buf()
        P.op("vector", lambda e: e.tensor_copy(out=selp[:], in_=cst[0:4, K_ID:K_ID + 4].unsqueeze(2).broadcast_to([4, 4, 128])), reads=[bcst], writes=[bsel])
        P.op("vector", lambda e: e.tensor_scalar(out=seln[:], in0=selp[:], scalar1=-1.0, scalar2=None, op0=ALU.mult), reads=[bsel], writes=[bsel])

        def V(fn, r=(), w=(), **kw):
            return P.op("vector", fn, reads=r, writes=w, **kw)

        def S(fn, r=(), w=(), **kw):
            return P.op("scalar", fn, reads=r, writes=w, **kw)

        def G(fn, r=(), w=(), **kw):
            return P.op("gpsimd", fn, reads=r, writes=w, **kw)

        def T(fn, r=(), w=(), **kw):
            return P.op("tensor", fn, reads=r, writes=w, **kw)

        def ln_stats(src, bsrc, s6, bs6, m, bm, r, brs):
            V(lambda e: e.bn_stats(out=s6[:, 0, :], in_=src[:, 0:512]), [bsrc], [bs6])
            V(lambda e: e.bn_stats(out=s6[:, 1, :], in_=src[:, 512:1024]), [bsrc], [bs6], nowaw=True)
            V(lambda e: e.bn_aggr(out=m[:], in_=s6[:].rearrange("p a b -> p (a b)")), [bs6], [bm])
            S(lambda e: e.activation(out=r[:], in_=m[:, 1:2], func=AF.Sqrt, bias=LN_EPS, scale=1.0), [bm], [brs])
            V(lambda e: e.reciprocal(out=r[:], in_=r[:]), [brs], [brs])

        def make_wload(wst, wbf):
            def load_w(src, c0, swap=False):
                wt, bw = wst.next()
                P.dma("sync", lambda e: e.dma_start(out=wt[:], in_=src[:, c0:c0 + 128].rearrange("(c p) n -> p c n", p=128)),
                      f"wst{wst.last}", writes=[bw])
                wb, bwb = wbf.next()
                S(lambda e: e.copy(out=wb[:], in_=wt[:]), [bw], [bwb])
                if swap:
                    wb2, bwb2 = wbf.next()
                    S(lambda e: e.copy(out=wb2[:, :, 0:64], in_=wt[:, :, 64:128]), [bw], [bwb2])
                    S(lambda e: e.copy(out=wb2[:, :, 64:128], in_=wt[:, :, 0:64]), [bw], [bwb2], nowaw=True)
                    return (wb, bwb), (wb2, bwb2)
                return wb, bwb
            return load_w

        for b in range(2):
            with P.phase():
                T2 = [P.sb(f"T2_{d}", [4, 2304], F32) for d in range(2)]
                bT2 = [Buf(), Buf()]
                acol = [P.sb(f"acol{d}", [128, 18, 4], F32) for d in range(2)]
                bacol = [Buf(), Buf()]
                em = [P.sb(f"em{d}", [128, 4], F32) for d in range(2)]
                bem = [Buf(), Buf()]
                pA = Ring(P, "pA", 3, [128, 512], F32, psum=True)
                pO = [P.ps(f"pO{i}", [128, 512], F32) for i in range(4)]
                bpO = [Buf() for _ in range(4)]
                pT = P.ps("pT", [128, 8, 128], BF16)
                bpT = Buf()
                with P.phase():
                    xr = Ring(P, "xr", 2, [128, 1024], F32)
                    xn = Ring(P, "xn", 2, [128, 1024], BF16)
                    st6 = Ring(P, "st6", 2, [128, 2, 6], F32)
                    mv = Ring(P, "mv", 2, [128, 2], F32)
                    rs = Ring(P, "rs", 2, [128, 1], F32)
                    tmpm = Ring(P, "tmpm", 2, [128, 8, 128], F32)
                    for j in range(18):
                        xt, bx = xr.next()
                        src = ctx_d[b, 128 * j:128 * j + 128, :] if j < 2 else x_d[b, 128 * (j - 2):128 * (j - 2) + 128, :]
                        P.dma("sync", lambda e, xt=xt, src=src: e.dma_start(out=xt[:], in_=src), f"xr{j % 2}", writes=[bx])
                        s6, bs6 = st6.next()
                        m, bm = mv.next()
                        r, brs = rs.next()
                        ln_stats(xt, bx, s6, bs6, m, bm, r, brs)
                        xb, bxb = xn.next()
                        V(lambda e, xb=xb, xt=xt, m=m, r=r: e.tensor_scalar(out=xb[:], in0=xt[:], scalar1=m[:, 0:1], scalar2=r[:, 0:1], op0=ALU.subtract, op1=ALU.mult),
                          [bx, bm, brs], [bxb])
                        for k in range(8):
                            T(lambda e, xb=xb, k=k: e.transpose(out=pT[:, k, :], in_=xb[:, 128 * k:128 * k + 128], identity=identb[:]),
                              [bxb, bidb], [bpT], nowaw=(k > 0))
                        v = 2 if j < 2 else b
                        tm, btm = tmpm.next()
                        V(lambda e, tm=tm, v=v: e.tensor_tensor(out=tm[:], in0=pT[:], in1=modc[:, 2 * v + 1, :].unsqueeze(2).broadcast_to([128, 8, 128]), op=ALU.mult),
                          [bpT, bmodc], [btm])
                        ch = 0 if j < 2 else 1 + (j - 2) // 4
                        G(lambda e, tm=tm, v=v, j=j: e.tensor_tensor(out=uT[:, :, 128 * j:128 * j + 128], in0=tm[:], in1=modc[:, 2 * v, :].unsqueeze(2).broadcast_to([128, 8, 128]), op=ALU.add),
                          [btm, bmodc], [buT[ch]], nowaw=True)

                    wgs = P.sb("wgs", [128, 8, 16], F32)
                    wgb = P.sb("wgb", [128, 8, 16], BF16)
                    bwg = Buf()
                    P.dma("sync", lambda e: e.dma_start(out=wgs[:], in_=w_in_d[:, C_MG:C_MG + 16].rearrange("(c p) n -> p c n", p=128)), "wg", writes=[bwg])
                    V(lambda e: e.tensor_copy(out=wgb[:], in_=wgs[:]), [bwg], [bwg])
                    T0 = P.sb("T0", [4, 2304], F32)
                    T1 = P.sb("T1", [4, 2304], F32)
                    ones4 = P.sb("ones4", [4, 2304], F32)
                    bT0 = Buf()
                    bT1 = Buf()
                    bones4 = Buf()
                    V(lambda e: e.memset(ones4[:], 1.0), [], [bones4])
                    mx = P.sb("mx", [4, 2], F32)
                    bmx = Buf()
                    mrow = P.sb("mrow", [4, 128], F32)
                    bmrow = Buf()
                    chunks = [(0, 256)] + [(256 + 512 * n, 512) for n in range(4)]
                    for d in range(2):
                        for gi, Tt, bT in ((2 * d, T0, bT0), (2 * d + 1, T1, bT1)):
                            for ci, (c0, cl) in enumerate(chunks):
                                pt, bp = pA.next()
                                for k in range(8):
                                    T(lambda e, pt=pt, k=k, gi=gi, c0=c0, cl=cl: e.matmul(pt[0:4, 0:cl], lhsT=wgb[:, k, 4 * gi:4 * gi + 4], rhs=uT[:, k, c0:c0 + cl], start=(k == 0), stop=(k == 7)),
                                      [bwg, buT[ci]], [bp])
                                if d == 0:
                                    o0 = c0
                                else:
                                    o0 = 2048 if ci == 0 else c0 - 256
                                V(lambda e, pt=pt, Tt=Tt, gi=gi, o0=o0, cl=cl: e.tensor_scalar(out=Tt[:, o0:o0 + cl], in0=pt[0:4, 0:cl], scalar1=bmg[:, gi:gi + 1], scalar2=None, op0=ALU.add),
                                  [bp, bbmg], [bT], nowaw=(ci > 0))
                        S(lambda e: e.activation(out=T1[:], in_=T1[:], func=AF.Exp, scale=-1.0), [bT1], [bT1])
                        S(lambda e: e.activation(out=T1[:], in_=T1[:], func=AF.Ln, bias=1.0, scale=1.0), [bT1], [bT1])
                        V(lambda e, d=d: e.tensor_tensor_scan(out=T2[d][:], data0=ones4[:], data1=T1[:], initial=0.0, op0=ALU.mult, op1=ALU.add),
                          [bT1, bones4], [bT2[d]])
                        if d == 1:
                            V(lambda e: e.tensor_tensor(out=T2[1][:], in0=T2[1][:], in1=T1[:], op=ALU.subtract), [bT2[1], bT1], [bT2[1]])
                        V(lambda e: e.tensor_reduce(out=mx[:, 0:1], in_=T0[:], axis=AX.X, op=ALU.max), [bT0], [bmx])
                        V(lambda e: e.tensor_scalar(out=mx[:, 1:2], in0=mx[:, 0:1], scalar1=LN16, scalar2=None, op0=ALU.add), [bmx], [bmx])
                        V(lambda e, d=d: e.scalar_tensor_tensor(out=T0[:], in0=T0[:], scalar=mx[:, 1:2], in1=T2[d][:], op0=ALU.subtract, op1=(ALU.add if d == 0 else ALU.subtract)),
                          [bT0, bmx, bT2[d]], [bT0])
                        pt, bp = pA.next()
                        for jo in range(18):
                            jf = jo if d == 0 else (jo + 2 if jo < 16 else jo - 16)
                            T(lambda e, pt=pt, jo=jo, jf=jf: e.transpose(out=pt[:, 4 * jf:4 * jf + 4], in_=T0[0:4, 128 * jo:128 * jo + 128], identity=identf[0:4, 0:4]),
                              [bT0, bcst], [bp], nowaw=(jo > 0))
                        S(lambda e, pt=pt, d=d: e.copy(out=acol[d][:].rearrange("p a b -> p (a b)"), in_=pt[:, 0:72]), [bp], [bacol[d]])
                        V(lambda e: e.tensor_scalar(out=mrow[:], in0=ones4[:, 0:128], scalar1=mx[:, 0:1], scalar2=-1.0, op0=ALU.mult, op1=ALU.mult), [bmx, bones4], [bmrow])
                        pt, bp = pA.next()
                        T(lambda e, pt=pt: e.transpose(out=pt[:, 0:4], in_=mrow[0:4, :], identity=identf[0:4, 0:4]), [bmrow, bcst], [bp])
                        S(lambda e, pt=pt, d=d: e.activation(out=em[d][:], in_=pt[:, 0:4], func=AF.Exp), [bp], [bem[d]])

                if dbg and "uT" in dbg and b == 0:
                    du = P.sb("dbg_u", [128, 2304], F32)
                    bdu = Buf()
                    V(lambda e: e.tensor_copy(out=du[:], in_=uT[:, 0, :]), buT, [bdu])
                    P.dma("gpsimd", lambda e: e.dma_start(out=dbg_outs["uT"], in_=du[:]), "dbg", reads=[bdu], writes=[bdbg], nowaw=True)
                    da = P.sb("dbg_a", [128, 2, 76], F32)
                    bda = Buf()
                    for d in range(2):
                        V(lambda e, d=d: e.tensor_copy(out=da[:, d, 0:72], in_=acol[d][:].rearrange("p a b -> p (a b)")), [bacol[d]], [bda], nowaw=True)
                        V(lambda e, d=d: e.tensor_copy(out=da[:, d, 72:76], in_=em[d][:]), [bem[d]], [bda], nowaw=True)
                    P.dma("gpsimd", lambda e: e.dma_start(out=dbg_outs["acol"], in_=da[:].rearrange("p a b -> p (a b)")), "dbg", reads=[bda], writes=[bdbg], nowaw=True)
                    P.dma("gpsimd", lambda e: e.dma_start(out=dbg_outs["T2"][0:4, :], in_=T2[0][:]), "dbg", reads=[bT2[0]], writes=[bdbg], nowaw=True)
                    P.dma("gpsimd", lambda e: e.dma_start(out=dbg_outs["T2"][4:8, :], in_=T2[1][:]), "dbg", reads=[bT2[1]], writes=[bdbg], nowaw=True)

                with P.phase():
                    wst = Ring(P, "wst", 3, [128, 8, 128], F32)
                    wbf = Ring(P, "wbf", 4, [128, 8, 128], BF16)
                    load_w = make_wload(wst, wbf)
                    wvt = P.sb("wvt", [128, 8, 256], BF16)
                    bwv = Buf()
                    qT = P.sb("qT", [128, 2, 2048], BF16)
                    kT = P.sb("kT", [128, 2, 2304], BF16)
                    vv = P.sb("vv", [128, 18, 257], BF16)
                    gT = P.sb("gT", [128, 2, 2048], BF16)
                    bq, bk, bv, bg = Buf(), Buf(), Buf(), Buf()
                    V(lambda e: e.memset(vv[:, :, 256:257], 1.0), [], [bv])
                    raw = P.sb("raw", [128, 2050], F32)
                    rawc = P.sb("rawc", [128, 258], F32)
                    acc = P.sb("acc", [128, 2048], F32)
                    braw, brawc, bacc = Buf(), Buf(), Buf()
                    V(lambda e: e.memset(raw[:], 0.0), [], [braw])
                    V(lambda e: e.memset(rawc[:], 0.0), [], [brawc])
                    tA = Ring(P, "tA", 2, [128, 512], F32)
                    tB = Ring(P, "tB", 2, [128, 512], F32)
                    rowr = Ring(P, "rowr", 2, [128, 512], F32)
                    prer = Ring(P, "prer", 2, [128, 512], F32)
                    Dr = Ring(P, "Dr", 3, [128, 512], BF16)
                    atr = Ring(P, "atr", 3, [128, 512], BF16)
                    mhalf = P.sb("mhalf", [128, 4], F32)
                    bmhalf = Buf()
                    V(lambda e: e.memset(mhalf[:], -0.5), [], [bmhalf])
                    acolr = [P.sb(f"acolr{d}", [128, 18], F32) for d in range(2)]
                    bacolr = [Buf(), Buf()]
                    rawr = Ring(P, "rawr", 2, [128, 4, 257], F32)
                    dn = P.sb("dn", [128, 4], F32)
                    bdn = Buf()
                    hf = P.sb("hf", [128, 4, 256], F32)
                    hs = P.sb("hs", [128, 4, 256], F32)
                    hn = P.sb("hn", [128, 4, 256], BF16)
                    bhf, bhs, bhn = Buf(), Buf(), Buf()
                    s6h = P.sb("s6h", [128, 4, 6], F32)
                    mvh = P.sb("mvh", [128, 4, 2], F32)
                    rsh = P.sb("rsh", [128, 4], F32)
                    bs6h, bmvh, brsh = Buf(), Buf(), Buf()
                    ofm = Ring(P, "ofm", 2, [128, 2, 512], BF16)

                    def proj_fm(wb, bwb, c0, cl, ci):
                        pt, bp = pA.next()
                        for k in range(8):
                            T(lambda e, k=k: e.matmul(pt[:, 0:cl], lhsT=wb[:, k, :], rhs=uT[:, k, c0:c0 + cl], start=(k == 0), stop=(k == 7)),
                              [bwb, buT[ci]], [bp])
                        return pt, bp

                    def proj_v(base):
                        for vu in range(2):
                            wt, bw = wst.next()
                            P.dma("sync", lambda e, wt=wt, vu=vu: e.dma_start(out=wt[:], in_=w_in_d[:, base + 128 * vu:base + 128 * vu + 128].rearrange("(c p) n -> p c n", p=128)),
                                  f"wst{wst.last}", writes=[bw])
                            S(lambda e, wt=wt, vu=vu: e.copy(out=wvt[:, :, 128 * vu:128 * vu + 128], in_=wt[:]), [bw], [bwv], nowaw=(vu > 0))
                        for q2 in range(9):
                            pt, bp = pA.next()
                            for jj in range(2):
                                j = 2 * q2 + jj
                                ci = 0 if j < 2 else 1 + (j - 2) // 4
                                for k in range(8):
                                    T(lambda e, pt=pt, jj=jj, j=j, k=k: e.matmul(pt[:, 256 * jj:256 * jj + 256], lhsT=uT[:, k, 128 * j:128 * j + 128], rhs=wvt[:, k, :], start=(k == 0), stop=(k == 7)),
                                      [bwv, buT[ci]], [bp], nowaw=not (jj == 0 and k == 0))
                            S(lambda e, pt=pt, q2=q2: e.copy(out=vv[:, 2 * q2:2 * q2 + 2, 0:256], in_=pt[:, :].rearrange("p (a b) -> p a b", b=256)),
                              [bp], [bv], nowaw=True)

                    def proj_g(base, func):
                        for dc in range(2):
                            wb, bwb = load_w(w_in_d, base + 128 * dc)
                            for n in range(4):
                                pt, bp = proj_fm(wb, bwb, 256 + 512 * n, 512, n + 1)
                                S(lambda e, pt=pt, dc=dc, n=n: e.activation(out=gT[:, dc, 512 * n:512 * n + 512], in_=pt[:], func=func), [bp], [bg], nowaw=True)

                    def proj_rot(base, dstT, bdst, is_k):
                        for dc in range(2):
                            (wb, bwb), (wb2, bwb2) = load_w(w_in_d, base + 128 * dc, swap=True)
                            if is_k:
                                pt, bp = proj_fm(wb, bwb, 0, 256, 0)
                                S(lambda e, pt=pt, dc=dc: e.copy(out=dstT[:, dc, 0:256], in_=pt[:, 0:256]), [bp], [bdst], nowaw=True)
                            for n in range(4):
                                p1, bp1 = proj_fm(wb, bwb, 256 + 512 * n, 512, n + 1)
                                p2, bp2 = proj_fm(wb2, bwb2, 256 + 512 * n, 512, n + 1)
                                if dc == 0:
                                    cosv = cst[:, K_COSR + 8 * n:K_COSR + 8 * n + 8].unsqueeze(2).broadcast_to([128, 8, 64])
                                    sinv = cst[:, K_SINR + 8 * n:K_SINR + 8 * n + 8].unsqueeze(2).broadcast_to([128, 8, 64])
                                else:
                                    cosv = cst[:, K_COSC:K_COSC + 64].unsqueeze(1).broadcast_to([128, 8, 64])
                                    sinv = cst[:, K_SINC:K_SINC + 64].unsqueeze(1).broadcast_to([128, 8, 64])
                                t1, bt1 = tA.next()
                                t2, bt2 = tB.next()
                                V(lambda e, t1=t1, p1=p1, cosv=cosv: e.tensor_tensor(out=t1[:].rearrange("p (a b) -> p a b", b=64), in0=p1[:].rearrange("p (a b) -> p a b", b=64), in1=cosv, op=ALU.mult),
                                  [bp1, bcst], [bt1])
                                V(lambda e, t2=t2, p2=p2, sinv=sinv: e.tensor_tensor(out=t2[:].rearrange("p (a b) -> p a b", b=64), in0=p2[:].rearrange("p (a b) -> p a b", b=64), in1=sinv, op=ALU.mult),
                                  [bp2, bcst], [bt2])
                                off = (256 if is_k else 0) + 512 * n
                                G(lambda e, t1=t1, t2=t2, dc=dc, off=off: e.tensor_tensor(out=dstT[:, dc, off:off + 512], in0=t1[:], in1=t2[:], op=ALU.add),
                                  [bt1, bt2], [bdst], nowaw=True)

                    def conv_silu(rw, brw, L, ch, dst_ap, bdst):
                        a = acc[:, 0:L]
                        V(lambda e: e.tensor_scalar(out=a, in0=rw[:, 0:L], scalar1=cw[:, ch, 0:1], scalar2=None, op0=ALU.mult), [brw, bcw], [bacc])
                        V(lambda e: e.scalar_tensor_tensor(out=a, in0=rw[:, 1:L + 1], scalar=cw[:, ch, 1:2], in1=a, op0=ALU.mult, op1=ALU.add), [brw, bcw, bacc], [bacc])
                        V(lambda e: e.scalar_tensor_tensor(out=a, in0=rw[:, 2:L + 2], scalar=cw[:, ch, 2:3], in1=a, op0=ALU.mult, op1=ALU.add), [brw, bcw, bacc], [bacc])
                        S(lambda e: e.activation(out=dst_ap, in_=a, func=AF.Silu, bias=cbias[:, ch:ch + 1], scale=1.0), [bacc, bcw], [bdst], nowaw=True)

                    def proj_conv(base, dstT, bdst, is_k, chbase):
                        for dc in range(2):
                            wb, bwb = load_w(w_in_d, base + 128 * dc)
                            ch = chbase + dc
                            if is_k:
                                pt, bp = proj_fm(wb, bwb, 0, 256, 0)
                                S(lambda e, pt=pt: e.copy(out=rawc[:, 1:257], in_=pt[:, 0:256]), [bp], [brawc], nowaw=True)
                                conv_silu(rawc, brawc, 256, ch, dstT[:, dc, 0:256], bdst)
                            for n in range(4):
                                pt, bp = proj_fm(wb, bwb, 256 + 512 * n, 512, n + 1)
                                S(lambda e, pt=pt, n=n: e.copy(out=raw[:, 1 + 512 * n:1 + 512 * n + 512], in_=pt[:]), [bp], [braw], nowaw=True)
                            off = 256 if is_k else 0
                            conv_silu(raw, braw, 2048, ch, dstT[:, dc, off:off + 2048], bdst)

                    def attention(h, is_ml, br):
                        dq = []
                        if not is_ml:
                            V(lambda e: e.tensor_scalar(out=acolr[0][:], in0=cst[:, K_PF:K_PF + 18], scalar1=nlgc[:, h:h + 1], scalar2=-LN16, op0=ALU.mult, op1=ALU.add),
                              [bcst, blgc], [bacolr[0]])
                            V(lambda e: e.tensor_scalar(out=acolr[1][:], in0=cst[:, K_PB:K_PB + 18], scalar1=lgc[:, 4 + h:5 + h], scalar2=-LN16, op0=ALU.mult, op1=ALU.add),
                              [bcst, blgc], [bacolr[1]])
                        ctxs = {}

                        def group_ctx(g, d):
                            rowt, brow = rowr.next()
                            if is_ml:
                                pt, bp = pA.next()
                                c0 = 256 + 512 * g if d == 0 else 512 * g
                                sl = seln if d == 0 else selp
                                T(lambda e: e.matmul(pt[:, :], lhsT=sl[:, h, :], rhs=T2[d][:, c0:c0 + 512], start=True, stop=True), [bsel, bT2[d]], [bp])
                                S(lambda e: e.copy(out=rowt[:], in_=pt[:]), [bp], [brow])
                            else:
                                base = float(256 + 512 * g) if d == 0 else float(512 * g)
                                sc = lgc[:, h:h + 1] if d == 0 else nlgc[:, 4 + h:5 + h]
                                V(lambda e: e.tensor_scalar(out=rowt[:], in0=cst[:, K_IOTA:K_IOTA + 512], scalar1=base, scalar2=sc, op0=ALU.add, op1=ALU.mult), [bcst, blgc], [brow])
                            keys = list(range(0, 4 * g + 6)) if d == 0 else [0, 1] + list(range(4 * g + 2, 18))

                            def applies(jf, sub):
                                if jf < 2:
                                    return True
                                jl = jf - 2
                                qi = 4 * g + sub
                                return jl <= qi if d == 0 else jl >= qi
                            first = {sub: [jf for jf in keys if applies(jf, sub)][0] for sub in range(4)}
                            last = {sub: [jf for jf in keys if applies(jf, sub)][-1] for sub in range(4)}
                            ctxs[(g, d)] = (rowt, brow, keys, applies, first, last)

                        def emit_S(g, d, jf):
                            rowt, brow, keys, applies, first, last = ctxs[(g, d)]
                            ps, bps = pA.next()
                            for dc in range(2):
                                T(lambda e, dc=dc: e.matmul(ps[:, :], lhsT=kT[:, dc, 128 * jf:128 * jf + 128], rhs=qT[:, dc, 512 * g:512 * g + 512], start=(dc == 0), stop=(dc == 1)),
                                  [bk, bq], [bps])
                            jl = jf - 2
                            masked = jf >= 2 and 4 * g <= jl <= 4 * g + 3
                            src, bsrc = rowt, brow
                            if masked:
                                jj = jl - 4 * g
                                mk = (K_MF if d == 0 else K_MB) + 384 - 128 * jj
                                pre, bpre = prer.next()
                                G(lambda e: e.tensor_tensor(out=pre[:], in0=rowt[:], in1=cst[:, mk:mk + 512], op=ALU.add), [brow, bcst], [bpre])
                                src, bsrc = pre, bpre
                            Dt, bD = Dr.next()
                            if is_ml:
                                bias_ap, bbias = acol[d][:, jf, h:h + 1], bacol[d]
                            else:
                                bias_ap, bbias = acolr[d][:, jf:jf + 1], bacolr[d]
                            S(lambda e: e.activation(out=Dt[:], in_=src[:], func=AF.Exp, bias=bias_ap, scale=1.0), [bsrc, bbias], [bD])
                            at, bat = atr.next()
                            V(lambda e: e.tensor_tensor(out=at[:], in0=ps[:], in1=Dt[:], op=ALU.mult), [bps, bD], [bat])
                            return at, bat

                        def emit_AV(g, d, jf, at, bat):
                            rowt, brow, keys, applies, first, last = ctxs[(g, d)]
                            for sub in range(4):
                                if not applies(jf, sub):
                                    continue
                                T(lambda e, sub=sub, st=(jf == first[sub]), sp=(jf == last[sub]): e.matmul(pO[sub][:, 0:257], lhsT=at[:, 128 * sub:128 * sub + 128], rhs=vv[:, jf, :], start=st, stop=sp),
                                  [bat, bv], [bpO[sub]])
                            if jf == keys[-1]:
                                group_done(g, d)

                        def group_done(g, d):
                            raw, braw_ = rawr.next()
                            for sub in range(4):
                                S(lambda e, sub=sub: e.copy(out=raw[:, sub, :], in_=pO[sub][:, 0:257]), [bpO[sub]], [braw_], nowaw=(sub > 0))
                            if is_ml:
                                dq.append(lambda: S(lambda e: e.activation(out=dn[:], in_=raw[:, :, 256], func=AF.Abs), [braw_], [bdn]))
                                dq.append(lambda: V(lambda e: e.tensor_tensor(out=dn[:], in0=dn[:], in1=em[d][:, h:h + 1].broadcast_to([128, 4]), op=ALU.max), [bdn, bem[d]], [bdn]))
                                dq.append(lambda: V(lambda e: e.reciprocal(out=dn[:], in_=dn[:]), [bdn], [bdn]))
                                if d == 0:
                                    dq.append(lambda: V(lambda e: e.tensor_tensor(out=hf[:], in0=raw[:, :, 0:256], in1=dn[:].unsqueeze(2).broadcast_to([128, 4, 256]), op=ALU.mult), [braw_, bdn], [bhf]))
                                else:
                                    dq.append(lambda: V(lambda e: e.tensor_tensor(out=hs[:], in0=raw[:, :, 0:256], in1=dn[:].unsqueeze(2).broadcast_to([128, 4, 256]), op=ALU.mult), [braw_, bdn], [bhs]))
                                    dq.append(lambda: V(lambda e: e.tensor_tensor(out=hs[:], in0=hs[:], in1=hf[:], op=ALU.add), [bhs, bhf], [bhs]))
                            else:
                                if d == 0:
                                    dq.append(lambda: S(lambda e: e.copy(out=hf[:], in_=raw[:, :, 0:256]), [braw_], [bhf]))
                                else:
                                    dq.append(lambda: V(lambda e: e.tensor_tensor(out=hs[:], in0=raw[:, :, 0:256], in1=hf[:], op=ALU.add), [braw_, bhf], [bhs]))
                            if d == 1:
                                def st_stats():
                                    for sub in range(4):
                                        V(lambda e, sub=sub: e.bn_stats(out=s6h[:, sub, :], in_=hs[:, sub, :]), [bhs], [bs6h], nowaw=(sub > 0))

                                def st_aggr():
                                    for sub in range(4):
                                        V(lambda e, sub=sub: e.bn_aggr(out=mvh[:, sub, :], in_=s6h[:, sub, :]), [bs6h], [bmvh], nowaw=(sub > 0))

                                def st_eps():
                                    V(lambda e: e.tensor_scalar(out=rsh[:], in0=mvh[:, :, 1], scalar1=LN_EPS, scalar2=None, op0=ALU.add), [bmvh], [brsh])

                                def st_pow():
                                    G(lambda e: e.tensor_tensor(out=rsh[:], in0=rsh[:], in1=mhalf[:], op=ALU.pow), [brsh, bmhalf], [brsh])

                                def st_norm():
                                    for sub in range(4):
                                        V(lambda e, sub=sub: e.tensor_scalar(out=hn[:, sub, :], in0=hs[:, sub, :], scalar1=mvh[:, sub, 0:1], scalar2=rsh[:, sub:sub + 1], op0=ALU.subtract, op1=ALU.mult),
                                          [bhs, bmvh, brsh], [bhn], nowaw=(sub > 0))

                                def st_tr():
                                    for sub in range(4):
                                        for dc in range(2):
                                            T(lambda e, sub=sub, dc=dc: e.transpose(out=pT[:, 4 * dc + sub, :], in_=hn[:, sub, 128 * dc:128 * dc + 128], identity=identb[:]),
                                              [bhn, bidb], [bpT], nowaw=not (sub == 0 and dc == 0))

                                def st_out():
                                    of, bof = ofm.next()
                                    for dc in range(2):
                                        V(lambda e, dc=dc: e.tensor_tensor(out=of[:, dc, :].rearrange("p (s c) -> p s c", c=128), in0=pT[:, 4 * dc:4 * dc + 4, :], in1=gT[:, dc, 512 * g:512 * g + 512].rearrange("p (s c) -> p s c", c=128), op=ALU.mult),
                                          [bpT, bg], [bof], nowaw=(dc > 0))
                                    P.dma("gpsimd", lambda e: e.dma_start(out=r_d[br, 2 * h:2 * h + 2, :, 512 * g:512 * g + 512].rearrange("c p t -> p c t"), in_=of[:]),
                                          f"rsp{ofm.last}", reads=[bof], writes=[br_d[br]], nowaw=True)
                                dq.extend([st_stats, st_aggr, st_eps, st_pow, st_norm, st_tr, st_out])

                        tiles = []
                        for g in range(4):
                            for d in range(2):
                                keys_ = list(range(0, 4 * g + 6)) if d == 0 else [0, 1] + list(range(4 * g + 2, 18))
                                for jf in keys_:
                                    tiles.append((g, d, jf))
                        queue = []
                        for ti_, (g, d, jf) in enumerate(tiles):
                            for (g2_, d2_, _) in tiles[ti_:ti_ + 4]:
                                if (g2_, d2_) not in ctxs:
                                    group_ctx(g2_, d2_)
                            at, bat = emit_S(g, d, jf)
                            queue.append((g, d, jf, at, bat))
                            if len(queue) > 2:
                                emit_AV(*queue.pop(0))
                            if dq:
                                dq.pop(0)()
                        while queue:
                            emit_AV(*queue.pop(0))
                        while dq:
                            dq.pop(0)()

                    for h in range(4):
                        proj_rot(C_RQ + 256 * h, qT, bq, False)
                        proj_rot(C_RK + 256 * h, kT, bk, True)
                        proj_v(C_RV + 256 * h)
                        proj_g(C_RG + 256 * h, AF.Silu)
                        attention(h, False, 0)
                    for h in range(4):
                        proj_conv(C_MQ + 256 * h, qT, bq, False, 2 * h)
                        proj_conv(C_MK + 256 * h, kT, bk, True, 8 + 2 * h)
                        proj_v(C_MV + 256 * h)
                        proj_g(C_MO + 256 * h, AF.Sigmoid)
                        attention(h, True, 1)

            if dbg and "r" in dbg and b == 0:
                with P.phase():
                    dr = P.sb("dbg_r", [128, 2048], BF16)
                    drf = P.sb("dbg_rf", [128, 2048], F32)
                    bdr = Buf()
                    for br in range(2):
                        for c in range(8):
                            P.dma("sync", lambda e, br=br, c=c: e.dma_start(out=dr[:], in_=r_d[br, c, :, :]), "dbgl", reads=[br_d[br]], writes=[bdr])
                            V(lambda e: e.tensor_copy(out=drf[:], in_=dr[:]), [bdr], [bdr])
                            P.dma("gpsimd", lambda e, br=br, c=c: e.dma_start(out=dbg_outs["r"][br, c, :, :], in_=drf[:]), "dbg", reads=[bdr], writes=[bdbg], nowaw=True)

            with P.phase():
                pA = Ring(P, "pA", 4, [128, 512], F32, psum=True)
                wst = Ring(P, "wst", 3, [128, 8, 128], F32)
                wbf = Ring(P, "wbf", 6, [128, 8, 128], BF16)
                load_w = make_wload(wst, wbf)
                rfull = P.sb("rfull", [128, 8, 2048], BF16)
                mfull = P.sb("mfull", [128, 8, 2048], BF16)
                brf, bmf = Buf(), Buf()
                for c in range(8):
                    P.dma("sync", lambda e, c=c: e.dma_start(out=rfull[:, c, :], in_=r_d[0, c, :, :]), "rfl", reads=[br_d[0]], writes=[brf], nowaw=True)
                    P.dma("sync", lambda e, c=c: e.dma_start(out=mfull[:, c, :], in_=r_d[1, c, :, :]), "mfl", reads=[br_d[1]], writes=[bmf], nowaw=True)
                sgr = Ring(P, "sgr", 2, [128, 512], F32)
                t1r = Ring(P, "t1r", 2, [128, 512], F32)
                yor = Ring(P, "yor", 2, [128, 512], BF16)
                for oc in range(8):
                    ws = []
                    for (wsrc, gbase) in ((w_rb_d, C_GR), (w_mb_d, C_GM)):
                        ws.append((load_w(wsrc, 128 * oc), load_w(w_in_d, gbase + 128 * oc)))
                    for n in range(4):
                        tt = []
                        for bi, (src_t, bsrc_t) in enumerate(((rfull, brf), (mfull, bmf))):
                            (wb, bwb), (wg_, bwg_) = ws[bi]
                            pa, bpa = pA.next()
                            for k in range(8):
                                T(lambda e, pa=pa, wb=wb, k=k, src_t=src_t, n=n: e.matmul(pa[:, :], lhsT=wb[:, k, :], rhs=src_t[:, k, 512 * n:512 * n + 512], start=(k == 0), stop=(k == 7)), [bwb, bsrc_t], [bpa])
                            pg, bpg = pA.next()
                            for k in range(8):
                                T(lambda e, pg=pg, wg_=wg_, k=k, n=n: e.matmul(pg[:, :], lhsT=wg_[:, k, :], rhs=uT[:, k, 256 + 512 * n:256 + 512 * n + 512], start=(k == 0), stop=(k == 7)), [bwg_, buT[n + 1]], [bpg])
                            sg, bsg = sgr.next()
                            S(lambda e, sg=sg, pg=pg: e.activation(out=sg[:], in_=pg[:], func=AF.Sigmoid), [bpg], [bsg])
                            t1, bt1 = t1r.next()
                            V(lambda e, t1=t1, pa=pa, sg=sg: e.tensor_tensor(out=t1[:], in0=pa[:], in1=sg[:], op=ALU.mult), [bpa, bsg], [bt1])
                            tt.append((t1, bt1))
                        yo, byo = yor.next()
                        G(lambda e, yo=yo, a=tt[0][0], c=tt[1][0]: e.tensor_tensor(out=yo[:], in0=a[:], in1=c[:], op=ALU.add), [tt[0][1], tt[1][1]], [byo])
                        P.dma("gpsimd", lambda e, yo=yo, oc=oc, n=n: e.dma_start(out=y_d[oc, :, 512 * n:512 * n + 512], in_=yo[:]), f"yspill{yor.last}", reads=[byo], writes=[by_d], nowaw=True)

            with P.phase():
                pA = Ring(P, "pA", 3, [128, 512], F32, psum=True)
                pO = [P.ps(f"pO{i}", [128, 512], F32) for i in range(4)]
                bpO = [Buf() for _ in range(4)]
                wst = Ring(P, "wst", 3, [128, 8, 128], F32)
                woutb = P.sb("woutb", [128, 8, 1024], BF16)
                bwout = Buf()
                for c in range(8):
                    wt, bw = wst.next()
                    P.dma("sync", lambda e, wt=wt, c=c: e.dma_start(out=wt[:], in_=w_out_d[:, 128 * c:128 * c + 128].rearrange("(c p) n -> p c n", p=128)), f"wst{wst.last}", writes=[bw])
                    S(lambda e, wt=wt, c=c: e.copy(out=woutb[:, :, 128 * c:128 * c + 128], in_=wt[:]), [bw], [bwout], nowaw=True)
                bct = {}
                bbc = Buf()
                for name, src in (("g1", mod_d[b:b + 1, 2048:3072]), ("sh2", mod_d[b:b + 1, 3072:4096]), ("sc2", mod_d[b:b + 1, 4096:5120]),
                                  ("l1g", lnp_d[0:1, :]), ("l1b", lnp_d[1:2, :])):
                    t = P.sb("bc_" + name, [128, 1024], F32)
                    bct[name] = t
                    P.dma("sync", lambda e, t=t, src=src: e.dma_start(out=t[:], in_=src.partition_broadcast(128)), "bc", reads=[bmod], writes=[bbc], nowaw=True)
                V(lambda e: e.tensor_scalar(out=bct["sc2"][:], in0=bct["sc2"][:], scalar1=1.0, scalar2=None, op0=ALU.add), [bbc], [bbc])
                yTr = Ring(P, "yT", 2, [128, 8, 512], BF16)
                xtr = Ring(P, "xt2", 2, [128, 1024], F32)
                ztr = Ring(P, "zt", 2, [128, 1024], F32)
                x1tr = Ring(P, "x1t", 2, [128, 1024], F32)
                u2tr = Ring(P, "u2t", 2, [128, 1024], F32)
                u2br = Ring(P, "u2b", 2, [128, 1024], BF16)
                u2Tr = Ring(P, "u2T", 2, [128, 8, 128], F32)
                s6r = Ring(P, "s6b", 2, [128, 2, 6], F32)
                m2r = Ring(P, "m2b", 2, [128, 2], F32)
                r2r = Ring(P, "r2b", 2, [128, 1], F32)
                yTs = {}
                u2s_ = {}

                def part1(i):
                    n, sub = i // 4, i % 4
                    gi = b * 16 + i
                    if sub == 0:
                        yT, byT = yTr.next()
                        P.dma("sync", lambda e: e.dma_start(out=yT[:], in_=y_d[:, :, 512 * n:512 * n + 512].rearrange("c p t -> p c t")), f"yT{yTr.last}", reads=[by_d], writes=[byT])
                        yTs[n] = (yT, byT)
                    yT, byT = yTs[n]
                    xt, bxt = xtr.next()
                    P.dma("sync", lambda e: e.dma_start(out=xt[:], in_=x_d[b, 128 * i:128 * i + 128, :]), f"xt2{xtr.last}", writes=[bxt])
                    for half in range(2):
                        for k in range(8):
                            T(lambda e, half=half, k=k: e.matmul(pO[half][:, :], lhsT=yT[:, k, 128 * sub:128 * sub + 128], rhs=woutb[:, k, 512 * half:512 * half + 512], start=(k == 0), stop=(k == 7)),
                              [byT, bwout], [bpO[half]])
                    zt, bzt = ztr.next()
                    for half in range(2):
                        V(lambda e, half=half: e.tensor_tensor(out=zt[:, 512 * half:512 * half + 512], in0=pO[half][:, :], in1=bct["g1"][:, 512 * half:512 * half + 512], op=ALU.mult),
                          [bpO[half], bbc], [bzt], nowaw=(half > 0))
                    V(lambda e: e.scalar_tensor_tensor(out=zt[:], in0=xt[:], scalar=DN_ALPHA, in1=zt[:], op0=ALU.mult, op1=ALU.add), [bxt, bzt], [bzt])
                    s6, bs6 = s6r.next()
                    m2, bm2 = m2r.next()
                    r2, br2 = r2r.next()
                    ln_stats(zt, bzt, s6, bs6, m2, bm2, r2, br2)
                    x1t, bx1t = x1tr.next()
                    V(lambda e: e.tensor_scalar(out=x1t[:], in0=zt[:], scalar1=m2[:, 0:1], scalar2=r2[:, 0:1], op0=ALU.subtract, op1=ALU.mult), [bzt, bm2, br2], [bx1t])
                    G(lambda e: e.tensor_tensor(out=x1t[:], in0=x1t[:], in1=bct["l1g"][:], op=ALU.mult), [bx1t, bbc], [bx1t])
                    G(lambda e: e.tensor_tensor(out=x1t[:], in0=x1t[:], in1=bct["l1b"][:], op=ALU.add), [bx1t, bbc], [bx1t])
                    P.dma("gpsimd", lambda e: e.dma_start(out=x1_d[128 * gi:128 * gi + 128, :], in_=x1t[:]), f"x1st{x1tr.last}", reads=[bx1t], writes=[bx1_d], nowaw=True)
                    s6, bs6 = s6r.next()
                    m2, bm2 = m2r.next()
                    r2, br2 = r2r.next()
                    ln_stats(x1t, bx1t, s6, bs6, m2, bm2, r2, br2)
                    u2t, bu2t = u2tr.next()
                    V(lambda e: e.tensor_scalar(out=u2t[:], in0=x1t[:], scalar1=m2[:, 0:1], scalar2=r2[:, 0:1], op0=ALU.subtract, op1=ALU.mult), [bx1t, bm2, br2], [bu2t])
                    G(lambda e: e.tensor_tensor(out=u2t[:], in0=u2t[:], in1=bct["sc2"][:], op=ALU.mult), [bu2t, bbc], [bu2t])
                    G(lambda e: e.tensor_tensor(out=u2t[:], in0=u2t[:], in1=bct["sh2"][:], op=ALU.add), [bu2t, bbc], [bu2t])
                    u2b, bu2b = u2br.next()
                    S(lambda e: e.copy(out=u2b[:], in_=u2t[:]), [bu2t], [bu2b])
                    P.dma("gpsimd", lambda e: e.dma_start(out=u2_d[128 * gi:128 * gi + 128, :], in_=u2b[:]), f"u2st{u2br.last}", reads=[bu2b], writes=[bu2_d], nowaw=True)
                    u2s_[i] = (u2t, bu2t)

                def part2(i):
                    gi = b * 16 + i
                    u2t, bu2t = u2s_.pop(i)
                    for k in range(8):
                        pp = pO[2 + k // 4]
                        T(lambda e, pp=pp, k=k: e.transpose(out=pp[:, 128 * (k % 4):128 * (k % 4) + 128], in_=u2t[:, 128 * k:128 * k + 128], identity=identf),
                          [bu2t, bcst], [bpO[2 + k // 4]], nowaw=(k % 4 > 0))
                    u2T, bu2T = u2Tr.next()
                    S(lambda e: e.copy(out=u2T[:, 0:4, :].rearrange("p a b -> p (a b)"), in_=pO[2][:, :]), [bpO[2]], [bu2T])
                    V(lambda e: e.tensor_copy(out=u2T[:, 4:8, :].rearrange("p a b -> p (a b)"), in_=pO[3][:, :]), [bpO[3]], [bu2T], nowaw=True)
                    pl, bpl = pA.next()
                    for k in range(8):
                        T(lambda e, k=k: e.matmul(pl[:, 0:36], lhsT=u2T[:, k, :], rhs=wrt[:, k, :], start=(k == 0), stop=(k == 7)), [bu2T, bwrt], [bpl])
                    V(lambda e: e.tensor_tensor(out=logits[:, gi, :], in0=pl[:, 0:36], in1=brt[:], op=ALU.add), [bpl, bwrt], [blog], nowaw=True)

                for i in range(17):
                    if i < 16:
                        part1(i)
                    if i >= 1:
                        part2(i - 1)

        if dbg and "x1" in dbg:
            with P.phase():
                t = P.sb("dbg_x1", [128, 1024], F32)
                bt = Buf()
                for gi in range(32):
                    P.dma("sync", lambda e, gi=gi: e.dma_start(out=t[:], in_=x1_d[128 * gi:128 * gi + 128, :]), "dbgl", reads=[bx1_d], writes=[bt])
                    P.dma("gpsimd", lambda e, gi=gi: e.dma_start(out=dbg_outs["x1"][128 * gi:128 * gi + 128, :], in_=t[:]), "dbg", reads=[bt], writes=[bdbg], nowaw=True)
                lt = P.sb("dbg_lg", [128, 32 * 36], F32)
                V(lambda e: e.tensor_copy(out=lt[:], in_=logits[:].rearrange("p a b -> p (a b)")), [blog], [bt])
                P.dma("gpsimd", lambda e: e.dma_start(out=dbg_outs["logits"], in_=lt[:]), "dbg", reads=[bt], writes=[bdbg], nowaw=True)

        MOE_PLACEHOLDER = True

        d1i = P.sb("d1i", [128, 32], I32)
        d2i = P.sb("d2i", [128, 32], I32)
        wt1 = P.sb("wt1", [128, 32], F32)
        wt2 = P.sb("wt2", [128, 32], F32)
        bei = P.sb("bei", [128, 96], I32)
        bd12, bwt12, bbe = Buf(), Buf(), Buf()
        with P.phase():
            pA = Ring(P, "pA", 3, [128, 512], F32, psum=True)
            ppos = [P.ps(f"ppos{i}", [128, 16, 32], F32) for i in range(2)]
            bppos = [Buf(), Buf()]

            def R(name, shape, dt=F32):
                return P.sb("rt_" + name, shape, dt), Buf()
            gmax, bgmax = R("gmax", [128, 32])
            ohg, bohg = R("ohg", [128, 32, 4])
            eg, beg = R("eg", [128, 32, 4])
            pg, bpg_ = R("pg", [128, 32])
            tmp4, btmp4 = R("tmp4", [128, 32, 4, 8])
            les, bles = R("les", [128, 32, 8])
            m1, bm1 = R("m1", [128, 32])
            mk1, bmk1 = R("mk1", [128, 32, 8])
            le2, ble2 = R("le2", [128, 32, 8])
            m2_, bm2_ = R("m2", [128, 32])
            mk2, bmk2 = R("mk2", [128, 32, 8])
            sg_, bsg_ = R("sg", [128, 32])
            A1, bA1 = R("A1", [128, 32, 4, 8])
            A2, bA2 = R("A2", [128, 32, 4, 8])
            Ab, bAb = R("Ab", [128, 32, 32], BF16)
            posf, bposf = R("posf", [128, 32, 32])
            cnt, bcnt = R("cnt", [128, 32])
            cnti, bcnti = R("cnti", [128, 32], I32)
            padf, bpadf = R("padf", [128, 32])
            pend, bpend = R("pend", [128, 32])
            poff, bpoff = R("poff", [128, 32])
            ones32, bones32 = R("ones32", [128, 32])
            dfl, bdfl = R("dfl", [128, 32])
            cmp, bcmp = R("cmp", [128, 48, 32])
            bef, bbef = R("bef", [128, 48])
            lgv = logits[:, :, 0:4]
            lev = logits[:, :, 4:36].rearrange("p t (g e) -> p t g e", e=8)

            def bc3(ap, n):
                return ap.unsqueeze(2).broadcast_to([128, 32, n])
            V(lambda e: e.memset(ones32[:], 1.0), [], [bones32])
            V(lambda e: e.tensor_reduce(out=gmax[:], in_=lgv, axis=AX.X, op=ALU.max), [blog], [bgmax])
            V(lambda e: e.tensor_tensor(out=ohg[:], in0=lgv, in1=bc3(gmax[:], 4), op=ALU.is_equal), [blog, bgmax], [bohg])
            V(lambda e: e.tensor_tensor(out=eg[:], in0=lgv, in1=bc3(gmax[:], 4), op=ALU.subtract), [blog, bgmax], [beg])
            S(lambda e: e.activation(out=eg[:], in_=eg[:], func=AF.Exp), [beg], [beg])
            V(lambda e: e.tensor_reduce(out=pg[:], in_=eg[:], axis=AX.X, op=ALU.add), [beg], [bpg_])
            V(lambda e: e.reciprocal(out=pg[:], in_=pg[:]), [bpg_], [bpg_])
            V(lambda e: e.tensor_tensor(out=tmp4[:], in0=lev, in1=ohg[:].unsqueeze(3).broadcast_to([128, 32, 4, 8]), op=ALU.mult), [blog, bohg], [btmp4])
            V(lambda e: e.tensor_reduce(out=les[:], in_=tmp4[:].rearrange("p t g e -> p t e g"), axis=AX.X, op=ALU.add), [btmp4], [bles])
            V(lambda e: e.tensor_reduce(out=m1[:], in_=les[:], axis=AX.X, op=ALU.max), [bles], [bm1])
            V(lambda e: e.tensor_tensor(out=mk1[:], in0=les[:], in1=bc3(m1[:], 8), op=ALU.is_equal), [bles, bm1], [bmk1])
            V(lambda e: e.scalar_tensor_tensor(out=le2[:], in0=mk1[:], scalar=-1e30, in1=les[:], op0=ALU.mult, op1=ALU.add), [bmk1, bles], [ble2])
            V(lambda e: e.tensor_reduce(out=m2_[:], in_=le2[:], axis=AX.X, op=ALU.max), [ble2], [bm2_])
            V(lambda e: e.tensor_tensor(out=mk2[:], in0=le2[:], in1=bc3(m2_[:], 8), op=ALU.is_equal), [ble2, bm2_], [bmk2])
            V(lambda e: e.tensor_tensor(out=sg_[:], in0=m1[:], in1=m2_[:], op=ALU.subtract), [bm1, bm2_], [bsg_])
            S(lambda e: e.activation(out=sg_[:], in_=sg_[:], func=AF.Sigmoid), [bsg_], [bsg_])
            V(lambda e: e.tensor_tensor(out=wt1[:], in0=pg[:], in1=sg_[:], op=ALU.mult), [bpg_, bsg_], [bwt12])
            V(lambda e: e.tensor_tensor(out=wt2[:], in0=pg[:], in1=wt1[:], op=ALU.subtract), [bpg_, bwt12], [bwt12])
            for (Ax, bAx, mk, bmk) in ((A1, bA1, mk1, bmk1), (A2, bA2, mk2, bmk2)):
                V(lambda e, Ax=Ax, mk=mk: e.tensor_tensor(out=Ax[:], in0=ohg[:].unsqueeze(3).broadcast_to([128, 32, 4, 8]), in1=mk[:].unsqueeze(2).broadcast_to([128, 32, 4, 8]), op=ALU.mult),
                  [bohg, bmk], [bAx])
            A1f = A1[:].rearrange("p t g e -> p t (g e)")
            A2f = A2[:].rearrange("p t g e -> p t (g e)")
            V(lambda e: e.tensor_tensor(out=Ab[:], in0=A1f, in1=A2f, op=ALU.add), [bA1, bA2], [bAb])
            for ti in range(32):
                pp = ppos[ti // 16]
                bpp = bppos[ti // 16]
                for tj in range(ti):
                    T(lambda e, pp=pp, ti=ti, tj=tj: e.matmul(pp[:, ti % 16, :], lhsT=onesb[:], rhs=Ab[:, tj, :], start=(tj == 0), stop=False), [bidb, bAb], [bpp], nowaw=True)
                T(lambda e, pp=pp, ti=ti: e.matmul(pp[:, ti % 16, :], lhsT=trib[:], rhs=Ab[:, ti, :], start=(ti == 0), stop=True), [bidb, bAb], [bpp], nowaw=True)
            pc, bpc = pA.next()
            for tj in range(32):
                T(lambda e, pc=pc, tj=tj: e.matmul(pc[:, 0:32], lhsT=onesb[:], rhs=Ab[:, tj, :], start=(tj == 0), stop=(tj == 31)), [bidb, bAb], [bpc])
            S(lambda e: e.copy(out=posf[:, 0:16, :], in_=ppos[0][:]), [bppos[0]], [bposf])
            V(lambda e: e.tensor_copy(out=posf[:, 16:32, :], in_=ppos[1][:]), [bppos[1]], [bposf], nowaw=True)
            V(lambda e: e.tensor_scalar(out=cnti[:], in0=pc[:, 0:32], scalar1=511.0, scalar2=None, op0=ALU.add), [bpc], [bcnti])
            V(lambda e: e.tensor_single_scalar(out=cnti[:], in_=cnti[:], scalar=9, op=ALU.arith_shift_right), [bcnti], [bcnti])
            V(lambda e: e.tensor_single_scalar(out=cnti[:], in_=cnti[:], scalar=9, op=ALU.logical_shift_left), [bcnti], [bcnti])
            V(lambda e: e.tensor_copy(out=padf[:], in_=cnti[:]), [bcnti], [bpadf])
            V(lambda e: e.tensor_tensor_scan(out=pend[:], data0=ones32[:], data1=padf[:], initial=0.0, op0=ALU.mult, op1=ALU.add), [bones32, bpadf], [bpend])
            V(lambda e: e.tensor_tensor(out=poff[:], in0=pend[:], in1=padf[:], op=ALU.subtract), [bpend, bpadf], [bpoff])
            V(lambda e: e.tensor_tensor(out=posf[:], in0=posf[:], in1=poff[:].unsqueeze(1).broadcast_to([128, 32, 32]), op=ALU.add), [bposf, bpoff], [bposf])
            for (Af, bAx, di) in ((A1f, bA1, d1i), (A2f, bA2, d2i)):
                V(lambda e, Af=Af: e.tensor_tensor(out=tmp4[:].rearrange("p t g e -> p t (g e)"), in0=Af, in1=posf[:], op=ALU.mult), [bAx, bposf], [btmp4])
                V(lambda e: e.tensor_reduce(out=dfl[:], in_=tmp4[:].rearrange("p t g e -> p t (g e)"), axis=AX.X, op=ALU.add), [btmp4], [bdfl])
                V(lambda e, di=di: e.tensor_copy(out=di[:], in_=dfl[:]), [bdfl], [bd12], nowaw=True)
            V(lambda e: e.tensor_tensor(out=cmp[:], in0=pend[:].unsqueeze(1).broadcast_to([128, 48, 32]), in1=cst[:, K_BLK:K_BLK + 48].unsqueeze(2).broadcast_to([128, 48, 32]), op=ALU.is_le),
              [bpend, bcst], [bcmp])
            V(lambda e: e.tensor_reduce(out=bef[:], in_=cmp[:], axis=AX.X, op=ALU.add), [bcmp], [bbef])
            V(lambda e: e.tensor_scalar(out=bei[:, 0:48], in0=bef[:], scalar1=31.0, scalar2=None, op0=ALU.min), [bbef], [bbe])
            if dbg and "route" in dbg:
                rt = P.sb("dbg_rt", [128, 4, 32], F32)
                brt_ = Buf()
                V(lambda e: e.tensor_copy(out=rt[:, 0, :], in_=d1i[:]), [bd12], [brt_], nowaw=True)
                V(lambda e: e.tensor_copy(out=rt[:, 1, :], in_=d2i[:]), [bd12], [brt_], nowaw=True)
                V(lambda e: e.tensor_copy(out=rt[:, 2, :], in_=wt1[:]), [bwt12], [brt_], nowaw=True)
                V(lambda e: e.tensor_copy(out=rt[:, 3, :], in_=wt2[:]), [bwt12], [brt_], nowaw=True)
                P.dma("gpsimd", lambda e: e.dma_start(out=dbg_outs["route"], in_=rt[:].rearrange("p a b -> p (a b)")), "dbg", reads=[brt_], writes=[bdbg], nowaw=True)
                bt_ = P.sb("dbg_be", [128, 96], F32)
                V(lambda e: e.memset(bt_[:], 0.0), [], [brt_], nowaw=True)
                V(lambda e: e.tensor_copy(out=bt_[:, 0:48], in_=bei[:, 0:48]), [bbe], [brt_])
                P.dma("gpsimd", lambda e: e.dma_start(out=dbg_outs["be"], in_=bt_[:]), "dbg", reads=[brt_], writes=[bdbg], nowaw=True)
            u2s = Ring(P, "u2s", 2, [128, 1024], BF16)
            for ti in range(32):
                ut, but = u2s.next()
                P.dma("sync", lambda e, ut=ut, ti=ti: e.dma_start(out=ut[:], in_=u2_d[128 * ti:128 * ti + 128, :]), f"u2s{ti % 2}", reads=[bu2_d], writes=[but])
                for di in (d1i, d2i):
                    P.dma("gpsimd", lambda e, ut=ut, ti=ti, di=di: e.indirect_dma_start(
                        out=xs_d, out_offset=bass.IndirectOffsetOnAxis(ap=di[:, ti:ti + 1], axis=0), in_=ut[:], in_offset=None),
                        f"xsc{ti % 2}", reads=[but, bd12], writes=[bxs_d], nowaw=True)

        with P.phase():
            pA = Ring(P, "pA", 2, [128, 512], F32, psum=True)
            pY = [P.ps(f"pY{i}", [128, 512], F32) for i in range(4)]
            bpY = [Buf(), Buf()]
            pT = P.ps("pT", [128, 8, 128], BF16)
            bpT = Buf()
            pT2 = P.ps("pT2", [128, 8, 128], BF16)
            bpT2 = Buf()
            wstg = Ring(P, "wstg", 4, [128, 2048], F32)
            w1b = Ring(P, "w1b", 2, [128, 8, 512], BF16)
            w3b = Ring(P, "w3b", 2, [128, 8, 512], BF16)
            w2b = Ring(P, "w2b", 2, [128, 4, 1024], BF16)
            xsb = Ring(P, "xsb", 4, [128, 1024], BF16)
            xsTr = Ring(P, "xsT", 3, [128, 8, 128], BF16)
            hsl = P.sb("hsl", [128, 512], F32)
            bhsl = Buf()
            hhr = Ring(P, "hh", 3, [128, 512], BF16)
            hTr = Ring(P, "hT", 2, [128, 4, 128], BF16)
            ysb = Ring(P, "ysb", 2, [128, 1024], F32)
            regs = {}
            blocks = [(sbk, sub_) for sbk in range(48) for sub_ in range(4)]
            wsets = {}
            hhs = {}

            def load_weights(sbk):
                w1t, bw1 = w1b.next()
                w3t, bw3 = w3b.next()
                w2t, bw2 = w2b.next()
                wsets[sbk] = (w1t, bw1, w3t, bw3, w2t, bw2)
                idx = 0
                for (wd, wt_, bwt_, eng, is2) in ((w_e1_d, w1t, bw1, "scalar", False), (w_e3_d, w3t, bw3, "scalar", False), (w_e2_d, w2t, bw2, "gpsimd", True)):
                    for hf_ in range(2):
                        stg, bstg = wstg.next()

                        def dma_fn(e, wd=wd, hf_=hf_, stg=stg, is2=is2, idx=idx, sbk=sbk):
                            if idx == 0:
                                if "r" not in regs:
                                    regs["r"] = e.alloc_register("r_exp")
                                    regs["a"] = e.alloc_register("r_expa")
                                    regs["b"] = e.alloc_register("r_expb")
                                e.reg_load(regs["r"], bei[0:1, sbk:sbk + 1])
                                e.reg_mul(regs["a"], regs["r"], 524288)
                                e.reg_add(regs["b"], regs["a"], 262144)
                            rr = regs["a"] if hf_ == 0 else regs["b"]
                            if not is2:
                                src = bass.AP(wd.tensor, rr, [[512, 128], [65536, 4], [1, 512]])
                                ins = e.dma_start(out=stg[:].rearrange("p (c n) -> p c n", n=512), in_=src)
                            else:
                                src = bass.AP(wd.tensor, rr, [[1024, 128], [131072, 2], [1, 1024]])
                                ins = e.dma_start(out=stg[:].rearrange("p (c n) -> p c n", n=1024), in_=src)
                            RH = type(rr)
                            tmpn = [nm for grp in ins.ins.regs_accessed() for nm in grp if "_tmp_" in nm][0]
                            kk = int(tmpn.split("_")[-1])
                            e.free_register(RH(tmpn, rr.engine))
                            e.free_register(RH(f"SP_{rr.name}_snap_{kk - 2}", rr.engine))
                            return ins
                        P.dma("sync", dma_fn, f"wstg{wstg.last}", reads=[bbe], writes=[bstg])
                        if not is2:
                            dst = wt_[:, 4 * hf_:4 * hf_ + 4, :].rearrange("p c n -> p (c n)")
                        else:
                            dst = wt_[:, 2 * hf_:2 * hf_ + 2, :].rearrange("p c n -> p (c n)")
                        if eng == "scalar":
                            S(lambda e, dst=dst, stg=stg: e.copy(out=dst, in_=stg[:]), [bstg], [bwt_], nowaw=(hf_ > 0))
                        else:
                            P.op(eng, lambda e, dst=dst, stg=stg: e.tensor_copy(out=dst, in_=stg[:]), reads=[bstg], writes=[bwt_], nowaw=(hf_ > 0))
                        idx += 1

            xsTs = {}

            def stageTx(i):
                xb_, bxb_ = xs_tiles.pop(i)
                for k in range(8):
                    T(lambda e, k=k: e.transpose(out=pT[:, k, :], in_=xb_[:, 128 * k:128 * k + 128], identity=identb[:]), [bxb_, bidb], [bpT], nowaw=(k > 0))
                xsT, bxsT = xsTr.next()
                V(lambda e: e.tensor_copy(out=xsT[:], in_=pT[:]), [bpT], [bxsT])
                xsTs[i] = (xsT, bxsT)

            def stageH(i):
                sbk, sub_ = blocks[i]
                w1t, bw1, w3t, bw3, w2t, bw2 = wsets[sbk]
                xsT, bxsT = xsTs.pop(i)
                p1, bp1 = pA.next()
                p3, bp3 = pA.next()
                for k in range(8):
                    T(lambda e, k=k: e.matmul(p1[:, :], lhsT=xsT[:, k, :], rhs=w1t[:, k, :], start=(k == 0), stop=(k == 7)), [bxsT, bw1], [bp1])
                for k in range(8):
                    T(lambda e, k=k: e.matmul(p3[:, :], lhsT=xsT[:, k, :], rhs=w3t[:, k, :], start=(k == 0), stop=(k == 7)), [bxsT, bw3], [bp3])
                S(lambda e: e.activation(out=hsl[:], in_=p1[:], func=AF.Silu), [bp1], [bhsl])
                hh, bhh = hhr.next()
                V(lambda e: e.tensor_tensor(out=hh[:], in0=p3[:], in1=hsl[:], op=ALU.mult), [bp3, bhsl], [bhh])
                hhs[i] = (hh, bhh)

            hTs = {}

            def stageTh(i):
                hh, bhh = hhs.pop(i)
                for f in range(4):
                    T(lambda e, f=f: e.transpose(out=pT2[:, f, :], in_=hh[:, 128 * f:128 * f + 128], identity=identb[:]), [bhh, bidb], [bpT2], nowaw=(f > 0))
                hT, bhT = hTr.next()
                V(lambda e: e.tensor_copy(out=hT[:], in_=pT2[:, 0:4, :]), [bpT2], [bhT])
                hTs[i] = (hT, bhT)

            def stageY(i):
                sbk, sub_ = blocks[i]
                bk = 4 * sbk + sub_
                w1t, bw1, w3t, bw3, w2t, bw2 = wsets[sbk]
                hT, bhT = hTs.pop(i)
                par = bk % 2
                for half in range(2):
                    for f in range(4):
                        T(lambda e, half=half, f=f: e.matmul(pY[2 * par + half][:, :], lhsT=hT[:, f, :], rhs=w2t[:, f, 512 * half:512 * half + 512], start=(f == 0), stop=(f == 3)),
                          [bhT, bw2], [bpY[par]], nowaw=not (half == 0 and f == 0))
                yt_, byt_ = ysb.next()
                S(lambda e: e.copy(out=yt_[:, 0:512], in_=pY[2 * par][:, :]), [bpY[par]], [byt_])
                V(lambda e: e.tensor_copy(out=yt_[:, 512:1024], in_=pY[2 * par + 1][:, :]), [bpY[par]], [byt_], nowaw=True)
                P.dma("gpsimd", lambda e: e.dma_start(out=ys_d[128 * bk:128 * bk + 128, :], in_=yt_[:]), f"yst{ysb.last}", reads=[byt_], writes=[bys_d], nowaw=True)

            xs_tiles = {}

            def load_xs(i):
                sbk, sub_ = blocks[i]
                bk = 4 * sbk + sub_
                xb_, bxb_ = xsb.next()
                P.dma("gpsimd", lambda e: e.dma_start(out=xb_[:], in_=xs_d[128 * bk:128 * bk + 128, :]), f"xsb{xsb.last}", reads=[bxs_d], writes=[bxb_])
                xs_tiles[i] = (xb_, bxb_)
            load_xs(0)
            load_xs(1)
            load_weights(0)
            NB_ = len(blocks)
            for i in range(NB_ + 3):
                if i + 2 < NB_:
                    load_xs(i + 2)
                if i < NB_:
                    stageTx(i)
                if 1 <= i < NB_ + 1:
                    stageH(i - 1)
                if 2 <= i < NB_ + 2:
                    stageTh(i - 2)
                if i >= 3:
                    stageY(i - 3)
                if i >= 2 and (i - 2) % 4 == 0 and (i - 2) // 4 + 1 < 48:
                    load_weights((i - 2) // 4 + 1)

        with P.phase():
            bct = {}
            bbc = Buf()
            for name, src in (("g2_0", mod_d[0:1, 5120:6144]), ("g2_1", mod_d[1:2, 5120:6144]), ("l2g", lnp_d[2:3, :]), ("l2b", lnp_d[3:4, :])):
                t = P.sb("bc2_" + name, [128, 1024], F32)
                bct[name] = t
                P.dma("sync", lambda e, t=t, src=src: e.dma_start(out=t[:], in_=src.partition_broadcast(128)), "bc2", reads=[bmod], writes=[bbc], nowaw=True)
            y1r = Ring(P, "y1r", 3, [128, 1024], F32)
            y2r = Ring(P, "y2r", 3, [128, 1024], F32)
            x1r = Ring(P, "x1r", 3, [128, 1024], F32)
            outr = Ring(P, "outr", 3, [128, 1024], F32)
            s6, m2, r2 = P.sb("s6c", [128, 2, 6], F32), P.sb("m2c", [128, 2], F32), P.sb("r2c", [128, 1], F32)
            bs6, bm2, br2 = Buf(), Buf(), Buf()
            gt = {}

            def gathers(ti):
                y1, by1 = y1r.next()
                y2, by2 = y2r.next()
                x1, bx1 = x1r.next()
                P.dma("gpsimd", lambda e: e.indirect_dma_start(out=y1[:], out_offset=None, in_=ys_d, in_offset=bass.IndirectOffsetOnAxis(ap=d1i[:, ti:ti + 1], axis=0)),
                      f"y1g{y1r.last}", reads=[bys_d, bd12], writes=[by1])
                P.dma("gpsimd", lambda e: e.indirect_dma_start(out=y2[:], out_offset=None, in_=ys_d, in_offset=bass.IndirectOffsetOnAxis(ap=d2i[:, ti:ti + 1], axis=0)),
                      f"y2g{y2r.last}", reads=[bys_d, bd12], writes=[by2])
                P.dma("sync", lambda e: e.dma_start(out=x1[:], in_=x1_d[128 * ti:128 * ti + 128, :]), f"x1l{x1r.last}", reads=[bx1_d], writes=[bx1])
                gt[ti] = (y1, by1, y2, by2, x1, bx1)

            def combine(ti):
                b = ti // 16
                i = ti % 16
                y1, by1, y2, by2, x1, bx1 = gt.pop(ti)
                V(lambda e: e.tensor_scalar(out=y1[:], in0=y1[:], scalar1=wt1[:, ti:ti + 1], scalar2=None, op0=ALU.mult), [by1, bwt12], [by1])
                V(lambda e: e.scalar_tensor_tensor(out=y1[:], in0=y2[:], scalar=wt2[:, ti:ti + 1], in1=y1[:], op0=ALU.mult, op1=ALU.add), [by1, by2, bwt12], [by1])
                g2 = bct["g2_%d" % b]
                V(lambda e: e.tensor_tensor(out=y1[:], in0=y1[:], in1=g2[:], op=ALU.mult), [by1, bbc], [by1])
                V(lambda e: e.scalar_tensor_tensor(out=y1[:], in0=x1[:], scalar=DN_ALPHA, in1=y1[:], op0=ALU.mult, op1=ALU.add), [by1, bx1], [by1])
                s6, bs6 = s6r.next()
                m2, bm2 = m2r.next()
                r2, br2 = r2r.next()
                ln_stats(y1, by1, s6, bs6, m2, bm2, r2, br2)
                ot, bot = outr.next()
                V(lambda e: e.tensor_scalar(out=ot[:], in0=y1[:], scalar1=m2[:, 0:1], scalar2=r2[:, 0:1], op0=ALU.subtract, op1=ALU.mult), [by1, bm2, br2], [bot])
                G(lambda e: e.tensor_tensor(out=ot[:], in0=ot[:], in1=bct["l2g"][:], op=ALU.mult), [bot, bbc], [bot])
                G(lambda e: e.tensor_tensor(out=ot[:], in0=ot[:], in1=bct["l2b"][:], op=ALU.add), [bot, bbc], [bot])
                P.dma("gpsimd", lambda e: e.dma_start(out=out_d[b, 128 * i:128 * i + 128, :], in_=ot[:]), f"ost{outr.last}", reads=[bot], writes=[bout], nowaw=True)
            s6r = Ring(P, "s6cr", 2, [128, 2, 6], F32)
            m2r = Ring(P, "m2cr", 2, [128, 2], F32)
            r2r = Ring(P, "r2cr", 2, [128, 1], F32)
            gathers(0)
            gathers(1)
            for ti in range(32):
                if ti + 2 < 32:
                    gathers(ti + 2)
                combine(ti)
        P.barrier()
        P.emit()
    return nc


_CONSTS = None


def _prep_shared(inp):
    global _CONSTS
    if _CONSTS is None:
        _CONSTS = make_consts()
    f = lambda a: np.ascontiguousarray(a, dtype=np.float32)
    cwT = np.ascontiguousarray(inp["ml_conv_w"][0].reshape(3, 16, 128).transpose(2, 1, 0), dtype=np.float32)
    cbT = np.ascontiguousarray(inp["ml_conv_b"][0].reshape(16, 128).T, dtype=np.float32)
    return {
        "w_ada": f(inp["w_ada"][0]), "b_ada": f(inp["b_ada"][0].reshape(1, 6144)), "w_in": f(inp["w_in"][0]),
        "bmg": f(inp["b_mgate"][0].reshape(4, 4).T), "cw": cwT, "cb": cbT,
        "rdl": f(inp["ret_decay_logit"][0].reshape(1, 8)),
        "w_rb": f(inp["w_ret_branch"][0]), "w_mb": f(inp["w_ml_branch"][0]), "w_out": f(inp["w_out"][0]),
        "lnp": f(np.stack([inp["ln1_g"][0], inp["ln1_b"][0], inp["ln2_g"][0], inp["ln2_b"][0]], 0)),
        "w_rt": f(np.concatenate([inp["w_rg"][0], inp["w_re"][0]], 1)),
        "b_rt": f(np.concatenate([inp["b_rg"][0], inp["b_re"][0]], 0).reshape(1, 36)),
        "w_e1": f(inp["w_e1"][0]), "w_e3": f(inp["w_e3"][0]), "w_e2": f(inp["w_e2"][0]),
        "consts": _CONSTS,
    }


def _core_inputs(inp, shared, c):
    x = np.asarray(inp["x"], dtype=np.float32)
    ctx = np.asarray(inp["ctx"], dtype=np.float32)
    cc = np.asarray(inp["c"], dtype=np.float32)
    c_ctx = np.asarray(inp["c_ctx"], dtype=np.float32)
    vecs = np.stack([cc[2 * c], cc[2 * c + 1], c_ctx], 0)
    cT = np.ascontiguousarray(vecs.reshape(3, 8, 128).transpose(2, 1, 0))
    m = dict(shared)
    m["x"] = np.ascontiguousarray(x[2 * c:2 * c + 2])
    m["ctx"] = np.ascontiguousarray(ctx[2 * c:2 * c + 2])
    m["cT"] = cT
    return m


def kernel(**inputs):
    nc = build()
    shared = _prep_shared(inputs)
    in_maps = [_core_inputs(inputs, shared, c) for c in range(NCORES)]
    res = run_bass_kernel_spmd(nc, in_maps, core_ids=list(range(NCORES)))
    out = np.concatenate([np.asarray(r["out"]) for r in res.results], axis=0)
    return out.astype(np.float32)
```

```python
import contextlib
import math
import numpy as np
import concourse.bass as bass
import concourse.mybir as mybir
from concourse.bass_utils import run_bass_kernel_spmd

F32 = mybir.dt.float32
BF16 = mybir.dt.bfloat16
I32 = mybir.dt.int32
AF = mybir.ActivationFunctionType
ALU = mybir.AluOpType
AX = mybir.AxisListType

ENGS = ["tensor", "vector", "scalar", "gpsimd", "sync"]
LN_EPS = 1e-5
DN_ALPHA = 2.0 ** 0.25
NEG = -30000.0
LN16 = math.log(16.0)
NCORES = 8


import types


def _freeze(fn):
    if fn is None or fn.__closure__ is None:
        return fn
    cells = []
    for c in fn.__closure__:
        try:
            cells.append(types.CellType(c.cell_contents))
        except ValueError:
            cells.append(c)
    return types.FunctionType(fn.__code__, fn.__globals__, fn.__name__, fn.__defaults__, tuple(cells))


class Buf:
    __slots__ = ("w", "r", "pr")

    def __init__(self):
        self.w = []
        self.r = []
        self.pr = []


class Prog:
    def __init__(self, nc):
        self.nc = nc
        self.es = contextlib.ExitStack()
        self.ops = {e: [] for e in ENGS}
        self.cnt = {e: 0 for e in ENGS}
        self.dsem = {}
        self.known = {e: {} for e in ENGS}
        self.semobj = {}
        self.stack = [self.es]

    def _nm(self, name):
        self.uid = getattr(self, "uid", 0) + 1
        return f"t{self.uid}_{name}"

    def sb(self, name, shape, dt):
        return self.stack[-1].enter_context(self.nc.sbuf_tensor(self._nm(name), list(shape), dt))

    def ps(self, name, shape, dt=F32):
        return self.stack[-1].enter_context(self.nc.psum_tensor(self._nm(name), list(shape), dt))

    @contextlib.contextmanager
    def phase(self):
        es = contextlib.ExitStack()
        self.stack.append(es)
        with es:
            yield
            self.barrier()
        self.stack.pop()

    def _deps(self, eng, reads, writes, nowaw):
        deps = {}

        def add(ev):
            k, v = ev
            if deps.get(k, 0) < v:
                deps[k] = v
        for b in reads:
            for ev in b.w:
                add(ev)
        for b in writes:
            for ev in b.r:
                add(ev)
            if nowaw:
                for ev in b.pr:
                    add(ev)
            else:
                for ev in b.w:
                    add(ev)
        out = []
        kn = self.known[eng]
        for k, v in deps.items():
            if k == ("E", "tensor") and eng == "tensor":
                continue
            if kn.get(k, 0) >= v:
                continue
            kn[k] = v
            out.append((k, v))
        return out

    def _commit(self, ev, reads, writes, nowaw):
        for b in reads:
            b.r.append(ev)
        for b in writes:
            if nowaw:
                b.w.append(ev)
            else:
                b.pr = b.r
                b.r = []
                b.w = [ev]

    def op(self, eng, fn, reads=(), writes=(), nowaw=False):
        waits = self._deps(eng, reads, writes, nowaw)
        self.cnt[eng] += 1
        ev = (("E", eng), self.cnt[eng])
        self.ops[eng].append((waits, _freeze(fn), ("E", eng), 1))
        self._commit(ev, reads, writes, nowaw)
        return ev

    def dma(self, eng, fn, slot, reads=(), writes=(), nowaw=False):
        waits = self._deps(eng, reads, writes, nowaw)
        self.dsem[slot] = self.dsem.get(slot, 0) + 16
        ev = (("D", slot), self.dsem[slot])
        self.ops[eng].append((waits, _freeze(fn), ("D", slot), 16))
        self._commit(ev, reads, writes, nowaw)
        return ev

    def barrier(self):
        for e in ENGS:
            waits = []
            kn = self.known[e]
            for e2 in ENGS:
                k = ("E", e2)
                v = self.cnt[e2]
                if e2 != e and v > 0 and kn.get(k, 0) < v:
                    kn[k] = v
                    waits.append((k, v))
            for slot, v in self.dsem.items():
                k = ("D", slot)
                if kn.get(k, 0) < v:
                    kn[k] = v
                    waits.append((k, v))
            if waits:
                self.ops[e].append((waits, None, None, 0))

    def emit(self):
        nc = self.nc
        for e in ENGS:
            self.semobj[("E", e)] = self.es.enter_context(nc.semaphore("e_" + e))
        for slot in self.dsem:
            self.semobj[("D", slot)] = self.es.enter_context(nc.semaphore("d_" + str(slot)))
        block = self.es.enter_context(nc.Block())
        for e in ENGS:
            ops = self.ops[e]
            if not ops:
                continue

            def body(engine, ops=ops):
                for waits, fn, sk, inc in ops:
                    for k, v in waits:
                        engine.wait_ge(self.semobj[k], v)
                    if fn is not None:
                        fn(engine).then_inc(self.semobj[sk], inc)
            getattr(block, e)(body)


class Ring:
    def __init__(self, P, name, n, shape, dt, psum=False):
        self.t = [(P.ps if psum else P.sb)(f"{name}{i}", shape, dt) for i in range(n)]
        self.b = [Buf() for _ in range(n)]
        self.i = 0
        self.n = n

    def next(self):
        i = self.i
        self.i = (i + 1) % self.n
        self.last = i
        return self.t[i], self.b[i]


C_RQ, C_RK, C_RV, C_RG, C_MQ, C_MK, C_MV, C_MO, C_MG, C_GR, C_GM = (
    0, 1024, 2048, 3072, 4096, 5120, 6144, 7168, 8192, 8208, 9232)

K_ID = 0
K_IOTA = 128
K_PF = 640
K_PB = 658
K_COSR = 676
K_SINR = 708
K_COSC = 740
K_SINC = 804
K_TRI = 868
K_BLK = 996
K_MF = 1092
K_MB = 1988
K_ONE = 2884
NCONST = 3012


def make_consts():
    c = np.zeros((128, NCONST), np.float32)
    p = np.arange(128)
    c[:, K_ID:K_ID + 128] = np.eye(128, dtype=np.float32)
    c[:, K_IOTA:K_IOTA + 512] = np.arange(512, dtype=np.float32)[None, :]
    for jf in range(18):
        c[:, K_PF + jf] = 128 * jf + p
        c[:, K_PB + jf] = (2048 + 128 * jf + p) if jf < 2 else (128 * (jf - 2) + p)
    q = (p % 64).astype(np.float64)
    freq = 10000.0 ** (-q / 64.0)
    sgn = np.where(p < 64, -1.0, 1.0)
    rows = np.arange(32, dtype=np.float64)
    cols = np.arange(64, dtype=np.float64)
    c[:, K_COSR:K_COSR + 32] = np.cos(freq[:, None] * rows[None, :])
    c[:, K_SINR:K_SINR + 32] = sgn[:, None] * np.sin(freq[:, None] * rows[None, :])
    c[:, K_COSC:K_COSC + 64] = np.cos(freq[:, None] * cols[None, :])
    c[:, K_SINC:K_SINC + 64] = sgn[:, None] * np.sin(freq[:, None] * cols[None, :])
    c[:, K_TRI:K_TRI + 128] = (p[:, None] < p[None, :]).astype(np.float32)
    c[:, K_BLK:K_BLK + 96] = 512.0 * np.arange(96, dtype=np.float32)[None, :]
    cc = np.arange(896)[None, :]
    c[:, K_MF:K_MF + 896] = np.where(cc - 384 - p[:, None] >= 0, 0.0, NEG)
    c[:, K_MB:K_MB + 896] = np.where(p[:, None] - (cc - 384) > 0, 0.0, NEG)
    c[:, K_ONE:K_ONE + 128] = 1.0
    return c


def build(dbg=None):
    nc = bass.Bass("TRN2", target_bir_lowering=False)
    P = Prog(nc)

    def din(name, shape, dt=F32):
        return nc.dram_tensor(name, list(shape), dt, kind="ExternalInput").ap()

    def dscr(name, shape, dt=F32):
        return nc.dram_tensor(name, list(shape), dt).ap()

    x_d = din("x", [2, 2048, 1024])
    ctx_d = din("ctx", [2, 256, 1024])
    cT_d = din("cT", [128, 8, 3])
    w_ada_d = din("w_ada", [1024, 6144])
    b_ada_d = din("b_ada", [1, 6144])
    w_in_d = din("w_in", [1024, 10256])
    bmg_d = din("bmg", [4, 4])
    cw_d = din("cw", [128, 16, 3])
    cb_d = din("cb", [128, 16])
    rdl_d = din("rdl", [1, 8])
    w_rb_d = din("w_rb", [1024, 1024])
    w_mb_d = din("w_mb", [1024, 1024])
    w_out_d = din("w_out", [1024, 1024])
    lnp_d = din("lnp", [4, 1024])
    w_rt_d = din("w_rt", [1024, 36])
    b_rt_d = din("b_rt", [1, 36])
    w_e1_d = din("w_e1", [32, 1024, 512])
    w_e3_d = din("w_e3", [32, 1024, 512])
    w_e2_d = din("w_e2", [32, 512, 1024])
    consts_d = din("consts", [128, NCONST])
    out_d = nc.dram_tensor("out", [2, 2048, 1024], F32, kind="ExternalOutput").ap()

    mod_d = dscr("mod_s", [3, 6144])
    r_d = dscr("r_s", [2, 8, 128, 2048], BF16)
    y_d = dscr("y_s", [8, 128, 2048], BF16)
    x1_d = dscr("x1_s", [4096, 1024])
    u2_d = dscr("u2_s", [4096, 1024], BF16)
    xs_d = dscr("xs_s", [24576, 1024], BF16)
    ys_d = dscr("ys_s", [24576, 1024])
    dbg_outs = {}
    if dbg:
        for name, shape in dbg.items():
            dbg_outs[name] = nc.dram_tensor(name, list(shape), F32, kind="ExternalOutput").ap()

    bmod = Buf()
    br_d = [Buf(), Buf()]
    bx1_d = Buf()
    by_d = Buf()
    bu2_d = Buf()
    bxs_d = Buf()
    bys_d = Buf()
    bout = Buf()
    bdbg = Buf()

    with P.es:
        cst = P.sb("cst", [128, NCONST], F32)
        bcst = Buf()
        P.dma("sync", lambda e: e.dma_start(out=cst[:], in_=consts_d), "cst", writes=[bcst])
        identb = P.sb("identb", [128, 128], BF16)
        bidb = Buf()
        P.op("vector", lambda e: e.tensor_copy(out=identb[:], in_=cst[:, K_ID:K_ID + 128]), reads=[bcst], writes=[bidb])
        identf = cst[:, K_ID:K_ID + 128]
        trib = P.sb("trib", [128, 128], BF16)
        onesb = P.sb("onesb", [128, 128], BF16)
        P.op("vector", lambda e: e.tensor_copy(out=trib[:], in_=cst[:, K_TRI:K_TRI + 128]), reads=[bcst], writes=[bidb], nowaw=True)
        P.op("vector", lambda e: e.tensor_copy(out=onesb[:], in_=cst[:, K_ONE:K_ONE + 128]), reads=[bcst], writes=[bidb], nowaw=True)
        uT = P.sb("uT", [128, 8, 2304], BF16)
        buT = [Buf() for _ in range(5)]
        modc = P.sb("modc", [128, 6, 8], F32)
        bmodc = Buf()
        lgc = P.sb("lgc", [128, 8], F32)
        nlgc = P.sb("nlgc", [128, 8], F32)
        blgc = Buf()
        cw = P.sb("cw", [128, 16, 3], F32)
        cbias = P.sb("cbias", [128, 16], F32)
        bcw = Buf()
        P.dma("sync", lambda e: e.dma_start(out=cw[:], in_=cw_d), "cw", writes=[bcw])
        P.dma("sync", lambda e: e.dma_start(out=cbias[:], in_=cb_d), "cw", writes=[bcw], nowaw=True)
        bmg = P.sb("bmg", [4, 4], F32)
        bbmg = Buf()
        P.dma("sync", lambda e: e.dma_start(out=bmg[:], in_=bmg_d), "bmg", writes=[bbmg])
        logits = P.sb("logits", [128, 32, 36], F32)
        blog = Buf()
        wrt = P.sb("wrt", [128, 8, 36], F32)
        brt = P.sb("brt", [128, 36], F32)
        bwrt = Buf()
        P.dma("sync", lambda e: e.dma_start(out=wrt[:], in_=w_rt_d.rearrange("(c p) n -> p c n", p=128)), "wrt", writes=[bwrt])
        P.dma("sync", lambda e: e.dma_start(out=brt[:], in_=b_rt_d.partition_broadcast(128)), "wrt", writes=[bwrt], nowaw=True)

        P.dma("sync", lambda e: e.dma_start(out=lgc[:], in_=rdl_d.partition_broadcast(128)), "lgc", writes=[blgc])
        P.op("scalar", lambda e: e.activation(out=nlgc[:], in_=lgc[:], func=AF.Exp, scale=-1.0), reads=[blgc], writes=[blgc])
        P.op("scalar", lambda e: e.activation(out=nlgc[:], in_=nlgc[:], func=AF.Ln, bias=1.0, scale=1.0), reads=[blgc], writes=[blgc])
        P.op("vector", lambda e: e.tensor_scalar(out=lgc[:], in0=nlgc[:], scalar1=-1.0, scalar2=None, op0=ALU.mult), reads=[blgc], writes=[blgc])

        with P.phase():
            cT = P.sb("cT", [128, 8, 3], F32)
            bcT = Buf()
            P.dma("sync", lambda e: e.dma_start(out=cT[:], in_=cT_d), "cT", writes=[bcT])
            P.op("scalar", lambda e: e.activation(out=cT[:], in_=cT[:], func=AF.Silu), reads=[bcT], writes=[bcT])
            bada = P.sb("bada", [3, 6144], F32)
            bbada = Buf()
            P.dma("sync", lambda e: e.dma_start(out=bada[:], in_=b_ada_d.partition_broadcast(3)), "bada", writes=[bbada])
            modsb = P.sb("modsb", [3, 6144], F32)
            bmodsb = Buf()
            wa = Ring(P, "wa", 2, [128, 8, 512], F32)
            pmod = Ring(P, "pmod", 2, [128, 512], F32, psum=True)
            for cg in range(12):
                wt, bw = wa.next()
                P.dma("sync", lambda e, wt=wt, cg=cg: e.dma_start(
                    out=wt[:], in_=w_ada_d[:, 512 * cg:512 * cg + 512].rearrange("(c p) n -> p c n", p=128)),
                    f"wa{cg % 2}", writes=[bw])
                pt, bp = pmod.next()
                for k in range(8):
                    P.op("tensor", lambda e, pt=pt, wt=wt, k=k: e.matmul(pt[0:3, :], lhsT=cT[:, k, :], rhs=wt[:, k, :], start=(k == 0), stop=(k == 7)),
                         reads=[bcT, bw], writes=[bp])
                P.op("vector", lambda e, pt=pt, cg=cg: e.tensor_tensor(out=modsb[:, 512 * cg:512 * cg + 512], in0=pt[0:3, :], in1=bada[:, 512 * cg:512 * cg + 512], op=ALU.add),
                     reads=[bp, bbada], writes=[bmodsb], nowaw=True)
            P.dma("gpsimd", lambda e: e.dma_start(out=mod_d, in_=modsb[:]), "mod", reads=[bmodsb], writes=[bmod])
            for v in range(3):
                for which in range(2):
                    P.dma("gpsimd", lambda e, v=v, which=which: e.dma_start(
                        out=modc[:, 2 * v + which, :], in_=mod_d[v, 1024 * which:1024 * which + 1024].rearrange("(c p) -> p c", p=128),
                        allow_slow_non_contiguous=True), "modc", reads=[bmod], writes=[bmodc], nowaw=True)
            for v in range(3):
                P.op("vector", lambda e, v=v: e.tensor_scalar(out=modc[:, 2 * v + 1, :], in0=modc[:, 2 * v + 1, :], scalar1=1.0, scalar2=None, op0=ALU.add),
                     reads=[bmodc], writes=[bmodc])


        selp = P.sb("selp", [4, 4, 128], F32)
        seln = P.sb("seln", [4, 4, 128], F32)
        bsel = Buf()
        P.op("vector", lambda e: e.tensor_copy(out=selp[:], in_=cst[0:4, K_ID:K_ID + 4].unsqueeze(2).broadcast_to([4, 4, 128])), reads=[bcst], writes=[bsel])
        P.op("vector", lambda e: e.tensor_scalar(out=seln[:], in0=selp[:], scalar1=-1.0, scalar2=None, op0=ALU.mult), reads=[bsel], writes=[bsel])

        def V(fn, r=(), w=(), **kw):
            return P.op("vector", fn, reads=r, writes=w, **kw)

        def S(fn, r=(), w=(), **kw):
            return P.op("scalar", fn, reads=r, writes=w, **kw)

        def G(fn, r=(), w=(), **kw):
            return P.op("gpsimd", fn, reads=r, writes=w, **kw)

        def T(fn, r=(), w=(), **kw):
            return P.op("tensor", fn, reads=r, writes=w, **kw)

        def ln_stats(src, bsrc, s6, bs6, m, bm, r, brs):
            V(lambda e: e.bn_stats(out=s6[:, 0, :], in_=src[:, 0:512]), [bsrc], [bs6])
            V(lambda e: e.bn_stats(out=s6[:, 1, :], in_=src[:, 512:1024]), [bsrc], [bs6], nowaw=True)
            V(lambda e: e.bn_aggr(out=m[:], in_=s6[:].rearrange("p a b -> p (a b)")), [bs6], [bm])
            S(lambda e: e.activation(out=r[:], in_=m[:, 1:2], func=AF.Sqrt, bias=LN_EPS, scale=1.0), [bm], [brs])
            V(lambda e: e.reciprocal(out=r[:], in_=r[:]), [brs], [brs])

        def make_wload(wst, wbf):
            def load_w(src, c0, swap=False):
                wt, bw = wst.next()
                P.dma("sync", lambda e: e.dma_start(out=wt[:], in_=src[:, c0:c0 + 128].rearrange("(c p) n -> p c n", p=128)),
                      f"wst{wst.last}", writes=[bw])
                wb, bwb = wbf.next()
                S(lambda e: e.copy(out=wb[:], in_=wt[:]), [bw], [bwb])
                if swap:
                    wb2, bwb2 = wbf.next()
                    S(lambda e: e.copy(out=wb2[:, :, 0:64], in_=wt[:, :, 64:128]), [bw], [bwb2])
                    S(lambda e: e.copy(out=wb2[:, :, 64:128], in_=wt[:, :, 0:64]), [bw], [bwb2], nowaw=True)
                    return (wb, bwb), (wb2, bwb2)
                return wb, bwb
            return load_w

        for b in range(2):
            with P.phase():
                T2 = [P.sb(f"T2_{d}", [4, 2304], F32) for d in range(2)]
                bT2 = [Buf(), Buf()]
                acol = [P.sb(f"acol{d}", [128, 18, 4], F32) for d in range(2)]
                bacol = [Buf(), Buf()]
                em = [P.sb(f"em{d}", [128, 4], F32) for d in range(2)]
                bem = [Buf(), Buf()]
                pA = Ring(P, "pA", 3, [128, 512], F32, psum=True)
                pO = [P.ps(f"pO{i}", [128, 512], F32) for i in range(4)]
                bpO = [Buf() for _ in range(4)]
                pT = P.ps("pT", [128, 8, 128], BF16)
                bpT = Buf()
                with P.phase():
                    xr = Ring(P, "xr", 2, [128, 1024], F32)
                    xn = Ring(P, "xn", 2, [128, 1024], BF16)
                    st6 = Ring(P, "st6", 2, [128, 2, 6], F32)
                    mv = Ring(P, "mv", 2, [128, 2], F32)
                    rs = Ring(P, "rs", 2, [128, 1], F32)
                    tmpm = Ring(P, "tmpm", 2, [128, 8, 128], F32)
                    for j in range(18):
                        xt, bx = xr.next()
                        src = ctx_d[b, 128 * j:128 * j + 128, :] if j < 2 else x_d[b, 128 * (j - 2):128 * (j - 2) + 128, :]
                        P.dma("sync", lambda e, xt=xt, src=src: e.dma_start(out=xt[:], in_=src), f"xr{j % 2}", writes=[bx])
                        s6, bs6 = st6.next()
                        m, bm = mv.next()
                        r, brs = rs.next()
                        ln_stats(xt, bx, s6, bs6, m, bm, r, brs)
                        xb, bxb = xn.next()
                        V(lambda e, xb=xb, xt=xt, m=m, r=r: e.tensor_scalar(out=xb[:], in0=xt[:], scalar1=m[:, 0:1], scalar2=r[:, 0:1], op0=ALU.subtract, op1=ALU.mult),
                          [bx, bm, brs], [bxb])
                        for k in range(8):
                            T(lambda e, xb=xb, k=k: e.transpose(out=pT[:, k, :], in_=xb[:, 128 * k:128 * k + 128], identity=identb[:]),
                              [bxb, bidb], [bpT], nowaw=(k > 0))
                        v = 2 if j < 2 else b
                        tm, btm = tmpm.next()
                        V(lambda e, tm=tm, v=v: e.tensor_tensor(out=tm[:], in0=pT[:], in1=modc[:, 2 * v + 1, :].unsqueeze(2).broadcast_to([128, 8, 128]), op=ALU.mult),
                          [bpT, bmodc], [btm])
                        ch = 0 if j < 2 else 1 + (j - 2) // 4
                        G(lambda e, tm=tm, v=v, j=j: e.tensor_tensor(out=uT[:, :, 128 * j:128 * j + 128], in0=tm[:], in1=modc[:, 2 * v, :].unsqueeze(2).broadcast_to([128, 8, 128]), op=ALU.add),
                          [btm, bmodc], [buT[ch]], nowaw=True)

                    wgs = P.sb("wgs", [128, 8, 16], F32)
                    wgb = P.sb("wgb", [128, 8, 16], BF16)
                    bwg = Buf()
                    P.dma("sync", lambda e: e.dma_start(out=wgs[:], in_=w_in_d[:, C_MG:C_MG + 16].rearrange("(c p) n -> p c n", p=128)), "wg", writes=[bwg])
                    V(lambda e: e.tensor_copy(out=wgb[:], in_=wgs[:]), [bwg], [bwg])
                    T0 = P.sb("T0", [4, 2304], F32)
                    T1 = P.sb("T1", [4, 2304], F32)
                    ones4 = P.sb("ones4", [4, 2304], F32)
                    bT0 = Buf()
                    bT1 = Buf()
                    bones4 = Buf()
                    V(lambda e: e.memset(ones4[:], 1.0), [], [bones4])
                    mx = P.sb("mx", [4, 2], F32)
                    bmx = Buf()
                    mrow = P.sb("mrow", [4, 128], F32)
                    bmrow = Buf()
                    chunks = [(0, 256)] + [(256 + 512 * n, 512) for n in range(4)]
                    for d in range(2):
                        for gi, Tt, bT in ((2 * d, T0, bT0), (2 * d + 1, T1, bT1)):
                            for ci, (c0, cl) in enumerate(chunks):
                                pt, bp = pA.next()
                                for k in range(8):
                                    T(lambda e, pt=pt, k=k, gi=gi, c0=c0, cl=cl: e.matmul(pt[0:4, 0:cl], lhsT=wgb[:, k, 4 * gi:4 * gi + 4], rhs=uT[:, k, c0:c0 + cl], start=(k == 0), stop=(k == 7)),
                                      [bwg, buT[ci]], [bp])
                                if d == 0:
                                    o0 = c0
                                else:
                                    o0 = 2048 if ci == 0 else c0 - 256
                                V(lambda e, pt=pt, Tt=Tt, gi=gi, o0=o0, cl=cl: e.tensor_scalar(out=Tt[:, o0:o0 + cl], in0=pt[0:4, 0:cl], scalar1=bmg[:, gi:gi + 1], scalar2=None, op0=ALU.add),
                                  [bp, bbmg], [bT], nowaw=(ci > 0))
                        S(lambda e: e.activation(out=T1[:], in_=T1[:], func=AF.Exp, scale=-1.0), [bT1], [bT1])
                        S(lambda e: e.activation(out=T1[:], in_=T1[:], func=AF.Ln, bias=1.0, scale=1.0), [bT1], [bT1])
                        V(lambda e, d=d: e.tensor_tensor_scan(out=T2[d][:], data0=ones4[:], data1=T1[:], initial=0.0, op0=ALU.mult, op1=ALU.add),
                          [bT1, bones4], [bT2[d]])
                        if d == 1:
                            V(lambda e: e.tensor_tensor(out=T2[1][:], in0=T2[1][:], in1=T1[:], op=ALU.subtract), [bT2[1], bT1], [bT2[1]])
                        V(lambda e: e.tensor_reduce(out=mx[:, 0:1], in_=T0[:], axis=AX.X, op=ALU.max), [bT0], [bmx])
                        V(lambda e: e.tensor_scalar(out=mx[:, 1:2], in0=mx[:, 0:1], scalar1=LN16, scalar2=None, op0=ALU.add), [bmx], [bmx])
                        V(lambda e, d=d: e.scalar_tensor_tensor(out=T0[:], in0=T0[:], scalar=mx[:, 1:2], in1=T2[d][:], op0=ALU.subtract, op1=(ALU.add if d == 0 else ALU.subtract)),
                          [bT0, bmx, bT2[d]], [bT0])
                        pt, bp = pA.next()
                        for jo in range(18):
                            jf = jo if d == 0 else (jo + 2 if jo < 16 else jo - 16)
                            T(lambda e, pt=pt, jo=jo, jf=jf: e.transpose(out=pt[:, 4 * jf:4 * jf + 4], in_=T0[0:4, 128 * jo:128 * jo + 128], identity=identf[0:4, 0:4]),
                              [bT0, bcst], [bp], nowaw=(jo > 0))
                        S(lambda e, pt=pt, d=d: e.copy(out=acol[d][:].rearrange("p a b -> p (a b)"), in_=pt[:, 0:72]), [bp], [bacol[d]])
                        V(lambda e: e.tensor_scalar(out=mrow[:], in0=ones4[:, 0:128], scalar1=mx[:, 0:1], scalar2=-1.0, op0=ALU.mult, op1=ALU.mult), [bmx, bones4], [bmrow])
                        pt, bp = pA.next()
                        T(lambda e, pt=pt: e.transpose(out=pt[:, 0:4], in_=mrow[0:4, :], identity=identf[0:4, 0:4]), [bmrow, bcst], [bp])
                        S(lambda e, pt=pt, d=d: e.activation(out=em[d][:], in_=pt[:, 0:4], func=AF.Exp), [bp], [bem[d]])

                if dbg and "uT" in dbg and b == 0:
                    du = P.sb("dbg_u", [128, 2304], F32)
                    bdu = Buf()
                    V(lambda e: e.tensor_copy(out=du[:], in_=uT[:, 0, :]), buT, [bdu])
                    P.dma("gpsimd", lambda e: e.dma_start(out=dbg_outs["uT"], in_=du[:]), "dbg", reads=[bdu], writes=[bdbg], nowaw=True)
                    da = P.sb("dbg_a", [128, 2, 76], F32)
                    bda = Buf()
                    for d in range(2):
                        V(lambda e, d=d: e.tensor_copy(out=da[:, d, 0:72], in_=acol[d][:].rearrange("p a b -> p (a b)")), [bacol[d]], [bda], nowaw=True)
                        V(lambda e, d=d: e.tensor_copy(out=da[:, d, 72:76], in_=em[d][:]), [bem[d]], [bda], nowaw=True)
                    P.dma("gpsimd", lambda e: e.dma_start(out=dbg_outs["acol"], in_=da[:].rearrange("p a b -> p (a b)")), "dbg", reads=[bda], writes=[bdbg], nowaw=True)
                    P.dma("gpsimd", lambda e: e.dma_start(out=dbg_outs["T2"][0:4, :], in_=T2[0][:]), "dbg", reads=[bT2[0]], writes=[bdbg], nowaw=True)
                    P.dma("gpsimd", lambda e: e.dma_start(out=dbg_outs["T2"][4:8, :], in_=T2[1][:]), "dbg", reads=[bT2[1]], writes=[bdbg], nowaw=True)

                with P.phase():
                    wst = Ring(P, "wst", 3, [128, 8, 128], F32)
                    wbf = Ring(P, "wbf", 4, [128, 8, 128], BF16)
                    load_w = make_wload(wst, wbf)
                    wvt = P.sb("wvt", [128, 8, 256], BF16)
                    bwv = Buf()
                    qT = P.sb("qT", [128, 2, 2048], BF16)
                    kT = P.sb("kT", [128, 2, 2304], BF16)
                    vv = P.sb("vv", [128, 18, 257], BF16)
                    gT = P.sb("gT", [128, 2, 2048], BF16)
                    bq, bk, bv, bg = Buf(), Buf(), Buf(), Buf()
                    V(lambda e: e.memset(vv[:, :, 256:257], 1.0), [], [bv])
                    raw = P.sb("raw", [128, 2050], F32)
                    rawc = P.sb("rawc", [128, 258], F32)
                    acc = P.sb("acc", [128, 2048], F32)
                    braw, brawc, bacc = Buf(), Buf(), Buf()
                    V(lambda e: e.memset(raw[:], 0.0), [], [braw])
                    V(lambda e: e.memset(rawc[:], 0.0), [], [brawc])
                    tA = Ring(P, "tA", 2, [128, 512], F32)
                    tB = Ring(P, "tB", 2, [128, 512], F32)
                    rowr = Ring(P, "rowr", 2, [128, 512], F32)
                    prer = Ring(P, "prer", 2, [128, 512], F32)
                    Dr = Ring(P, "Dr", 3, [128, 512], BF16)
                    atr = Ring(P, "atr", 3, [128, 512], BF16)
                    mhalf = P.sb("mhalf", [128, 4], F32)
                    bmhalf = Buf()
                    V(lambda e: e.memset(mhalf[:], -0.5), [], [bmhalf])
                    acolr = [P.sb(f"acolr{d}", [128, 18], F32) for d in range(2)]
                    bacolr = [Buf(), Buf()]
                    rawr = Ring(P, "rawr", 2, [128, 4, 257], F32)
                    dn = P.sb("dn", [128, 4], F32)
                    bdn = Buf()
                    hf = P.sb("hf", [128, 4, 256], F32)
                    hs = P.sb("hs", [128, 4, 256], F32)
                    hn = P.sb("hn", [128, 4, 256], BF16)
                    bhf, bhs, bhn = Buf(), Buf(), Buf()
                    s6h = P.sb("s6h", [128, 4, 6], F32)
                    mvh = P.sb("mvh", [128, 4, 2], F32)
                    rsh = P.sb("rsh", [128, 4], F32)
                    bs6h, bmvh, brsh = Buf(), Buf(), Buf()
                    ofm = Ring(P, "ofm", 2, [128, 2, 512], BF16)

                    def proj_fm(wb, bwb, c0, cl, ci):
                        pt, bp = pA.next()
                        for k in range(8):
                            T(lambda e, k=k: e.matmul(pt[:, 0:cl], lhsT=wb[:, k, :], rhs=uT[:, k, c0:c0 + cl], start=(k == 0), stop=(k == 7)),
                              [bwb, buT[ci]], [bp])
                        return pt, bp

                    def proj_v(base):
                        for vu in range(2):
                            wt, bw = wst.next()
                            P.dma("sync", lambda e, wt=wt, vu=vu: e.dma_start(out=wt[:], in_=w_in_d[:, base + 128 * vu:base + 128 * vu + 128].rearrange("(c p) n -> p c n", p=128)),
                                  f"wst{wst.last}", writes=[bw])
                            S(lambda e, wt=wt, vu=vu: e.copy(out=wvt[:, :, 128 * vu:128 * vu + 128], in_=wt[:]), [bw], [bwv], nowaw=(vu > 0))
                        for q2 in range(9):
                            pt, bp = pA.next()
                            for jj in range(2):
                                j = 2 * q2 + jj
                                ci = 0 if j < 2 else 1 + (j - 2) // 4
                                for k in range(8):
                                    T(lambda e, pt=pt, jj=jj, j=j, k=k: e.matmul(pt[:, 256 * jj:256 * jj + 256], lhsT=uT[:, k, 128 * j:128 * j + 128], rhs=wvt[:, k, :], start=(k == 0), stop=(k == 7)),
                                      [bwv, buT[ci]], [bp], nowaw=not (jj == 0 and k == 0))
                            S(lambda e, pt=pt, q2=q2: e.copy(out=vv[:, 2 * q2:2 * q2 + 2, 0:256], in_=pt[:, :].rearrange("p (a b) -> p a b", b=256)),
                              [bp], [bv], nowaw=True)

                    def proj_g(base, func):
                        for dc in range(2):
                            wb, bwb = load_w(w_in_d, base + 128 * dc)
                            for n in range(4):
                                pt, bp = proj_fm(wb, bwb, 256 + 512 * n, 512, n + 1)
                                S(lambda e, pt=pt, dc=dc, n=n: e.activation(out=gT[:, dc, 512 * n:512 * n + 512], in_=pt[:], func=func), [bp], [bg], nowaw=True)

                    def proj_rot(base, dstT, bdst, is_k):
                        for dc in range(2):
                            (wb, bwb), (wb2, bwb2) = load_w(w_in_d, base + 128 * dc, swap=True)
                            if is_k:
                                pt, bp = proj_fm(wb, bwb, 0, 256, 0)
                                S(lambda e, pt=pt, dc=dc: e.copy(out=dstT[:, dc, 0:256], in_=pt[:, 0:256]), [bp], [bdst], nowaw=True)
                            for n in range(4):
                                p1, bp1 = proj_fm(wb, bwb, 256 + 512 * n, 512, n + 1)
                                p2, bp2 = proj_fm(wb2, bwb2, 256 + 512 * n, 512, n + 1)
                                if dc == 0:
                                    cosv = cst[:, K_COSR + 8 * n:K_COSR + 8 * n + 8].unsqueeze(2).broadcast_to([128, 8, 64])
                                    sinv = cst[:, K_SINR + 8 * n:K_SINR + 8 * n + 8].unsqueeze(2).broadcast_to([128, 8, 64])
                                else:
                                    cosv = cst[:, K_COSC:K_COSC + 64].unsqueeze(1).broadcast_to([128, 8, 64])
                                    sinv = cst[:, K_SINC:K_SINC + 64].unsqueeze(1).broadcast_to([128, 8, 64])
                                t1, bt1 = tA.next()
                                t2, bt2 = tB.next()
                                V(lambda e, t1=t1, p1=p1, cosv=cosv: e.tensor_tensor(out=t1[:].rearrange("p (a b) -> p a b", b=64), in0=p1[:].rearrange("p (a b) -> p a b", b=64), in1=cosv, op=ALU.mult),
                                  [bp1, bcst], [bt1])
                                V(lambda e, t2=t2, p2=p2, sinv=sinv: e.tensor_tensor(out=t2[:].rearrange("p (a b) -> p a b", b=64), in0=p2[:].rearrange("p (a b) -> p a b", b=64), in1=sinv, op=ALU.mult),
                                  [bp2, bcst], [bt2])
                                off = (256 if is_k else 0) + 512 * n
                                G(lambda e, t1=t1, t2=t2, dc=dc, off=off: e.tensor_tensor(out=dstT[:, dc, off:off + 512], in0=t1[:], in1=t2[:], op=ALU.add),
                                  [bt1, bt2], [bdst], nowaw=True)

                    def conv_silu(rw, brw, L, ch, dst_ap, bdst):
                        a = acc[:, 0:L]
                        V(lambda e: e.tensor_scalar(out=a, in0=rw[:, 0:L], scalar1=cw[:, ch, 0:1], scalar2=None, op0=ALU.mult), [brw, bcw], [bacc])
                        V(lambda e: e.scalar_tensor_tensor(out=a, in0=rw[:, 1:L + 1], scalar=cw[:, ch, 1:2], in1=a, op0=ALU.mult, op1=ALU.add), [brw, bcw, bacc], [bacc])
                        V(lambda e: e.scalar_tensor_tensor(out=a, in0=rw[:, 2:L + 2], scalar=cw[:, ch, 2:3], in1=a, op0=ALU.mult, op1=ALU.add), [brw, bcw, bacc], [bacc])
                        S(lambda e: e.activation(out=dst_ap, in_=a, func=AF.Silu, bias=cbias[:, ch:ch + 1], scale=1.0), [bacc, bcw], [bdst], nowaw=True)

                    def proj_conv(base, dstT, bdst, is_k, chbase):
                        for dc in range(2):
                            wb, bwb = load_w(w_in_d, base + 128 * dc)
                            ch = chbase + dc
                            if is_k:
                                pt, bp = proj_fm(wb, bwb, 0, 256, 0)
                                S(lambda e, pt=pt: e.copy(out=rawc[:, 1:257], in_=pt[:, 0:256]), [bp], [brawc], nowaw=True)
                                conv_silu(rawc, brawc, 256, ch, dstT[:, dc, 0:256], bdst)
                            for n in range(4):
                                pt, bp = proj_fm(wb, bwb, 256 + 512 * n, 512, n + 1)
                                S(lambda e, pt=pt, n=n: e.copy(out=raw[:, 1 + 512 * n:1 + 512 * n + 512], in_=pt[:]), [bp], [braw], nowaw=True)
                            off = 256 if is_k else 0
                            conv_silu(raw, braw, 2048, ch, dstT[:, dc, off:off + 2048], bdst)

                    def attention(h, is_ml, br):
                        dq = []
                        if not is_ml:
                            V(lambda e: e.tensor_scalar(out=acolr[0][:], in0=cst[:, K_PF:K_PF + 18], scalar1=nlgc[:, h:h + 1], scalar2=-LN16, op0=ALU.mult, op1=ALU.add),
                              [bcst, blgc], [bacolr[0]])
                            V(lambda e: e.tensor_scalar(out=acolr[1][:], in0=cst[:, K_PB:K_PB + 18], scalar1=lgc[:, 4 + h:5 + h], scalar2=-LN16, op0=ALU.mult, op1=ALU.add),
                              [bcst, blgc], [bacolr[1]])
                        ctxs = {}

                        def group_ctx(g, d):
                            rowt, brow = rowr.next()
                            if is_ml:
                                pt, bp = pA.next()
                                c0 = 256 + 512 * g if d == 0 else 512 * g
                                sl = seln if d == 0 else selp
                                T(lambda e: e.matmul(pt[:, :], lhsT=sl[:, h, :], rhs=T2[d][:, c0:c0 + 512], start=True, stop=True), [bsel, bT2[d]], [bp])
                                S(lambda e: e.copy(out=rowt[:], in_=pt[:]), [bp], [brow])
                            else:
                                base = float(256 + 512 * g) if d == 0 else float(512 * g)
                                sc = lgc[:, h:h + 1] if d == 0 else nlgc[:, 4 + h:5 + h]
                                V(lambda e: e.tensor_scalar(out=rowt[:], in0=cst[:, K_IOTA:K_IOTA + 512], scalar1=base, scalar2=sc, op0=ALU.add, op1=ALU.mult), [bcst, blgc], [brow])
                            keys = list(range(0, 4 * g + 6)) if d == 0 else [0, 1] + list(range(4 * g + 2, 18))

                            def applies(jf, sub):
                                if jf < 2:
                                    return True
                                jl = jf - 2
                                qi = 4 * g + sub
                                return jl <= qi if d == 0 else jl >= qi
                            first = {sub: [jf for jf in keys if applies(jf, sub)][0] for sub in range(4)}
                            last = {sub: [jf for jf in keys if applies(jf, sub)][-1] for sub in range(4)}
                            ctxs[(g, d)] = (rowt, brow, keys, applies, first, last)

                        def emit_S(g, d, jf):
                            rowt, brow, keys, applies, first, last = ctxs[(g, d)]
                            ps, bps = pA.next()
                            for dc in range(2):
                                T(lambda e, dc=dc: e.matmul(ps[:, :], lhsT=kT[:, dc, 128 * jf:128 * jf + 128], rhs=qT[:, dc, 512 * g:512 * g + 512], start=(dc == 0), stop=(dc == 1)),
                                  [bk, bq], [bps])
                            jl = jf - 2
                            masked = jf >= 2 and 4 * g <= jl <= 4 * g + 3
                            src, bsrc = rowt, brow
                            if masked:
                                jj = jl - 4 * g
                                mk = (K_MF if d == 0 else K_MB) + 384 - 128 * jj
                                pre, bpre = prer.next()
                                G(lambda e: e.tensor_tensor(out=pre[:], in0=rowt[:], in1=cst[:, mk:mk + 512], op=ALU.add), [brow, bcst], [bpre])
                                src, bsrc = pre, bpre
                            Dt, bD = Dr.next()
                            if is_ml:
                                bias_ap, bbias = acol[d][:, jf, h:h + 1], bacol[d]
                            else:
                                bias_ap, bbias = acolr[d][:, jf:jf + 1], bacolr[d]
                            S(lambda e: e.activation(out=Dt[:], in_=src[:], func=AF.Exp, bias=bias_ap, scale=1.0), [bsrc, bbias], [bD])
                            at, bat = atr.next()
                            V(lambda e: e.tensor_tensor(out=at[:], in0=ps[:], in1=Dt[:], op=ALU.mult), [bps, bD], [bat])
                            return at, bat

                        def emit_AV(g, d, jf, at, bat):
                            rowt, brow, keys, applies, first, last = ctxs[(g, d)]
                            for sub in range(4):
                                if not applies(jf, sub):
                                    continue
                                T(lambda e, sub=sub, st=(jf == first[sub]), sp=(jf == last[sub]): e.matmul(pO[sub][:, 0:257], lhsT=at[:, 128 * sub:128 * sub + 128], rhs=vv[:, jf, :], start=st, stop=sp),
                                  [bat, bv], [bpO[sub]])
                            if jf == keys[-1]:
                                group_done(g, d)

                        def group_done(g, d):
                            raw, braw_ = rawr.next()
                            for sub in range(4):
                                S(lambda e, sub=sub: e.copy(out=raw[:, sub, :], in_=pO[sub][:, 0:257]), [bpO[sub]], [braw_], nowaw=(sub > 0))
                            if is_ml:
                                dq.append(lambda: S(lambda e: e.activation(out=dn[:], in_=raw[:, :, 256], func=AF.Abs), [braw_], [bdn]))
                                dq.append(lambda: V(lambda e: e.tensor_tensor(out=dn[:], in0=dn[:], in1=em[d][:, h:h + 1].broadcast_to([128, 4]), op=ALU.max), [bdn, bem[d]], [bdn]))
                                dq.append(lambda: V(lambda e: e.reciprocal(out=dn[:], in_=dn[:]), [bdn], [bdn]))
                                if d == 0:
                                    dq.append(lambda: V(lambda e: e.tensor_tensor(out=hf[:], in0=raw[:, :, 0:256], in1=dn[:].unsqueeze(2).broadcast_to([128, 4, 256]), op=ALU.mult), [braw_, bdn], [bhf]))
                                else:
                                    dq.append(lambda: V(lambda e: e.tensor_tensor(out=hs[:], in0=raw[:, :, 0:256], in1=dn[:].unsqueeze(2).broadcast_to([128, 4, 256]), op=ALU.mult), [braw_, bdn], [bhs]))
                                    dq.append(lambda: V(lambda e: e.tensor_tensor(out=hs[:], in0=hs[:], in1=hf[:], op=ALU.add), [bhs, bhf], [bhs]))
                            else:
                                if d == 0:
                                    dq.append(lambda: S(lambda e: e.copy(out=hf[:], in_=raw[:, :, 0:256]), [braw_], [bhf]))
                                else:
                                    dq.append(lambda: V(lambda e: e.tensor_tensor(out=hs[:], in0=raw[:, :, 0:256], in1=hf[:], op=ALU.add), [braw_, bhf], [bhs]))
                            if d == 1:
                                def st_stats():
                                    for sub in range(4):
                                        V(lambda e, sub=sub: e.bn_stats(out=s6h[:, sub, :], in_=hs[:, sub, :]), [bhs], [bs6h], nowaw=(sub > 0))

                                def st_aggr():
                                    for sub in range(4):
                                        V(lambda e, sub=sub: e.bn_aggr(out=mvh[:, sub, :], in_=s6h[:, sub, :]), [bs6h], [bmvh], nowaw=(sub > 0))

                                def st_eps():
                                    V(lambda e: e.tensor_scalar(out=rsh[:], in0=mvh[:, :, 1], scalar1=LN_EPS, scalar2=None, op0=ALU.add), [bmvh], [brsh])

                                def st_pow():
                                    G(lambda e: e.tensor_tensor(out=rsh[:], in0=rsh[:], in1=mhalf[:], op=ALU.pow), [brsh, bmhalf], [brsh])

                                def st_norm():
                                    for sub in range(4):
                                        V(lambda e, sub=sub: e.tensor_scalar(out=hn[:, sub, :], in0=hs[:, sub, :], scalar1=mvh[:, sub, 0:1], scalar2=rsh[:, sub:sub + 1], op0=ALU.subtract, op1=ALU.mult),
                                          [bhs, bmvh, brsh], [bhn], nowaw=(sub > 0))

                                def st_tr():
                                    for sub in range(4):
                                        for dc in range(2):
                                            T(lambda e, sub=sub, dc=dc: e.transpose(out=pT[:, 4 * dc + sub, :], in_=hn[:, sub, 128 * dc:128 * dc + 128], identity=identb[:]),
                                              [bhn, bidb], [bpT], nowaw=not (sub == 0 and dc == 0))

                                def st_out():
                                    of, bof = ofm.next()
                                    for dc in range(2):
                                        V(lambda e, dc=dc: e.tensor_tensor(out=of[:, dc, :].rearrange("p (s c) -> p s c", c=128), in0=pT[:, 4 * dc:4 * dc + 4, :], in1=gT[:, dc, 512 * g:512 * g + 512].rearrange("p (s c) -> p s c", c=128), op=ALU.mult),
                                          [bpT, bg], [bof], nowaw=(dc > 0))
                                    P.dma("gpsimd", lambda e: e.dma_start(out=r_d[br, 2 * h:2 * h + 2, :, 512 * g:512 * g + 512].rearrange("c p t -> p c t"), in_=of[:]),
                                          f"rsp{ofm.last}", reads=[bof], writes=[br_d[br]], nowaw=True)
                                dq.extend([st_stats, st_aggr, st_eps, st_pow, st_norm, st_tr, st_out])

                        tiles = []
                        for g in range(4):
                            for d in range(2):
                                keys_ = list(range(0, 4 * g + 6)) if d == 0 else [0, 1] + list(range(4 * g + 2, 18))
                                for jf in keys_:
                                    tiles.append((g, d, jf))
                        queue = []
                        for ti_, (g, d, jf) in enumerate(tiles):
                            for (g2_, d2_, _) in tiles[ti_:ti_ + 4]:
                                if (g2_, d2_) not in ctxs:
                                    group_ctx(g2_, d2_)
                            at, bat = emit_S(g, d, jf)
                            queue.append((g, d, jf, at, bat))
                            if len(queue) > 2:
                                emit_AV(*queue.pop(0))
                            if dq:
                                dq.pop(0)()
                        while queue:
                            emit_AV(*queue.pop(0))
                        while dq:
                            dq.pop(0)()

                    for h in range(4):
                        proj_rot(C_RQ + 256 * h, qT, bq, False)
                        proj_rot(C_RK + 256 * h, kT, bk, True)
                        proj_v(C_RV + 256 * h)
                        proj_g(C_RG + 256 * h, AF.Silu)
                        attention(h, False, 0)
                    for h in range(4):
                        proj_conv(C_MQ + 256 * h, qT, bq, False, 2 * h)
                        proj_conv(C_MK + 256 * h, kT, bk, True, 8 + 2 * h)
                        proj_v(C_MV + 256 * h)
                        proj_g(C_MO + 256 * h, AF.Sigmoid)
                        attention(h, True, 1)

            if dbg and "r" in dbg and b == 0:
                with P.phase():
                    dr = P.sb("dbg_r", [128, 2048], BF16)
                    drf = P.sb("dbg_rf", [128, 2048], F32)
                    bdr = Buf()
                    for br in range(2):
                        for c in range(8):
                            P.dma("sync", lambda e, br=br, c=c: e.dma_start(out=dr[:], in_=r_d[br, c, :, :]), "dbgl", reads=[br_d[br]], writes=[bdr])
                            V(lambda e: e.tensor_copy(out=drf[:], in_=dr[:]), [bdr], [bdr])
                            P.dma("gpsimd", lambda e, br=br, c=c: e.dma_start(out=dbg_outs["r"][br, c, :, :], in_=drf[:]), "dbg", reads=[bdr], writes=[bdbg], nowaw=True)

            with P.phase():
                pA = Ring(P, "pA", 4, [128, 512], F32, psum=True)
                wst = Ring(P, "wst", 3, [128, 8, 128], F32)
                wbf = Ring(P, "wbf", 8, [128, 8, 128], BF16)
                load_w = make_wload(wst, wbf)
                rfull = P.sb("rfull", [128, 8, 2048], BF16)
                mfull = P.sb("mfull", [128, 8, 2048], BF16)
                brf, bmf = Buf(), Buf()
                for c in range(8):
                    P.dma("sync", lambda e, c=c: e.dma_start(out=rfull[:, c, :], in_=r_d[0, c, :, :]), "rfl", reads=[br_d[0]], writes=[brf], nowaw=True)
                    P.dma("sync", lambda e, c=c: e.dma_start(out=mfull[:, c, :], in_=r_d[1, c, :, :]), "mfl", reads=[br_d[1]], writes=[bmf], nowaw=True)
                sgr = Ring(P, "sgr", 2, [128, 512], F32)
                t1r = Ring(P, "t1r", 2, [128, 512], F32)
                yor = Ring(P, "yor", 2, [128, 512], BF16)
                def load_oc(oc):
                    ws = []
                    for (wsrc, gbase) in ((w_rb_d, C_GR), (w_mb_d, C_GM)):
                        ws.append((load_w(wsrc, 128 * oc), load_w(w_in_d, gbase + 128 * oc)))
                    return ws
                ws_all = {0: load_oc(0)}
                for oc in range(8):
                    ws = ws_all.pop(oc)
                    for n in range(4):
                        if n == 1 and oc + 1 < 8:
                            ws_all[oc + 1] = load_oc(oc + 1)
                        tt = []
                        for bi, (src_t, bsrc_t) in enumerate(((rfull, brf), (mfull, bmf))):
                            (wb, bwb), (wg_, bwg_) = ws[bi]
                            pa, bpa = pA.next()
                            for k in range(8):
                                T(lambda e, pa=pa, wb=wb, k=k, src_t=src_t, n=n: e.matmul(pa[:, :], lhsT=wb[:, k, :], rhs=src_t[:, k, 512 * n:512 * n + 512], start=(k == 0), stop=(k == 7)), [bwb, bsrc_t], [bpa])
                            pg, bpg = pA.next()
                            for k in range(8):
                                T(lambda e, pg=pg, wg_=wg_, k=k, n=n: e.matmul(pg[:, :], lhsT=wg_[:, k, :], rhs=uT[:, k, 256 + 512 * n:256 + 512 * n + 512], start=(k == 0), stop=(k == 7)), [bwg_, buT[n + 1]], [bpg])
                            sg, bsg = sgr.next()
                            S(lambda e, sg=sg, pg=pg: e.activation(out=sg[:], in_=pg[:], func=AF.Sigmoid), [bpg], [bsg])
                            t1, bt1 = t1r.next()
                            V(lambda e, t1=t1, pa=pa, sg=sg: e.tensor_tensor(out=t1[:], in0=pa[:], in1=sg[:], op=ALU.mult), [bpa, bsg], [bt1])
                            tt.append((t1, bt1))
                        yo, byo = yor.next()
                        G(lambda e, yo=yo, a=tt[0][0], c=tt[1][0]: e.tensor_tensor(out=yo[:], in0=a[:], in1=c[:], op=ALU.add), [tt[0][1], tt[1][1]], [byo])
                        P.dma("gpsimd", lambda e, yo=yo, oc=oc, n=n: e.dma_start(out=y_d[oc, :, 512 * n:512 * n + 512], in_=yo[:]), f"yspill{yor.last}", reads=[byo], writes=[by_d], nowaw=True)

            with P.phase():
                pA = Ring(P, "pA", 3, [128, 512], F32, psum=True)
                pO = [P.ps(f"pO{i}", [128, 512], F32) for i in range(4)]
                bpO = [Buf() for _ in range(4)]
                wst = Ring(P, "wst", 3, [128, 8, 128], F32)
                woutb = P.sb("woutb", [128, 8, 1024], BF16)
                bwout = Buf()
                for c in range(8):
                    wt, bw = wst.next()
                    P.dma("sync", lambda e, wt=wt, c=c: e.dma_start(out=wt[:], in_=w_out_d[:, 128 * c:128 * c + 128].rearrange("(c p) n -> p c n", p=128)), f"wst{wst.last}", writes=[bw])
                    S(lambda e, wt=wt, c=c: e.copy(out=woutb[:, :, 128 * c:128 * c + 128], in_=wt[:]), [bw], [bwout], nowaw=True)
                bct = {}
                bbc = Buf()
                for name, src in (("g1", mod_d[b:b + 1, 2048:3072]), ("sh2", mod_d[b:b + 1, 3072:4096]), ("sc2", mod_d[b:b + 1, 4096:5120]),
                                  ("l1g", lnp_d[0:1, :]), ("l1b", lnp_d[1:2, :])):
                    t = P.sb("bc_" + name, [128, 1024], F32)
                    bct[name] = t
                    P.dma("sync", lambda e, t=t, src=src: e.dma_start(out=t[:], in_=src.partition_broadcast(128)), "bc", reads=[bmod], writes=[bbc], nowaw=True)
                V(lambda e: e.tensor_scalar(out=bct["sc2"][:], in0=bct["sc2"][:], scalar1=1.0, scalar2=None, op0=ALU.add), [bbc], [bbc])
                yTr = Ring(P, "yT", 2, [128, 8, 512], BF16)
                xtr = Ring(P, "xt2", 2, [128, 1024], F32)
                ztr = Ring(P, "zt", 2, [128, 1024], F32)
                x1tr = Ring(P, "x1t", 2, [128, 1024], F32)
                u2tr = Ring(P, "u2t", 2, [128, 1024], F32)
                u2br = Ring(P, "u2b", 2, [128, 1024], BF16)
                u2Tr = Ring(P, "u2T", 2, [128, 8, 128], F32)
                s6r = Ring(P, "s6b", 2, [128, 2, 6], F32)
                m2r = Ring(P, "m2b", 2, [128, 2], F32)
                r2r = Ring(P, "r2b", 2, [128, 1], F32)
                yTs = {}
                u2s_ = {}

                def part1(i):
                    n, sub = i // 4, i % 4
                    gi = b * 16 + i
                    if sub == 0:
                        yT, byT = yTr.next()
                        P.dma("sync", lambda e: e.dma_start(out=yT[:], in_=y_d[:, :, 512 * n:512 * n + 512].rearrange("c p t -> p c t")), f"yT{yTr.last}", reads=[by_d], writes=[byT])
                        yTs[n] = (yT, byT)
                    yT, byT = yTs[n]
                    xt, bxt = xtr.next()
                    P.dma("sync", lambda e: e.dma_start(out=xt[:], in_=x_d[b, 128 * i:128 * i + 128, :]), f"xt2{xtr.last}", writes=[bxt])
                    for half in range(2):
                        for k in range(8):
                            T(lambda e, half=half, k=k: e.matmul(pO[half][:, :], lhsT=yT[:, k, 128 * sub:128 * sub + 128], rhs=woutb[:, k, 512 * half:512 * half + 512], start=(k == 0), stop=(k == 7)),
                              [byT, bwout], [bpO[half]])
                    zt, bzt = ztr.next()
                    for half in range(2):
                        V(lambda e, half=half: e.tensor_tensor(out=zt[:, 512 * half:512 * half + 512], in0=pO[half][:, :], in1=bct["g1"][:, 512 * half:512 * half + 512], op=ALU.mult),
                          [bpO[half], bbc], [bzt], nowaw=(half > 0))
                    V(lambda e: e.scalar_tensor_tensor(out=zt[:], in0=xt[:], scalar=DN_ALPHA, in1=zt[:], op0=ALU.mult, op1=ALU.add), [bxt, bzt], [bzt])
                    s6, bs6 = s6r.next()
                    m2, bm2 = m2r.next()
                    r2, br2 = r2r.next()
                    ln_stats(zt, bzt, s6, bs6, m2, bm2, r2, br2)
                    x1t, bx1t = x1tr.next()
                    V(lambda e: e.tensor_scalar(out=x1t[:], in0=zt[:], scalar1=m2[:, 0:1], scalar2=r2[:, 0:1], op0=ALU.subtract, op1=ALU.mult), [bzt, bm2, br2], [bx1t])
                    G(lambda e: e.tensor_tensor(out=x1t[:], in0=x1t[:], in1=bct["l1g"][:], op=ALU.mult), [bx1t, bbc], [bx1t])
                    G(lambda e: e.tensor_tensor(out=x1t[:], in0=x1t[:], in1=bct["l1b"][:], op=ALU.add), [bx1t, bbc], [bx1t])
                    P.dma("gpsimd", lambda e: e.dma_start(out=x1_d[128 * gi:128 * gi + 128, :], in_=x1t[:]), f"x1st{x1tr.last}", reads=[bx1t], writes=[bx1_d], nowaw=True)
                    s6, bs6 = s6r.next()
                    m2, bm2 = m2r.next()
                    r2, br2 = r2r.next()
                    ln_stats(x1t, bx1t, s6, bs6, m2, bm2, r2, br2)
                    u2t, bu2t = u2tr.next()
                    V(lambda e: e.tensor_scalar(out=u2t[:], in0=x1t[:], scalar1=m2[:, 0:1], scalar2=r2[:, 0:1], op0=ALU.subtract, op1=ALU.mult), [bx1t, bm2, br2], [bu2t])
                    G(lambda e: e.tensor_tensor(out=u2t[:], in0=u2t[:], in1=bct["sc2"][:], op=ALU.mult), [bu2t, bbc], [bu2t])
                    G(lambda e: e.tensor_tensor(out=u2t[:], in0=u2t[:], in1=bct["sh2"][:], op=ALU.add), [bu2t, bbc], [bu2t])
                    u2b, bu2b = u2br.next()
                    S(lambda e: e.copy(out=u2b[:], in_=u2t[:]), [bu2t], [bu2b])
                    P.dma("gpsimd", lambda e: e.dma_start(out=u2_d[128 * gi:128 * gi + 128, :], in_=u2b[:]), f"u2st{u2br.last}", reads=[bu2b], writes=[bu2_d], nowaw=True)
                    u2s_[i] = (u2t, bu2t)

                def part2(i):
                    gi = b * 16 + i
                    u2t, bu2t = u2s_.pop(i)
                    for k in range(8):
                        pp = pO[2 + k // 4]
                        T(lambda e, pp=pp, k=k: e.transpose(out=pp[:, 128 * (k % 4):128 * (k % 4) + 128], in_=u2t[:, 128 * k:128 * k + 128], identity=identf),
                          [bu2t, bcst], [bpO[2 + k // 4]], nowaw=(k % 4 > 0))
                    u2T, bu2T = u2Tr.next()
                    S(lambda e: e.copy(out=u2T[:, 0:4, :].rearrange("p a b -> p (a b)"), in_=pO[2][:, :]), [bpO[2]], [bu2T])
                    V(lambda e: e.tensor_copy(out=u2T[:, 4:8, :].rearrange("p a b -> p (a b)"), in_=pO[3][:, :]), [bpO[3]], [bu2T], nowaw=True)
                    pl, bpl = pA.next()
                    for k in range(8):
                        T(lambda e, k=k: e.matmul(pl[:, 0:36], lhsT=u2T[:, k, :], rhs=wrt[:, k, :], start=(k == 0), stop=(k == 7)), [bu2T, bwrt], [bpl])
                    V(lambda e: e.tensor_tensor(out=logits[:, gi, :], in0=pl[:, 0:36], in1=brt[:], op=ALU.add), [bpl, bwrt], [blog], nowaw=True)

                for i in range(17):
                    if i < 16:
                        part1(i)
                    if i >= 1:
                        part2(i - 1)

        if dbg and "x1" in dbg:
            with P.phase():
                t = P.sb("dbg_x1", [128, 1024], F32)
                bt = Buf()
                for gi in range(32):
                    P.dma("sync", lambda e, gi=gi: e.dma_start(out=t[:], in_=x1_d[128 * gi:128 * gi + 128, :]), "dbgl", reads=[bx1_d], writes=[bt])
                    P.dma("gpsimd", lambda e, gi=gi: e.dma_start(out=dbg_outs["x1"][128 * gi:128 * gi + 128, :], in_=t[:]), "dbg", reads=[bt], writes=[bdbg], nowaw=True)
                lt = P.sb("dbg_lg", [128, 32 * 36], F32)
                V(lambda e: e.tensor_copy(out=lt[:], in_=logits[:].rearrange("p a b -> p (a b)")), [blog], [bt])
                P.dma("gpsimd", lambda e: e.dma_start(out=dbg_outs["logits"], in_=lt[:]), "dbg", reads=[bt], writes=[bdbg], nowaw=True)

        MOE_PLACEHOLDER = True

        d1i = P.sb("d1i", [128, 32], I32)
        d2i = P.sb("d2i", [128, 32], I32)
        wt1 = P.sb("wt1", [128, 32], F32)
        wt2 = P.sb("wt2", [128, 32], F32)
        bei = P.sb("bei", [128, 96], I32)
        bd12, bwt12, bbe = Buf(), Buf(), Buf()
        with P.phase():
            pA = Ring(P, "pA", 3, [128, 512], F32, psum=True)
            ppos = [P.ps(f"ppos{i}", [128, 16, 32], F32) for i in range(2)]
            bppos = [Buf(), Buf()]

            def R(name, shape, dt=F32):
                return P.sb("rt_" + name, shape, dt), Buf()
            gmax, bgmax = R("gmax", [128, 32])
            ohg, bohg = R("ohg", [128, 32, 4])
            eg, beg = R("eg", [128, 32, 4])
            pg, bpg_ = R("pg", [128, 32])
            tmp4, btmp4 = R("tmp4", [128, 32, 4, 8])
            les, bles = R("les", [128, 32, 8])
            m1, bm1 = R("m1", [128, 32])
            mk1, bmk1 = R("mk1", [128, 32, 8])
            le2, ble2 = R("le2", [128, 32, 8])
            m2_, bm2_ = R("m2", [128, 32])
            mk2, bmk2 = R("mk2", [128, 32, 8])
            sg_, bsg_ = R("sg", [128, 32])
            A1, bA1 = R("A1", [128, 32, 4, 8])
            A2, bA2 = R("A2", [128, 32, 4, 8])
            Ab, bAb = R("Ab", [128, 32, 32], BF16)
            posf, bposf = R("posf", [128, 32, 32])
            cnt, bcnt = R("cnt", [128, 32])
            cnti, bcnti = R("cnti", [128, 32], I32)
            padf, bpadf = R("padf", [128, 32])
            pend, bpend = R("pend", [128, 32])
            poff, bpoff = R("poff", [128, 32])
            ones32, bones32 = R("ones32", [128, 32])
            dfl, bdfl = R("dfl", [128, 32])
            cmp, bcmp = R("cmp", [128, 48, 32])
            bef, bbef = R("bef", [128, 48])
            lgv = logits[:, :, 0:4]
            lev = logits[:, :, 4:36].rearrange("p t (g e) -> p t g e", e=8)

            def bc3(ap, n):
                return ap.unsqueeze(2).broadcast_to([128, 32, n])
            V(lambda e: e.memset(ones32[:], 1.0), [], [bones32])
            V(lambda e: e.tensor_reduce(out=gmax[:], in_=lgv, axis=AX.X, op=ALU.max), [blog], [bgmax])
            V(lambda e: e.tensor_tensor(out=ohg[:], in0=lgv, in1=bc3(gmax[:], 4), op=ALU.is_equal), [blog, bgmax], [bohg])
            V(lambda e: e.tensor_tensor(out=eg[:], in0=lgv, in1=bc3(gmax[:], 4), op=ALU.subtract), [blog, bgmax], [beg])
            S(lambda e: e.activation(out=eg[:], in_=eg[:], func=AF.Exp), [beg], [beg])
            V(lambda e: e.tensor_reduce(out=pg[:], in_=eg[:], axis=AX.X, op=ALU.add), [beg], [bpg_])
            V(lambda e: e.reciprocal(out=pg[:], in_=pg[:]), [bpg_], [bpg_])
            V(lambda e: e.tensor_tensor(out=tmp4[:], in0=lev, in1=ohg[:].unsqueeze(3).broadcast_to([128, 32, 4, 8]), op=ALU.mult), [blog, bohg], [btmp4])
            V(lambda e: e.tensor_reduce(out=les[:], in_=tmp4[:].rearrange("p t g e -> p t e g"), axis=AX.X, op=ALU.add), [btmp4], [bles])
            V(lambda e: e.tensor_reduce(out=m1[:], in_=les[:], axis=AX.X, op=ALU.max), [bles], [bm1])
            V(lambda e: e.tensor_tensor(out=mk1[:], in0=les[:], in1=bc3(m1[:], 8), op=ALU.is_equal), [bles, bm1], [bmk1])
            V(lambda e: e.scalar_tensor_tensor(out=le2[:], in0=mk1[:], scalar=-1e30, in1=les[:], op0=ALU.mult, op1=ALU.add), [bmk1, bles], [ble2])
            V(lambda e: e.tensor_reduce(out=m2_[:], in_=le2[:], axis=AX.X, op=ALU.max), [ble2], [bm2_])
            V(lambda e: e.tensor_tensor(out=mk2[:], in0=le2[:], in1=bc3(m2_[:], 8), op=ALU.is_equal), [ble2, bm2_], [bmk2])
            V(lambda e: e.tensor_tensor(out=sg_[:], in0=m1[:], in1=m2_[:], op=ALU.subtract), [bm1, bm2_], [bsg_])
            S(lambda e: e.activation(out=sg_[:], in_=sg_[:], func=AF.Sigmoid), [bsg_], [bsg_])
            V(lambda e: e.tensor_tensor(out=wt1[:], in0=pg[:], in1=sg_[:], op=ALU.mult), [bpg_, bsg_], [bwt12])
            V(lambda e: e.tensor_tensor(out=wt2[:], in0=pg[:], in1=wt1[:], op=ALU.subtract), [bpg_, bwt12], [bwt12])
            for (Ax, bAx, mk, bmk) in ((A1, bA1, mk1, bmk1), (A2, bA2, mk2, bmk2)):
                V(lambda e, Ax=Ax, mk=mk: e.tensor_tensor(out=Ax[:], in0=ohg[:].unsqueeze(3).broadcast_to([128, 32, 4, 8]), in1=mk[:].unsqueeze(2).broadcast_to([128, 32, 4, 8]), op=ALU.mult),
                  [bohg, bmk], [bAx])
            A1f = A1[:].rearrange("p t g e -> p t (g e)")
            A2f = A2[:].rearrange("p t g e -> p t (g e)")
            V(lambda e: e.tensor_tensor(out=Ab[:], in0=A1f, in1=A2f, op=ALU.add), [bA1, bA2], [bAb])
            for ti in range(32):
                pp = ppos[ti // 16]
                bpp = bppos[ti // 16]
                for tj in range(ti):
                    T(lambda e, pp=pp, ti=ti, tj=tj: e.matmul(pp[:, ti % 16, :], lhsT=onesb[:], rhs=Ab[:, tj, :], start=(tj == 0), stop=False), [bidb, bAb], [bpp], nowaw=True)
                T(lambda e, pp=pp, ti=ti: e.matmul(pp[:, ti % 16, :], lhsT=trib[:], rhs=Ab[:, ti, :], start=(ti == 0), stop=True), [bidb, bAb], [bpp], nowaw=True)
            pc, bpc = pA.next()
            for tj in range(32):
                T(lambda e, pc=pc, tj=tj: e.matmul(pc[:, 0:32], lhsT=onesb[:], rhs=Ab[:, tj, :], start=(tj == 0), stop=(tj == 31)), [bidb, bAb], [bpc])
            S(lambda e: e.copy(out=posf[:, 0:16, :], in_=ppos[0][:]), [bppos[0]], [bposf])
            V(lambda e: e.tensor_copy(out=posf[:, 16:32, :], in_=ppos[1][:]), [bppos[1]], [bposf], nowaw=True)
            V(lambda e: e.tensor_scalar(out=cnti[:], in0=pc[:, 0:32], scalar1=511.0, scalar2=None, op0=ALU.add), [bpc], [bcnti])
            V(lambda e: e.tensor_single_scalar(out=cnti[:], in_=cnti[:], scalar=9, op=ALU.arith_shift_right), [bcnti], [bcnti])
            V(lambda e: e.tensor_single_scalar(out=cnti[:], in_=cnti[:], scalar=9, op=ALU.logical_shift_left), [bcnti], [bcnti])
            V(lambda e: e.tensor_copy(out=padf[:], in_=cnti[:]), [bcnti], [bpadf])
            V(lambda e: e.tensor_tensor_scan(out=pend[:], data0=ones32[:], data1=padf[:], initial=0.0, op0=ALU.mult, op1=ALU.add), [bones32, bpadf], [bpend])
            V(lambda e: e.tensor_tensor(out=poff[:], in0=pend[:], in1=padf[:], op=ALU.subtract), [bpend, bpadf], [bpoff])
            V(lambda e: e.tensor_tensor(out=posf[:], in0=posf[:], in1=poff[:].unsqueeze(1).broadcast_to([128, 32, 32]), op=ALU.add), [bposf, bpoff], [bposf])
            for (Af, bAx, di) in ((A1f, bA1, d1i), (A2f, bA2, d2i)):
                V(lambda e, Af=Af: e.tensor_tensor(out=tmp4[:].rearrange("p t g e -> p t (g e)"), in0=Af, in1=posf[:], op=ALU.mult), [bAx, bposf], [btmp4])
                V(lambda e: e.tensor_reduce(out=dfl[:], in_=tmp4[:].rearrange("p t g e -> p t (g e)"), axis=AX.X, op=ALU.add), [btmp4], [bdfl])
                V(lambda e, di=di: e.tensor_copy(out=di[:], in_=dfl[:]), [bdfl], [bd12], nowaw=True)
            V(lambda e: e.tensor_tensor(out=cmp[:], in0=pend[:].unsqueeze(1).broadcast_to([128, 48, 32]), in1=cst[:, K_BLK:K_BLK + 48].unsqueeze(2).broadcast_to([128, 48, 32]), op=ALU.is_le),
              [bpend, bcst], [bcmp])
            V(lambda e: e.tensor_reduce(out=bef[:], in_=cmp[:], axis=AX.X, op=ALU.add), [bcmp], [bbef])
            V(lambda e: e.tensor_scalar(out=bei[:, 0:48], in0=bef[:], scalar1=31.0, scalar2=None, op0=ALU.min), [bbef], [bbe])
            if dbg and "route" in dbg:
                rt = P.sb("dbg_rt", [128, 4, 32], F32)
                brt_ = Buf()
                V(lambda e: e.tensor_copy(out=rt[:, 0, :], in_=d1i[:]), [bd12], [brt_], nowaw=True)
                V(lambda e: e.tensor_copy(out=rt[:, 1, :], in_=d2i[:]), [bd12], [brt_], nowaw=True)
                V(lambda e: e.tensor_copy(out=rt[:, 2, :], in_=wt1[:]), [bwt12], [brt_], nowaw=True)
                V(lambda e: e.tensor_copy(out=rt[:, 3, :], in_=wt2[:]), [bwt12], [brt_], nowaw=True)
                P.dma("gpsimd", lambda e: e.dma_start(out=dbg_outs["route"], in_=rt[:].rearrange("p a b -> p (a b)")), "dbg", reads=[brt_], writes=[bdbg], nowaw=True)
                bt_ = P.sb("dbg_be", [128, 96], F32)
                V(lambda e: e.memset(bt_[:], 0.0), [], [brt_], nowaw=True)
                V(lambda e: e.tensor_copy(out=bt_[:, 0:48], in_=bei[:, 0:48]), [bbe], [brt_])
                P.dma("gpsimd", lambda e: e.dma_start(out=dbg_outs["be"], in_=bt_[:]), "dbg", reads=[brt_], writes=[bdbg], nowaw=True)
            u2s = Ring(P, "u2s", 2, [128, 1024], BF16)
            for ti in range(32):
                ut, but = u2s.next()
                P.dma("sync", lambda e, ut=ut, ti=ti: e.dma_start(out=ut[:], in_=u2_d[128 * ti:128 * ti + 128, :]), f"u2s{ti % 2}", reads=[bu2_d], writes=[but])
                for di in (d1i, d2i):
                    P.dma("gpsimd", lambda e, ut=ut, ti=ti, di=di: e.indirect_dma_start(
                        out=xs_d, out_offset=bass.IndirectOffsetOnAxis(ap=di[:, ti:ti + 1], axis=0), in_=ut[:], in_offset=None),
                        f"xsc{ti % 2}", reads=[but, bd12], writes=[bxs_d], nowaw=True)

        with P.phase():
            pA = Ring(P, "pA", 2, [128, 512], F32, psum=True)
            pY = [P.ps(f"pY{i}", [128, 512], F32) for i in range(4)]
            bpY = [Buf(), Buf()]
            pT = P.ps("pT", [128, 8, 128], BF16)
            bpT = Buf()
            pT2 = P.ps("pT2", [128, 8, 128], BF16)
            bpT2 = Buf()
            wstg = Ring(P, "wstg", 4, [128, 2048], F32)
            w1b = Ring(P, "w1b", 2, [128, 8, 512], BF16)
            w3b = Ring(P, "w3b", 2, [128, 8, 512], BF16)
            w2b = Ring(P, "w2b", 2, [128, 4, 1024], BF16)
            xsb = Ring(P, "xsb", 4, [128, 1024], BF16)
            xsTr = Ring(P, "xsT", 3, [128, 8, 128], BF16)
            hsl = P.sb("hsl", [128, 512], F32)
            bhsl = Buf()
            hhr = Ring(P, "hh", 3, [128, 512], BF16)
            hTr = Ring(P, "hT", 2, [128, 4, 128], BF16)
            ysb = Ring(P, "ysb", 2, [128, 1024], F32)
            regs = {}
            blocks = [(sbk, sub_) for sbk in range(48) for sub_ in range(4)]
            wsets = {}
            hhs = {}

            def load_weights(sbk):
                w1t, bw1 = w1b.next()
                w3t, bw3 = w3b.next()
                w2t, bw2 = w2b.next()
                wsets[sbk] = (w1t, bw1, w3t, bw3, w2t, bw2)
                idx = 0
                for (wd, wt_, bwt_, eng, is2) in ((w_e1_d, w1t, bw1, "scalar", False), (w_e3_d, w3t, bw3, "scalar", False), (w_e2_d, w2t, bw2, "vector", True)):
                    for hf_ in range(2):
                        stg, bstg = wstg.next()

                        def dma_fn(e, wd=wd, hf_=hf_, stg=stg, is2=is2, idx=idx, sbk=sbk):
                            if idx == 0:
                                if "r" not in regs:
                                    regs["r"] = e.alloc_register("r_exp")
                                    regs["a"] = e.alloc_register("r_expa")
                                    regs["b"] = e.alloc_register("r_expb")
                                e.reg_load(regs["r"], bei[0:1, sbk:sbk + 1])
                                e.reg_mul(regs["a"], regs["r"], 524288)
                                e.reg_add(regs["b"], regs["a"], 262144)
                            rr = regs["a"] if hf_ == 0 else regs["b"]
                            if not is2:
                                src = bass.AP(wd.tensor, rr, [[512, 128], [65536, 4], [1, 512]])
                                ins = e.dma_start(out=stg[:].rearrange("p (c n) -> p c n", n=512), in_=src)
                            else:
                                src = bass.AP(wd.tensor, rr, [[1024, 128], [131072, 2], [1, 1024]])
                                ins = e.dma_start(out=stg[:].rearrange("p (c n) -> p c n", n=1024), in_=src)
                            RH = type(rr)
                            tmpn = [nm for grp in ins.ins.regs_accessed() for nm in grp if "_tmp_" in nm][0]
                            kk = int(tmpn.split("_")[-1])
                            e.free_register(RH(tmpn, rr.engine))
                            e.free_register(RH(f"SP_{rr.name}_snap_{kk - 2}", rr.engine))
                            return ins
                        P.dma("sync", dma_fn, f"wstg{wstg.last}", reads=[bbe], writes=[bstg])
                        if not is2:
                            dst = wt_[:, 4 * hf_:4 * hf_ + 4, :].rearrange("p c n -> p (c n)")
                        else:
                            dst = wt_[:, 2 * hf_:2 * hf_ + 2, :].rearrange("p c n -> p (c n)")
                        if eng == "scalar":
                            S(lambda e, dst=dst, stg=stg: e.copy(out=dst, in_=stg[:]), [bstg], [bwt_], nowaw=(hf_ > 0))
                        else:
                            P.op(eng, lambda e, dst=dst, stg=stg: e.tensor_copy(out=dst, in_=stg[:]), reads=[bstg], writes=[bwt_], nowaw=(hf_ > 0))
                        idx += 1

            xsTs = {}

            def stageTx(i):
                xb_, bxb_ = xs_tiles.pop(i)
                for k in range(8):
                    T(lambda e, k=k: e.transpose(out=pT[:, k, :], in_=xb_[:, 128 * k:128 * k + 128], identity=identb[:]), [bxb_, bidb], [bpT], nowaw=(k > 0))
                xsT, bxsT = xsTr.next()
                V(lambda e: e.tensor_copy(out=xsT[:], in_=pT[:]), [bpT], [bxsT])
                xsTs[i] = (xsT, bxsT)

            def stageH(i):
                sbk, sub_ = blocks[i]
                w1t, bw1, w3t, bw3, w2t, bw2 = wsets[sbk]
                xsT, bxsT = xsTs.pop(i)
                p1, bp1 = pA.next()
                p3, bp3 = pA.next()
                for k in range(8):
                    T(lambda e, k=k: e.matmul(p1[:, :], lhsT=xsT[:, k, :], rhs=w1t[:, k, :], start=(k == 0), stop=(k == 7)), [bxsT, bw1], [bp1])
                for k in range(8):
                    T(lambda e, k=k: e.matmul(p3[:, :], lhsT=xsT[:, k, :], rhs=w3t[:, k, :], start=(k == 0), stop=(k == 7)), [bxsT, bw3], [bp3])
                S(lambda e: e.activation(out=hsl[:], in_=p1[:], func=AF.Silu), [bp1], [bhsl])
                hh, bhh = hhr.next()
                V(lambda e: e.tensor_tensor(out=hh[:], in0=p3[:], in1=hsl[:], op=ALU.mult), [bp3, bhsl], [bhh])
                hhs[i] = (hh, bhh)

            hTs = {}

            def stageTh(i):
                hh, bhh = hhs.pop(i)
                for f in range(4):
                    T(lambda e, f=f: e.transpose(out=pT2[:, f, :], in_=hh[:, 128 * f:128 * f + 128], identity=identb[:]), [bhh, bidb], [bpT2], nowaw=(f > 0))
                hT, bhT = hTr.next()
                V(lambda e: e.tensor_copy(out=hT[:], in_=pT2[:, 0:4, :]), [bpT2], [bhT])
                hTs[i] = (hT, bhT)

            def stageY(i):
                sbk, sub_ = blocks[i]
                bk = 4 * sbk + sub_
                w1t, bw1, w3t, bw3, w2t, bw2 = wsets[sbk]
                hT, bhT = hTs.pop(i)
                par = bk % 2
                for half in range(2):
                    for f in range(4):
                        T(lambda e, half=half, f=f: e.matmul(pY[2 * par + half][:, :], lhsT=hT[:, f, :], rhs=w2t[:, f, 512 * half:512 * half + 512], start=(f == 0), stop=(f == 3)),
                          [bhT, bw2], [bpY[par]], nowaw=not (half == 0 and f == 0))
                yt_, byt_ = ysb.next()
                S(lambda e: e.copy(out=yt_[:, 0:512], in_=pY[2 * par][:, :]), [bpY[par]], [byt_])
                V(lambda e: e.tensor_copy(out=yt_[:, 512:1024], in_=pY[2 * par + 1][:, :]), [bpY[par]], [byt_], nowaw=True)
                P.dma("gpsimd", lambda e: e.dma_start(out=ys_d[128 * bk:128 * bk + 128, :], in_=yt_[:]), f"yst{ysb.last}", reads=[byt_], writes=[bys_d], nowaw=True)

            xs_tiles = {}

            def load_xs(i):
                sbk, sub_ = blocks[i]
                bk = 4 * sbk + sub_
                xb_, bxb_ = xsb.next()
                P.dma("gpsimd", lambda e: e.dma_start(out=xb_[:], in_=xs_d[128 * bk:128 * bk + 128, :]), f"xsb{xsb.last}", reads=[bxs_d], writes=[bxb_])
                xs_tiles[i] = (xb_, bxb_)
            load_xs(0)
            load_xs(1)
            load_weights(0)
            NB_ = len(blocks)
            for i in range(NB_ + 3):
                if i + 2 < NB_:
                    load_xs(i + 2)
                if i < NB_:
                    stageTx(i)
                if 1 <= i < NB_ + 1:
                    stageH(i - 1)
                if 2 <= i < NB_ + 2:
                    stageTh(i - 2)
                if i >= 3:
                    stageY(i - 3)
                if i >= 2 and (i - 2) % 4 == 0 and (i - 2) // 4 + 1 < 48:
                    load_weights((i - 2) // 4 + 1)

        with P.phase():
            bct = {}
            bbc = Buf()
            for name, src in (("g2_0", mod_d[0:1, 5120:6144]), ("g2_1", mod_d[1:2, 5120:6144]), ("l2g", lnp_d[2:3, :]), ("l2b", lnp_d[3:4, :])):
                t = P.sb("bc2_" + name, [128, 1024], F32)
                bct[name] = t
                P.dma("sync", lambda e, t=t, src=src: e.dma_start(out=t[:], in_=src.partition_broadcast(128)), "bc2", reads=[bmod], writes=[bbc], nowaw=True)
            y1r = Ring(P, "y1r", 3, [128, 1024], F32)
            y2r = Ring(P, "y2r", 3, [128, 1024], F32)
            x1r = Ring(P, "x1r", 3, [128, 1024], F32)
            outr = Ring(P, "outr", 3, [128, 1024], F32)
            s6, m2, r2 = P.sb("s6c", [128, 2, 6], F32), P.sb("m2c", [128, 2], F32), P.sb("r2c", [128, 1], F32)
            bs6, bm2, br2 = Buf(), Buf(), Buf()
            gt = {}

            def gathers(ti):
                y1, by1 = y1r.next()
                y2, by2 = y2r.next()
                x1, bx1 = x1r.next()
                P.dma("gpsimd", lambda e: e.indirect_dma_start(out=y1[:], out_offset=None, in_=ys_d, in_offset=bass.IndirectOffsetOnAxis(ap=d1i[:, ti:ti + 1], axis=0)),
                      f"y1g{y1r.last}", reads=[bys_d, bd12], writes=[by1])
                P.dma("gpsimd", lambda e: e.indirect_dma_start(out=y2[:], out_offset=None, in_=ys_d, in_offset=bass.IndirectOffsetOnAxis(ap=d2i[:, ti:ti + 1], axis=0)),
                      f"y2g{y2r.last}", reads=[bys_d, bd12], writes=[by2])
                P.dma("sync", lambda e: e.dma_start(out=x1[:], in_=x1_d[128 * ti:128 * ti + 128, :]), f"x1l{x1r.last}", reads=[bx1_d], writes=[bx1])
                gt[ti] = (y1, by1, y2, by2, x1, bx1)

            def combine(ti):
                b = ti // 16
                i = ti % 16
                y1, by1, y2, by2, x1, bx1 = gt.pop(ti)
                V(lambda e: e.tensor_scalar(out=y1[:], in0=y1[:], scalar1=wt1[:, ti:ti + 1], scalar2=None, op0=ALU.mult), [by1, bwt12], [by1])
                V(lambda e: e.scalar_tensor_tensor(out=y1[:], in0=y2[:], scalar=wt2[:, ti:ti + 1], in1=y1[:], op0=ALU.mult, op1=ALU.add), [by1, by2, bwt12], [by1])
                g2 = bct["g2_%d" % b]
                V(lambda e: e.tensor_tensor(out=y1[:], in0=y1[:], in1=g2[:], op=ALU.mult), [by1, bbc], [by1])
                V(lambda e: e.scalar_tensor_tensor(out=y1[:], in0=x1[:], scalar=DN_ALPHA, in1=y1[:], op0=ALU.mult, op1=ALU.add), [by1, bx1], [by1])
                s6, bs6 = s6r.next()
                m2, bm2 = m2r.next()
                r2, br2 = r2r.next()
                ln_stats(y1, by1, s6, bs6, m2, bm2, r2, br2)
                ot, bot = outr.next()
                V(lambda e: e.tensor_scalar(out=ot[:], in0=y1[:], scalar1=m2[:, 0:1], scalar2=r2[:, 0:1], op0=ALU.subtract, op1=ALU.mult), [by1, bm2, br2], [bot])
                G(lambda e: e.tensor_tensor(out=ot[:], in0=ot[:], in1=bct["l2g"][:], op=ALU.mult), [bot, bbc], [bot])
                G(lambda e: e.tensor_tensor(out=ot[:], in0=ot[:], in1=bct["l2b"][:], op=ALU.add), [bot, bbc], [bot])
                P.dma("gpsimd", lambda e: e.dma_start(out=out_d[b, 128 * i:128 * i + 128, :], in_=ot[:]), f"ost{outr.last}", reads=[bot], writes=[bout], nowaw=True)
            s6r = Ring(P, "s6cr", 2, [128, 2, 6], F32)
            m2r = Ring(P, "m2cr", 2, [128, 2], F32)
            r2r = Ring(P, "r2cr", 2, [128, 1], F32)
            gathers(0)
            gathers(1)
            for ti in range(32):
                if ti + 2 < 32:
                    gathers(ti + 2)
                combine(ti)
        P.barrier()
        P.emit()
    return nc


_CONSTS = None


def _prep_shared(inp):
    global _CONSTS
    if _CONSTS is None:
        _CONSTS = make_consts()
    f = lambda a: np.ascontiguousarray(a, dtype=np.float32)
    cwT = np.ascontiguousarray(inp["ml_conv_w"][0].reshape(3, 16, 128).transpose(2, 1, 0), dtype=np.float32)
    cbT = np.ascontiguousarray(inp["ml_conv_b"][0].reshape(16, 128).T, dtype=np.float32)
    return {
        "w_ada": f(inp["w_ada"][0]), "b_ada": f(inp["b_ada"][0].reshape(1, 6144)), "w_in": f(inp["w_in"][0]),
        "bmg": f(inp["b_mgate"][0].reshape(4, 4).T), "cw": cwT, "cb": cbT,
        "rdl": f(inp["ret_decay_logit"][0].reshape(1, 8)),
        "w_rb": f(inp["w_ret_branch"][0]), "w_mb": f(inp["w_ml_branch"][0]), "w_out": f(inp["w_out"][0]),
        "lnp": f(np.stack([inp["ln1_g"][0], inp["ln1_b"][0], inp["ln2_g"][0], inp["ln2_b"][0]], 0)),
        "w_rt": f(np.concatenate([inp["w_rg"][0], inp["w_re"][0]], 1)),
        "b_rt": f(np.concatenate([inp["b_rg"][0], inp["b_re"][0]], 0).reshape(1, 36)),
        "w_e1": f(inp["w_e1"][0]), "w_e3": f(inp["w_e3"][0]), "w_e2": f(inp["w_e2"][0]),
        "consts": _CONSTS,
    }


def _core_inputs(inp, shared, c):
    x = np.asarray(inp["x"], dtype=np.float32)
    ctx = np.asarray(inp["ctx"], dtype=np.float32)
    cc = np.asarray(inp["c"], dtype=np.float32)
    c_ctx = np.asarray(inp["c_ctx"], dtype=np.float32)
    vecs = np.stack([cc[2 * c], cc[2 * c + 1], c_ctx], 0)
    cT = np.ascontiguousarray(vecs.reshape(3, 8, 128).transpose(2, 1, 0))
    m = dict(shared)
    m["x"] = np.ascontiguousarray(x[2 * c:2 * c + 2])
    m["ctx"] = np.ascontiguousarray(ctx[2 * c:2 * c + 2])
    m["cT"] = cT
    return m


def kernel(**inputs):
    nc = build()
    shared = _prep_shared(inputs)
    in_maps = [_core_inputs(inputs, shared, c) for c in range(NCORES)]
    res = run_bass_kernel_spmd(nc, in_maps, core_ids=list(range(NCORES)))
    out = np.concatenate([np.asarray(r["out"]) for r in res.results], axis=0)
    return out.astype(np.float32)
```

```python
import contextlib
import math
import numpy as np
import concourse.bass as bass
import concourse.mybir as mybir
from concourse.bass_utils import run_bass_kernel_spmd

F32 = mybir.dt.float32
BF16 = mybir.dt.bfloat16
I32 = mybir.dt.int32
AF = mybir.ActivationFunctionType
ALU = mybir.AluOpType
AX = mybir.AxisListType

ENGS = ["tensor", "vector", "scalar", "gpsimd", "sync"]
LN_EPS = 1e-5
DN_ALPHA = 2.0 ** 0.25
NEG = -30000.0
LN16 = math.log(16.0)
NCORES = 8


import types


def _freeze(fn):
    if fn is None or fn.__closure__ is None:
        return fn
    cells = []
    for c in fn.__closure__:
        try:
            cells.append(types.CellType(c.cell_contents))
        except ValueError:
            cells.append(c)
    return types.FunctionType(fn.__code__, fn.__globals__, fn.__name__, fn.__defaults__, tuple(cells))


class Buf:
    __slots__ = ("w", "r", "pr")

    def __init__(self):
        self.w = []
        self.r = []
        self.pr = []


class Prog:
    def __init__(self, nc):
        self.nc = nc
        self.es = contextlib.ExitStack()
        self.ops = {e: [] for e in ENGS}
        self.cnt = {e: 0 for e in ENGS}
        self.dsem = {}
        self.known = {e: {} for e in ENGS}
        self.semobj = {}
        self.stack = [self.es]

    def _nm(self, name):
        self.uid = getattr(self, "uid", 0) + 1
        return f"t{self.uid}_{name}"

    def sb(self, name, shape, dt):
        return self.stack[-1].enter_context(self.nc.sbuf_tensor(self._nm(name), list(shape), dt))

    def ps(self, name, shape, dt=F32):
        return self.stack[-1].enter_context(self.nc.psum_tensor(self._nm(name), list(shape), dt))

    @contextlib.contextmanager
    def phase(self):
        es = contextlib.ExitStack()
        self.stack.append(es)
        with es:
            yield
            self.barrier()
        self.stack.pop()

    def _deps(self, eng, reads, writes, nowaw):
        deps = {}

        def add(ev):
            k, v = ev
            if deps.get(k, 0) < v:
                deps[k] = v
        for b in reads:
            for ev in b.w:
                add(ev)
        for b in writes:
            for ev in b.r:
                add(ev)
            if nowaw:
                for ev in b.pr:
                    add(ev)
            else:
                for ev in b.w:
                    add(ev)
        out = []
        kn = self.known[eng]
        for k, v in deps.items():
            if k == ("E", "tensor") and eng == "tensor":
                continue
            if kn.get(k, 0) >= v:
                continue
            kn[k] = v
            out.append((k, v))
        return out

    def _commit(self, ev, reads, writes, nowaw):
        for b in reads:
            b.r.append(ev)
        for b in writes:
            if nowaw:
                b.w.append(ev)
            else:
                b.pr = b.r
                b.r = []
                b.w = [ev]

    def op(self, eng, fn, reads=(), writes=(), nowaw=False):
        waits = self._deps(eng, reads, writes, nowaw)
        self.cnt[eng] += 1
        ev = (("E", eng), self.cnt[eng])
        self.ops[eng].append((waits, _freeze(fn), ("E", eng), 1))
        self._commit(ev, reads, writes, nowaw)
        return ev

    def dma(self, eng, fn, slot, reads=(), writes=(), nowaw=False):
        waits = self._deps(eng, reads, writes, nowaw)
        self.dsem[slot] = self.dsem.get(slot, 0) + 16
        ev = (("D", slot), self.dsem[slot])
        self.ops[eng].append((waits, _freeze(fn), ("D", slot), 16))
        self._commit(ev, reads, writes, nowaw)
        return ev

    def barrier(self):
        for e in ENGS:
            waits = []
            kn = self.known[e]
            for e2 in ENGS:
                k = ("E", e2)
                v = self.cnt[e2]
                if e2 != e and v > 0 and kn.get(k, 0) < v:
                    kn[k] = v
                    waits.append((k, v))
            for slot, v in self.dsem.items():
                k = ("D", slot)
                if kn.get(k, 0) < v:
                    kn[k] = v
                    waits.append((k, v))
            if waits:
                self.ops[e].append((waits, None, None, 0))

    def emit(self):
        nc = self.nc
        for e in ENGS:
            self.semobj[("E", e)] = self.es.enter_context(nc.semaphore("e_" + e))
        for slot in self.dsem:
            self.semobj[("D", slot)] = self.es.enter_context(nc.semaphore("d_" + str(slot)))
        block = self.es.enter_context(nc.Block())
        for e in ENGS:
            ops = self.ops[e]
            if not ops:
                continue

            def body(engine, ops=ops):
                for waits, fn, sk, inc in ops:
                    for k, v in waits:
                        engine.wait_ge(self.semobj[k], v)
                    if fn is not None:
                        fn(engine).then_inc(self.semobj[sk], inc)
            getattr(block, e)(body)


class Ring:
    def __init__(self, P, name, n, shape, dt, psum=False):
        self.t = [(P.ps if psum else P.sb)(f"{name}{i}", shape, dt) for i in range(n)]
        self.b = [Buf() for _ in range(n)]
        self.i = 0
        self.n = n

    def next(self):
        i = self.i
        self.i = (i + 1) % self.n
        self.last = i
        return self.t[i], self.b[i]


C_RQ, C_RK, C_RV, C_RG, C_MQ, C_MK, C_MV, C_MO, C_MG, C_GR, C_GM = (
    0, 1024, 2048, 3072, 4096, 5120, 6144, 7168, 8192, 8208, 9232)

K_ID = 0
K_IOTA = 128
K_PF = 640
K_PB = 658
K_COSR = 676
K_SINR = 708
K_COSC = 740
K_SINC = 804
K_TRI = 868
K_BLK = 996
K_MF = 1092
K_MB = 1988
K_ONE = 2884
NCONST = 3012


def make_consts():
    c = np.zeros((128, NCONST), np.float32)
    p = np.arange(128)
    c[:, K_ID:K_ID + 128] = np.eye(128, dtype=np.float32)
    c[:, K_IOTA:K_IOTA + 512] = np.arange(512, dtype=np.float32)[None, :]
    for jf in range(18):
        c[:, K_PF + jf] = 128 * jf + p
        c[:, K_PB + jf] = (2048 + 128 * jf + p) if jf < 2 else (128 * (jf - 2) + p)
    q = (p % 64).astype(np.float64)
    freq = 10000.0 ** (-q / 64.0)
    sgn = np.where(p < 64, -1.0, 1.0)
    rows = np.arange(32, dtype=np.float64)
    cols = np.arange(64, dtype=np.float64)
    c[:, K_COSR:K_COSR + 32] = np.cos(freq[:, None] * rows[None, :])
    c[:, K_SINR:K_SINR + 32] = sgn[:, None] * np.sin(freq[:, None] * rows[None, :])
    c[:, K_COSC:K_COSC + 64] = np.cos(freq[:, None] * cols[None, :])
    c[:, K_SINC:K_SINC + 64] = sgn[:, None] * np.sin(freq[:, None] * cols[None, :])
    c[:, K_TRI:K_TRI + 128] = (p[:, None] < p[None, :]).astype(np.float32)
    c[:, K_BLK:K_BLK + 96] = 512.0 * np.arange(96, dtype=np.float32)[None, :]
    cc = np.arange(896)[None, :]
    c[:, K_MF:K_MF + 896] = np.where(cc - 384 - p[:, None] >= 0, 0.0, NEG)
    c[:, K_MB:K_MB + 896] = np.where(p[:, None] - (cc - 384) > 0, 0.0, NEG)
    c[:, K_ONE:K_ONE + 128] = 1.0
    return c


def build(dbg=None):
    nc = bass.Bass("TRN2", target_bir_lowering=False)
    P = Prog(nc)

    def din(name, shape, dt=F32):
        return nc.dram_tensor(name, list(shape), dt, kind="ExternalInput").ap()

    def dscr(name, shape, dt=F32):
        return nc.dram_tensor(name, list(shape), dt).ap()

    x_d = din("x", [2, 2048, 1024])
    ctx_d = din("ctx", [2, 256, 1024])
    cT_d = din("cT", [128, 8, 3])
    w_ada_d = din("w_ada", [1024, 6144])
    b_ada_d = din("b_ada", [1, 6144])
    w_in_d = din("w_in", [1024, 10256])
    bmg_d = din("bmg", [4, 4])
    cw_d = din("cw", [128, 16, 3])
    cb_d = din("cb", [128, 16])
    rdl_d = din("rdl", [1, 8])
    w_rb_d = din("w_rb", [1024, 1024])
    w_mb_d = din("w_mb", [1024, 1024])
    w_out_d = din("w_out", [1024, 1024])
    lnp_d = din("lnp", [4, 1024])
    w_rt_d = din("w_rt", [1024, 36])
    b_rt_d = din("b_rt", [1, 36])
    w_e1_d = din("w_e1", [32, 1024, 512])
    w_e3_d = din("w_e3", [32, 1024, 512])
    w_e2_d = din("w_e2", [32, 512, 1024])
    consts_d = din("consts", [128, NCONST])
    out_d = nc.dram_tensor("out", [2, 2048, 1024], F32, kind="ExternalOutput").ap()

    mod_d = dscr("mod_s", [3, 6144])
    r_d = dscr("r_s", [2, 8, 128, 2048], BF16)
    y_d = dscr("y_s", [8, 128, 2048], BF16)
    x1_d = dscr("x1_s", [4096, 1024])
    u2_d = dscr("u2_s", [4096, 1024], BF16)
    xs_d = dscr("xs_s", [24576, 1024], BF16)
    ys_d = dscr("ys_s", [24576, 1024])
    dbg_outs = {}
    if dbg:
        for name, shape in dbg.items():
            dbg_outs[name] = nc.dram_tensor(name, list(shape), F32, kind="ExternalOutput").ap()

    bmod = Buf()
    br_d = [Buf(), Buf()]
    bx1_d = Buf()
    by_d = Buf()
    bu2_d = Buf()
    bxs_d = Buf()
    bys_d = Buf()
    bout = Buf()
    bdbg = Buf()

    with P.es:
        cst = P.sb("cst", [128, NCONST], F32)
        bcst = Buf()
        P.dma("sync", lambda e: e.dma_start(out=cst[:], in_=consts_d), "cst", writes=[bcst])
        identb = P.sb("identb", [128, 128], BF16)
        bidb = Buf()
        P.op("vector", lambda e: e.tensor_copy(out=identb[:], in_=cst[:, K_ID:K_ID + 128]), reads=[bcst], writes=[bidb])
        identf = cst[:, K_ID:K_ID + 128]
        trib = P.sb("trib", [128, 128], BF16)
        onesb = P.sb("onesb", [128, 128], BF16)
        P.op("vector", lambda e: e.tensor_copy(out=trib[:], in_=cst[:, K_TRI:K_TRI + 128]), reads=[bcst], writes=[bidb], nowaw=True)
        P.op("vector", lambda e: e.tensor_copy(out=onesb[:], in_=cst[:, K_ONE:K_ONE + 128]), reads=[bcst], writes=[bidb], nowaw=True)
        uT = P.sb("uT", [128, 8, 2304], BF16)
        buT = [Buf() for _ in range(5)]
        modc = P.sb("modc", [128, 6, 8], F32)
        bmodc = Buf()
        lgc = P.sb("lgc", [128, 8], F32)
        nlgc = P.sb("nlgc", [128, 8], F32)
        blgc = Buf()
        cw = P.sb("cw", [128, 16, 3], F32)
        cbias = P.sb("cbias", [128, 16], F32)
        bcw = Buf()
        P.dma("sync", lambda e: e.dma_start(out=cw[:], in_=cw_d), "cw", writes=[bcw])
        P.dma("sync", lambda e: e.dma_start(out=cbias[:], in_=cb_d), "cw", writes=[bcw], nowaw=True)
        bmg = P.sb("bmg", [4, 4], F32)
        bbmg = Buf()
        P.dma("sync", lambda e: e.dma_start(out=bmg[:], in_=bmg_d), "bmg", writes=[bbmg])
        logits = P.sb("logits", [128, 32, 36], F32)
        blog = Buf()
        wrt = P.sb("wrt", [128, 8, 36], F32)
        brt = P.sb("brt", [128, 36], F32)
        bwrt = Buf()
        P.dma("sync", lambda e: e.dma_start(out=wrt[:], in_=w_rt_d.rearrange("(c p) n -> p c n", p=128)), "wrt", writes=[bwrt])
        P.dma("sync", lambda e: e.dma_start(out=brt[:], in_=b_rt_d.partition_broadcast(128)), "wrt", writes=[bwrt], nowaw=True)

        P.dma("sync", lambda e: e.dma_start(out=lgc[:], in_=rdl_d.partition_broadcast(128)), "lgc", writes=[blgc])
        P.op("scalar", lambda e: e.activation(out=nlgc[:], in_=lgc[:], func=AF.Exp, scale=-1.0), reads=[blgc], writes=[blgc])
        P.op("scalar", lambda e: e.activation(out=nlgc[:], in_=nlgc[:], func=AF.Ln, bias=1.0, scale=1.0), reads=[blgc], writes=[blgc])
        P.op("vector", lambda e: e.tensor_scalar(out=lgc[:], in0=nlgc[:], scalar1=-1.0, scalar2=None, op0=ALU.mult), reads=[blgc], writes=[blgc])

        with P.phase():
            cT = P.sb("cT", [128, 8, 3], F32)
            bcT = Buf()
            P.dma("sync", lambda e: e.dma_start(out=cT[:], in_=cT_d), "cT", writes=[bcT])
            P.op("scalar", lambda e: e.activation(out=cT[:], in_=cT[:], func=AF.Silu), reads=[bcT], writes=[bcT])
            bada = P.sb("bada", [3, 6144], F32)
            bbada = Buf()
            P.dma("sync", lambda e: e.dma_start(out=bada[:], in_=b_ada_d.partition_broadcast(3)), "bada", writes=[bbada])
            modsb = P.sb("modsb", [3, 6144], F32)
            bmodsb = Buf()
            wa = Ring(P, "wa", 2, [128, 8, 512], F32)
            pmod = Ring(P, "pmod", 2, [128, 512], F32, psum=True)
            for cg in range(12):
                wt, bw = wa.next()
                P.dma("sync", lambda e, wt=wt, cg=cg: e.dma_start(
                    out=wt[:], in_=w_ada_d[:, 512 * cg:512 * cg + 512].rearrange("(c p) n -> p c n", p=128)),
                    f"wa{cg % 2}", writes=[bw])
                pt, bp = pmod.next()
                for k in range(8):
                    P.op("tensor", lambda e, pt=pt, wt=wt, k=k: e.matmul(pt[0:3, :], lhsT=cT[:, k, :], rhs=wt[:, k, :], start=(k == 0), stop=(k == 7)),
                         reads=[bcT, bw], writes=[bp])
                P.op("vector", lambda e, pt=pt, cg=cg: e.tensor_tensor(out=modsb[:, 512 * cg:512 * cg + 512], in0=pt[0:3, :], in1=bada[:, 512 * cg:512 * cg + 512], op=ALU.add),
                     reads=[bp, bbada], writes=[bmodsb], nowaw=True)
            P.dma("gpsimd", lambda e: e.dma_start(out=mod_d, in_=modsb[:]), "mod", reads=[bmodsb], writes=[bmod])
            for v in range(3):
                for which in range(2):
                    P.dma("gpsimd", lambda e, v=v, which=which: e.dma_start(
                        out=modc[:, 2 * v + which, :], in_=mod_d[v, 1024 * which:1024 * which + 1024].rearrange("(c p) -> p c", p=128),
                        allow_slow_non_contiguous=True), "modc", reads=[bmod], writes=[bmodc], nowaw=True)
            for v in range(3):
                P.op("vector", lambda e, v=v: e.tensor_scalar(out=modc[:, 2 * v + 1, :], in0=modc[:, 2 * v + 1, :], scalar1=1.0, scalar2=None, op0=ALU.add),
                     reads=[bmodc], writes=[bmodc])


        selp = P.sb("selp", [4, 4, 128], F32)
        seln = P.sb("seln", [4, 4, 128], F32)
        bsel = Buf()
        P.op("vector", lambda e: e.tensor_copy(out=selp[:], in_=cst[0:4, K_ID:K_ID + 4].unsqueeze(2).broadcast_to([4, 4, 128])), reads=[bcst], writes=[bsel])
        P.op("vector", lambda e: e.tensor_scalar(out=seln[:], in0=selp[:], scalar1=-1.0, scalar2=None, op0=ALU.mult), reads=[bsel], writes=[bsel])

        def V(fn, r=(), w=(), **kw):
            return P.op("vector", fn, reads=r, writes=w, **kw)

        def S(fn, r=(), w=(), **kw):
            return P.op("scalar", fn, reads=r, writes=w, **kw)

        def G(fn, r=(), w=(), **kw):
            return P.op("gpsimd", fn, reads=r, writes=w, **kw)

        def T(fn, r=(), w=(), **kw):
            return P.op("tensor", fn, reads=r, writes=w, **kw)

        def ln_stats(src, bsrc, s6, bs6, m, bm, r, brs):
            V(lambda e: e.bn_stats(out=s6[:, 0, :], in_=src[:, 0:512]), [bsrc], [bs6])
            V(lambda e: e.bn_stats(out=s6[:, 1, :], in_=src[:, 512:1024]), [bsrc], [bs6], nowaw=True)
            V(lambda e: e.bn_aggr(out=m[:], in_=s6[:].rearrange("p a b -> p (a b)")), [bs6], [bm])
            S(lambda e: e.activation(out=r[:], in_=m[:, 1:2], func=AF.Sqrt, bias=LN_EPS, scale=1.0), [bm], [brs])
            V(lambda e: e.reciprocal(out=r[:], in_=r[:]), [brs], [brs])

        def make_wload(wst, wbf):
            def load_w(src, c0, swap=False):
                wt, bw = wst.next()
                P.dma("sync", lambda e: e.dma_start(out=wt[:], in_=src[:, c0:c0 + 128].rearrange("(c p) n -> p c n", p=128)),
                      f"wst{wst.last}", writes=[bw])
                wb, bwb = wbf.next()
                S(lambda e: e.copy(out=wb[:], in_=wt[:]), [bw], [bwb])
                if swap:
                    wb2, bwb2 = wbf.next()
                    S(lambda e: e.copy(out=wb2[:, :, 0:64], in_=wt[:, :, 64:128]), [bw], [bwb2])
                    S(lambda e: e.copy(out=wb2[:, :, 64:128], in_=wt[:, :, 0:64]), [bw], [bwb2], nowaw=True)
                    return (wb, bwb), (wb2, bwb2)
                return wb, bwb
            return load_w

        for b in range(2):
            with P.phase():
                T2 = [P.sb(f"T2_{d}", [4, 2304], F32) for d in range(2)]
                bT2 = [Buf(), Buf()]
                acol = [P.sb(f"acol{d}", [128, 18, 4], F32) for d in range(2)]
                bacol = [Buf(), Buf()]
                em = [P.sb(f"em{d}", [128, 4], F32) for d in range(2)]
                bem = [Buf(), Buf()]
                pA = Ring(P, "pA", 3, [128, 512], F32, psum=True)
                pO = [P.ps(f"pO{i}", [128, 512], F32) for i in range(4)]
                bpO = [Buf() for _ in range(4)]
                pT = P.ps("pT", [128, 8, 128], BF16)
                bpT = Buf()
                with P.phase():
                    xr = Ring(P, "xr", 2, [128, 1024], F32)
                    xn = Ring(P, "xn", 2, [128, 1024], BF16)
                    st6 = Ring(P, "st6", 2, [128, 2, 6], F32)
                    mv = Ring(P, "mv", 2, [128, 2], F32)
                    rs = Ring(P, "rs", 2, [128, 1], F32)
                    tmpm = Ring(P, "tmpm", 2, [128, 8, 128], F32)
                    for j in range(18):
                        xt, bx = xr.next()
                        src = ctx_d[b, 128 * j:128 * j + 128, :] if j < 2 else x_d[b, 128 * (j - 2):128 * (j - 2) + 128, :]
                        P.dma("sync", lambda e, xt=xt, src=src: e.dma_start(out=xt[:], in_=src), f"xr{j % 2}", writes=[bx])
                        s6, bs6 = st6.next()
                        m, bm = mv.next()
                        r, brs = rs.next()
                        ln_stats(xt, bx, s6, bs6, m, bm, r, brs)
                        xb, bxb = xn.next()
                        V(lambda e, xb=xb, xt=xt, m=m, r=r: e.tensor_scalar(out=xb[:], in0=xt[:], scalar1=m[:, 0:1], scalar2=r[:, 0:1], op0=ALU.subtract, op1=ALU.mult),
                          [bx, bm, brs], [bxb])
                        for k in range(8):
                            T(lambda e, xb=xb, k=k: e.transpose(out=pT[:, k, :], in_=xb[:, 128 * k:128 * k + 128], identity=identb[:]),
                              [bxb, bidb], [bpT], nowaw=(k > 0))
                        v = 2 if j < 2 else b
                        tm, btm = tmpm.next()
                        V(lambda e, tm=tm, v=v: e.tensor_tensor(out=tm[:], in0=pT[:], in1=modc[:, 2 * v + 1, :].unsqueeze(2).broadcast_to([128, 8, 128]), op=ALU.mult),
                          [bpT, bmodc], [btm])
                        ch = 0 if j < 2 else 1 + (j - 2) // 4
                        G(lambda e, tm=tm, v=v, j=j: e.tensor_tensor(out=uT[:, :, 128 * j:128 * j + 128], in0=tm[:], in1=modc[:, 2 * v, :].unsqueeze(2).broadcast_to([128, 8, 128]), op=ALU.add),
                          [btm, bmodc], [buT[ch]], nowaw=True)

                    wgs = P.sb("wgs", [128, 8, 16], F32)
                    wgb = P.sb("wgb", [128, 8, 16], BF16)
                    bwg = Buf()
                    P.dma("sync", lambda e: e.dma_start(out=wgs[:], in_=w_in_d[:, C_MG:C_MG + 16].rearrange("(c p) n -> p c n", p=128)), "wg", writes=[bwg])
                    V(lambda e: e.tensor_copy(out=wgb[:], in_=wgs[:]), [bwg], [bwg])
                    T0 = P.sb("T0", [4, 2304], F32)
                    T1 = P.sb("T1", [4, 2304], F32)
                    ones4 = P.sb("ones4", [4, 2304], F32)
                    bT0 = Buf()
                    bT1 = Buf()
                    bones4 = Buf()
                    V(lambda e: e.memset(ones4[:], 1.0), [], [bones4])
                    mx = P.sb("mx", [4, 2], F32)
                    bmx = Buf()
                    mrow = P.sb("mrow", [4, 128], F32)
                    bmrow = Buf()
                    chunks = [(0, 256)] + [(256 + 512 * n, 512) for n in range(4)]
                    for d in range(2):
                        for gi, Tt, bT in ((2 * d, T0, bT0), (2 * d + 1, T1, bT1)):
                            for ci, (c0, cl) in enumerate(chunks):
                                pt, bp = pA.next()
                                for k in range(8):
                                    T(lambda e, pt=pt, k=k, gi=gi, c0=c0, cl=cl: e.matmul(pt[0:4, 0:cl], lhsT=wgb[:, k, 4 * gi:4 * gi + 4], rhs=uT[:, k, c0:c0 + cl], start=(k == 0), stop=(k == 7)),
                                      [bwg, buT[ci]], [bp])
                                if d == 0:
                                    o0 = c0
                                else:
                                    o0 = 2048 if ci == 0 else c0 - 256
                                V(lambda e, pt=pt, Tt=Tt, gi=gi, o0=o0, cl=cl: e.tensor_scalar(out=Tt[:, o0:o0 + cl], in0=pt[0:4, 0:cl], scalar1=bmg[:, gi:gi + 1], scalar2=None, op0=ALU.add),
                                  [bp, bbmg], [bT], nowaw=(ci > 0))
                        S(lambda e: e.activation(out=T1[:], in_=T1[:], func=AF.Exp, scale=-1.0), [bT1], [bT1])
                        S(lambda e: e.activation(out=T1[:], in_=T1[:], func=AF.Ln, bias=1.0, scale=1.0), [bT1], [bT1])
                        V(lambda e, d=d: e.tensor_tensor_scan(out=T2[d][:], data0=ones4[:], data1=T1[:], initial=0.0, op0=ALU.mult, op1=ALU.add),
                          [bT1, bones4], [bT2[d]])
                        if d == 1:
                            V(lambda e: e.tensor_tensor(out=T2[1][:], in0=T2[1][:], in1=T1[:], op=ALU.subtract), [bT2[1], bT1], [bT2[1]])
                        V(lambda e: e.tensor_reduce(out=mx[:, 0:1], in_=T0[:], axis=AX.X, op=ALU.max), [bT0], [bmx])
                        V(lambda e: e.tensor_scalar(out=mx[:, 1:2], in0=mx[:, 0:1], scalar1=LN16, scalar2=None, op0=ALU.add), [bmx], [bmx])
                        V(lambda e, d=d: e.scalar_tensor_tensor(out=T0[:], in0=T0[:], scalar=mx[:, 1:2], in1=T2[d][:], op0=ALU.subtract, op1=(ALU.add if d == 0 else ALU.subtract)),
                          [bT0, bmx, bT2[d]], [bT0])
                        pt, bp = pA.next()
                        for jo in range(18):
                            jf = jo if d == 0 else (jo + 2 if jo < 16 else jo - 16)
                            T(lambda e, pt=pt, jo=jo, jf=jf: e.transpose(out=pt[:, 4 * jf:4 * jf + 4], in_=T0[0:4, 128 * jo:128 * jo + 128], identity=identf[0:4, 0:4]),
                              [bT0, bcst], [bp], nowaw=(jo > 0))
                        S(lambda e, pt=pt, d=d: e.copy(out=acol[d][:].rearrange("p a b -> p (a b)"), in_=pt[:, 0:72]), [bp], [bacol[d]])
                        V(lambda e: e.tensor_scalar(out=mrow[:], in0=ones4[:, 0:128], scalar1=mx[:, 0:1], scalar2=-1.0, op0=ALU.mult, op1=ALU.mult), [bmx, bones4], [bmrow])
                        pt, bp = pA.next()
                        T(lambda e, pt=pt: e.transpose(out=pt[:, 0:4], in_=mrow[0:4, :], identity=identf[0:4, 0:4]), [bmrow, bcst], [bp])
                        S(lambda e, pt=pt, d=d: e.activation(out=em[d][:], in_=pt[:, 0:4], func=AF.Exp), [bp], [bem[d]])

                if dbg and "uT" in dbg and b == 0:
                    du = P.sb("dbg_u", [128, 2304], F32)
                    bdu = Buf()
                    V(lambda e: e.tensor_copy(out=du[:], in_=uT[:, 0, :]), buT, [bdu])
                    P.dma("gpsimd", lambda e: e.dma_start(out=dbg_outs["uT"], in_=du[:]), "dbg", reads=[bdu], writes=[bdbg], nowaw=True)
                    da = P.sb("dbg_a", [128, 2, 76], F32)
                    bda = Buf()
                    for d in range(2):
                        V(lambda e, d=d: e.tensor_copy(out=da[:, d, 0:72], in_=acol[d][:].rearrange("p a b -> p (a b)")), [bacol[d]], [bda], nowaw=True)
                        V(lambda e, d=d: e.tensor_copy(out=da[:, d, 72:76], in_=em[d][:]), [bem[d]], [bda], nowaw=True)
                    P.dma("gpsimd", lambda e: e.dma_start(out=dbg_outs["acol"], in_=da[:].rearrange("p a b -> p (a b)")), "dbg", reads=[bda], writes=[bdbg], nowaw=True)
                    P.dma("gpsimd", lambda e: e.dma_start(out=dbg_outs["T2"][0:4, :], in_=T2[0][:]), "dbg", reads=[bT2[0]], writes=[bdbg], nowaw=True)
                    P.dma("gpsimd", lambda e: e.dma_start(out=dbg_outs["T2"][4:8, :], in_=T2[1][:]), "dbg", reads=[bT2[1]], writes=[bdbg], nowaw=True)

                with P.phase():
                    wst = Ring(P, "wst", 3, [128, 8, 128], F32)
                    wbf = Ring(P, "wbf", 4, [128, 8, 128], BF16)
                    load_w = make_wload(wst, wbf)
                    wvt = P.sb("wvt", [128, 8, 256], BF16)
                    bwv = Buf()
                    qT = P.sb("qT", [128, 2, 2048], BF16)
                    kT = P.sb("kT", [128, 2, 2304], BF16)
                    vv = P.sb("vv", [128, 18, 257], BF16)
                    gT = P.sb("gT", [128, 2, 2048], BF16)
                    bq, bk, bv, bg = Buf(), Buf(), Buf(), Buf()
                    V(lambda e: e.memset(vv[:, :, 256:257], 1.0), [], [bv])
                    raw = P.sb("raw", [128, 2050], F32)
                    rawc = P.sb("rawc", [128, 258], F32)
                    acc = P.sb("acc", [128, 2048], F32)
                    braw, brawc, bacc = Buf(), Buf(), Buf()
                    V(lambda e: e.memset(raw[:], 0.0), [], [braw])
                    V(lambda e: e.memset(rawc[:], 0.0), [], [brawc])
                    tA = Ring(P, "tA", 2, [128, 512], F32)
                    tB = Ring(P, "tB", 2, [128, 512], F32)
                    rowr = Ring(P, "rowr", 2, [128, 512], F32)
                    prer = Ring(P, "prer", 2, [128, 512], F32)
                    Dr = Ring(P, "Dr", 3, [128, 512], BF16)
                    atr = Ring(P, "atr", 3, [128, 512], BF16)
                    nmr = P.sb("nmr", [128, 4], F32)
                    bnmr = Buf()
                    mhalf = P.sb("mhalf", [128, 4], F32)
                    bmhalf = Buf()
                    V(lambda e: e.memset(mhalf[:], -0.5), [], [bmhalf])
                    acolr = [P.sb(f"acolr{d}", [128, 18], F32) for d in range(2)]
                    bacolr = [Buf(), Buf()]
                    rawr = Ring(P, "rawr", 2, [128, 4, 257], F32)
                    dn = P.sb("dn", [128, 4], F32)
                    bdn = Buf()
                    hf = P.sb("hf", [128, 4, 256], F32)
                    hs = P.sb("hs", [128, 4, 256], F32)
                    hn = P.sb("hn", [128, 4, 256], BF16)
                    bhf, bhs, bhn = Buf(), Buf(), Buf()
                    s6h = P.sb("s6h", [128, 4, 6], F32)
                    mvh = P.sb("mvh", [128, 4, 2], F32)
                    rsh = P.sb("rsh", [128, 4], F32)
                    bs6h, bmvh, brsh = Buf(), Buf(), Buf()
                    ofm = Ring(P, "ofm", 2, [128, 2, 512], BF16)

                    def proj_fm(wb, bwb, c0, cl, ci):
                        pt, bp = pA.next()
                        for k in range(8):
                            T(lambda e, k=k: e.matmul(pt[:, 0:cl], lhsT=wb[:, k, :], rhs=uT[:, k, c0:c0 + cl], start=(k == 0), stop=(k == 7)),
                              [bwb, buT[ci]], [bp])
                        return pt, bp

                    def proj_v(base):
                        for vu in range(2):
                            wt, bw = wst.next()
                            P.dma("sync", lambda e, wt=wt, vu=vu: e.dma_start(out=wt[:], in_=w_in_d[:, base + 128 * vu:base + 128 * vu + 128].rearrange("(c p) n -> p c n", p=128)),
                                  f"wst{wst.last}", writes=[bw])
                            S(lambda e, wt=wt, vu=vu: e.copy(out=wvt[:, :, 128 * vu:128 * vu + 128], in_=wt[:]), [bw], [bwv], nowaw=(vu > 0))
                        for q2 in range(9):
                            pt, bp = pA.next()
                            for jj in range(2):
                                j = 2 * q2 + jj
                                ci = 0 if j < 2 else 1 + (j - 2) // 4
                                for k in range(8):
                                    T(lambda e, pt=pt, jj=jj, j=j, k=k: e.matmul(pt[:, 256 * jj:256 * jj + 256], lhsT=uT[:, k, 128 * j:128 * j + 128], rhs=wvt[:, k, :], start=(k == 0), stop=(k == 7)),
                                      [bwv, buT[ci]], [bp], nowaw=not (jj == 0 and k == 0))
                            S(lambda e, pt=pt, q2=q2: e.copy(out=vv[:, 2 * q2:2 * q2 + 2, 0:256], in_=pt[:, :].rearrange("p (a b) -> p a b", b=256)),
                              [bp], [bv], nowaw=True)

                    def proj_g(base, func):
                        for dc in range(2):
                            wb, bwb = load_w(w_in_d, base + 128 * dc)
                            for n in range(4):
                                pt, bp = proj_fm(wb, bwb, 256 + 512 * n, 512, n + 1)
                                S(lambda e, pt=pt, dc=dc, n=n: e.activation(out=gT[:, dc, 512 * n:512 * n + 512], in_=pt[:], func=func), [bp], [bg], nowaw=True)

                    def proj_rot(base, dstT, bdst, is_k):
                        for dc in range(2):
                            (wb, bwb), (wb2, bwb2) = load_w(w_in_d, base + 128 * dc, swap=True)
                            if is_k:
                                pt, bp = proj_fm(wb, bwb, 0, 256, 0)
                                S(lambda e, pt=pt, dc=dc: e.copy(out=dstT[:, dc, 0:256], in_=pt[:, 0:256]), [bp], [bdst], nowaw=True)
                            for n in range(4):
                                p1, bp1 = proj_fm(wb, bwb, 256 + 512 * n, 512, n + 1)
                                p2, bp2 = proj_fm(wb2, bwb2, 256 + 512 * n, 512, n + 1)
                                if dc == 0:
                                    cosv = cst[:, K_COSR + 8 * n:K_COSR + 8 * n + 8].unsqueeze(2).broadcast_to([128, 8, 64])
                                    sinv = cst[:, K_SINR + 8 * n:K_SINR + 8 * n + 8].unsqueeze(2).broadcast_to([128, 8, 64])
                                else:
                                    cosv = cst[:, K_COSC:K_COSC + 64].unsqueeze(1).broadcast_to([128, 8, 64])
                                    sinv = cst[:, K_SINC:K_SINC + 64].unsqueeze(1).broadcast_to([128, 8, 64])
                                t1, bt1 = tA.next()
                                t2, bt2 = tB.next()
                                V(lambda e, t1=t1, p1=p1, cosv=cosv: e.tensor_tensor(out=t1[:].rearrange("p (a b) -> p a b", b=64), in0=p1[:].rearrange("p (a b) -> p a b", b=64), in1=cosv, op=ALU.mult),
                                  [bp1, bcst], [bt1])
                                V(lambda e, t2=t2, p2=p2, sinv=sinv: e.tensor_tensor(out=t2[:].rearrange("p (a b) -> p a b", b=64), in0=p2[:].rearrange("p (a b) -> p a b", b=64), in1=sinv, op=ALU.mult),
                                  [bp2, bcst], [bt2])
                                off = (256 if is_k else 0) + 512 * n
                                G(lambda e, t1=t1, t2=t2, dc=dc, off=off: e.tensor_tensor(out=dstT[:, dc, off:off + 512], in0=t1[:], in1=t2[:], op=ALU.add),
                                  [bt1, bt2], [bdst], nowaw=True)

                    def conv_silu(rw, brw, L, ch, dst_ap, bdst):
                        a = acc[:, 0:L]
                        V(lambda e: e.tensor_scalar(out=a, in0=rw[:, 0:L], scalar1=cw[:, ch, 0:1], scalar2=None, op0=ALU.mult), [brw, bcw], [bacc])
                        V(lambda e: e.scalar_tensor_tensor(out=a, in0=rw[:, 1:L + 1], scalar=cw[:, ch, 1:2], in1=a, op0=ALU.mult, op1=ALU.add), [brw, bcw, bacc], [bacc])
                        V(lambda e: e.scalar_tensor_tensor(out=a, in0=rw[:, 2:L + 2], scalar=cw[:, ch, 2:3], in1=a, op0=ALU.mult, op1=ALU.add), [brw, bcw, bacc], [bacc])
                        S(lambda e: e.activation(out=dst_ap, in_=a, func=AF.Silu, bias=cbias[:, ch:ch + 1], scale=1.0), [bacc, bcw], [bdst], nowaw=True)

                    def proj_conv(base, dstT, bdst, is_k, chbase):
                        for dc in range(2):
                            wb, bwb = load_w(w_in_d, base + 128 * dc)
                            ch = chbase + dc
                            if is_k:
                                pt, bp = proj_fm(wb, bwb, 0, 256, 0)
                                S(lambda e, pt=pt: e.copy(out=rawc[:, 1:257], in_=pt[:, 0:256]), [bp], [brawc], nowaw=True)
                                conv_silu(rawc, brawc, 256, ch, dstT[:, dc, 0:256], bdst)
                            for n in range(4):
                                pt, bp = proj_fm(wb, bwb, 256 + 512 * n, 512, n + 1)
                                S(lambda e, pt=pt, n=n: e.copy(out=raw[:, 1 + 512 * n:1 + 512 * n + 512], in_=pt[:]), [bp], [braw], nowaw=True)
                            off = 256 if is_k else 0
                            conv_silu(raw, braw, 2048, ch, dstT[:, dc, off:off + 2048], bdst)

                    def attention(h, is_ml, br):
                        dq = []
                        if not is_ml:
                            V(lambda e: e.tensor_scalar(out=acolr[0][:], in0=cst[:, K_PF:K_PF + 18], scalar1=nlgc[:, h:h + 1], scalar2=-LN16, op0=ALU.mult, op1=ALU.add),
                              [bcst, blgc], [bacolr[0]])
                            V(lambda e: e.tensor_scalar(out=acolr[1][:], in0=cst[:, K_PB:K_PB + 18], scalar1=lgc[:, 4 + h:5 + h], scalar2=-LN16, op0=ALU.mult, op1=ALU.add),
                              [bcst, blgc], [bacolr[1]])
                        ctxs = {}

                        def group_ctx(g, d):
                            rowt, brow = rowr.next()
                            if is_ml:
                                pt, bp = pA.next()
                                c0 = 256 + 512 * g if d == 0 else 512 * g
                                sl = seln if d == 0 else selp
                                T(lambda e: e.matmul(pt[:, :], lhsT=sl[:, h, :], rhs=T2[d][:, c0:c0 + 512], start=True, stop=True), [bsel, bT2[d]], [bp])
                                S(lambda e: e.copy(out=rowt[:], in_=pt[:]), [bp], [brow])
                            else:
                                base = float(256 + 512 * g) if d == 0 else float(512 * g)
                                sc = lgc[:, h:h + 1] if d == 0 else nlgc[:, 4 + h:5 + h]
                                V(lambda e: e.tensor_scalar(out=rowt[:], in0=cst[:, K_IOTA:K_IOTA + 512], scalar1=base, scalar2=sc, op0=ALU.add, op1=ALU.mult), [bcst, blgc], [brow])
                            keys = list(range(0, 4 * g + 6)) if d == 0 else [0, 1] + list(range(4 * g + 2, 18))

                            def applies(jf, sub):
                                if jf < 2:
                                    return True
                                jl = jf - 2
                                qi = 4 * g + sub
                                return jl <= qi if d == 0 else jl >= qi
                            first = {sub: [jf for jf in keys if applies(jf, sub)][0] for sub in range(4)}
                            last = {sub: [jf for jf in keys if applies(jf, sub)][-1] for sub in range(4)}
                            ctxs[(g, d)] = (rowt, brow, keys, applies, first, last)

                        def emit_S(g, d, jf):
                            rowt, brow, keys, applies, first, last = ctxs[(g, d)]
                            ps, bps = pA.next()
                            for dc in range(2):
                                T(lambda e, dc=dc: e.matmul(ps[:, :], lhsT=kT[:, dc, 128 * jf:128 * jf + 128], rhs=qT[:, dc, 512 * g:512 * g + 512], start=(dc == 0), stop=(dc == 1)),
                                  [bk, bq], [bps])
                            jl = jf - 2
                            masked = jf >= 2 and 4 * g <= jl <= 4 * g + 3
                            src, bsrc = rowt, brow
                            if masked:
                                jj = jl - 4 * g
                                mk = (K_MF if d == 0 else K_MB) + 384 - 128 * jj
                                pre, bpre = prer.next()
                                G(lambda e: e.tensor_tensor(out=pre[:], in0=rowt[:], in1=cst[:, mk:mk + 512], op=ALU.add), [brow, bcst], [bpre])
                                src, bsrc = pre, bpre
                            Dt, bD = Dr.next()
                            if is_ml:
                                bias_ap, bbias = acol[d][:, jf, h:h + 1], bacol[d]
                            else:
                                bias_ap, bbias = acolr[d][:, jf:jf + 1], bacolr[d]
                            S(lambda e: e.activation(out=Dt[:], in_=src[:], func=AF.Exp, bias=bias_ap, scale=1.0), [bsrc, bbias], [bD])
                            at, bat = atr.next()
                            V(lambda e: e.tensor_tensor(out=at[:], in0=ps[:], in1=Dt[:], op=ALU.mult), [bps, bD], [bat])
                            return at, bat

                        def emit_AV(g, d, jf, at, bat):
                            rowt, brow, keys, applies, first, last = ctxs[(g, d)]
                            for sub in range(4):
                                if not applies(jf, sub):
                                    continue
                                T(lambda e, sub=sub, st=(jf == first[sub]), sp=(jf == last[sub]): e.matmul(pO[sub][:, 0:257], lhsT=at[:, 128 * sub:128 * sub + 128], rhs=vv[:, jf, :], start=st, stop=sp),
                                  [bat, bv], [bpO[sub]])
                            if jf == keys[-1]:
                                group_done(g, d)

                        def group_done(g, d):
                            raw, braw_ = rawr.next()
                            for sub in range(4):
                                S(lambda e, sub=sub: e.copy(out=raw[:, sub, :], in_=pO[sub][:, 0:257]), [bpO[sub]], [braw_], nowaw=(sub > 0))
                            if is_ml:
                                dq.append(lambda: S(lambda e: e.activation(out=dn[:], in_=raw[:, :, 256], func=AF.Abs), [braw_], [bdn]))
                                dq.append(lambda: V(lambda e: e.tensor_tensor(out=dn[:], in0=dn[:], in1=em[d][:, h:h + 1].broadcast_to([128, 4]), op=ALU.max), [bdn, bem[d]], [bdn]))
                                dq.append(lambda: V(lambda e: e.reciprocal(out=dn[:], in_=dn[:]), [bdn], [bdn]))
                                if d == 0:
                                    dq.append(lambda: V(lambda e: e.tensor_tensor(out=hf[:], in0=raw[:, :, 0:256], in1=dn[:].unsqueeze(2).broadcast_to([128, 4, 256]), op=ALU.mult), [braw_, bdn], [bhf]))
                                else:
                                    dq.append(lambda: V(lambda e: e.tensor_tensor(out=hs[:], in0=raw[:, :, 0:256], in1=dn[:].unsqueeze(2).broadcast_to([128, 4, 256]), op=ALU.mult), [braw_, bdn], [bhs]))
                                    dq.append(lambda: V(lambda e: e.tensor_tensor(out=hs[:], in0=hs[:], in1=hf[:], op=ALU.add), [bhs, bhf], [bhs]))
                            else:
                                if d == 0:
                                    dq.append(lambda: S(lambda e: e.copy(out=hf[:], in_=raw[:, :, 0:256]), [braw_], [bhf]))
                                else:
                                    dq.append(lambda: V(lambda e: e.tensor_tensor(out=hs[:], in0=raw[:, :, 0:256], in1=hf[:], op=ALU.add), [braw_, bhf], [bhs]))
                            if d == 1:
                                def st_stats():
                                    for sub in range(4):
                                        V(lambda e, sub=sub: e.bn_stats(out=s6h[:, sub, :], in_=hs[:, sub, :]), [bhs], [bs6h], nowaw=(sub > 0))

                                def st_aggr():
                                    for sub in range(4):
                                        V(lambda e, sub=sub: e.bn_aggr(out=mvh[:, sub, :], in_=s6h[:, sub, :]), [bs6h], [bmvh], nowaw=(sub > 0))

                                def st_eps():
                                    V(lambda e: e.tensor_scalar(out=rsh[:], in0=mvh[:, :, 1], scalar1=LN_EPS, scalar2=None, op0=ALU.add), [bmvh], [brsh])

                                def st_pow():
                                    G(lambda e: e.tensor_tensor(out=rsh[:], in0=rsh[:], in1=mhalf[:], op=ALU.pow), [brsh, bmhalf], [brsh])

                                def st_norm():
                                    V(lambda e: e.scalar_tensor_tensor(out=nmr[:], in0=mvh[:, :, 0], scalar=-1.0, in1=rsh[:], op0=ALU.mult, op1=ALU.mult), [bmvh, brsh], [bnmr])
                                    for sub in range(4):
                                        S(lambda e, sub=sub: e.activation(out=hn[:, sub, :], in_=hs[:, sub, :], func=AF.Identity, bias=nmr[:, sub:sub + 1], scale=rsh[:, sub:sub + 1]),
                                          [bhs, bnmr, brsh], [bhn], nowaw=(sub > 0))

                                def st_tr():
                                    for sub in range(4):
                                        for dc in range(2):
                                            T(lambda e, sub=sub, dc=dc: e.transpose(out=pT[:, 4 * dc + sub, :], in_=hn[:, sub, 128 * dc:128 * dc + 128], identity=identb[:]),
                                              [bhn, bidb], [bpT], nowaw=not (sub == 0 and dc == 0))

                                def st_out():
                                    of, bof = ofm.next()
                                    for dc in range(2):
                                        V(lambda e, dc=dc: e.tensor_tensor(out=of[:, dc, :].rearrange("p (s c) -> p s c", c=128), in0=pT[:, 4 * dc:4 * dc + 4, :], in1=gT[:, dc, 512 * g:512 * g + 512].rearrange("p (s c) -> p s c", c=128), op=ALU.mult),
                                          [bpT, bg], [bof], nowaw=(dc > 0))
                                    P.dma("gpsimd", lambda e: e.dma_start(out=r_d[br, 2 * h:2 * h + 2, :, 512 * g:512 * g + 512].rearrange("c p t -> p c t"), in_=of[:]),
                                          f"rsp{ofm.last}", reads=[bof], writes=[br_d[br]], nowaw=True)
                                dq.extend([st_stats, st_aggr, st_eps, st_pow, st_norm, st_tr, st_out])

                        tiles = []
                        for g in range(4):
                            for d in range(2):
                                keys_ = list(range(0, 4 * g + 6)) if d == 0 else [0, 1] + list(range(4 * g + 2, 18))
                                for jf in keys_:
                                    tiles.append((g, d, jf))
                        queue = []
                        for ti_, (g, d, jf) in enumerate(tiles):
                            for (g2_, d2_, _) in tiles[ti_:ti_ + 4]:
                                if (g2_, d2_) not in ctxs:
                                    group_ctx(g2_, d2_)
                            at, bat = emit_S(g, d, jf)
                            queue.append((g, d, jf, at, bat))
                            if len(queue) > 2:
                                emit_AV(*queue.pop(0))
                            if dq:
                                dq.pop(0)()
                        while queue:
                            emit_AV(*queue.pop(0))
                        while dq:
                            dq.pop(0)()

                    for h in range(4):
                        proj_rot(C_RQ + 256 * h, qT, bq, False)
                        proj_rot(C_RK + 256 * h, kT, bk, True)
                        proj_v(C_RV + 256 * h)
                        proj_g(C_RG + 256 * h, AF.Silu)
                        attention(h, False, 0)
                    for h in range(4):
                        proj_conv(C_MQ + 256 * h, qT, bq, False, 2 * h)
                        proj_conv(C_MK + 256 * h, kT, bk, True, 8 + 2 * h)
                        proj_v(C_MV + 256 * h)
                        proj_g(C_MO + 256 * h, AF.Sigmoid)
                        attention(h, True, 1)

            if dbg and "r" in dbg and b == 0:
                with P.phase():
                    dr = P.sb("dbg_r", [128, 2048], BF16)
                    drf = P.sb("dbg_rf", [128, 2048], F32)
                    bdr = Buf()
                    for br in range(2):
                        for c in range(8):
                            P.dma("sync", lambda e, br=br, c=c: e.dma_start(out=dr[:], in_=r_d[br, c, :, :]), "dbgl", reads=[br_d[br]], writes=[bdr])
                            V(lambda e: e.tensor_copy(out=drf[:], in_=dr[:]), [bdr], [bdr])
                            P.dma("gpsimd", lambda e, br=br, c=c: e.dma_start(out=dbg_outs["r"][br, c, :, :], in_=drf[:]), "dbg", reads=[bdr], writes=[bdbg], nowaw=True)

            with P.phase():
                pA = Ring(P, "pA", 8, [128, 512], F32, psum=True)
                wst = Ring(P, "wst", 3, [128, 8, 128], F32)
                wbf = Ring(P, "wbf", 8, [128, 8, 128], BF16)
                load_w = make_wload(wst, wbf)
                rfull = P.sb("rfull", [128, 8, 2048], BF16)
                mfull = P.sb("mfull", [128, 8, 2048], BF16)
                brf, bmf = Buf(), Buf()
                for c in range(8):
                    P.dma("sync", lambda e, c=c: e.dma_start(out=rfull[:, c, :], in_=r_d[0, c, :, :]), "rfl", reads=[br_d[0]], writes=[brf], nowaw=True)
                    P.dma("sync", lambda e, c=c: e.dma_start(out=mfull[:, c, :], in_=r_d[1, c, :, :]), "mfl", reads=[br_d[1]], writes=[bmf], nowaw=True)
                sgr = Ring(P, "sgr", 4, [128, 512], F32)
                t1r = Ring(P, "t1r", 4, [128, 512], F32)
                yor = Ring(P, "yor", 4, [128, 512], BF16)
                def load_oc(oc):
                    ws = []
                    for (wsrc, gbase) in ((w_rb_d, C_GR), (w_mb_d, C_GM)):
                        ws.append((load_w(wsrc, 128 * oc), load_w(w_in_d, gbase + 128 * oc)))
                    return ws
                ws_all = {0: load_oc(0)}
                for oc in range(8):
                    ws = ws_all.pop(oc)
                    for n in range(4):
                        if n == 1 and oc + 1 < 8:
                            ws_all[oc + 1] = load_oc(oc + 1)
                        tt = []
                        for bi, (src_t, bsrc_t) in enumerate(((rfull, brf), (mfull, bmf))):
                            (wb, bwb), (wg_, bwg_) = ws[bi]
                            pa, bpa = pA.next()
                            for k in range(8):
                                T(lambda e, pa=pa, wb=wb, k=k, src_t=src_t, n=n: e.matmul(pa[:, :], lhsT=wb[:, k, :], rhs=src_t[:, k, 512 * n:512 * n + 512], start=(k == 0), stop=(k == 7)), [bwb, bsrc_t], [bpa])
                            pg, bpg = pA.next()
                            for k in range(8):
                                T(lambda e, pg=pg, wg_=wg_, k=k, n=n: e.matmul(pg[:, :], lhsT=wg_[:, k, :], rhs=uT[:, k, 256 + 512 * n:256 + 512 * n + 512], start=(k == 0), stop=(k == 7)), [bwg_, buT[n + 1]], [bpg])
                            sg, bsg = sgr.next()
                            S(lambda e, sg=sg, pg=pg: e.activation(out=sg[:], in_=pg[:], func=AF.Sigmoid), [bpg], [bsg])
                            t1, bt1 = t1r.next()
                            V(lambda e, t1=t1, pa=pa, sg=sg: e.tensor_tensor(out=t1[:], in0=pa[:], in1=sg[:], op=ALU.mult), [bpa, bsg], [bt1])
                            tt.append((t1, bt1))
                        yo, byo = yor.next()
                        G(lambda e, yo=yo, a=tt[0][0], c=tt[1][0]: e.tensor_tensor(out=yo[:], in0=a[:], in1=c[:], op=ALU.add), [tt[0][1], tt[1][1]], [byo])
                        P.dma("gpsimd", lambda e, yo=yo, oc=oc, n=n: e.dma_start(out=y_d[oc, :, 512 * n:512 * n + 512], in_=yo[:]), f"yspill{yor.last}", reads=[byo], writes=[by_d], nowaw=True)

            with P.phase():
                pA = Ring(P, "pA", 3, [128, 512], F32, psum=True)
                pO = [P.ps(f"pO{i}", [128, 512], F32) for i in range(4)]
                bpO = [Buf() for _ in range(4)]
                wst = Ring(P, "wst", 3, [128, 8, 128], F32)
                woutb = P.sb("woutb", [128, 8, 1024], BF16)
                bwout = Buf()
                for c in range(8):
                    wt, bw = wst.next()
                    P.dma("sync", lambda e, wt=wt, c=c: e.dma_start(out=wt[:], in_=w_out_d[:, 128 * c:128 * c + 128].rearrange("(c p) n -> p c n", p=128)), f"wst{wst.last}", writes=[bw])
                    S(lambda e, wt=wt, c=c: e.copy(out=woutb[:, :, 128 * c:128 * c + 128], in_=wt[:]), [bw], [bwout], nowaw=True)
                bct = {}
                bbc = Buf()
                for name, src in (("g1", mod_d[b:b + 1, 2048:3072]), ("sh2", mod_d[b:b + 1, 3072:4096]), ("sc2", mod_d[b:b + 1, 4096:5120]),
                                  ("l1g", lnp_d[0:1, :]), ("l1b", lnp_d[1:2, :])):
                    t = P.sb("bc_" + name, [128, 1024], F32)
                    bct[name] = t
                    P.dma("sync", lambda e, t=t, src=src: e.dma_start(out=t[:], in_=src.partition_broadcast(128)), "bc", reads=[bmod], writes=[bbc], nowaw=True)
                V(lambda e: e.tensor_scalar(out=bct["sc2"][:], in0=bct["sc2"][:], scalar1=1.0, scalar2=None, op0=ALU.add), [bbc], [bbc])
                yTr = Ring(P, "yT", 2, [128, 8, 512], BF16)
                xtr = Ring(P, "xt2", 2, [128, 1024], F32)
                ztr = Ring(P, "zt", 2, [128, 1024], F32)
                x1tr = Ring(P, "x1t", 2, [128, 1024], F32)
                u2tr = Ring(P, "u2t", 2, [128, 1024], F32)
                u2br = Ring(P, "u2b", 2, [128, 1024], BF16)
                u2Tr = Ring(P, "u2T", 2, [128, 8, 128], F32)
                s6r = Ring(P, "s6b", 2, [128, 2, 6], F32)
                m2r = Ring(P, "m2b", 2, [128, 2], F32)
                r2r = Ring(P, "r2b", 2, [128, 1], F32)
                yTs = {}
                u2s_ = {}

                def part1(i):
                    n, sub = i // 4, i % 4
                    gi = b * 16 + i
                    if sub == 0:
                        yT, byT = yTr.next()
                        P.dma("sync", lambda e: e.dma_start(out=yT[:], in_=y_d[:, :, 512 * n:512 * n + 512].rearrange("c p t -> p c t")), f"yT{yTr.last}", reads=[by_d], writes=[byT])
                        yTs[n] = (yT, byT)
                    yT, byT = yTs[n]
                    xt, bxt = xtr.next()
                    P.dma("sync", lambda e: e.dma_start(out=xt[:], in_=x_d[b, 128 * i:128 * i + 128, :]), f"xt2{xtr.last}", writes=[bxt])
                    for half in range(2):
                        for k in range(8):
                            T(lambda e, half=half, k=k: e.matmul(pO[half][:, :], lhsT=yT[:, k, 128 * sub:128 * sub + 128], rhs=woutb[:, k, 512 * half:512 * half + 512], start=(k == 0), stop=(k == 7)),
                              [byT, bwout], [bpO[half]])
                    zt, bzt = ztr.next()
                    for half in range(2):
                        V(lambda e, half=half: e.tensor_tensor(out=zt[:, 512 * half:512 * half + 512], in0=pO[half][:, :], in1=bct["g1"][:, 512 * half:512 * half + 512], op=ALU.mult),
                          [bpO[half], bbc], [bzt], nowaw=(half > 0))
                    V(lambda e: e.scalar_tensor_tensor(out=zt[:], in0=xt[:], scalar=DN_ALPHA, in1=zt[:], op0=ALU.mult, op1=ALU.add), [bxt, bzt], [bzt])
                    s6, bs6 = s6r.next()
                    m2, bm2 = m2r.next()
                    r2, br2 = r2r.next()
                    ln_stats(zt, bzt, s6, bs6, m2, bm2, r2, br2)
                    x1t, bx1t = x1tr.next()
                    V(lambda e: e.tensor_scalar(out=x1t[:], in0=zt[:], scalar1=m2[:, 0:1], scalar2=r2[:, 0:1], op0=ALU.subtract, op1=ALU.mult), [bzt, bm2, br2], [bx1t])
                    G(lambda e: e.tensor_tensor(out=x1t[:], in0=x1t[:], in1=bct["l1g"][:], op=ALU.mult), [bx1t, bbc], [bx1t])
                    V(lambda e: e.tensor_tensor(out=x1t[:], in0=x1t[:], in1=bct["l1b"][:], op=ALU.add), [bx1t, bbc], [bx1t])
                    P.dma("gpsimd", lambda e: e.dma_start(out=x1_d[128 * gi:128 * gi + 128, :], in_=x1t[:]), f"x1st{x1tr.last}", reads=[bx1t], writes=[bx1_d], nowaw=True)
                    s6, bs6 = s6r.next()
                    m2, bm2 = m2r.next()
                    r2, br2 = r2r.next()
                    ln_stats(x1t, bx1t, s6, bs6, m2, bm2, r2, br2)
                    u2t, bu2t = u2tr.next()
                    V(lambda e: e.tensor_scalar(out=u2t[:], in0=x1t[:], scalar1=m2[:, 0:1], scalar2=r2[:, 0:1], op0=ALU.subtract, op1=ALU.mult), [bx1t, bm2, br2], [bu2t])
                    G(lambda e: e.tensor_tensor(out=u2t[:], in0=u2t[:], in1=bct["sc2"][:], op=ALU.mult), [bu2t, bbc], [bu2t])
                    V(lambda e: e.tensor_tensor(out=u2t[:], in0=u2t[:], in1=bct["sh2"][:], op=ALU.add), [bu2t, bbc], [bu2t])
                    u2b, bu2b = u2br.next()
                    S(lambda e: e.copy(out=u2b[:], in_=u2t[:]), [bu2t], [bu2b])
                    P.dma("gpsimd", lambda e: e.dma_start(out=u2_d[128 * gi:128 * gi + 128, :], in_=u2b[:]), f"u2st{u2br.last}", reads=[bu2b], writes=[bu2_d], nowaw=True)
                    u2s_[i] = (u2t, bu2t)

                def part2(i):
                    gi = b * 16 + i
                    u2t, bu2t = u2s_.pop(i)
                    for k in range(8):
                        pp = pO[2 + k // 4]
                        T(lambda e, pp=pp, k=k: e.transpose(out=pp[:, 128 * (k % 4):128 * (k % 4) + 128], in_=u2t[:, 128 * k:128 * k + 128], identity=identf),
                          [bu2t, bcst], [bpO[2 + k // 4]], nowaw=(k % 4 > 0))
                    u2T, bu2T = u2Tr.next()
                    S(lambda e: e.copy(out=u2T[:, 0:4, :].rearrange("p a b -> p (a b)"), in_=pO[2][:, :]), [bpO[2]], [bu2T])
                    V(lambda e: e.tensor_copy(out=u2T[:, 4:8, :].rearrange("p a b -> p (a b)"), in_=pO[3][:, :]), [bpO[3]], [bu2T], nowaw=True)
                    pl, bpl = pA.next()
                    for k in range(8):
                        T(lambda e, k=k: e.matmul(pl[:, 0:36], lhsT=u2T[:, k, :], rhs=wrt[:, k, :], start=(k == 0), stop=(k == 7)), [bu2T, bwrt], [bpl])
                    V(lambda e: e.tensor_tensor(out=logits[:, gi, :], in0=pl[:, 0:36], in1=brt[:], op=ALU.add), [bpl, bwrt], [blog], nowaw=True)

                for i in range(17):
                    if i < 16:
                        part1(i)
                    if i >= 1:
                        part2(i - 1)

        if dbg and "x1" in dbg:
            with P.phase():
                t = P.sb("dbg_x1", [128, 1024], F32)
                bt = Buf()
                for gi in range(32):
                    P.dma("sync", lambda e, gi=gi: e.dma_start(out=t[:], in_=x1_d[128 * gi:128 * gi + 128, :]), "dbgl", reads=[bx1_d], writes=[bt])
                    P.dma("gpsimd", lambda e, gi=gi: e.dma_start(out=dbg_outs["x1"][128 * gi:128 * gi + 128, :], in_=t[:]), "dbg", reads=[bt], writes=[bdbg], nowaw=True)
                lt = P.sb("dbg_lg", [128, 32 * 36], F32)
                V(lambda e: e.tensor_copy(out=lt[:], in_=logits[:].rearrange("p a b -> p (a b)")), [blog], [bt])
                P.dma("gpsimd", lambda e: e.dma_start(out=dbg_outs["logits"], in_=lt[:]), "dbg", reads=[bt], writes=[bdbg], nowaw=True)

        MOE_PLACEHOLDER = True

        d1i = P.sb("d1i", [128, 32], I32)
        d2i = P.sb("d2i", [128, 32], I32)
        wt1 = P.sb("wt1", [128, 32], F32)
        wt2 = P.sb("wt2", [128, 32], F32)
        bei = P.sb("bei", [128, 96], I32)
        bd12, bwt12, bbe = Buf(), Buf(), Buf()
        with P.phase():
            pA = Ring(P, "pA", 3, [128, 512], F32, psum=True)
            ppos = [P.ps(f"ppos{i}", [128, 16, 32], F32) for i in range(2)]
            bppos = [Buf(), Buf()]

            def R(name, shape, dt=F32):
                return P.sb("rt_" + name, shape, dt), Buf()
            gmax, bgmax = R("gmax", [128, 32])
            ohg, bohg = R("ohg", [128, 32, 4])
            eg, beg = R("eg", [128, 32, 4])
            pg, bpg_ = R("pg", [128, 32])
            tmp4, btmp4 = R("tmp4", [128, 32, 4, 8])
            les, bles = R("les", [128, 32, 8])
            m1, bm1 = R("m1", [128, 32])
            mk1, bmk1 = R("mk1", [128, 32, 8])
            le2, ble2 = R("le2", [128, 32, 8])
            m2_, bm2_ = R("m2", [128, 32])
            mk2, bmk2 = R("mk2", [128, 32, 8])
            sg_, bsg_ = R("sg", [128, 32])
            A1, bA1 = R("A1", [128, 32, 4, 8])
            A2, bA2 = R("A2", [128, 32, 4, 8])
            Ab, bAb = R("Ab", [128, 32, 32], BF16)
            posf, bposf = R("posf", [128, 32, 32])
            cnt, bcnt = R("cnt", [128, 32])
            cnti, bcnti = R("cnti", [128, 32], I32)
            padf, bpadf = R("padf", [128, 32])
            pend, bpend = R("pend", [128, 32])
            poff, bpoff = R("poff", [128, 32])
            ones32, bones32 = R("ones32", [128, 32])
            dfl, bdfl = R("dfl", [128, 32])
            cmp, bcmp = R("cmp", [128, 48, 32])
            bef, bbef = R("bef", [128, 48])
            lgv = logits[:, :, 0:4]
            lev = logits[:, :, 4:36].rearrange("p t (g e) -> p t g e", e=8)

            def bc3(ap, n):
                return ap.unsqueeze(2).broadcast_to([128, 32, n])
            V(lambda e: e.memset(ones32[:], 1.0), [], [bones32])
            V(lambda e: e.tensor_reduce(out=gmax[:], in_=lgv, axis=AX.X, op=ALU.max), [blog], [bgmax])
            V(lambda e: e.tensor_tensor(out=ohg[:], in0=lgv, in1=bc3(gmax[:], 4), op=ALU.is_equal), [blog, bgmax], [bohg])
            V(lambda e: e.tensor_tensor(out=eg[:], in0=lgv, in1=bc3(gmax[:], 4), op=ALU.subtract), [blog, bgmax], [beg])
            S(lambda e: e.activation(out=eg[:], in_=eg[:], func=AF.Exp), [beg], [beg])
            V(lambda e: e.tensor_reduce(out=pg[:], in_=eg[:], axis=AX.X, op=ALU.add), [beg], [bpg_])
            V(lambda e: e.reciprocal(out=pg[:], in_=pg[:]), [bpg_], [bpg_])
            V(lambda e: e.tensor_tensor(out=tmp4[:], in0=lev, in1=ohg[:].unsqueeze(3).broadcast_to([128, 32, 4, 8]), op=ALU.mult), [blog, bohg], [btmp4])
            V(lambda e: e.tensor_reduce(out=les[:], in_=tmp4[:].rearrange("p t g e -> p t e g"), axis=AX.X, op=ALU.add), [btmp4], [bles])
            V(lambda e: e.tensor_reduce(out=m1[:], in_=les[:], axis=AX.X, op=ALU.max), [bles], [bm1])
            V(lambda e: e.tensor_tensor(out=mk1[:], in0=les[:], in1=bc3(m1[:], 8), op=ALU.is_equal), [bles, bm1], [bmk1])
            V(lambda e: e.scalar_tensor_tensor(out=le2[:], in0=mk1[:], scalar=-1e30, in1=les[:], op0=ALU.mult, op1=ALU.add), [bmk1, bles], [ble2])
            V(lambda e: e.tensor_reduce(out=m2_[:], in_=le2[:], axis=AX.X, op=ALU.max), [ble2], [bm2_])
            V(lambda e: e.tensor_tensor(out=mk2[:], in0=le2[:], in1=bc3(m2_[:], 8), op=ALU.is_equal), [ble2, bm2_], [bmk2])
            V(lambda e: e.tensor_tensor(out=sg_[:], in0=m1[:], in1=m2_[:], op=ALU.subtract), [bm1, bm2_], [bsg_])
            S(lambda e: e.activation(out=sg_[:], in_=sg_[:], func=AF.Sigmoid), [bsg_], [bsg_])
            V(lambda e: e.tensor_tensor(out=wt1[:], in0=pg[:], in1=sg_[:], op=ALU.mult), [bpg_, bsg_], [bwt12])
            V(lambda e: e.tensor_tensor(out=wt2[:], in0=pg[:], in1=wt1[:], op=ALU.subtract), [bpg_, bwt12], [bwt12])
            for (Ax, bAx, mk, bmk) in ((A1, bA1, mk1, bmk1), (A2, bA2, mk2, bmk2)):
                V(lambda e, Ax=Ax, mk=mk: e.tensor_tensor(out=Ax[:], in0=ohg[:].unsqueeze(3).broadcast_to([128, 32, 4, 8]), in1=mk[:].unsqueeze(2).broadcast_to([128, 32, 4, 8]), op=ALU.mult),
                  [bohg, bmk], [bAx])
            A1f = A1[:].rearrange("p t g e -> p t (g e)")
            A2f = A2[:].rearrange("p t g e -> p t (g e)")
            V(lambda e: e.tensor_tensor(out=Ab[:], in0=A1f, in1=A2f, op=ALU.add), [bA1, bA2], [bAb])
            for ti in range(32):
                pp = ppos[ti // 16]
                bpp = bppos[ti // 16]
                for tj in range(ti):
                    T(lambda e, pp=pp, ti=ti, tj=tj: e.matmul(pp[:, ti % 16, :], lhsT=onesb[:], rhs=Ab[:, tj, :], start=(tj == 0), stop=False), [bidb, bAb], [bpp], nowaw=True)
                T(lambda e, pp=pp, ti=ti: e.matmul(pp[:, ti % 16, :], lhsT=trib[:], rhs=Ab[:, ti, :], start=(ti == 0), stop=True), [bidb, bAb], [bpp], nowaw=True)
            pc, bpc = pA.next()
            for tj in range(32):
                T(lambda e, pc=pc, tj=tj: e.matmul(pc[:, 0:32], lhsT=onesb[:], rhs=Ab[:, tj, :], start=(tj == 0), stop=(tj == 31)), [bidb, bAb], [bpc])
            S(lambda e: e.copy(out=posf[:, 0:16, :], in_=ppos[0][:]), [bppos[0]], [bposf])
            V(lambda e: e.tensor_copy(out=posf[:, 16:32, :], in_=ppos[1][:]), [bppos[1]], [bposf], nowaw=True)
            V(lambda e: e.tensor_scalar(out=cnti[:], in0=pc[:, 0:32], scalar1=511.0, scalar2=None, op0=ALU.add), [bpc], [bcnti])
            V(lambda e: e.tensor_single_scalar(out=cnti[:], in_=cnti[:], scalar=9, op=ALU.arith_shift_right), [bcnti], [bcnti])
            V(lambda e: e.tensor_single_scalar(out=cnti[:], in_=cnti[:], scalar=9, op=ALU.logical_shift_left), [bcnti], [bcnti])
            V(lambda e: e.tensor_copy(out=padf[:], in_=cnti[:]), [bcnti], [bpadf])
            V(lambda e: e.tensor_tensor_scan(out=pend[:], data0=ones32[:], data1=padf[:], initial=0.0, op0=ALU.mult, op1=ALU.add), [bones32, bpadf], [bpend])
            V(lambda e: e.tensor_tensor(out=poff[:], in0=pend[:], in1=padf[:], op=ALU.subtract), [bpend, bpadf], [bpoff])
            V(lambda e: e.tensor_tensor(out=posf[:], in0=posf[:], in1=poff[:].unsqueeze(1).broadcast_to([128, 32, 32]), op=ALU.add), [bposf, bpoff], [bposf])
            for (Af, bAx, di) in ((A1f, bA1, d1i), (A2f, bA2, d2i)):
                V(lambda e, Af=Af: e.tensor_tensor(out=tmp4[:].rearrange("p t g e -> p t (g e)"), in0=Af, in1=posf[:], op=ALU.mult), [bAx, bposf], [btmp4])
                V(lambda e: e.tensor_reduce(out=dfl[:], in_=tmp4[:].rearrange("p t g e -> p t (g e)"), axis=AX.X, op=ALU.add), [btmp4], [bdfl])
                V(lambda e, di=di: e.tensor_copy(out=di[:], in_=dfl[:]), [bdfl], [bd12], nowaw=True)
            V(lambda e: e.tensor_tensor(out=cmp[:], in0=pend[:].unsqueeze(1).broadcast_to([128, 48, 32]), in1=cst[:, K_BLK:K_BLK + 48].unsqueeze(2).broadcast_to([128, 48, 32]), op=ALU.is_le),
              [bpend, bcst], [bcmp])
            V(lambda e: e.tensor_reduce(out=bef[:], in_=cmp[:], axis=AX.X, op=ALU.add), [bcmp], [bbef])
            V(lambda e: e.tensor_scalar(out=bei[:, 0:48], in0=bef[:], scalar1=31.0, scalar2=None, op0=ALU.min), [bbef], [bbe])
            if dbg and "route" in dbg:
                rt = P.sb("dbg_rt", [128, 4, 32], F32)
                brt_ = Buf()
                V(lambda e: e.tensor_copy(out=rt[:, 0, :], in_=d1i[:]), [bd12], [brt_], nowaw=True)
                V(lambda e: e.tensor_copy(out=rt[:, 1, :], in_=d2i[:]), [bd12], [brt_], nowaw=True)
                V(lambda e: e.tensor_copy(out=rt[:, 2, :], in_=wt1[:]), [bwt12], [brt_], nowaw=True)
                V(lambda e: e.tensor_copy(out=rt[:, 3, :], in_=wt2[:]), [bwt12], [brt_], nowaw=True)
                P.dma("gpsimd", lambda e: e.dma_start(out=dbg_outs["route"], in_=rt[:].rearrange("p a b -> p (a b)")), "dbg", reads=[brt_], writes=[bdbg], nowaw=True)
                bt_ = P.sb("dbg_be", [128, 96], F32)
                V(lambda e: e.memset(bt_[:], 0.0), [], [brt_], nowaw=True)
                V(lambda e: e.tensor_copy(out=bt_[:, 0:48], in_=bei[:, 0:48]), [bbe], [brt_])
                P.dma("gpsimd", lambda e: e.dma_start(out=dbg_outs["be"], in_=bt_[:]), "dbg", reads=[brt_], writes=[bdbg], nowaw=True)
            u2s = Ring(P, "u2s", 2, [128, 1024], BF16)
            for ti in range(32):
                ut, but = u2s.next()
                P.dma("sync", lambda e, ut=ut, ti=ti: e.dma_start(out=ut[:], in_=u2_d[128 * ti:128 * ti + 128, :]), f"u2s{ti % 2}", reads=[bu2_d], writes=[but])
                for di in (d1i, d2i):
                    P.dma("gpsimd", lambda e, ut=ut, ti=ti, di=di: e.indirect_dma_start(
                        out=xs_d, out_offset=bass.IndirectOffsetOnAxis(ap=di[:, ti:ti + 1], axis=0), in_=ut[:], in_offset=None),
                        f"xsc{ti % 2}", reads=[but, bd12], writes=[bxs_d], nowaw=True)

        with P.phase():
            pA = Ring(P, "pA", 2, [128, 512], F32, psum=True)
            pY = [P.ps(f"pY{i}", [128, 512], F32) for i in range(4)]
            bpY = [Buf(), Buf()]
            pT = P.ps("pT", [128, 8, 128], BF16)
            bpT = Buf()
            pT2 = P.ps("pT2", [128, 8, 128], BF16)
            bpT2 = Buf()
            wstg = Ring(P, "wstg", 6, [128, 2048], F32)
            w1b = Ring(P, "w1b", 2, [128, 8, 512], BF16)
            w3b = Ring(P, "w3b", 2, [128, 8, 512], BF16)
            w2b = Ring(P, "w2b", 2, [128, 4, 1024], BF16)
            xsb = Ring(P, "xsb", 4, [128, 1024], BF16)
            xsTr = Ring(P, "xsT", 3, [128, 8, 128], BF16)
            hsl = P.sb("hsl", [128, 512], F32)
            bhsl = Buf()
            hhr = Ring(P, "hh", 3, [128, 512], BF16)
            hTr = Ring(P, "hT", 2, [128, 4, 128], BF16)
            ysb = Ring(P, "ysb", 2, [128, 1024], F32)
            regs = {}
            blocks = [(sbk, sub_) for sbk in range(48) for sub_ in range(4)]
            wsets = {}
            hhs = {}

            def load_weights(sbk):
                w1t, bw1 = w1b.next()
                w3t, bw3 = w3b.next()
                w2t, bw2 = w2b.next()
                wsets[sbk] = (w1t, bw1, w3t, bw3, w2t, bw2)
                idx = 0
                for (wd, wt_, bwt_, eng, is2) in ((w_e1_d, w1t, bw1, "scalar", False), (w_e3_d, w3t, bw3, "scalar", False), (w_e2_d, w2t, bw2, "vector", True)):
                    for hf_ in range(2):
                        stg, bstg = wstg.next()

                        def dma_fn(e, wd=wd, hf_=hf_, stg=stg, is2=is2, idx=idx, sbk=sbk):
                            if idx == 0:
                                if "r" not in regs:
                                    regs["r"] = e.alloc_register("r_exp")
                                    regs["a"] = e.alloc_register("r_expa")
                                    regs["b"] = e.alloc_register("r_expb")
                                e.reg_load(regs["r"], bei[0:1, sbk:sbk + 1])
                                e.reg_mul(regs["a"], regs["r"], 524288)
                                e.reg_add(regs["b"], regs["a"], 262144)
                            rr = regs["a"] if hf_ == 0 else regs["b"]
                            if not is2:
                                src = bass.AP(wd.tensor, rr, [[512, 128], [65536, 4], [1, 512]])
                                ins = e.dma_start(out=stg[:].rearrange("p (c n) -> p c n", n=512), in_=src)
                            else:
                                src = bass.AP(wd.tensor, rr, [[1024, 128], [131072, 2], [1, 1024]])
                                ins = e.dma_start(out=stg[:].rearrange("p (c n) -> p c n", n=1024), in_=src)
                            RH = type(rr)
                            tmpn = [nm for grp in ins.ins.regs_accessed() for nm in grp if "_tmp_" in nm][0]
                            kk = int(tmpn.split("_")[-1])
                            e.free_register(RH(tmpn, rr.engine))
                            e.free_register(RH(f"SP_{rr.name}_snap_{kk - 2}", rr.engine))
                            return ins
                        P.dma("sync", dma_fn, f"wstg{wstg.last}", reads=[bbe], writes=[bstg])
                        if not is2:
                            dst = wt_[:, 4 * hf_:4 * hf_ + 4, :].rearrange("p c n -> p (c n)")
                        else:
                            dst = wt_[:, 2 * hf_:2 * hf_ + 2, :].rearrange("p c n -> p (c n)")
                        if eng == "scalar":
                            S(lambda e, dst=dst, stg=stg: e.copy(out=dst, in_=stg[:]), [bstg], [bwt_], nowaw=(hf_ > 0))
                        else:
                            P.op(eng, lambda e, dst=dst, stg=stg: e.tensor_copy(out=dst, in_=stg[:]), reads=[bstg], writes=[bwt_], nowaw=(hf_ > 0))
                        idx += 1

            xsTs = {}

            def stageTx(i):
                xb_, bxb_ = xs_tiles.pop(i)
                for k in range(8):
                    T(lambda e, k=k: e.transpose(out=pT[:, k, :], in_=xb_[:, 128 * k:128 * k + 128], identity=identb[:]), [bxb_, bidb], [bpT], nowaw=(k > 0))
                xsT, bxsT = xsTr.next()
                V(lambda e: e.tensor_copy(out=xsT[:], in_=pT[:]), [bpT], [bxsT])
                xsTs[i] = (xsT, bxsT)

            def stageH(i):
                sbk, sub_ = blocks[i]
                w1t, bw1, w3t, bw3, w2t, bw2 = wsets[sbk]
                xsT, bxsT = xsTs.pop(i)
                p1, bp1 = pA.next()
                p3, bp3 = pA.next()
                for k in range(8):
                    T(lambda e, k=k: e.matmul(p1[:, :], lhsT=xsT[:, k, :], rhs=w1t[:, k, :], start=(k == 0), stop=(k == 7)), [bxsT, bw1], [bp1])
                for k in range(8):
                    T(lambda e, k=k: e.matmul(p3[:, :], lhsT=xsT[:, k, :], rhs=w3t[:, k, :], start=(k == 0), stop=(k == 7)), [bxsT, bw3], [bp3])
                S(lambda e: e.activation(out=hsl[:], in_=p1[:], func=AF.Silu), [bp1], [bhsl])
                hh, bhh = hhr.next()
                V(lambda e: e.tensor_tensor(out=hh[:], in0=p3[:], in1=hsl[:], op=ALU.mult), [bp3, bhsl], [bhh])
                hhs[i] = (hh, bhh)

            hTs = {}

            def stageTh(i):
                hh, bhh = hhs.pop(i)
                for f in range(4):
                    T(lambda e, f=f: e.transpose(out=pT2[:, f, :], in_=hh[:, 128 * f:128 * f + 128], identity=identb[:]), [bhh, bidb], [bpT2], nowaw=(f > 0))
                hT, bhT = hTr.next()
                V(lambda e: e.tensor_copy(out=hT[:], in_=pT2[:, 0:4, :]), [bpT2], [bhT])
                hTs[i] = (hT, bhT)

            def stageY(i):
                sbk, sub_ = blocks[i]
                bk = 4 * sbk + sub_
                w1t, bw1, w3t, bw3, w2t, bw2 = wsets[sbk]
                hT, bhT = hTs.pop(i)
                par = bk % 2
                for half in range(2):
                    for f in range(4):
                        T(lambda e, half=half, f=f: e.matmul(pY[2 * par + half][:, :], lhsT=hT[:, f, :], rhs=w2t[:, f, 512 * half:512 * half + 512], start=(f == 0), stop=(f == 3)),
                          [bhT, bw2], [bpY[par]], nowaw=not (half == 0 and f == 0))
                yt_, byt_ = ysb.next()
                S(lambda e: e.copy(out=yt_[:, 0:512], in_=pY[2 * par][:, :]), [bpY[par]], [byt_])
                V(lambda e: e.tensor_copy(out=yt_[:, 512:1024], in_=pY[2 * par + 1][:, :]), [bpY[par]], [byt_], nowaw=True)
                P.dma("gpsimd", lambda e: e.dma_start(out=ys_d[128 * bk:128 * bk + 128, :], in_=yt_[:]), f"yst{ysb.last}", reads=[byt_], writes=[bys_d], nowaw=True)

            xs_tiles = {}

            def load_xs(i):
                sbk, sub_ = blocks[i]
                bk = 4 * sbk + sub_
                xb_, bxb_ = xsb.next()
                P.dma("gpsimd", lambda e: e.dma_start(out=xb_[:], in_=xs_d[128 * bk:128 * bk + 128, :]), f"xsb{xsb.last}", reads=[bxs_d], writes=[bxb_])
                xs_tiles[i] = (xb_, bxb_)
            load_xs(0)
            load_xs(1)
            load_weights(0)
            NB_ = len(blocks)
            for i in range(NB_ + 3):
                if i + 2 < NB_:
                    load_xs(i + 2)
                if i < NB_:
                    stageTx(i)
                if 1 <= i < NB_ + 1:
                    stageH(i - 1)
                if 2 <= i < NB_ + 2:
                    stageTh(i - 2)
                if i >= 3:
                    stageY(i - 3)
                if i >= 2 and (i - 2) % 4 == 0 and (i - 2) // 4 + 1 < 48:
                    load_weights((i - 2) // 4 + 1)

        with P.phase():
            bct = {}
            bbc = Buf()
            for name, src in (("g2_0", mod_d[0:1, 5120:6144]), ("g2_1", mod_d[1:2, 5120:6144]), ("l2g", lnp_d[2:3, :]), ("l2b", lnp_d[3:4, :])):
                t = P.sb("bc2_" + name, [128, 1024], F32)
                bct[name] = t
                P.dma("sync", lambda e, t=t, src=src: e.dma_start(out=t[:], in_=src.partition_broadcast(128)), "bc2", reads=[bmod], writes=[bbc], nowaw=True)
            y1r = Ring(P, "y1r", 3, [128, 1024], F32)
            y2r = Ring(P, "y2r", 3, [128, 1024], F32)
            x1r = Ring(P, "x1r", 3, [128, 1024], F32)
            outr = Ring(P, "outr", 3, [128, 1024], F32)
            s6, m2, r2 = P.sb("s6c", [128, 2, 6], F32), P.sb("m2c", [128, 2], F32), P.sb("r2c", [128, 1], F32)
            bs6, bm2, br2 = Buf(), Buf(), Buf()
            gt = {}

            def gathers(ti):
                y1, by1 = y1r.next()
                y2, by2 = y2r.next()
                x1, bx1 = x1r.next()
                P.dma("gpsimd", lambda e: e.indirect_dma_start(out=y1[:], out_offset=None, in_=ys_d, in_offset=bass.IndirectOffsetOnAxis(ap=d1i[:, ti:ti + 1], axis=0)),
                      f"y1g{y1r.last}", reads=[bys_d, bd12], writes=[by1])
                P.dma("gpsimd", lambda e: e.indirect_dma_start(out=y2[:], out_offset=None, in_=ys_d, in_offset=bass.IndirectOffsetOnAxis(ap=d2i[:, ti:ti + 1], axis=0)),
                      f"y2g{y2r.last}", reads=[bys_d, bd12], writes=[by2])
                P.dma("sync", lambda e: e.dma_start(out=x1[:], in_=x1_d[128 * ti:128 * ti + 128, :]), f"x1l{x1r.last}", reads=[bx1_d], writes=[bx1])
                gt[ti] = (y1, by1, y2, by2, x1, bx1)

            def combine(ti):
                b = ti // 16
                i = ti % 16
                y1, by1, y2, by2, x1, bx1 = gt.pop(ti)
                V(lambda e: e.tensor_scalar(out=y1[:], in0=y1[:], scalar1=wt1[:, ti:ti + 1], scalar2=None, op0=ALU.mult), [by1, bwt12], [by1])
                V(lambda e: e.scalar_tensor_tensor(out=y1[:], in0=y2[:], scalar=wt2[:, ti:ti + 1], in1=y1[:], op0=ALU.mult, op1=ALU.add), [by1, by2, bwt12], [by1])
                g2 = bct["g2_%d" % b]
                V(lambda e: e.tensor_tensor(out=y1[:], in0=y1[:], in1=g2[:], op=ALU.mult), [by1, bbc], [by1])
                V(lambda e: e.scalar_tensor_tensor(out=y1[:], in0=x1[:], scalar=DN_ALPHA, in1=y1[:], op0=ALU.mult, op1=ALU.add), [by1, bx1], [by1])
                s6, bs6 = s6r.next()
                m2, bm2 = m2r.next()
                r2, br2 = r2r.next()
                ln_stats(y1, by1, s6, bs6, m2, bm2, r2, br2)
                ot, bot = outr.next()
                V(lambda e: e.tensor_scalar(out=ot[:], in0=y1[:], scalar1=m2[:, 0:1], scalar2=r2[:, 0:1], op0=ALU.subtract, op1=ALU.mult), [by1, bm2, br2], [bot])
                G(lambda e: e.tensor_tensor(out=ot[:], in0=ot[:], in1=bct["l2g"][:], op=ALU.mult), [bot, bbc], [bot])
                G(lambda e: e.tensor_tensor(out=ot[:], in0=ot[:], in1=bct["l2b"][:], op=ALU.add), [bot, bbc], [bot])
                P.dma("gpsimd", lambda e: e.dma_start(out=out_d[b, 128 * i:128 * i + 128, :], in_=ot[:]), f"ost{outr.last}", reads=[bot], writes=[bout], nowaw=True)
            s6r = Ring(P, "s6cr", 2, [128, 2, 6], F32)
            m2r = Ring(P, "m2cr", 2, [128, 2], F32)
            r2r = Ring(P, "r2cr", 2, [128, 1], F32)
            gathers(0)
            gathers(1)
            for ti in range(32):
                if ti + 2 < 32:
                    gathers(ti + 2)
                combine(ti)
        P.barrier()
        P.emit()
    return nc


_CONSTS = None


def _prep_shared(inp):
    global _CONSTS
    if _CONSTS is None:
        _CONSTS = make_consts()
    f = lambda a: np.ascontiguousarray(a, dtype=np.float32)
    cwT = np.ascontiguousarray(inp["ml_conv_w"][0].reshape(3, 16, 128).transpose(2, 1, 0), dtype=np.float32)
    cbT = np.ascontiguousarray(inp["ml_conv_b"][0].reshape(16, 128).T, dtype=np.float32)
    return {
        "w_ada": f(inp["w_ada"][0]), "b_ada": f(inp["b_ada"][0].reshape(1, 6144)), "w_in": f(inp["w_in"][0]),
        "bmg": f(inp["b_mgate"][0].reshape(4, 4).T), "cw": cwT, "cb": cbT,
        "rdl": f(inp["ret_decay_logit"][0].reshape(1, 8)),
        "w_rb": f(inp["w_ret_branch"][0]), "w_mb": f(inp["w_ml_branch"][0]), "w_out": f(inp["w_out"][0]),
        "lnp": f(np.stack([inp["ln1_g"][0], inp["ln1_b"][0], inp["ln2_g"][0], inp["ln2_b"][0]], 0)),
        "w_rt": f(np.concatenate([inp["w_rg"][0], inp["w_re"][0]], 1)),
        "b_rt": f(np.concatenate([inp["b_rg"][0], inp["b_re"][0]], 0).reshape(1, 36)),
        "w_e1": f(inp["w_e1"][0]), "w_e3": f(inp["w_e3"][0]), "w_e2": f(inp["w_e2"][0]),
        "consts": _CONSTS,
    }


def _core_inputs(inp, shared, c):
    x = np.asarray(inp["x"], dtype=np.float32)
    ctx = np.asarray(inp["ctx"], dtype=np.float32)
    cc = np.asarray(inp["c"], dtype=np.float32)
    c_ctx = np.asarray(inp["c_ctx"], dtype=np.float32)
    vecs = np.stack([cc[2 * c], cc[2 * c + 1], c_ctx], 0)
    cT = np.ascontiguousarray(vecs.reshape(3, 8, 128).transpose(2, 1, 0))
    m = dict(shared)
    m["x"] = np.ascontiguousarray(x[2 * c:2 * c + 2])
    m["ctx"] = np.ascontiguousarray(ctx[2 * c:2 * c + 2])
    m["cT"] = cT
    return m


def kernel(**inputs):
    nc = build()
    shared = _prep_shared(inputs)
    in_maps = [_core_inputs(inputs, shared, c) for c in range(NCORES)]
    res = run_bass_kernel_spmd(nc, in_maps, core_ids=list(range(NCORES)))
    out = np.concatenate([np.asarray(r["out"]) for r in res.results], axis=0)
    return out.astype(np.float32)
```
